# Optimizing a Trainium2 kernel written in Bass

```python
import math
import jax, jax.numpy as jnp
from jax import lax
import numpy as np

D_MODEL = 1024
BATCH = 2
SEQ = 8192
DEPTH = 1

MEM_LEN = 256
ATTN_HEADS = 8
ATTN_HEAD_DIM = D_MODEL // 16
ATTN_WIDTH = ATTN_HEADS * ATTN_HEAD_DIM
SSM_WIDTH = D_MODEL - ATTN_WIDTH
SSM_GROUP = 16
SSM_GROUPS = SSM_WIDTH // SSM_GROUP
SSM_STATE = 64
MIX_WIDTH = ATTN_WIDTH + SSM_WIDTH
IN_WIDTH = 3 * ATTN_WIDTH + SSM_WIDTH
Q_BLOCK = 128
DT_MIN = 1e-3
DT_MAX = 1e-1
XATTN_HEADS = 4
XATTN_HEAD_DIM = D_MODEL // XATTN_HEADS
PEER_HEADS = 8
PEER_KEYS = 128
PEER_EXPERTS = PEER_KEYS * PEER_KEYS
PEER_TOPK = 16
PEER_KEY_DIM = 128
PEER_QUERY_DIM = 2 * PEER_KEY_DIM
TOKEN_BLOCK = 128
EPS = 1e-6

kernel_name = "hybrid_sba_s5_peer_block"


def rmsnorm(x, g):
    xf = x.astype(jnp.float32)
    y = xf * lax.rsqrt(jnp.mean(xf * xf, axis=-1, keepdims=True) + EPS)
    return (y * g.astype(jnp.float32)).astype(x.dtype)


def stick_breaking_attention(q, k, v):
    b, h, s, dh = q.shape
    nb = s // Q_BLOCK
    scale = dh ** -0.5
    q_blocks = q.reshape(b, h, nb, Q_BLOCK, dh).transpose(2, 0, 1, 3, 4)
    key_pos = jnp.arange(s)

    def block(args):
        q_blk, blk = args
        z = jnp.einsum('bhqd,bhkd->bhqk', q_blk, k, preferred_element_type=jnp.float32) * scale
        q_pos = blk * Q_BLOCK + jnp.arange(Q_BLOCK)
        mask = key_pos[None, :] < q_pos[:, None]
        log_stay = jnp.where(mask, jax.nn.log_sigmoid(-z), 0.0)
        later = lax.cumsum(log_stay, axis=3, reverse=True) - log_stay
        w = jnp.where(mask, jnp.exp(jax.nn.log_sigmoid(z) + later), 0.0)
        return jnp.einsum('bhqk,bhkd->bhqd', w.astype(v.dtype), v)

    out = lax.map(block, (q_blocks, jnp.arange(nb)))
    return out.transpose(1, 2, 0, 3, 4).reshape(b, h, s, dh)


def s5_mixer(u, a_re, a_im, log_dt, b_re, b_im, c_re, c_im, d_skip, w_glu, b_glu):
    bsz, s, _ = u.shape
    f32 = jnp.float32
    uf = u.astype(f32).reshape(bsz, s, SSM_GROUPS, SSM_GROUP)
    lam = lax.complex(a_re.astype(f32), a_im.astype(f32))
    dt = jnp.exp(log_dt.astype(f32))[:, None]
    lam_bar = jnp.exp(lam * dt)
    b_mat = lax.complex(b_re.astype(f32), b_im.astype(f32))
    b_bar = ((lam_bar - 1.0) / lam)[..., None] * b_mat
    bu = jnp.einsum('gpc,bsgc->bsgp', b_bar, uf.astype(jnp.complex64))
    a_seq = jnp.broadcast_to(lam_bar, bu.shape)

    def combine(left, right):
        a_l, b_l = left
        a_r, b_r = right
        return a_r * a_l, a_r * b_l + b_r

    _, states = lax.associative_scan(combine, (a_seq, bu), axis=1)
    c_mat = lax.complex(c_re.astype(f32), c_im.astype(f32))
    y = jnp.real(jnp.einsum('gcp,bsgp->bsgc', c_mat, states)) + d_skip.astype(f32) * uf
    y = jax.nn.gelu(y.reshape(bsz, s, SSM_WIDTH))
    gate = jax.nn.sigmoid(y @ w_glu.astype(f32) + b_glu.astype(f32))
    return (y * gate).astype(u.dtype)


def memory_cross_attention(h, mem_n, w_xq, w_xkv, w_xo):
    b, s, _ = h.shape
    m = mem_n.shape[1]
    q = (h @ w_xq).reshape(b, s, XATTN_HEADS, XATTN_HEAD_DIM)
    kv = (mem_n @ w_xkv).reshape(b, m, 2, XATTN_HEADS, XATTN_HEAD_DIM)
    k, v = kv[:, :, 0], kv[:, :, 1]
    scores = jnp.einsum('bshd,bmhd->bhsm', q, k, preferred_element_type=jnp.float32) * (XATTN_HEAD_DIM ** -0.5)
    p = jax.nn.softmax(scores, axis=-1).astype(v.dtype)
    out = jnp.einsum('bhsm,bmhd->bshd', p, v).reshape(b, s, D_MODEL)
    return out @ w_xo


def peer_ffn(h, w_pq, sub_keys, peer_u, peer_v):
    b, s, d = h.shape
    q = (h @ w_pq).reshape(b, s, PEER_HEADS, 2, PEER_KEY_DIM)
    scores = jnp.einsum('bshid,hikd->bshik', q, sub_keys, preferred_element_type=jnp.float32)
    half_val, half_idx = lax.top_k(scores, PEER_TOPK)
    cand = half_val[..., 0, :, None] + half_val[..., 1, None, :]
    cand = cand.reshape(b, s, PEER_HEADS, PEER_TOPK * PEER_TOPK)
    best_val, best_pos = lax.top_k(cand, PEER_TOPK)
    i1 = jnp.take_along_axis(half_idx[..., 0, :], best_pos // PEER_TOPK, axis=-1)
    i2 = jnp.take_along_axis(half_idx[..., 1, :], best_pos % PEER_TOPK, axis=-1)
    expert = i1 * PEER_KEYS + i2
    gate = jax.nn.softmax(best_val, axis=-1)
    nb = s // TOKEN_BLOCK

    def to_blocks(t):
        return t.reshape(b, nb, TOKEN_BLOCK, *t.shape[2:]).swapaxes(0, 1)

    def block(args):
        h_blk, e_blk, g_blk = args
        u_sel = peer_u[e_blk]
        act = jnp.einsum('btd,bthkd->bthk', h_blk, u_sel, preferred_element_type=jnp.float32)
        w = (g_blk * jax.nn.gelu(act)).astype(h.dtype)
        v_sel = peer_v[e_blk]
        return jnp.einsum('bthk,bthkd->btd', w, v_sel)

    out = lax.map(block, (to_blocks(h), to_blocks(expert), to_blocks(gate)))
    return out.swapaxes(0, 1).reshape(b, s, d)


def setup_inputs(seed: int = 0) -> dict:
    key = jax.random.key(seed)
    ks = jax.random.split(key, 32)
    f32 = jnp.float32
    L = DEPTH

    def nrm(k, shape, scale):
        return jax.random.normal(k, shape, f32) * scale

    def gain(k, shape):
        return 1.0 + 0.02 * jax.random.normal(k, shape, f32)

    n = jnp.arange(SSM_STATE, dtype=f32)
    a_re = -0.5 + 0.01 * jax.random.normal(ks[4], (L, SSM_GROUPS, SSM_STATE), f32)
    a_im = math.pi * n + 0.01 * jax.random.normal(ks[5], (L, SSM_GROUPS, SSM_STATE), f32)
    log_dt = jax.random.uniform(ks[6], (L, SSM_GROUPS), f32, math.log(DT_MIN), math.log(DT_MAX))
    return {
        "x": nrm(ks[0], (BATCH, SEQ, D_MODEL), 1.0),
        "mem": nrm(ks[1], (BATCH, MEM_LEN, D_MODEL), 1.0),
        "g_mix": gain(ks[2], (L, D_MODEL)),
        "w_in": nrm(ks[3], (L, D_MODEL, IN_WIDTH), D_MODEL ** -0.5),
        "a_re": a_re,
        "a_im": a_im,
        "log_dt": log_dt,
        "b_re": nrm(ks[7], (L, SSM_GROUPS, SSM_STATE, SSM_GROUP), (2 * SSM_GROUP) ** -0.5),
        "b_im": nrm(ks[8], (L, SSM_GROUPS, SSM_STATE, SSM_GROUP), (2 * SSM_GROUP) ** -0.5),
        "c_re": nrm(ks[9], (L, SSM_GROUPS, SSM_GROUP, SSM_STATE), (2 * SSM_STATE) ** -0.5),
        "c_im": nrm(ks[10], (L, SSM_GROUPS, SSM_GROUP, SSM_STATE), (2 * SSM_STATE) ** -0.5),
        "d_skip": nrm(ks[11], (L, SSM_GROUPS, SSM_GROUP), 1.0),
        "w_glu": nrm(ks[12], (L, SSM_WIDTH, SSM_WIDTH), SSM_WIDTH ** -0.5),
        "b_glu": nrm(ks[13], (L, SSM_WIDTH), 0.01),
        "g_attn_out": gain(ks[14], (L, ATTN_WIDTH)),
        "g_ssm_out": gain(ks[15], (L, SSM_WIDTH)),
        "w_out": nrm(ks[16], (L, MIX_WIDTH, D_MODEL), MIX_WIDTH ** -0.5),
        "g_xattn": gain(ks[17], (L, D_MODEL)),
        "g_mem": gain(ks[18], (L, D_MODEL)),
        "w_xq": nrm(ks[19], (L, D_MODEL, D_MODEL), D_MODEL ** -0.5),
        "w_xkv": nrm(ks[20], (L, D_MODEL, 2 * D_MODEL), D_MODEL ** -0.5),
        "w_xo": nrm(ks[21], (L, D_MODEL, D_MODEL), D_MODEL ** -0.5),
        "g_ffn": gain(ks[22], (L, D_MODEL)),
        "w_pq": nrm(ks[23], (L, D_MODEL, PEER_HEADS * PEER_QUERY_DIM), D_MODEL ** -0.5),
        "sub_keys": nrm(ks[24], (L, PEER_HEADS, 2, PEER_KEYS, PEER_KEY_DIM), PEER_KEY_DIM ** -0.5),
        "peer_u": nrm(ks[25], (L, PEER_EXPERTS, D_MODEL), D_MODEL ** -0.5),
        "peer_v": nrm(ks[26], (L, PEER_EXPERTS, D_MODEL), PEER_HEADS ** -0.5),
        "g_final": gain(ks[27], (D_MODEL,)),
    }


def reference(x, mem, g_mix, w_in, a_re, a_im, log_dt, b_re, b_im, c_re, c_im, d_skip,
              w_glu, b_glu, g_attn_out, g_ssm_out, w_out, g_xattn, g_mem, w_xq, w_xkv, w_xo,
              g_ffn, w_pq, sub_keys, peer_u, peer_v, g_final):
    b, s, _ = x.shape
    h = x
    for layer in range(DEPTH):
        xn = rmsnorm(h, g_mix[layer])
        proj = xn @ w_in[layer]
        q = proj[..., :ATTN_WIDTH]
        k = proj[..., ATTN_WIDTH:2 * ATTN_WIDTH]
        v = proj[..., 2 * ATTN_WIDTH:3 * ATTN_WIDTH]
        u = proj[..., 3 * ATTN_WIDTH:]

        def heads(t):
            return t.reshape(b, s, ATTN_HEADS, ATTN_HEAD_DIM).transpose(0, 2, 1, 3)

        attn = stick_breaking_attention(heads(q), heads(k), heads(v))
        attn = attn.transpose(0, 2, 1, 3).reshape(b, s, ATTN_WIDTH)
        ssm = s5_mixer(u, a_re[layer], a_im[layer], log_dt[layer], b_re[layer], b_im[layer],
                       c_re[layer], c_im[layer], d_skip[layer], w_glu[layer], b_glu[layer])
        mixed = jnp.concatenate([rmsnorm(attn, g_attn_out[layer]),
                                 rmsnorm(ssm, g_ssm_out[layer])], axis=-1)
        h = h + mixed @ w_out[layer]
        h = h + memory_cross_attention(rmsnorm(h, g_xattn[layer]), rmsnorm(mem, g_mem[layer]),
                                       w_xq[layer], w_xkv[layer], w_xo[layer])
        h = h + peer_ffn(rmsnorm(h, g_ffn[layer]), w_pq[layer], sub_keys[layer],
                         peer_u[layer], peer_v[layer])
    return rmsnorm(h, g_final)
```

```python
from contextlib import ExitStack
import numpy as np
import concourse.bass as bass
import concourse.mybir as mybir
from concourse.bass_utils import run_bass_kernel_spmd

F32 = mybir.dt.float32
BF16 = mybir.dt.bfloat16
U32 = mybir.dt.uint32
I32 = mybir.dt.int32
AF = mybir.ActivationFunctionType
ALU = mybir.AluOpType
AX = mybir.AxisListType

D = 1024
SEQ = 8192
NSEG = 16
SEGT = 512
NOWN = 4
EPS = 1e-6
KB_TOP = [15, 31, 47, 63]
NEG = -1.0e30


def own_tiles(j):
    return [j, 7 - j, 8 + j, 15 - j]


class Tok:
    __slots__ = ("w", "r")

    def __init__(self):
        self.w = None
        self.r = {}


class _Eng:
    def __init__(self, name, h, sem):
        self.name = name
        self.h = h
        self.sem = sem
        self.n = 0
        self.waited = {}


class _Chan:
    def __init__(self, sem):
        self.sem = sem
        self.n = 0


class Sched:
    def __init__(self, nc, es):
        self.nc = nc
        self.es = es
        self.E = {}
        for name, h in (("pe", nc.tensor), ("act", nc.scalar), ("dve", nc.vector),
                        ("pool", nc.gpsimd), ("sp", nc.sync)):
            sem = es.enter_context(nc.semaphore("sem_" + name))
            self.E[name] = _Eng(name, h, sem)
        self.nchan = 0
        self.chans = []

    def chan(self):
        self.nchan += 1
        c = _Chan(self.es.enter_context(self.nc.semaphore("ch%d" % self.nchan)))
        self.chans.append(c)
        return c

    def barrier(self):
        for E in self.E.values():
            for F in self.E.values():
                if F is E or F.n == 0:
                    continue
                if E.waited.get(id(F.sem), 0) < F.n:
                    E.h.wait_ge(F.sem, F.n)
                    E.waited[id(F.sem)] = F.n
            for c in self.chans:
                if c.n > 0 and E.waited.get(id(c.sem), 0) < c.n:
                    E.h.wait_ge(c.sem, c.n)
                    E.waited[id(c.sem)] = c.n

    def _wait(self, E, reads, writes, weak=()):
        deps = {}
        for t in weak:
            for d in [t.w] + list(t.r.values()):
                if d is not None and d[0] is not E.sem:
                    k_ = id(d[0])
                    if k_ not in deps or deps[k_][1] < d[1]:
                        deps[k_] = d

        def add(d):
            if d is None:
                return
            s, v = d
            k = id(s)
            if k not in deps or deps[k][1] < v:
                deps[k] = (s, v)

        for t in reads:
            add(t.w)
        for t in writes:
            add(t.w)
            for d in t.r.values():
                add(d)
        for k, (s, v) in deps.items():
            if E.name == "pe" and s is E.sem:
                continue
            if E.waited.get(k, 0) < v:
                E.h.wait_ge(s, v)
                E.waited[k] = v

    def op(self, eng, fn, reads=(), writes=(), weak=()):
        E = self.E[eng]
        self._wait(E, reads, writes, weak)
        ins = fn(E.h)
        E.n += 1
        ins.then_inc(E.sem, 1)
        me = (E.sem, E.n)
        for t in reads:
            t.r[id(E.sem)] = me
        for t in writes:
            t.w = me
            t.r = {}
        for t in weak:
            t.w = me
            t.r = {}
        return ins

    def dma(self, queue, ch, out, in_, reads=(), writes=(), indirect=None, **kw):
        Q = self.E[queue]
        self._wait(Q, reads, writes)
        if indirect is not None:
            ins = Q.h.indirect_dma_start(out=out, out_offset=None, in_=in_, in_offset=indirect)
        else:
            ins = Q.h.dma_start(out=out, in_=in_, **kw)
        ch.n += 16
        ins.then_inc(ch.sem, 16)
        me = (ch.sem, ch.n)
        for t in reads:
            t.r[id(ch.sem)] = me
        for t in writes:
            t.w = me
            t.r = {}
        return ins

    def wait_tok(self, eng, toks):
        self._wait(self.E[eng], toks, ())


class Ctx:
    pass


_NAME_CNT = [0]


def _sb(nc, es, name, shape, dt):
    _NAME_CNT[0] += 1
    return es.enter_context(nc.sbuf_tensor("%s_%d" % (name, _NAME_CNT[0]), list(shape), dt))


def rms_scale(C, xt, xt_tok, rstd, rstd_tok, junk, junk_tok, ss, ss_tok):
    S = C.S
    S.op("act", lambda e: e.activation(out=junk, in_=xt, func=AF.Square, accum_out=ss),
         reads=[xt_tok], writes=[junk_tok, ss_tok])
    S.op("act", lambda e: e.activation(out=rstd, in_=ss, func=AF.Ln, bias=C.eps_col[:], scale=1.0 / D),
         reads=[ss_tok, C.const_tok], writes=[rstd_tok])
    S.op("act", lambda e: e.activation(out=rstd, in_=rstd, func=AF.Exp, scale=-0.5),
         reads=[rstd_tok], writes=[rstd_tok])


def transpose_to(C, xn, xn_tok, dst_fn, dst_tok, nkt=8):
    S = C.S
    force = getattr(C, "tp_force", None)
    if force is not None:
        pt, pt_tok = C.tp_ps[force], C.tp_tok[force]
    else:
        pt, pt_tok = C.tp_ps[C.tp_i % 2], C.tp_tok[C.tp_i % 2]
        C.tp_i += 1
    ptb = pt[:].bitcast(BF16)
    for kt in range(nkt):
        S.op("pe", lambda e, kt=kt: e.transpose(out=ptb[:, kt * 128:(kt + 1) * 128],
                                                in_=xn[:, kt * 128:(kt + 1) * 128],
                                                identity=C.ident_bf[:]),
             reads=[xn_tok, C.const_tok], writes=[pt_tok])
    return ptb, pt_tok


TWO_PI = 6.283185307179586
CW1 = 6.28125
CW2 = TWO_PI - CW1


def phase_ssm(C, nc, S, ps, ps_tok, I, ssmT, ssm_tok, gmix_sb, gmix_tok):
    with ExitStack() as e2:
        cosT = _sb(nc, e2, "cosT", [128, 16, SEGT], F32)
        sinT = _sb(nc, e2, "sinT", [128, 16, SEGT], F32)
        tab_tok = Tok()
        bre = _sb(nc, e2, "bre", [128, 16, 128], BF16)
        bim = _sb(nc, e2, "bim", [128, 16, 128], BF16)
        cre = _sb(nc, e2, "cre", [128, 16, 128], BF16)
        ncim = _sb(nc, e2, "ncim", [128, 16, 128], BF16)
        bc_tok = Tok()
        wu = _sb(nc, e2, "wu", [128, 8, 512], BF16)
        wglu = _sb(nc, e2, "wglu", [128, 4, 512], BF16)
        diagD = _sb(nc, e2, "diagD", [128, 4, 128], BF16)
        w_tok = Tok()
        P = {}
        for nm in ("are", "aim", "ldt", "dt", "rho", "th", "gre", "gim", "ngim", "pa", "pb", "pc", "pd", "adt", "nadt",
                   "ilr", "ili", "q1", "q2", "q3", "q4"):
            P[nm] = _sb(nc, e2, "p_" + nm, [128, 16], F32)
        p_tok = Tok()
        bglu = _sb(nc, e2, "bglu", [128, 4], F32)
        dsk = _sb(nc, e2, "dsk", [128, 4], F32)
        segsel = _sb(nc, e2, "segsel_sb", [128, 4, 16], F32)
        halfpi = _sb(nc, e2, "halfpi", [128, 1], F32)
        Xst = _sb(nc, e2, "Xst", [128, 2, 16, 17], F32)
        tau1p = _sb(nc, e2, "tau1p", [128, SEGT], F32)
        magt = _sb(nc, e2, "magt", [128, SEGT], F32)
        mag_tok = Tok()
        S.op("pool", lambda e: e.iota(tau1p[:], pattern=[[1, SEGT]], base=1, channel_multiplier=0,
                                      allow_small_or_imprecise_dtypes=True), writes=[mag_tok])
        xst_tok = Tok()
        small_tok = Tok()
        ch0 = S.chan()
        for dst, src in ((P["are"], I["a_re"]), (P["aim"], I["a_im"]), (P["ldt"], I["log_dt"]),
                         (bglu, I["b_glu"]), (dsk, I["d_skip"])):
            S.dma("sp", ch0, dst[:], src[:, :], writes=[small_tok])
        S.dma("sp", ch0, segsel[:], I["segsel"][:, :, :], writes=[small_tok])
        S.op("pool", lambda e: e.memset(halfpi[:], float(np.pi / 2)), writes=[small_tok])
        S.op("pool", lambda e: e.memset(Xst[:], 0.0), writes=[xst_tok])
        S.op("act", lambda e: e.activation(out=P["dt"][:], in_=P["ldt"][:], func=AF.Exp), reads=[small_tok], writes=[p_tok])
        S.op("dve", lambda e: e.tensor_tensor(out=P["pa"][:], in0=P["are"][:], in1=P["dt"][:], op=ALU.mult),
             reads=[small_tok, p_tok], writes=[p_tok])
        S.op("act", lambda e: e.activation(out=P["rho"][:], in_=P["pa"][:], func=AF.Exp), reads=[p_tok], writes=[p_tok])
        S.op("dve", lambda e: e.tensor_copy(out=P["adt"][:], in_=P["pa"][:]), reads=[p_tok], writes=[p_tok])
        S.op("dve", lambda e: e.tensor_scalar(P["nadt"][:], P["pa"][:], -1.0, None, ALU.mult), reads=[p_tok], writes=[p_tok])
        S.op("dve", lambda e: e.tensor_tensor(out=P["th"][:], in0=P["aim"][:], in1=P["dt"][:], op=ALU.mult),
             reads=[small_tok, p_tok], writes=[p_tok])
        with ExitStack() as e3:
            tau1 = _sb(nc, e3, "tau1", [128, SEGT], F32)
            ang = _sb(nc, e3, "ang", [128, 4, SEGT], F32)
            kf = _sb(nc, e3, "kf", [128, 4, SEGT], F32)
            ki = _sb(nc, e3, "ki", [128, 4, SEGT], I32)
            s1 = _sb(nc, e3, "s1", [128, 4, SEGT], F32)
            t_tok = Tok()
            S.op("pool", lambda e: e.iota(tau1[:], pattern=[[1, SEGT]], base=1, channel_multiplier=0,
                                          allow_small_or_imprecise_dtypes=True), writes=[t_tok])
            for gq in range(4):
                for kk in range(4):
                    k = gq * 4 + kk
                    S.op("dve", lambda e, kk=kk, k=k: e.tensor_scalar(ang[:, kk, :], tau1[:], P["th"][:, k:k + 1], None, ALU.mult),
                         reads=[p_tok, t_tok], writes=[t_tok])
                S.op("dve", lambda e: e.tensor_scalar(kf[:], ang[:], 1.0 / TWO_PI, None, ALU.mult), reads=[t_tok], writes=[t_tok])
                S.op("dve", lambda e: e.tensor_copy(out=ki[:], in_=kf[:]), reads=[t_tok], writes=[t_tok])
                S.op("dve", lambda e: e.tensor_copy(out=kf[:], in_=ki[:]), reads=[t_tok], writes=[t_tok])
                S.op("dve", lambda e: e.scalar_tensor_tensor(out=ang[:], in0=kf[:], scalar=-CW1, in1=ang[:], op0=ALU.mult, op1=ALU.add),
                     reads=[t_tok], writes=[t_tok])
                S.op("dve", lambda e: e.scalar_tensor_tensor(out=ang[:], in0=kf[:], scalar=-CW2, in1=ang[:], op0=ALU.mult, op1=ALU.add),
                     reads=[t_tok], writes=[t_tok])
                S.op("act", lambda e: e.activation(out=s1[:], in_=ang[:], func=AF.Sin, scale=0.25), reads=[t_tok], writes=[t_tok])
                S.op("act", lambda e: e.activation(out=kf[:], in_=ang[:], func=AF.Sin, scale=0.25, bias=halfpi[:]),
                     reads=[t_tok, small_tok], writes=[t_tok])
                S.op("dve", lambda e: e.scalar_tensor_tensor(out=ang[:], in0=s1[:], scalar=2.0, in1=kf[:], op0=ALU.mult, op1=ALU.mult),
                     reads=[t_tok], writes=[t_tok])
                S.op("dve", lambda e: e.tensor_tensor(out=s1[:], in0=s1[:], in1=s1[:], op=ALU.mult), reads=[t_tok], writes=[t_tok])
                S.op("dve", lambda e: e.tensor_scalar(s1[:], s1[:], -2.0, 1.0, ALU.mult, ALU.add), reads=[t_tok], writes=[t_tok])
                S.op("dve", lambda e, gq=gq: e.scalar_tensor_tensor(out=sinT[:, gq * 4:(gq + 1) * 4, :], in0=ang[:], scalar=2.0, in1=s1[:],
                                                                    op0=ALU.mult, op1=ALU.mult),
                     reads=[t_tok], writes=[tab_tok])
                S.op("dve", lambda e: e.tensor_tensor(out=ang[:], in0=ang[:], in1=ang[:], op=ALU.mult), reads=[t_tok], writes=[t_tok])
                S.op("dve", lambda e, gq=gq: e.tensor_scalar(cosT[:, gq * 4:(gq + 1) * 4, :], ang[:], -2.0, 1.0, ALU.mult, ALU.add),
                     reads=[t_tok], writes=[tab_tok])
            c0 = cosT[:, :, 0]
            s0 = sinT[:, :, 0]
            tt = lambda o, a, b, op: S.op("dve", lambda e: e.tensor_tensor(out=o, in0=a, in1=b, op=op),
                                          reads=[p_tok, tab_tok, small_tok], writes=[p_tok])
            tt(P["pa"][:], P["rho"][:], c0, ALU.mult)
            S.op("dve", lambda e: e.tensor_scalar(P["pa"][:], P["pa"][:], -1.0, None, ALU.add), reads=[p_tok], writes=[p_tok])
            tt(P["pb"][:], P["rho"][:], s0, ALU.mult)
            tt(P["pc"][:], P["are"][:], P["are"][:], ALU.mult)
            tt(P["pd"][:], P["aim"][:], P["aim"][:], ALU.mult)
            tt(P["pc"][:], P["pc"][:], P["pd"][:], ALU.add)
            S.op("dve", lambda e: e.reciprocal(out=P["pc"][:], in_=P["pc"][:]), reads=[p_tok], writes=[p_tok])
            tt(P["gre"][:], P["pa"][:], P["are"][:], ALU.mult)
            tt(P["pd"][:], P["pb"][:], P["aim"][:], ALU.mult)
            tt(P["gre"][:], P["gre"][:], P["pd"][:], ALU.add)
            tt(P["gre"][:], P["gre"][:], P["pc"][:], ALU.mult)
            tt(P["gim"][:], P["pb"][:], P["are"][:], ALU.mult)
            tt(P["pd"][:], P["pa"][:], P["aim"][:], ALU.mult)
            tt(P["gim"][:], P["gim"][:], P["pd"][:], ALU.subtract)
            tt(P["gim"][:], P["gim"][:], P["pc"][:], ALU.mult)
            S.op("dve", lambda e: e.tensor_scalar(P["ngim"][:], P["gim"][:], -1.0, None, ALU.mult), reads=[p_tok], writes=[p_tok])
            S.barrier()
        with ExitStack() as e3:
            f1 = _sb(nc, e3, "f1", [128, 16, 128], F32)
            f2 = _sb(nc, e3, "f2", [128, 16, 128], F32)
            f3 = _sb(nc, e3, "f3", [128, 128], F32)
            f_tok = Tok()
            chf = S.chan()
            for src, dst in ((I["bpad_re"], bre), (I["bpad_im"], bim)):
                S.dma("sp", chf, f1[:], src[:, :, :], writes=[f_tok])
                S.op("dve", lambda e, dst=dst: e.tensor_copy(out=dst[:], in_=f1[:]), reads=[f_tok], writes=[bc_tok, f_tok])
            S.dma("sp", chf, f1[:], I["cpad_re"][:, :, :], writes=[f_tok])
            S.dma("sp", chf, f2[:], I["cpad_im"][:, :, :], writes=[f_tok])
            for k in range(16):
                S.op("dve", lambda e, k=k: e.tensor_scalar(f3[:], f2[:, k, :], P["gim"][:, k:k + 1], None, ALU.mult),
                     reads=[f_tok, p_tok], writes=[f_tok])
                S.op("dve", lambda e, k=k: e.scalar_tensor_tensor(out=cre[:, k, :], in0=f1[:, k, :], scalar=P["gre"][:, k:k + 1],
                                                                  in1=f3[:], op0=ALU.mult, op1=ALU.subtract),
                     reads=[f_tok, p_tok], writes=[bc_tok])
                S.op("dve", lambda e, k=k: e.tensor_scalar(f3[:], f2[:, k, :], P["gre"][:, k:k + 1], None, ALU.mult),
                     reads=[f_tok, p_tok, bc_tok], writes=[f_tok])
                S.op("dve", lambda e, k=k: e.scalar_tensor_tensor(out=ncim[:, k, :], in0=f1[:, k, :], scalar=P["ngim"][:, k:k + 1],
                                                                  in1=f3[:], op0=ALU.mult, op1=ALU.subtract),
                     reads=[f_tok, p_tok], writes=[bc_tok])
            for kt in range(8):
                for q4 in range(4):
                    S.dma("sp", chf, f3[:], I["w_in"][:, kt, 1536 + q4 * 128:1536 + (q4 + 1) * 128], writes=[f_tok])
                    S.op("dve", lambda e, kt=kt, q4=q4: e.tensor_scalar(wu[:, kt, q4 * 128:(q4 + 1) * 128], f3[:],
                                                                      gmix_sb[:, kt:kt + 1], None, ALU.mult),
                         reads=[f_tok, gmix_tok], writes=[w_tok, f_tok])
            for kt in range(4):
                S.dma("sp", chf, f1[:, 0:4, :], I["w_glu"][:, kt, :].rearrange("p (a b) -> p a b", a=4), writes=[f_tok])
                S.op("dve", lambda e, kt=kt: e.tensor_copy(out=wglu[:, kt, :].rearrange("p (a b) -> p a b", a=4), in_=f1[:, 0:4, :]),
                     reads=[f_tok], writes=[w_tok, f_tok])
            for ct in range(4):
                S.op("dve", lambda e, ct=ct: e.tensor_scalar(diagD[:, ct, :], C.ident_f[:], dsk[:, ct:ct + 1], None, ALU.mult),
                     reads=[small_tok, C.const_tok], writes=[w_tok])
            S.barrier()

        def scale_tables(sign_key):
            for k in range(16):
                S.op("act", lambda e, k=k: e.activation(out=magt[:], in_=tau1p[:], func=AF.Exp, scale=P[sign_key][:, k:k + 1]),
                     reads=[mag_tok, p_tok], writes=[mag_tok])
                S.op("dve", lambda e, k=k: e.tensor_tensor(out=cosT[:, k, :], in0=cosT[:, k, :], in1=magt[:], op=ALU.mult),
                     reads=[mag_tok, tab_tok], writes=[tab_tok])
                S.op("dve", lambda e, k=k: e.tensor_tensor(out=sinT[:, k, :], in0=sinT[:, k, :], in1=magt[:], op=ALU.mult),
                     reads=[mag_tok, tab_tok], writes=[tab_tok])

        scale_tables("adt")
        with ExitStack() as e3:
            xt = [_sb(nc, e3, "axt%d" % i, [128, D], F32) for i in range(2)]
            xt_tok = [Tok(), Tok()]
            xt_ch = [S.chan(), S.chan()]
            xn = [_sb(nc, e3, "axn%d" % i, [128, D], BF16) for i in range(2)]
            xn_tok = [Tok(), Tok()]
            ss = [_sb(nc, e3, "ass%d" % i, [128, 1], F32) for i in range(2)]
            ss_tok = [Tok(), Tok()]
            rstd = [_sb(nc, e3, "arstd%d" % i, [128, 1], F32) for i in range(2)]
            rstd_tok = [Tok(), Tok()]
            xnT = [_sb(nc, e3, "axnT%d" % i, [128, 8, SEGT], BF16) for i in range(2)]
            xnT_tok = [Tok(), Tok()]
            uT = [_sb(nc, e3, "auT%d" % i, [128, 4, SEGT], BF16) for i in range(2)]
            uT_tok = [Tok(), Tok()]
            acc4 = [_sb(nc, e3, "acc4_%d" % i, [128, 4, 16], F32) for i in range(2)]
            acc4_tok = [[[Tok() for _ in range(16)] for _ in range(4)] for _ in range(2)]
            junkS = [_sb(nc, e3, "junkS%d" % i, [128, SEGT], BF16) for i in range(4)]
            junkS_tok = [Tok() for _ in range(4)]
            jc = [0]
            sm_ = {nm: _sb(nc, e3, "sm_" + nm, [128, 16], F32) for nm in ("a", "b", "c", "d", "sr", "si")}
            sm_tok = Tok()
            t0r, t0i = cosT[:, :, 0], sinT[:, :, 0]
            Lr, Li = cosT[:, :, SEGT - 1], sinT[:, :, SEGT - 1]
            tt2 = lambda o, a, b, op: S.op("dve", lambda e: e.tensor_tensor(out=o, in0=a, in1=b, op=op),
                                           reads=[p_tok, tab_tok, sm_tok], writes=[p_tok])
            tt2(P["q1"][:], t0r, t0r, ALU.mult)
            tt2(P["q2"][:], t0i, t0i, ALU.mult)
            tt2(P["q1"][:], P["q1"][:], P["q2"][:], ALU.add)
            S.op("dve", lambda e: e.reciprocal(out=P["q1"][:], in_=P["q1"][:]), reads=[p_tok], writes=[p_tok])
            tt2(P["ilr"][:], t0r, P["q1"][:], ALU.mult)
            tt2(P["ili"][:], t0i, P["q1"][:], ALU.mult)
            S.op("dve", lambda e: e.tensor_scalar(P["ili"][:], P["ili"][:], -1.0, None, ALU.mult), reads=[p_tok], writes=[p_tok])
            ti = 0
            for sg in range(NSEG):
                xb = sg % 2
                for m in range(4):
                    a = ti % 2
                    ti += 1
                    r0 = sg * SEGT + m * 128
                    S.dma("sp", xt_ch[a], xt[a][:], I["x_rev"][r0:r0 + 128, :], writes=[xt_tok[a]])
                    rms_scale(C, xt[a][:], xt_tok[a], rstd[a][:], rstd_tok[a], xn[a][:], xn_tok[a], ss[a][:], ss_tok[a])
                    S.op("dve", lambda e, a=a: e.tensor_scalar(xn[a][:], xt[a][:], rstd[a][:], None, ALU.mult),
                         reads=[xt_tok[a], rstd_tok[a]], writes=[xn_tok[a]])
                    ptb, pt_tok = transpose_to(C, xn[a], xn_tok[a], None, None)
                    S.op("act", lambda e, xb=xb, m=m, ptb=ptb: e.copy(out=xnT[xb][:, :, m * 128:(m + 1) * 128],
                                                                      in_=ptb.rearrange("p (k t) -> p k t", k=8)),
                         reads=[pt_tok], writes=[xnT_tok[xb]])
                for ct in range(4):
                    pb = 4 + ct % 2
                    for kt in range(8):
                        S.op("pe", lambda e, kt=kt, ct=ct, pb=pb, xb=xb: e.matmul(
                            ps[pb][:], lhsT=wu[:, kt, ct * 128:(ct + 1) * 128], rhs=xnT[xb][:, kt, :],
                            start=(kt == 0), stop=(kt == 7)), reads=[w_tok, xnT_tok[xb]], writes=[ps_tok[pb]])
                    S.op("act", lambda e, ct=ct, pb=pb, xb=xb: e.copy(out=uT[xb][:, ct, :], in_=ps[pb][:]),
                         reads=[ps_tok[pb]], writes=[uT_tok[xb]])
                ab = sg % 2
                for k in range(16):
                    ct = k // 4
                    bur, bui = (0, 1) if k % 2 == 0 else (2, 3)
                    S.op("pe", lambda e, k=k, ct=ct, bur=bur: e.matmul(ps[bur][:], lhsT=bre[:, k, :], rhs=uT[xb][:, ct, :], start=True, stop=True),
                         reads=[bc_tok, uT_tok[xb]], writes=[ps_tok[bur]])
                    S.op("pe", lambda e, k=k, ct=ct, bui=bui: e.matmul(ps[bui][:], lhsT=bim[:, k, :], rhs=uT[xb][:, ct, :], start=True, stop=True),
                         reads=[bc_tok, uT_tok[xb]], writes=[ps_tok[bui]])
                    for j, (bk, tab) in enumerate(((bur, cosT), (bui, sinT), (bur, sinT), (bui, cosT))):
                        jb = jc[0] % 4
                        jc[0] += 1
                        S.op("dve", lambda e, j=j, bk=bk, tab=tab, k=k, jb=jb: e.scalar_tensor_tensor(
                            out=junkS[jb][:], in0=ps[bk][:], scalar=1.0, in1=tab[:, k, :], op0=ALU.mult, op1=ALU.mult,
                            accum_out=acc4[ab][:, j, k:k + 1]),
                            reads=[ps_tok[bk], tab_tok], writes=[junkS_tok[jb], acc4_tok[ab][j][k]])
                A_ = acc4[ab]
                rd = [t_ for row_ in acc4_tok[ab] for t_ in row_] + [p_tok, tab_tok, xst_tok, sm_tok]
                tt3 = lambda o, a_, b_, op: S.op("dve", lambda e: e.tensor_tensor(out=o, in0=a_, in1=b_, op=op), reads=rd, writes=[sm_tok])
                tt3(sm_["a"][:], A_[:, 0, :], A_[:, 1, :], ALU.subtract)
                tt3(sm_["b"][:], A_[:, 2, :], A_[:, 3, :], ALU.add)
                tt3(sm_["c"][:], P["ilr"][:], sm_["a"][:], ALU.mult)
                tt3(sm_["d"][:], P["ili"][:], sm_["b"][:], ALU.mult)
                tt3(sm_["sr"][:], sm_["c"][:], sm_["d"][:], ALU.subtract)
                tt3(sm_["c"][:], P["ilr"][:], sm_["b"][:], ALU.mult)
                tt3(sm_["d"][:], P["ili"][:], sm_["a"][:], ALU.mult)
                tt3(sm_["si"][:], sm_["c"][:], sm_["d"][:], ALU.add)
                Xr, Xi = Xst[:, 0, :, sg], Xst[:, 1, :, sg]
                tt3(sm_["a"][:], Lr, Xr, ALU.mult)
                tt3(sm_["b"][:], Li, Xi, ALU.mult)
                tt3(sm_["a"][:], sm_["a"][:], sm_["b"][:], ALU.subtract)
                S.op("dve", lambda e, sg=sg: e.tensor_tensor(out=Xst[:, 0, :, sg + 1], in0=sm_["a"][:], in1=sm_["sr"][:], op=ALU.add),
                     reads=[sm_tok], writes=[xst_tok])
                tt3(sm_["c"][:], Lr, Xi, ALU.mult)
                tt3(sm_["d"][:], Li, Xr, ALU.mult)
                tt3(sm_["c"][:], sm_["c"][:], sm_["d"][:], ALU.add)
                S.op("dve", lambda e, sg=sg: e.tensor_tensor(out=Xst[:, 1, :, sg + 1], in0=sm_["c"][:], in1=sm_["si"][:], op=ALU.add),
                     reads=[sm_tok], writes=[xst_tok])
            S.barrier()
        scale_tables("nadt")
        S.barrier()
        with ExitStack() as e3:
            xt = [_sb(nc, e3, "sxt%d" % i, [128, D], F32) for i in range(2)]
            xt_tok = [Tok(), Tok()]
            xt_ch = [S.chan(), S.chan()]
            xn = [_sb(nc, e3, "sxn%d" % i, [128, D], BF16) for i in range(2)]
            xn_tok = [Tok(), Tok()]
            ss = [_sb(nc, e3, "sss%d" % i, [128, 1], F32) for i in range(2)]
            ss_tok = [Tok(), Tok()]
            rstd = [_sb(nc, e3, "srstd%d" % i, [128, 1], F32) for i in range(2)]
            rstd_tok = [Tok(), Tok()]
            xnT = [_sb(nc, e3, "sxnT", [128, 8, SEGT], BF16)] * 2
            xnT_tok = [Tok()] * 2
            uT = [_sb(nc, e3, "uT%d" % i, [128, 4, SEGT], BF16) for i in range(2)]
            uT_tok = [Tok(), Tok()]
            tmp4 = [_sb(nc, e3, "tm%d" % i, [128, SEGT], F32) for i in range(4)]
            tmp4_tok = [Tok() for _ in range(4)]
            Wr = [_sb(nc, e3, "Wr%d" % i, [128, SEGT], F32) for i in range(2)]
            Wi = [_sb(nc, e3, "Wi%d" % i, [128, SEGT], F32) for i in range(2)]
            Vr = [_sb(nc, e3, "Vr%d" % i, [128, SEGT], F32) for i in range(2)]
            Vi = [_sb(nc, e3, "Vi%d" % i, [128, SEGT], F32) for i in range(2)]
            W_tok = [Tok(), Tok()]
            V_tok = [Tok(), Tok()]
            Xb = [_sb(nc, e3, "Xb%d" % i, [128, 2, SEGT], BF16) for i in range(2)]
            Xb_tok = [Tok(), Tok()]
            xin = _sb(nc, e3, "xin", [128, 32], F32)
            xin_tok = Tok()
            selt = _sb(nc, e3, "selt", [128, 32, 16], F32)
            rot = _sb(nc, e3, "rot", [128, 4], F32)
            rot_tok = Tok()
            yb = _sb(nc, e3, "yb", [128, 4, SEGT], BF16)
            y_tok = [Tok() for _ in range(4)]
            g1 = _sb(nc, e3, "g1", [128, SEGT], F32)
            g2 = _sb(nc, e3, "g2", [128, SEGT], F32)
            g_tok = Tok()
            ti = 0
            pcount = 0
            for sgi in range(NSEG, NSEG + NOWN):
                own = sgi >= NSEG
                sg = sgi - NSEG if own else sgi
                src = I["x_own"] if own else I["x_full"]
                xb = sgi % 2
                for m in range(4):
                    a = ti % 2
                    ti += 1
                    r0 = sg * SEGT + m * 128
                    S.dma("sp", xt_ch[a], xt[a][:], src[r0:r0 + 128, :], writes=[xt_tok[a]])
                    rms_scale(C, xt[a][:], xt_tok[a], rstd[a][:], rstd_tok[a], xn[a][:], xn_tok[a], ss[a][:], ss_tok[a])
                    S.op("dve", lambda e, a=a: e.tensor_scalar(xn[a][:], xt[a][:], rstd[a][:], None, ALU.mult),
                         reads=[xt_tok[a], rstd_tok[a]], writes=[xn_tok[a]])
                    ptb, pt_tok = transpose_to(C, xn[a], xn_tok[a], None, None)
                    S.op("act", lambda e, xb=xb, m=m, ptb=ptb: e.copy(out=xnT[xb][:, :, m * 128:(m + 1) * 128],
                                                                      in_=ptb.rearrange("p (k t) -> p k t", k=8)),
                         reads=[pt_tok], writes=[xnT_tok[xb]])
                for ct in range(4):
                    pb = ct % 2
                    for kt in range(8):
                        S.op("pe", lambda e, kt=kt, ct=ct, pb=pb, xb=xb: e.matmul(
                            ps[pb][:], lhsT=wu[:, kt, ct * 128:(ct + 1) * 128], rhs=xnT[xb][:, kt, :],
                            start=(kt == 0), stop=(kt == 7)), reads=[w_tok, xnT_tok[xb]], writes=[ps_tok[pb]])
                    S.op("act", lambda e, ct=ct, pb=pb, xb=xb: e.copy(out=uT[xb][:, ct, :], in_=ps[pb][:]),
                         reads=[ps_tok[pb]], writes=[uT_tok[xb]])
                if own:
                    S.op("dve", lambda e, sg=sg: e.tensor_tensor(out=selt[:], in0=Xst[:].rearrange("p r k s -> p (r k) s")[:, :, 0:16],
                                                                in1=segsel[:, sg:sg + 1, :].to_broadcast([128, 32, 16]), op=ALU.mult),
                         reads=[xst_tok, small_tok], writes=[xin_tok])
                    S.op("dve", lambda e: e.tensor_reduce(out=xin[:], in_=selt[:], axis=AX.X, op=ALU.add),
                         reads=[xin_tok], writes=[xin_tok])
                for ct in range(4):
                    yps = 4 + ct % 2
                    for kk in range(4):
                        k = ct * 4 + kk
                        pp = pcount % 2
                        pcount += 1
                        bur, bui = 2 * pp, 2 * pp + 1
                        bur, bui = (2, 3) if pp == 0 else (0, 1)
                        S.op("pe", lambda e: e.matmul(ps[bur][:], lhsT=bre[:, k, :], rhs=uT[xb][:, ct, :], start=True, stop=True),
                             reads=[bc_tok, uT_tok[xb]], writes=[ps_tok[bur]])
                        S.op("pe", lambda e: e.matmul(ps[bui][:], lhsT=bim[:, k, :], rhs=uT[xb][:, ct, :], start=True, stop=True),
                             reads=[bc_tok, uT_tok[xb]], writes=[ps_tok[bui]])
                        ck, sk = cosT[:, k, :], sinT[:, k, :]
                        S.op("dve", lambda e: e.tensor_tensor(out=tmp4[0][:], in0=ps[bur][:], in1=ck, op=ALU.mult),
                             reads=[ps_tok[bur], tab_tok], writes=[tmp4_tok[0]])
                        S.op("dve", lambda e: e.tensor_tensor(out=tmp4[1][:], in0=ps[bui][:], in1=sk, op=ALU.mult),
                             reads=[ps_tok[bui], tab_tok], writes=[tmp4_tok[1]])
                        S.op("pool", lambda e: e.tensor_tensor(out=Wr[pp][:], in0=tmp4[0][:], in1=tmp4[1][:], op=ALU.add),
                             reads=[tmp4_tok[0], tmp4_tok[1]], writes=[W_tok[pp]])
                        S.op("dve", lambda e: e.tensor_tensor(out=tmp4[2][:], in0=ps[bui][:], in1=ck, op=ALU.mult),
                             reads=[ps_tok[bui], tab_tok], writes=[tmp4_tok[2]])
                        S.op("dve", lambda e: e.tensor_tensor(out=tmp4[3][:], in0=ps[bur][:], in1=sk, op=ALU.mult),
                             reads=[ps_tok[bur], tab_tok], writes=[tmp4_tok[3]])
                        S.op("pool", lambda e: e.tensor_tensor(out=Wi[pp][:], in0=tmp4[2][:], in1=tmp4[3][:], op=ALU.subtract),
                             reads=[tmp4_tok[2], tmp4_tok[3]], writes=[W_tok[pp]])
                        if own:
                            ir, ii = xin[:, k:k + 1], xin[:, 16 + k:16 + k + 1]
                            itoks = [xin_tok]
                        else:
                            ir, ii = Xst[:, 2 * k, sg:sg + 1], Xst[:, 2 * k + 1, sg:sg + 1]
                            itoks = [xst_tok]
                        rb = P["rho"][:, k:k + 1].to_broadcast([128, SEGT])
                        S.op("dve", lambda e: e.tensor_tensor_scan(out=Vr[pp][:], data0=rb, data1=Wr[pp][:], initial=ir,
                                                                   op0=ALU.mult, op1=ALU.add),
                             reads=[W_tok[pp], p_tok] + itoks, writes=[V_tok[pp]])
                        S.op("dve", lambda e: e.tensor_tensor_scan(out=Vi[pp][:], data0=rb, data1=Wi[pp][:], initial=ii,
                                                                   op0=ALU.mult, op1=ALU.add),
                             reads=[W_tok[pp], p_tok] + itoks, writes=[V_tok[pp]])
                        if not own:
                            cl, sl_ = cosT[:, k, SEGT - 1:SEGT], sinT[:, k, SEGT - 1:SEGT]
                            vr, vi = Vr[pp][:, SEGT - 1:SEGT], Vi[pp][:, SEGT - 1:SEGT]
                            S.op("dve", lambda e: e.tensor_tensor(out=rot[:, 0:1], in0=vi, in1=sl_, op=ALU.mult),
                                 reads=[V_tok[pp], tab_tok], writes=[rot_tok])
                            S.op("dve", lambda e: e.scalar_tensor_tensor(out=Xst[:, 2 * k, sg + 1:sg + 2], in0=vr, scalar=cl, in1=rot[:, 0:1],
                                                                         op0=ALU.mult, op1=ALU.subtract),
                                 reads=[V_tok[pp], tab_tok, rot_tok], writes=[xst_tok])
                            S.op("dve", lambda e: e.tensor_tensor(out=rot[:, 1:2], in0=vr, in1=sl_, op=ALU.mult),
                                 reads=[V_tok[pp], tab_tok], writes=[rot_tok])
                            S.op("dve", lambda e: e.scalar_tensor_tensor(out=Xst[:, 2 * k + 1, sg + 1:sg + 2], in0=vi, scalar=cl, in1=rot[:, 1:2],
                                                                         op0=ALU.mult, op1=ALU.add),
                                 reads=[V_tok[pp], tab_tok, rot_tok], writes=[xst_tok])
                        else:
                            S.op("dve", lambda e: e.tensor_tensor(out=tmp4[0][:], in0=Vr[pp][:], in1=ck, op=ALU.mult),
                                 reads=[V_tok[pp], tab_tok], writes=[tmp4_tok[0]])
                            S.op("pool", lambda e: e.tensor_tensor(out=tmp4[1][:], in0=Vi[pp][:], in1=sk, op=ALU.mult),
                                 reads=[V_tok[pp], tab_tok], writes=[tmp4_tok[1]])
                            S.op("pool", lambda e: e.tensor_tensor(out=Xb[pp][:, 0, :], in0=tmp4[0][:], in1=tmp4[1][:], op=ALU.subtract),
                                 reads=[tmp4_tok[0], tmp4_tok[1]], writes=[Xb_tok[pp]])
                            S.op("dve", lambda e: e.tensor_tensor(out=tmp4[2][:], in0=Vr[pp][:], in1=sk, op=ALU.mult),
                                 reads=[V_tok[pp], tab_tok], writes=[tmp4_tok[2]])
                            S.op("pool", lambda e: e.tensor_tensor(out=tmp4[3][:], in0=Vi[pp][:], in1=ck, op=ALU.mult),
                                 reads=[V_tok[pp], tab_tok], writes=[tmp4_tok[3]])
                            S.op("pool", lambda e: e.tensor_tensor(out=Xb[pp][:, 1, :], in0=tmp4[2][:], in1=tmp4[3][:], op=ALU.add),
                                 reads=[tmp4_tok[2], tmp4_tok[3]], writes=[Xb_tok[pp]])
                            S.op("pe", lambda e: e.matmul(ps[yps][:], lhsT=cre[:, k, :], rhs=Xb[pp][:, 0, :], start=(kk == 0), stop=False),
                                 reads=[bc_tok, Xb_tok[pp]], writes=[ps_tok[yps]])
                            S.op("pe", lambda e: e.matmul(ps[yps][:], lhsT=ncim[:, k, :], rhs=Xb[pp][:, 1, :], start=False, stop=False),
                                 reads=[bc_tok, Xb_tok[pp]], writes=[ps_tok[yps]])
                    if own:
                        S.op("pe", lambda e: e.matmul(ps[yps][:], lhsT=diagD[:, ct, :], rhs=uT[xb][:, ct, :], start=False, stop=True),
                             reads=[w_tok, uT_tok[xb]], writes=[ps_tok[yps]])
                        S.op("act", lambda e: e.activation(out=g1[:], in_=ps[yps][:], func=AF.Square), reads=[ps_tok[yps]], writes=[g_tok])
                        S.op("dve", lambda e: e.tensor_scalar(g1[:], g1[:], 0.044715, 1.0, ALU.mult, ALU.add), reads=[g_tok], writes=[g_tok])
                        S.op("dve", lambda e: e.tensor_tensor(out=g1[:], in0=ps[yps][:], in1=g1[:], op=ALU.mult),
                             reads=[g_tok, ps_tok[yps]], writes=[g_tok])
                        S.op("act", lambda e: e.activation(out=g2[:], in_=g1[:], func=AF.Sigmoid, scale=1.5957691216057308),
                             reads=[g_tok], writes=[g_tok])
                        S.op("dve", lambda e, ct=ct: e.tensor_tensor(out=yb[:, ct, :], in0=ps[yps][:], in1=g2[:], op=ALU.mult),
                             reads=[g_tok, ps_tok[yps]], writes=[y_tok[ct]])
                if own:
                    for c2 in range(4):
                        gp = 4 + c2 % 2
                        for kt in range(4):
                            S.op("pe", lambda e, kt=kt, c2=c2, gp=gp: e.matmul(ps[gp][:], lhsT=wglu[:, kt, c2 * 128:(c2 + 1) * 128],
                                                                              rhs=yb[:, kt, :], start=(kt == 0), stop=(kt == 3)),
                                 reads=[w_tok] + y_tok, writes=[ps_tok[gp]])
                        S.op("act", lambda e, c2=c2, gp=gp: e.activation(out=g2[:], in_=ps[gp][:], func=AF.Sigmoid, bias=bglu[:, c2:c2 + 1]),
                             reads=[ps_tok[gp], small_tok], writes=[g_tok])
                        S.op("dve", lambda e, c2=c2, sg=sg: e.tensor_tensor(out=ssmT[:, c2, sg * SEGT:(sg + 1) * SEGT], in0=yb[:, c2, :],
                                                                           in1=g2[:], op=ALU.mult),
                             reads=[g_tok, y_tok[c2]], writes=[ssm_tok[sg]])
            S.barrier()
        S.barrier()


def _rms_rstd(C, S, src_ap, src_tok, junk_ap, junk_tok, ss, ss_tok, rstd, rstd_tok, n=D):
    S.op("act", lambda e: e.activation(out=junk_ap, in_=src_ap, func=AF.Square, accum_out=ss[:]),
         reads=[src_tok], writes=[junk_tok, ss_tok])
    S.op("act", lambda e: e.activation(out=rstd[:], in_=ss[:], func=AF.Ln, bias=C.eps_col[:], scale=1.0 / n),
         reads=[ss_tok, C.const_tok], writes=[rstd_tok])
    S.op("act", lambda e: e.activation(out=rstd[:], in_=rstd[:], func=AF.Exp, scale=-0.5),
         reads=[rstd_tok], writes=[rstd_tok])


def _load_cast(S, nc, dst, src, gcol, gtok, stg, stg_tok, stg_ch, dst_tok, nkt, ncols, cnt):
    for kt in range(nkt):
        for c0 in range(0, ncols, 512):
            sl = cnt[0] % 2
            cnt[0] += 1
            w = min(512, ncols - c0)
            S.dma("sp", stg_ch[sl], stg[sl][:, 0:w], src[:, kt, c0:c0 + w], writes=[stg_tok[sl]])
            eng = "dve" if sl == 0 else "pool"
            if gcol is not None:
                S.op(eng, lambda e, kt=kt, c0=c0, w=w, sl=sl: e.tensor_scalar(dst[:, kt, c0:c0 + w], stg[sl][:, 0:w],
                                                                            gcol[:, kt:kt + 1], None, ALU.mult),
                     reads=[stg_tok[sl], gtok], writes=[dst_tok])
            else:
                S.op(eng, lambda e, kt=kt, c0=c0, w=w, sl=sl: e.tensor_copy(out=dst[:, kt, c0:c0 + w], in_=stg[sl][:, 0:w]),
                     reads=[stg_tok[sl]], writes=[dst_tok])


def phase_post(C, nc, S, ps, ps_tok, I, attnT, attn_tok, ssmT, ssm_tok, y, dbg):
    NT = NOWN * 4
    h2buf = nc.dram_tensor("h2buf", [NOWN * SEGT, D], F32, kind="Internal").ap()
    h2_tok = [Tok() for _ in range(NT)]
    alla = [t for row in attn_tok for t in row]
    bank = [0]

    def nbk():
        b = bank[0] % 6
        bank[0] += 1
        return b

    uvbf = nc.dram_tensor("uvbf", [16384, 2048], BF16, kind="Internal").ap()
    uv_toks = [Tok() for _ in range(32)]
    if dbg not in ("h1", "h2"):
        with ExitStack() as e2:
            cin = [_sb(nc, e2, "cin%d" % i, [128, 8, D], F32) for i in range(2)]
            cin_tok = [Tok(), Tok()]
            cin_ch = [S.chan(), S.chan()]
            cout = [_sb(nc, e2, "cout%d" % i, [128, 8, D], BF16) for i in range(2)]
            cout_tok = [[Tok() for _ in range(8)] for _ in range(2)]
            cout_ch = [S.chan(), S.chan()]
            ci = 0
            engs = ("act", "dve", "pool", "dve", "act", "dve", "act", "dve")
            for tbl, src in enumerate((I["peer_u"], I["peer_v"])):
                for chk in range(16):
                    sl = ci % 2
                    r0 = chk * 1024
                    S.dma("sp", cin_ch[sl], cin[sl][:], src[r0:r0 + 1024, :].rearrange("(p a) d -> p a d", a=8),
                          writes=[cin_tok[sl]])
                    for a in range(8):
                        if engs[a] == "act":
                            S.op("act", lambda e, a=a, sl=sl: e.copy(out=cout[sl][:, a, :], in_=cin[sl][:, a, :]),
                                 reads=[cin_tok[sl]], writes=[cout_tok[sl][a]])
                        else:
                            S.op(engs[a], lambda e, a=a, sl=sl: e.tensor_copy(out=cout[sl][:, a, :], in_=cin[sl][:, a, :]),
                                 reads=[cin_tok[sl]], writes=[cout_tok[sl][a]])
                    S.dma("sp", cout_ch[sl],
                          uvbf[r0:r0 + 1024, tbl * 1024:(tbl + 1) * 1024].rearrange("(p a) d -> p a d", a=8), cout[sl][:],
                          reads=cout_tok[sl], writes=[uv_toks[ci]])
                    ci += 1
            S.barrier()

    with ExitStack() as e2:
        wo = _sb(nc, e2, "wo", [128, 8, D], BF16)
        wxq = _sb(nc, e2, "wxq", [128, 8, D], BF16)
        wxo = _sb(nc, e2, "wxo", [128, 8, D], BF16)
        KmT = _sb(nc, e2, "KmT", [128, 8, 256], BF16)
        Vm = _sb(nc, e2, "Vm", [128, 2, D], BF16)
        w_tok = Tok()
        kv_tok = Tok()
        gcols = _sb(nc, e2, "gcols", [128, 3, 8], F32)
        g_tok = Tok()
        chg = S.chan()
        S.dma("sp", chg, gcols[:, 0, :], I["g_out"][:, :], writes=[g_tok])
        S.dma("sp", chg, gcols[:, 1, :], I["g_xattn"][:, :], writes=[g_tok])
        S.dma("sp", chg, gcols[:, 2, :], I["g_mem"][:, :], writes=[g_tok])
        stg = [_sb(nc, e2, "pstg%d" % i, [128, 512], F32) for i in range(2)]
        stg_tok = [Tok(), Tok()]
        stg_ch = [S.chan(), S.chan()]
        cnt = [0]
        _load_cast(S, nc, wo, I["w_out"], gcols[:, 0, :], g_tok, stg, stg_tok, stg_ch, w_tok, 8, D, cnt)
        _load_cast(S, nc, wxq, I["w_xq"], gcols[:, 1, :], g_tok, stg, stg_tok, stg_ch, w_tok, 8, D, cnt)
        _load_cast(S, nc, wxo, I["w_xo"], None, None, stg, stg_tok, stg_ch, w_tok, 8, D, cnt)
        h = _sb(nc, e2, "h", [128, D], F32)
        h_tok = Tok()
        h_ch = S.chan()
        hn = _sb(nc, e2, "hn", [128, D], BF16)
        hn_tok = Tok()
        hnT = _sb(nc, e2, "hnT", [128, 8, 128], BF16)
        hnT_tok = Tok()
        ss = _sb(nc, e2, "pss", [128, 1], F32)
        ss_tok = Tok()
        rstd = _sb(nc, e2, "prstd", [128, 1], F32)
        rstd_tok = Tok()
        with ExitStack() as e3:
            memT = _sb(nc, e3, "memT", [128, 8, 256], BF16)
            memT_tok = Tok()
            wch = _sb(nc, e3, "wch", [128, 8, 512], BF16)
            wch_tok = Tok()
            for mt in range(2):
                S.dma("sp", h_ch, h[:], I["mem"][mt * 128:(mt + 1) * 128, :], writes=[h_tok])
                _rms_rstd(C, S, h[:], h_tok, hn[:], hn_tok, ss, ss_tok, rstd, rstd_tok)
                S.op("dve", lambda e: e.tensor_scalar(hn[:], h[:], rstd[:], None, ALU.mult), reads=[h_tok, rstd_tok], writes=[hn_tok])
                ptb, pt_tok = transpose_to(C, hn, hn_tok, None, None)
                S.op("act", lambda e, mt=mt, ptb=ptb: e.copy(out=memT[:, :, mt * 128:(mt + 1) * 128],
                                                           in_=ptb.rearrange("p (k t) -> p k t", k=8)),
                     reads=[pt_tok], writes=[memT_tok])
            for cc in range(4):
                _load_cast(S, nc, wch, I["w_xkv"][:, :, cc * 512:(cc + 1) * 512], gcols[:, 2, :], g_tok, stg, stg_tok, stg_ch,
                           wch_tok, 8, 512, cnt)
                if cc < 2:
                    for j4 in range(4):
                        b_ = nbk()
                        for kt in range(8):
                            S.op("pe", lambda e, kt=kt, j4=j4, b_=b_: e.matmul(ps[b_][:, 0:256], lhsT=wch[:, kt, j4 * 128:(j4 + 1) * 128],
                                                                             rhs=memT[:, kt, :], start=(kt == 0), stop=(kt == 7)),
                                 reads=[wch_tok, memT_tok], writes=[ps_tok[b_]])
                        S.op("act", lambda e, j4=j4, b_=b_, cc=cc: e.copy(out=KmT[:, cc * 4 + j4, :], in_=ps[b_][:, 0:256]),
                             reads=[ps_tok[b_]], writes=[kv_tok])
                else:
                    for mt in range(2):
                        b_ = nbk()
                        for kt in range(8):
                            S.op("pe", lambda e, kt=kt, mt=mt, b_=b_: e.matmul(ps[b_][:], lhsT=memT[:, kt, mt * 128:(mt + 1) * 128],
                                                                             rhs=wch[:, kt, :], start=(kt == 0), stop=(kt == 7)),
                                 reads=[wch_tok, memT_tok], writes=[ps_tok[b_]])
                        S.op("act", lambda e, mt=mt, b_=b_, cc=cc: e.copy(out=Vm[:, mt, (cc - 2) * 512:(cc - 1) * 512], in_=ps[b_][:]),
                             reads=[ps_tok[b_]], writes=[kv_tok])
            S.barrier()
        def mkset(tag):
            B = {}
            for nm, shp, dt_ in (("sq4", [128, 4, 128], BF16), ("rs2", [128, 2], F32), ("qT", [128, 8, 128], BF16),
                                 ("pp", [128, 4, 256], BF16), ("pT", [128, 8, 128], BF16), ("oT", [128, 8, 128], BF16),
                                 ("mx", [128, 4], F32), ("sm", [128, 4], F32), ("h", [128, D], F32), ("hn", [128, D], BF16),
                                 ("hnT", [128, 8, 128], BF16), ("ss", [128, 1], F32), ("rstd", [128, 1], F32)):
                B[nm] = _sb(nc, e2, nm + tag, shp, dt_)
            for nm in ("sq_tok", "rs2_tok", "qT_tok", "pp_tok", "pT_tok", "oT_tok", "sm_tok", "h_tok", "hn_tok", "hnT_tok",
                       "ss_tok", "rstd_tok"):
                B[nm] = Tok()
            B["h_ch"] = S.chan()
            return B

        class _Rec1:
            def __init__(self):
                self.items = []

            def op(self, *a, **k):
                self.items.append(("op", a, k))

            def dma(self, *a, **k):
                self.items.append(("dma", a, k))

        def emit_tile(S_, tile, nbk_, sq4, rs2, qT, pp, pT, oT, mx, sm, h, hn, hnT, ss, rstd, sq_tok, rs2_tok, qT_tok, pp_tok,
                      pT_tok, oT_tok, sm_tok, h_tok, hn_tok, hnT_tok, ss_tok, rstd_tok, h_ch):
            saved = C.S
            C.S = S_
            t0 = tile * 128
            slot = tile // 4
            tsl = slice(t0, t0 + 128)
            for which, (src, toks) in enumerate(((attnT, attn_tok[slot]), (ssmT, [ssm_tok[slot]]))):
                S_.op("act", lambda e, src=src: e.activation(out=sq4[:], in_=src[:, :, tsl], func=AF.Square),
                     reads=toks, writes=[sq_tok])
                b_ = nbk_()
                for hp in range(4):
                    S_.op("pe", lambda e, hp=hp, b_=b_: e.matmul(ps[b_][:, 0:1], lhsT=sq4[:, hp, :], rhs=C.ones_bf[:, 0:1],
                                                                start=(hp == 0), stop=(hp == 3)),
                         reads=[sq_tok, C.const_tok], writes=[ps_tok[b_]])
                S_.op("act", lambda e, which=which, b_=b_: e.activation(out=rs2[:, which:which + 1], in_=ps[b_][:, 0:1], func=AF.Ln,
                                                                       bias=C.eps_col[:], scale=1.0 / 512),
                     reads=[ps_tok[b_], C.const_tok], writes=[rs2_tok])
            S_.op("act", lambda e: e.activation(out=rs2[:], in_=rs2[:], func=AF.Exp, scale=-0.5), reads=[rs2_tok], writes=[rs2_tok])
            S_.dma("sp", h_ch, h[:], I["x_own"][t0:t0 + 128, :], writes=[h_tok])
            for which, (src, toks) in enumerate(((attnT, attn_tok[slot]), (ssmT, [ssm_tok[slot]]))):
                for n2 in range(2):
                    b_ = nbk_()
                    for hp in range(4):
                        S_.op("pe", lambda e, hp=hp, b_=b_, src=src, which=which, n2=n2: e.matmul(
                            ps[b_][:], lhsT=src[:, hp, tsl], rhs=wo[:, which * 4 + hp, n2 * 512:(n2 + 1) * 512],
                            start=(hp == 0), stop=(hp == 3)), reads=toks + [w_tok], writes=[ps_tok[b_]])
                    S_.op("dve", lambda e, b_=b_, which=which, n2=n2: e.scalar_tensor_tensor(
                        out=h[:, n2 * 512:(n2 + 1) * 512], in0=ps[b_][:], scalar=rs2[:, which:which + 1],
                        in1=h[:, n2 * 512:(n2 + 1) * 512], op0=ALU.mult, op1=ALU.add),
                        reads=[ps_tok[b_], rs2_tok], writes=[h_tok])
            if dbg == "h1":
                S_.dma("sp", h_ch, y[t0:t0 + 128, :], h[:], reads=[h_tok], writes=[h2_tok[tile]])
                C.S = saved
                return
            _rms_rstd(C, S_, h[:], h_tok, hn[:], hn_tok, ss, ss_tok, rstd, rstd_tok)
            S_.op("dve", lambda e: e.tensor_scalar(hn[:], h[:], rstd[:], None, ALU.mult), reads=[h_tok, rstd_tok], writes=[hn_tok])
            ptb, pt_tok = transpose_to(C, hn, hn_tok, None, None)
            S_.op("act", lambda e, ptb=ptb: e.copy(out=hnT[:], in_=ptb.rearrange("p (k t) -> p k t", k=8)),
                 reads=[pt_tok], writes=[hnT_tok])
            for half in range(2):
                b_ = nbk_()
                for j4 in range(4):
                    hc = half * 4 + j4
                    for kt in range(8):
                        S_.op("pe", lambda e, kt=kt, hc=hc, j4=j4, b_=b_: e.matmul(
                            ps[b_][:, j4 * 128:(j4 + 1) * 128], lhsT=wxq[:, kt, hc * 128:(hc + 1) * 128], rhs=hnT[:, kt, :],
                            start=(kt == 0), stop=(kt == 7)), reads=[w_tok, hnT_tok], writes=[ps_tok[b_]])
                S_.op("act", lambda e, half=half, b_=b_: e.mul(out=qT[:, half * 4:(half + 1) * 4, :],
                                                             in_=ps[b_][:].rearrange("p (a b) -> p a b", a=4), mul=0.0625),
                     reads=[ps_tok[b_]], writes=[qT_tok])
            sb_ = [nbk_(), nbk_()]
            for hh in range(4):
                b_ = sb_[hh // 2]
                for c2 in range(2):
                    S_.op("pe", lambda e, hh=hh, c2=c2, b_=b_: e.matmul(
                        ps[b_][:, (hh % 2) * 256:(hh % 2 + 1) * 256], lhsT=qT[:, 2 * hh + c2, :], rhs=KmT[:, 2 * hh + c2, :],
                        start=(c2 == 0), stop=(c2 == 1)), reads=[qT_tok, kv_tok], writes=[ps_tok[b_]])
            for i2 in range(2):
                b_ = sb_[i2]
                S_.op("dve", lambda e, i2=i2, b_=b_: e.tensor_reduce(out=mx[:, 2 * i2:2 * i2 + 2],
                                                                    in_=ps[b_][:].rearrange("p (a b) -> p a b", a=2),
                                                                    axis=AX.X, op=ALU.max),
                     reads=[ps_tok[b_]], writes=[sm_tok])
            S_.op("dve", lambda e: e.tensor_scalar(mx[:], mx[:], -1.0, None, ALU.mult), reads=[sm_tok], writes=[sm_tok])
            for hh in range(4):
                b_ = sb_[hh // 2]
                S_.op("act", lambda e, hh=hh, b_=b_: e.activation(out=pp[:, hh, :], in_=ps[b_][:, (hh % 2) * 256:(hh % 2 + 1) * 256],
                                                                 func=AF.Exp, bias=mx[:, hh:hh + 1], accum_out=sm[:, hh:hh + 1]),
                     reads=[ps_tok[b_], sm_tok], writes=[pp_tok, sm_tok])
            S_.op("dve", lambda e: e.reciprocal(out=sm[:], in_=sm[:]), reads=[sm_tok], writes=[sm_tok])
            for hh in range(4):
                S_.op("dve", lambda e, hh=hh: e.tensor_scalar(pp[:, hh, :], pp[:, hh, :], sm[:, hh:hh + 1], None, ALU.mult),
                     reads=[sm_tok, pp_tok], writes=[pp_tok])
            ptb, pt_tok = transpose_to(C, pp[:].rearrange("p a b -> p (a b)"), pp_tok, None, None)
            S_.op("act", lambda e, ptb=ptb: e.copy(out=pT[:], in_=ptb.rearrange("p (k t) -> p k t", k=8)),
                 reads=[pt_tok], writes=[pT_tok])
            for half in range(2):
                b_ = nbk_()
                for j4 in range(4):
                    hc = half * 4 + j4
                    hh, c2 = hc // 2, hc % 2
                    for mt in range(2):
                        S_.op("pe", lambda e, mt=mt, hh=hh, c2=c2, j4=j4, b_=b_: e.matmul(
                            ps[b_][:, j4 * 128:(j4 + 1) * 128], lhsT=Vm[:, mt, hh * 256 + c2 * 128:hh * 256 + (c2 + 1) * 128],
                            rhs=pT[:, 2 * hh + mt, :], start=(mt == 0), stop=(mt == 1)),
                            reads=[kv_tok, pT_tok], writes=[ps_tok[b_]])
                S_.op("act", lambda e, half=half, b_=b_: e.copy(out=oT[:, half * 4:(half + 1) * 4, :],
                                                              in_=ps[b_][:].rearrange("p (a b) -> p a b", a=4)),
                     reads=[ps_tok[b_]], writes=[oT_tok])
            for n2 in range(2):
                b_ = nbk_()
                for hc in range(8):
                    S_.op("pe", lambda e, hc=hc, n2=n2, b_=b_: e.matmul(ps[b_][:], lhsT=oT[:, hc, :], rhs=wxo[:, hc, n2 * 512:(n2 + 1) * 512],
                                                                      start=(hc == 0), stop=(hc == 7)),
                         reads=[oT_tok, w_tok], writes=[ps_tok[b_]])
                S_.op("dve", lambda e, n2=n2, b_=b_: e.tensor_tensor(out=h[:, n2 * 512:(n2 + 1) * 512], in0=ps[b_][:],
                                                                    in1=h[:, n2 * 512:(n2 + 1) * 512], op=ALU.add),
                     reads=[ps_tok[b_]], writes=[h_tok])
            dst = y if dbg == "h2" else h2buf
            S_.dma("sp", h_ch, dst[t0:t0 + 128, :], h[:], reads=[h_tok], writes=[h2_tok[tile]])

            C.S = saved

        sets1 = [mkset("_e"), mkset("_o")]
        bk = [[0], [0]]

        def mk_nbk(par):
            def f():
                b = 3 * par + bk[par][0] % 3
                bk[par][0] += 1
                return b
            return f

        for t2 in range(0, NT, 2):
            recs = []
            for par in range(2):
                r_ = _Rec1()
                C.tp_force = par
                emit_tile(r_, t2 + par, mk_nbk(par), **sets1[par])
                C.tp_force = None
                recs.append(r_.items)
            while recs[0] or recs[1]:
                for par in range(2):
                    for _ in range(6):
                        if recs[par]:
                            kind, a, k = recs[par].pop(0)
                            getattr(S, kind)(*a, **k)
        S.barrier()
    if dbg in ("h1", "h2"):
        S.wait_tok("sp", h2_tok)
        return

    with ExitStack() as e2:
        wpq = _sb(nc, e2, "wpq", [128, 8, 2048], BF16)
        skT = _sb(nc, e2, "skT_sb", [128, 16, 128], BF16)
        w_tok = Tok()
        gffn = _sb(nc, e2, "gffn", [128, 8], F32)
        gffn_rep = _sb(nc, e2, "gffn_rep_sb", [128, D], F32)
        gfin_rep = _sb(nc, e2, "gfin_rep_sb", [128, D], F32)
        iota256 = _sb(nc, e2, "iota256", [128, 256], F32)
        g_tok = Tok()
        chg = S.chan()
        S.dma("sp", chg, gffn[:], I["g_ffn"][:, :], writes=[g_tok])
        S.dma("sp", chg, gffn_rep[:], I["gffn_rep"][:, :], writes=[g_tok])
        S.dma("sp", chg, gfin_rep[:], I["gfin_rep"][:, :], writes=[g_tok])
        S.op("pool", lambda e: e.iota(iota256[:], pattern=[[1, 256]], base=0, channel_multiplier=0,
                                      allow_small_or_imprecise_dtypes=True), writes=[g_tok])
        stg = [_sb(nc, e2, "qstg%d" % i, [128, 512], F32) for i in range(2)]
        stg_tok = [Tok(), Tok()]
        stg_ch = [S.chan(), S.chan()]
        cnt = [0]
        _load_cast(S, nc, wpq, I["w_pq"], gffn, g_tok, stg, stg_tok, stg_ch, w_tok, 8, 2048, cnt)
        _load_cast(S, nc, skT, I["skT"], None, None, stg, stg_tok, stg_ch, w_tok, 16, 128, cnt)
        h = _sb(nc, e2, "h_b", [128, D], F32)
        h_tok = Tok()
        h_ch = S.chan()
        hn = _sb(nc, e2, "hn_b", [128, D], BF16)
        hn_tok = Tok()
        hnT = _sb(nc, e2, "hnT_b", [128, 8, 128], BF16)
        hnT_tok = Tok()
        hn3 = _sb(nc, e2, "hn3", [128, D], F32)
        hn3_tok = Tok()
        ss = _sb(nc, e2, "qss", [128, 1], F32)
        ss_tok = Tok()
        rstd = _sb(nc, e2, "qrstd", [128, 1], F32)
        rstd_tok = Tok()
        qpT = _sb(nc, e2, "qpT", [128, 16, 128], BF16)
        qpT_tok = Tok()
        sc = _sb(nc, e2, "sc", [128, 16, 128], F32)
        sc_tok = Tok()
        scr = _sb(nc, e2, "scr", [128, 2048], F32)
        scr_tok = Tok()
        hv = _sb(nc, e2, "hv", [128, 16, 16], F32)
        hi = _sb(nc, e2, "hi", [128, 16, 16], U32)
        hif = _sb(nc, e2, "hif", [128, 16, 16], F32)
        hv_tok = Tok()
        cand = _sb(nc, e2, "cand", [128, 8, 256], F32)
        eidx = _sb(nc, e2, "eidx", [128, 8, 256], F32)
        e0 = _sb(nc, e2, "e0", [128, 8, 16], F32)
        cand_tok = Tok()
        bv = _sb(nc, e2, "bv", [128, 8, 16], F32)
        bp = _sb(nc, e2, "bp", [128, 8, 16], U32)
        bpf = _sb(nc, e2, "bpf", [128, 8, 16], F32)
        bv_tok = Tok()
        junk = [_sb(nc, e2, "junk256_%d" % i, [128, 256], F32) for i in range(4)]
        junk_tok = [Tok() for _ in range(4)]
        eidc_tok = [Tok() for _ in range(128)]
        jq = [0]
        eidf = _sb(nc, e2, "eidf", [128, 128], F32)
        eid = _sb(nc, e2, "eid", [128, 128], U32)
        eid_tok = Tok()
        gt = _sb(nc, e2, "gt", [128, 8, 16], F32)
        gs = _sb(nc, e2, "gs", [128, 8], F32)
        nb0 = _sb(nc, e2, "nb0", [128, 8], F32)
        gt_tok = Tok()
        actc = _sb(nc, e2, "actc", [128, 128], F32)
        act_tok = Tok()
        wgt = _sb(nc, e2, "wgt", [128, 128], F32)
        wg2 = _sb(nc, e2, "wg2", [128, 128], F32)
        wgt_tok = Tok()
        NG = 8
        gbuf = [_sb(nc, e2, "gbuf%d" % i, [128, 2 * D], BF16) for i in range(NG)]
        gbuf_tok = [Tok() for _ in range(NG)]
        gbuf_ch = [S.chan() for _ in range(NG)]
        junk2 = [_sb(nc, e2, "junk2_%d" % i, [128, D], BF16) for i in range(2)]
        junk2_tok = [Tok(), Tok()]
        j2 = [0]
        NDG = 4
        dg = [_sb(nc, e2, "dg%d" % i, [128, 128], BF16) for i in range(NDG)]
        dg_tok = [Tok() for _ in range(NDG)]
        slot_tok = [Tok() for _ in range(128)]
        grp_tok = [Tok() for _ in range(32)]
        di = 0
        ytok = Tok()
        gi = 0
        PLAY_N = 15
        bankA = [0]

        def nbkA():
            b = bankA[0] % 4
            bankA[0] += 1
            return b

        class _Rec:
            def __init__(self):
                self.items = []

            def op(self, *a, **k):
                self.items.append(("op", a, k))

            def dma(self, *a, **k):
                self.items.append(("dma", a, k))

        junkF = _sb(nc, e2, "junkF", [128, D], BF16)
        junkF_tok = Tok()
        ss2 = _sb(nc, e2, "ss2", [128, 1], F32)
        ss2_tok = Tok()
        rstd2 = _sb(nc, e2, "rstd2", [128, 1], F32)
        rstd2_tok = Tok()
        obuf = _sb(nc, e2, "obuf", [128, D], F32)
        obuf_tok = Tok()
        o_ch = S.chan()
        h_b2 = _sb(nc, e2, "h_b2", [128, D], F32)
        hn3_b2 = _sb(nc, e2, "hn3_b2", [128, D], F32)
        eid_b2 = _sb(nc, e2, "eid_b2", [128, 128], U32)
        gt_b2 = _sb(nc, e2, "gt_b2", [128, 8, 16], F32)
        sets = [dict(h=h, h_tok=h_tok, h_ch=h_ch, hn3=hn3, hn3_tok=hn3_tok, eid=eid, eid_tok=eid_tok, gt=gt, gt_tok=gt_tok),
                dict(h=h_b2, h_tok=Tok(), h_ch=S.chan(), hn3=hn3_b2, hn3_tok=Tok(), eid=eid_b2, eid_tok=Tok(), gt=gt_b2, gt_tok=Tok())]

        def emitA(S_, tile, h, h_tok, h_ch, hn3, hn3_tok, eid, eid_tok, gt, gt_tok):
            saved = C.S
            C.S = S_
            t0 = tile * 128
            S_.dma("sp", h_ch, h[:], h2buf[t0:t0 + 128, :], reads=[h2_tok[tile]], writes=[h_tok])
            _rms_rstd(C, S_, h[:], h_tok, hn[:], hn_tok, ss, ss_tok, rstd, rstd_tok)
            S_.op("dve", lambda e: e.tensor_scalar(hn[:], h[:], rstd[:], None, ALU.mult), reads=[h_tok, rstd_tok], writes=[hn_tok])
            S_.op("dve", lambda e: e.scalar_tensor_tensor(out=hn3[:], in0=h[:], scalar=rstd[:], in1=gffn_rep[:], op0=ALU.mult, op1=ALU.mult),
                 reads=[h_tok, rstd_tok, g_tok], writes=[hn3_tok])
            ptb, pt_tok = transpose_to(C, hn, hn_tok, None, None)
            S_.op("act", lambda e, ptb=ptb: e.copy(out=hnT[:], in_=ptb.rearrange("p (k t) -> p k t", k=8)),
                 reads=[pt_tok], writes=[hnT_tok])
            for q4 in range(4):
                b_ = nbkA()
                for j4 in range(4):
                    ch = q4 * 4 + j4
                    for kt in range(8):
                        S_.op("pe", lambda e, kt=kt, ch=ch, j4=j4, b_=b_: e.matmul(
                            ps[b_][:, j4 * 128:(j4 + 1) * 128], lhsT=wpq[:, kt, ch * 128:(ch + 1) * 128], rhs=hnT[:, kt, :],
                            start=(kt == 0), stop=(kt == 7)), reads=[w_tok, hnT_tok], writes=[ps_tok[b_]])
                S_.op("act", lambda e, q4=q4, b_=b_: e.copy(out=qpT[:, q4 * 4:(q4 + 1) * 4, :],
                                                          in_=ps[b_][:].rearrange("p (a b) -> p a b", a=4)),
                     reads=[ps_tok[b_]], writes=[qpT_tok])
            for q4 in range(4):
                b_ = nbkA()
                for j4 in range(4):
                    ch = q4 * 4 + j4
                    S_.op("pe", lambda e, ch=ch, j4=j4, b_=b_: e.matmul(ps[b_][:, j4 * 128:(j4 + 1) * 128], lhsT=qpT[:, ch, :],
                                                                      rhs=skT[:, ch, :], start=True, stop=True),
                         reads=[w_tok, qpT_tok], writes=[ps_tok[b_]])
                S_.op("act", lambda e, q4=q4, b_=b_: e.copy(out=sc[:, q4 * 4:(q4 + 1) * 4, :],
                                                          in_=ps[b_][:].rearrange("p (a b) -> p a b", a=4)),
                     reads=[ps_tok[b_]], writes=[sc_tok])
            scr3 = scr[:].rearrange("p (a b) -> p a b", a=16)
            for ch in range(16):
                S_.op("dve", lambda e, ch=ch: e.max(out=hv[:, ch, 0:8], in_=sc[:, ch, :]), reads=[sc_tok], writes=[hv_tok])
                S_.op("dve", lambda e, ch=ch: e.max_index(out=hi[:, ch, 0:8], in_max=hv[:, ch, 0:8], in_values=sc[:, ch, :]),
                     reads=[sc_tok, hv_tok], writes=[hv_tok])
                S_.op("dve", lambda e, ch=ch: e.match_replace(out=scr3[:, ch, :], in_to_replace=hv[:, ch, 0:8], in_values=sc[:, ch, :],
                                                             imm_value=NEG), reads=[sc_tok, hv_tok], writes=[scr_tok])
                S_.op("dve", lambda e, ch=ch: e.max(out=hv[:, ch, 8:16], in_=scr3[:, ch, :]), reads=[scr_tok], writes=[hv_tok])
                S_.op("dve", lambda e, ch=ch: e.max_index(out=hi[:, ch, 8:16], in_max=hv[:, ch, 8:16], in_values=scr3[:, ch, :]),
                     reads=[scr_tok, hv_tok], writes=[hv_tok])
            S_.op("dve", lambda e: e.tensor_copy(out=hif[:], in_=hi[:]), reads=[hv_tok], writes=[hv_tok])
            hv4 = hv[:].rearrange("p (h i) k -> p h i k", i=2)
            hif4 = hif[:].rearrange("p (h i) k -> p h i k", i=2)
            cand4 = cand[:].rearrange("p h (a b) -> p h a b", a=16)
            eidx4 = eidx[:].rearrange("p h (a b) -> p h a b", a=16)
            S_.op("dve", lambda e: e.tensor_tensor(out=cand4, in0=hv4[:, :, 0, :].unsqueeze(3).to_broadcast([128, 8, 16, 16]),
                                                  in1=hv4[:, :, 1, :].unsqueeze(2).to_broadcast([128, 8, 16, 16]), op=ALU.add),
                 reads=[hv_tok], writes=[cand_tok])
            S_.op("dve", lambda e: e.tensor_scalar(e0[:], hif4[:, :, 0, :], 128.0, None, ALU.mult), reads=[hv_tok], writes=[cand_tok])
            S_.op("dve", lambda e: e.tensor_tensor(out=eidx4, in0=e0[:].unsqueeze(3).to_broadcast([128, 8, 16, 16]),
                                                  in1=hif4[:, :, 1, :].unsqueeze(2).to_broadcast([128, 8, 16, 16]), op=ALU.add),
                 reads=[hv_tok, cand_tok], writes=[cand_tok])
            scr8 = scr[:].rearrange("p (a b) -> p a b", a=8)
            for hh in range(8):
                S_.op("dve", lambda e, hh=hh: e.max(out=bv[:, hh, 0:8], in_=cand[:, hh, :]), reads=[cand_tok], writes=[bv_tok])
                S_.op("dve", lambda e, hh=hh: e.max_index(out=bp[:, hh, 0:8], in_max=bv[:, hh, 0:8], in_values=cand[:, hh, :]),
                     reads=[cand_tok, bv_tok], writes=[bv_tok])
                S_.op("dve", lambda e, hh=hh: e.match_replace(out=scr8[:, hh, :], in_to_replace=bv[:, hh, 0:8], in_values=cand[:, hh, :],
                                                             imm_value=NEG), reads=[cand_tok, bv_tok], writes=[scr_tok])
                S_.op("dve", lambda e, hh=hh: e.max(out=bv[:, hh, 8:16], in_=scr8[:, hh, :]), reads=[scr_tok], writes=[bv_tok])
                S_.op("dve", lambda e, hh=hh: e.max_index(out=bp[:, hh, 8:16], in_max=bv[:, hh, 8:16], in_values=scr8[:, hh, :]),
                     reads=[scr_tok, bv_tok], writes=[bv_tok])
            S_.op("dve", lambda e: e.tensor_copy(out=bpf[:], in_=bp[:]), reads=[bv_tok], writes=[bv_tok])
            for hh in range(8):
                for k in range(16):
                    s_ = hh * 16 + k
                    jb = jq[0] % 4
                    jq[0] += 1
                    S_.op("dve", lambda e, hh=hh, k=k, s_=s_, jb=jb: e.scalar_tensor_tensor(
                        out=junk[jb][:], in0=iota256[:], scalar=bpf[:, hh, k:k + 1], in1=eidx[:, hh, :],
                        op0=ALU.is_equal, op1=ALU.mult, accum_out=eidf[:, s_:s_ + 1]),
                        reads=[bv_tok, cand_tok, g_tok], writes=[junk_tok[jb], eidc_tok[s_]])
            S_.op("dve", lambda e: e.tensor_scalar(eidf[:], eidf[:], 16383.0, 0.0, ALU.min, ALU.max), reads=[eid_tok] + eidc_tok,
                  writes=[eid_tok] + eidc_tok)
            S_.op("dve", lambda e: e.tensor_copy(out=eid[:], in_=eidf[:]), reads=[eid_tok], writes=[eid_tok])
            S_.op("dve", lambda e: e.tensor_scalar(nb0[:], bv[:, :, 0], -1.0, None, ALU.mult), reads=[bv_tok], writes=[gt_tok])
            for hh in range(8):
                S_.op("act", lambda e, hh=hh: e.activation(out=gt[:, hh, :], in_=bv[:, hh, :], func=AF.Exp, bias=nb0[:, hh:hh + 1],
                                                          accum_out=gs[:, hh:hh + 1]), reads=[bv_tok, gt_tok], writes=[gt_tok])
            S_.op("dve", lambda e: e.reciprocal(out=gs[:], in_=gs[:]), reads=[gt_tok], writes=[gt_tok])
            S_.op("dve", lambda e: e.tensor_tensor(out=gt[:], in0=gt[:], in1=gs[:].unsqueeze(2).to_broadcast([128, 8, 16]), op=ALU.mult),
                 reads=[gt_tok], writes=[gt_tok])

            C.S = saved

        def emitB(tile, play, h, h_tok, h_ch, hn3, hn3_tok, eid, eid_tok, gt, gt_tok):
            nonlocal gi, di
            t0 = tile * 128
            pa, pb_ = 4, 5
            gtf = gt[:].rearrange("p a b -> p (a b)")
            for grp in range(32):
                used = []
                for q in range(4):
                    s_ = grp * 4 + q
                    g_ = gi % NG
                    gi += 1
                    used.append(g_)
                    S.dma("pool", gbuf_ch[g_], gbuf[g_][:], uvbf[:, :], reads=[eid_tok] + uv_toks, writes=[gbuf_tok[g_]],
                          indirect=bass.IndirectOffsetOnAxis(ap=eid[:, s_:s_ + 1], axis=0))
                    jb = j2[0] % 2
                    j2[0] += 1
                    S.op("dve", lambda e, g_=g_, s_=s_, jb=jb: e.scalar_tensor_tensor(out=junk2[jb][:], in0=gbuf[g_][:, 0:D], scalar=1.0, in1=hn3[:],
                                                                                     op0=ALU.mult, op1=ALU.mult, accum_out=actc[:, s_:s_ + 1]),
                         reads=[gbuf_tok[g_], hn3_tok], writes=[junk2_tok[jb], slot_tok[s_]])
                cs = slice(grp * 4, grp * 4 + 4)
                gk = grp_tok[grp]
                S.op("dve", lambda e: e.tensor_tensor(out=wg2[:, cs], in0=actc[:, cs], in1=actc[:, cs], op=ALU.mult),
                     reads=[slot_tok[grp * 4 + q] for q in range(4)], writes=[gk])
                S.op("dve", lambda e: e.tensor_scalar(wg2[:, cs], wg2[:, cs], 0.044715, 1.0, ALU.mult, ALU.add), reads=[gk], writes=[gk])
                S.op("dve", lambda e: e.tensor_tensor(out=wg2[:, cs], in0=wg2[:, cs], in1=actc[:, cs], op=ALU.mult),
                     reads=[gk] + [slot_tok[grp * 4 + q] for q in range(4)], writes=[gk])
                S.op("act", lambda e: e.activation(out=wg2[:, cs], in_=wg2[:, cs], func=AF.Sigmoid, scale=1.5957691216057308),
                     reads=[gk], writes=[gk])
                S.op("dve", lambda e: e.tensor_tensor(out=wgt[:, cs], in0=wg2[:, cs], in1=actc[:, cs], op=ALU.mult),
                     reads=[gk] + [slot_tok[grp * 4 + q] for q in range(4)], writes=[gk])
                S.op("dve", lambda e: e.tensor_tensor(out=wgt[:, cs], in0=wgt[:, cs], in1=gtf[:, cs], op=ALU.mult),
                     reads=[gk, gt_tok], writes=[gk])
                for q in range(4):
                    s_ = grp * 4 + q
                    g_ = used[q]
                    d_ = di % NDG
                    di += 1
                    S.op("act", lambda e, d_=d_, s_=s_: e.activation(out=dg[d_][:], in_=C.ident_bf[:], func=AF.Copy,
                                                                    scale=wgt[:, s_:s_ + 1]),
                         reads=[gk, C.const_tok], writes=[dg_tok[d_]])
                    for n2, pbk in enumerate((pa, pb_)):
                        S.op("pe", lambda e, d_=d_, g_=g_, n2=n2, pbk=pbk, s_=s_: e.matmul(
                            ps[pbk][:], lhsT=dg[d_][:], rhs=gbuf[g_][:, D + n2 * 512:D + (n2 + 1) * 512],
                            start=(s_ == 0), stop=(s_ == 127)), reads=[dg_tok[d_], gbuf_tok[g_]], writes=[ps_tok[pbk]])
                play(PLAY_N)
            for n2, pbk in enumerate((pa, pb_)):
                S.op("dve", lambda e, n2=n2, pbk=pbk: e.tensor_tensor(out=h[:, n2 * 512:(n2 + 1) * 512], in0=ps[pbk][:],
                                                                      in1=h[:, n2 * 512:(n2 + 1) * 512], op=ALU.add),
                     reads=[ps_tok[pbk]], writes=[h_tok])
            if dbg == "h3":
                S.dma("sp", h_ch, y[t0:t0 + 128, :], h[:], reads=[h_tok], writes=[ytok])
                play(10 ** 9)
                return
            _rms_rstd(C, S, h[:], h_tok, junkF[:], junkF_tok, ss2, ss2_tok, rstd2, rstd2_tok)
            S.op("dve", lambda e: e.scalar_tensor_tensor(out=obuf[:], in0=h[:], scalar=rstd2[:], in1=gfin_rep[:], op0=ALU.mult, op1=ALU.mult),
                 reads=[h_tok, rstd2_tok, g_tok], writes=[obuf_tok])
            S.dma("sp", o_ch, y[t0:t0 + 128, :], obuf[:], reads=[obuf_tok], writes=[ytok])
            play(10 ** 9)

        rec = _Rec()
        emitA(rec, 0, **sets[0])
        for kind, a, k in rec.items:
            getattr(S, kind)(*a, **k)
        for tile in range(NT):
            rec = _Rec()
            if tile + 1 < NT:
                emitA(rec, tile + 1, **sets[(tile + 1) % 2])
            items = rec.items

            def play(n, items=items):
                while n > 0 and items:
                    kind, a, k = items.pop(0)
                    getattr(S, kind)(*a, **k)
                    n -= 1

            emitB(tile, play, **sets[tile % 2])
            play(10 ** 9)
        S.wait_tok("sp", [ytok])
        S.barrier()


def build(dbg=None):
    nc = bass.Bass("TRN2", target_bir_lowering=False)
    C = Ctx()
    C.nc = nc
    C.dbg = dbg

    def din(name, shape, dt=F32):
        return nc.dram_tensor(name, list(shape), dt, kind="ExternalInput").ap()

    x_full = din("x_full", [SEQ, D])
    x_own = din("x_own", [NOWN * SEGT, D])
    qpos = din("qpos", [128, NOWN, SEGT])
    w_in = din("w_in", [128, 8, 2048])
    g_mix = din("g_mix", [128, 8])
    x_rev = din("x_rev", [SEQ, D])
    I = {"x_full": x_full, "x_own": x_own, "w_in": w_in, "x_rev": x_rev}
    for nm, shp in (("a_re", [128, 16]), ("a_im", [128, 16]), ("log_dt", [128, 16]),
                    ("bpad_re", [128, 16, 128]), ("bpad_im", [128, 16, 128]),
                    ("cpad_re", [128, 16, 128]), ("cpad_im", [128, 16, 128]),
                    ("d_skip", [128, 4]), ("w_glu", [128, 4, 512]), ("b_glu", [128, 4]),
                    ("segsel", [128, 4, 16]),
                    ("w_out", [128, 8, D]), ("g_out", [128, 8]), ("mem", [256, D]), ("g_mem", [128, 8]),
                    ("w_xkv", [128, 8, 2048]), ("g_xattn", [128, 8]), ("w_xq", [128, 8, D]), ("w_xo", [128, 8, D]),
                    ("g_ffn", [128, 8]), ("gffn_rep", [128, D]), ("gfin_rep", [128, D]), ("w_pq", [128, 8, 2048]),
                    ("skT", [128, 16, 128]), ("peer_u", [16384, D]), ("peer_v", [16384, D])):
        I[nm] = din(nm, shp)
    y = nc.dram_tensor("y", [NOWN * SEGT, D], F32, kind="ExternalOutput").ap()
    if dbg == "attn":
        dbg_out = nc.dram_tensor("dbg_attn", [128, 4, NOWN * SEGT], F32, kind="ExternalOutput").ap()

    with ExitStack() as es:
        S = Sched(nc, es)
        C.S = S
        C.const_tok = Tok()
        ident_f = _sb(nc, es, "ident_f", [128, 128], F32)
        C.ident_f = ident_f
        C.ident_bf = _sb(nc, es, "ident_bf", [128, 128], BF16)
        ones_f = _sb(nc, es, "ones_f", [128, 128], F32)
        C.ones_bf = _sb(nc, es, "ones_bf", [128, 128], BF16)
        C.tri_bf = _sb(nc, es, "tri_bf", [128, 128], BF16)
        C.atri_bf = _sb(nc, es, "atri_bf", [128, 128], BF16)
        C.eps_col = _sb(nc, es, "eps_col", [128, 1], F32)
        C.kpos = _sb(nc, es, "kpos", [128, 64], F32)
        S.op("pool", lambda e: e.memset(ones_f[:], 1.0), writes=[C.const_tok])
        S.op("pool", lambda e: e.memset(C.eps_col[:], EPS), writes=[C.const_tok])
        S.op("pool", lambda e: e.affine_select(out=ident_f[:], in_=ones_f[:], pattern=[[-1, 128]],
                                               compare_op=ALU.is_equal, fill=0.0, base=0,
                                               channel_multiplier=1),
             reads=[C.const_tok], writes=[C.const_tok])
        S.op("pool", lambda e: e.tensor_copy(out=C.ident_bf[:], in_=ident_f[:]),
             reads=[C.const_tok], writes=[C.const_tok])
        S.op("pool", lambda e: e.tensor_copy(out=C.ones_bf[:], in_=ones_f[:]),
             reads=[C.const_tok], writes=[C.const_tok])
        S.op("pool", lambda e: e.affine_select(out=C.tri_bf[:], in_=ones_f[:], pattern=[[-1, 128]],
                                               compare_op=ALU.is_ge, fill=0.0, base=0,
                                               channel_multiplier=1),
             reads=[C.const_tok], writes=[C.const_tok])
        S.op("pool", lambda e: e.affine_select(out=C.atri_bf[:], in_=ones_f[:], pattern=[[1, 128]],
                                               compare_op=ALU.is_gt, fill=0.0, base=0,
                                               channel_multiplier=-1),
             reads=[C.const_tok], writes=[C.const_tok])
        S.op("pool", lambda e: e.iota(C.kpos[:], pattern=[[128, 64]], base=0, channel_multiplier=1,
                                      allow_small_or_imprecise_dtypes=True),
             writes=[C.const_tok])

        ps = [es.enter_context(nc.psum_tensor("ps%d" % i, [128, 512], F32)) for i in range(8)]
        ps_tok = [Tok() for _ in range(8)]
        C.tp_ps = [ps[6], ps[7]]
        C.tp_tok = [ps_tok[6], ps_tok[7]]
        C.tp_i = 0

        attnT = _sb(nc, es, "attnT", [128, 4, NOWN * SEGT], BF16)
        attn_tok = [[Tok() for _ in range(8)] for _ in range(NOWN)]

        gmix_sb = _sb(nc, es, "gmix", [128, 8], F32)
        gmix_tok = Tok()
        S.dma("sp", S.chan(), gmix_sb[:], g_mix[:, :], writes=[gmix_tok])

        with ExitStack() as e2:
          if dbg != "ssm":
              KT = _sb(nc, e2, "KT", [128, 4, SEQ], BF16)
              V = _sb(nc, e2, "V", [128, 64, 512], BF16)
              QT = _sb(nc, e2, "QT", [128, 4, NOWN * SEGT], BF16)
              kt_tok = [Tok() for _ in range(NSEG)]
              v_tok = [Tok() for _ in range(NSEG)]
              q_tok = [Tok() for _ in range(NOWN)]
              with ExitStack() as e3:
                  wk = _sb(nc, e3, "wk", [128, 8, 512], BF16)
                  wq = wk
                  wv = _sb(nc, e3, "wv", [128, 8, 512], BF16)
                  w_tok = Tok()
                  wk_tok = Tok()
                  wv_tok = Tok()
                  xt = [_sb(nc, e3, "xt%d" % i, [128, D], F32) for i in range(2)]
                  xt_tok = [Tok() for _ in range(2)]
                  xt_ch = [S.chan() for _ in range(2)]
                  wi_box = [0]

                  def load_w(wdst, c0, wtok):
                      for k2 in range(4):
                          sl = wi_box[0] % 2
                          wi_box[0] += 1
                          S.dma("sp", xt_ch[sl], xt[sl][:].rearrange("p (a b) -> p a b", a=2),
                                w_in[:, 2 * k2:2 * k2 + 2, c0:c0 + 512], writes=[xt_tok[sl]])
                          for a2 in range(2):
                              kt = 2 * k2 + a2
                              eng = "dve" if a2 == 0 else "pool"
                              S.op(eng, lambda e, kt=kt, sl=sl, wdst=wdst, a2=a2: e.tensor_scalar(
                                  wdst[:, kt, :], xt[sl][:, a2 * 512:(a2 + 1) * 512], gmix_sb[:, kt:kt + 1], None, ALU.mult),
                                  reads=[xt_tok[sl], gmix_tok], writes=[wtok])

                  load_w(wk, 512, wk_tok)
                  load_w(wv, 1024, wv_tok)

                  xn = [_sb(nc, e3, "xn%d" % i, [128, D], BF16) for i in range(2)]
                  xn_tok = [Tok(), Tok()]
                  ss = [_sb(nc, e3, "ss%d" % i, [128, 1], F32) for i in range(2)]
                  ss_tok = [Tok(), Tok()]
                  rstd = [_sb(nc, e3, "rstd%d" % i, [128, 1], F32) for i in range(2)]
                  rstd_tok = [Tok(), Tok()]
                  xnT = [_sb(nc, e3, "xnT%d" % i, [128, 8, SEGT], BF16) for i in range(2)]
                  xnT_tok = [Tok(), Tok()]
                  ti = 0
                  for sgi in range(NSEG + NOWN):
                      own = sgi >= NSEG
                      sg = sgi - NSEG if own else sgi
                      if sgi == NSEG:
                          load_w(wq, 0, wk_tok)
                      src = x_own if own else x_full
                      xb = sgi % 2
                      for m in range(4):
                          a = ti % 2
                          b = ti % 2
                          ti += 1
                          r0 = sg * SEGT + m * 128
                          S.dma("sp", xt_ch[a], xt[a][:], src[r0:r0 + 128, :], writes=[xt_tok[a]])
                          rms_scale(C, xt[a][:], xt_tok[a], rstd[b][:], rstd_tok[b], xn[b][:], xn_tok[b],
                                    ss[b][:], ss_tok[b])
                          S.op("dve", lambda e, a=a, b=b: e.tensor_scalar(xn[b][:], xt[a][:], rstd[b][:], None, ALU.mult),
                               reads=[xt_tok[a], rstd_tok[b]], writes=[xn_tok[b]])
                          ptb, pt_tok = transpose_to(C, xn[b], xn_tok[b], None, None)
                          S.op("dve", lambda e, xb=xb, m=m, ptb=ptb: e.tensor_copy(
                              out=xnT[xb][:, :, m * 128:(m + 1) * 128],
                              in_=ptb.rearrange("p (k t) -> p k t", k=8)),
                              reads=[pt_tok], writes=[xnT_tok[xb]])
                      if not own:
                          for hp in range(4):
                              pb = hp % 2
                              for kt in range(8):
                                  S.op("pe", lambda e, kt=kt, hp=hp, pb=pb, xb=xb: e.matmul(
                                      ps[pb][:], lhsT=wk[:, kt, hp * 128:(hp + 1) * 128], rhs=xnT[xb][:, kt, :],
                                      start=(kt == 0), stop=(kt == 7)),
                                      reads=[wk_tok, xnT_tok[xb]], writes=[ps_tok[pb]])
                              S.op("act", lambda e, hp=hp, pb=pb, sg=sg: e.copy(
                                  out=KT[:, hp, sg * SEGT:(sg + 1) * SEGT], in_=ps[pb][:]),
                                  reads=[ps_tok[pb]], writes=[kt_tok[sg]])
                          for m in range(4):
                              pb = 2 + m % 2
                              for kt in range(8):
                                  S.op("pe", lambda e, kt=kt, m=m, pb=pb, xb=xb: e.matmul(
                                      ps[pb][:], lhsT=xnT[xb][:, kt, m * 128:(m + 1) * 128], rhs=wv[:, kt, :],
                                      start=(kt == 0), stop=(kt == 7)),
                                      reads=[wv_tok, xnT_tok[xb]], writes=[ps_tok[pb]])
                              S.op("act", lambda e, m=m, pb=pb, sg=sg: e.copy(
                                  out=V[:, sg * 4 + m, :], in_=ps[pb][:]),
                                  reads=[ps_tok[pb]], writes=[v_tok[sg]])
                      else:
                          for hp in range(4):
                              pb = hp % 2
                              for kt in range(8):
                                  S.op("pe", lambda e, kt=kt, hp=hp, pb=pb, xb=xb: e.matmul(
                                      ps[pb][:], lhsT=wq[:, kt, hp * 128:(hp + 1) * 128], rhs=xnT[xb][:, kt, :],
                                      start=(kt == 0), stop=(kt == 7)),
                                      reads=[wk_tok, xnT_tok[xb]], writes=[ps_tok[pb]])
                              S.op("act", lambda e, hp=hp, pb=pb, sg=sg: e.mul(
                                  out=QT[:, hp, sg * SEGT:(sg + 1) * SEGT], in_=ps[pb][:], mul=0.125),
                                  reads=[ps_tok[pb]], writes=[q_tok[sg]])

              S.barrier()
              if dbg == "kv":
                  dch = S.chan()
                  dk = nc.dram_tensor("dbg_kt", [128, 4, SEQ], BF16, kind="ExternalOutput").ap()
                  dv = nc.dram_tensor("dbg_v", [128, 64, 512], BF16, kind="ExternalOutput").ap()
                  dq = nc.dram_tensor("dbg_q", [128, 4, NOWN * SEGT], BF16, kind="ExternalOutput").ap()
                  dtok = Tok()
                  S.dma("sp", dch, dk[:, :, :], KT[:], reads=kt_tok, writes=[dtok])
                  S.dma("sp", dch, dv[:, :, :], V[:], reads=v_tok, writes=[dtok])
                  S.dma("sp", dch, dq[:, :, :], QT[:], reads=q_tok, writes=[dtok])
                  S.wait_tok("sp", [dtok])
              with ExitStack() as e3:
                if dbg != "kv":
                    NE = 3
                    e_sb = [_sb(nc, e3, "e%d" % i, [128, 512], F32) for i in range(NE)]
                    e_tok = [Tok() for _ in range(NE)]
                    sp_sb = [_sb(nc, e3, "sp%d" % i, [128, 512], BF16) for i in range(3)]
                    sp_tok = [Tok(), Tok(), Tok()]
                    ex_sb = [_sb(nc, e3, "ex%d" % i, [128, 512], BF16) for i in range(2)]
                    ex_tok = [Tok(), Tok()]
                    w_sb = [_sb(nc, e3, "w%d" % i, [128, 512], BF16) for i in range(2)]
                    w_tok2 = [Tok(), Tok()]
                    masks = _sb(nc, e3, "masks", [128, 16, 512], BF16)
                    mask_tok = Tok()
                    qp = _sb(nc, e3, "qp", [128, NOWN, SEGT], F32)
                    qp_tok = Tok()
                    S.dma("sp", S.chan(), qp[:], qpos[:, :, :], writes=[qp_tok])
                    zps = [ps[0], ps[1]]
                    zps_tok = [ps_tok[0], ps_tok[1]]
                    cps = [ps[2], ps[3]]
                    cps_tok = [ps_tok[2], ps_tok[3]]
                    ops_ = [ps[4], ps[5]]
                    ops_tok = [ps_tok[4], ps_tok[5]]

                    for slot in range(NOWN):
                        top = KB_TOP[slot]
                        nb = top + 1
                        for r in range(16):
                            kb = top - r
                            S.op("dve", lambda e, r=r, kb=kb, slot=slot: e.tensor_scalar(
                                masks[:, r, :], qp[:, slot, :], C.kpos[:, kb:kb + 1], None, ALU.is_gt),
                                reads=[qp_tok, C.const_tok], writes=[mask_tok])
                        blocks = [(h, top - r, r) for h in range(8) for r in range(nb)]
                        nblk = len(blocks)

                        def s1(i):
                            h, kb, r = blocks[i]
                            hp, hh = h // 2, h % 2
                            zb = i % 2
                            S.op("pe", lambda e: e.matmul(
                                zps[zb][:], lhsT=KT[hh * 64:(hh + 1) * 64, hp, kb * 128:(kb + 1) * 128],
                                rhs=QT[hh * 64:(hh + 1) * 64, hp, slot * SEGT:(slot + 1) * SEGT],
                                start=True, stop=True),
                                reads=[kt_tok[kb // 4], q_tok[slot]], writes=[zps_tok[zb]])

                        def s2(i):
                            h, kb, r = blocks[i]
                            zb, eb, sb = i % 2, i % NE, i % 3
                            S.op("act", lambda e: e.activation(out=e_sb[eb][:], in_=zps[zb][:], func=AF.Exp),
                                 reads=[zps_tok[zb]], writes=[e_tok[eb]])
                            if r < 16:
                                S.op("dve", lambda e: e.tensor_tensor(out=e_sb[eb][:], in0=e_sb[eb][:],
                                                                       in1=masks[:, r, :], op=ALU.mult),
                                     reads=[mask_tok], writes=[e_tok[eb]])
                            S.op("act", lambda e: e.activation(out=sp_sb[sb][:], in_=e_sb[eb][:], func=AF.Ln, bias=1.0),
                                 reads=[e_tok[eb]], writes=[sp_tok[sb]])

                        def s3(i):
                            h, kb, r = blocks[i]
                            sb = i % 3
                            cb = h % 2
                            S.op("pe", lambda e: e.matmul(cps[cb][:], lhsT=C.tri_bf[:], rhs=sp_sb[sb][:],
                                                          start=(r == 0), stop=True, skip_group_check=(r != 0)),
                                 reads=[sp_tok[sb], C.const_tok], writes=[cps_tok[cb]])

                        def s3b(i):
                            h, kb, r = blocks[i]
                            if r == nb - 1:
                                return
                            sb = i % 3
                            cb = h % 2
                            S.op("pe", lambda e: e.matmul(cps[cb][:], lhsT=C.atri_bf[:], rhs=sp_sb[sb][:],
                                                          start=False, stop=True, skip_group_check=True),
                                 reads=[sp_tok[sb], C.const_tok], writes=[cps_tok[cb]])

                        def s4(i):
                            h, kb, r = blocks[i]
                            cb, xb_, eb, wb = h % 2, i % 2, i % NE, i % 2
                            S.op("act", lambda e: e.activation(out=ex_sb[xb_][:], in_=cps[cb][:], func=AF.Exp, scale=-1.0),
                                 reads=[cps_tok[cb]], writes=[ex_tok[xb_]])
                            S.op("dve" if i % 2 == 0 else "pool", lambda e: e.tensor_tensor(out=w_sb[wb][:], in0=e_sb[eb][:], in1=ex_sb[xb_][:],
                                                                   op=ALU.mult),
                                 reads=[e_tok[eb], ex_tok[xb_]], writes=[w_tok2[wb]])

                        def s5(i):
                            h, kb, r = blocks[i]
                            wb = i % 2
                            ob = h % 2
                            hp, hh = h // 2, h % 2
                            S.op("pe", lambda e: e.matmul(ops_[ob][hh * 64:(hh + 1) * 64, :], lhsT=V[:, kb, h * 64:(h + 1) * 64],
                                                          rhs=w_sb[wb][:], start=(r == 0), stop=(r == nb - 1)),
                                 reads=[w_tok2[wb], v_tok[kb // 4]], writes=[ops_tok[ob]])
                            if r == nb - 1:
                                S.op("act", lambda e: e.copy(out=attnT[hh * 64:(hh + 1) * 64, hp, slot * SEGT:(slot + 1) * SEGT],
                                                             in_=ops_[ob][hh * 64:(hh + 1) * 64, :]),
                                     reads=[ops_tok[ob]], writes=[attn_tok[slot][h]])

                        for t in range(nblk + 2):
                            if t < nblk:
                                s1(t)
                                s2(t)
                            if 0 <= t - 2 < nblk:
                                s3b(t - 2)
                            if 0 <= t - 1 < nblk:
                                s3(t - 1)
                                s4(t - 1)
                            if 0 <= t - 2 < nblk:
                                s5(t - 2)

        S.barrier()
        ssmT = _sb(nc, es, "ssmT", [128, 4, NOWN * SEGT], BF16)
        ssm_tok = [Tok() for _ in range(NOWN)]
        if dbg in ("ssm", None, "h1", "h2", "h3"):
            phase_ssm(C, nc, S, ps, ps_tok, I, ssmT, ssm_tok, gmix_sb, gmix_tok)
        S.barrier()
        och = S.chan()
        if dbg == "ssm":
            dbg_ssm = nc.dram_tensor("dbg_ssm", [128, 4, NOWN * SEGT], BF16, kind="ExternalOutput").ap()
            ytok = Tok()
            S.dma("sp", och, dbg_ssm[:, :, :], ssmT[:], reads=ssm_tok, writes=[ytok])
            S.wait_tok("sp", [ytok])
        if dbg == "attn":
            with ExitStack() as e2:
                tmp = _sb(nc, e2, "dbgtmp", [128, 4, NOWN * SEGT], F32)
                tmp_tok = Tok()
                alltoks = [t for row in attn_tok for t in row]
                S.op("dve", lambda e: e.tensor_copy(out=tmp[:], in_=attnT[:]), reads=alltoks, writes=[tmp_tok])
                ytok = Tok()
                S.dma("sp", och, dbg_out[:, :, :], tmp[:], reads=[tmp_tok], writes=[ytok])
                S.wait_tok("sp", [ytok])
        S.barrier()
        if dbg in (None, "h1", "h2", "h3"):
            phase_post(C, nc, S, ps, ps_tok, I, attnT, attn_tok, ssmT, ssm_tok, y, dbg)
            return nc
        with ExitStack() as e2:
            zt = _sb(nc, e2, "zt", [128, D], F32)
            zt_tok = Tok()
            S.op("pool", lambda e: e.memset(zt[:], 0.0), writes=[zt_tok])
            ytok = Tok()
            for i in range(NOWN * 4):
                S.dma("sp", och, y[i * 128:(i + 1) * 128, :], zt[:], reads=[zt_tok], writes=[ytok])
            S.wait_tok("sp", [ytok])
    return nc


def make_in_maps(inputs):
    x = np.ascontiguousarray(inputs["x"], dtype=np.float32)
    w_in = np.ascontiguousarray(inputs["w_in"][0].reshape(8, 128, 2048).transpose(1, 0, 2))
    g_mix = np.ascontiguousarray(inputs["g_mix"][0].reshape(8, 128).T)
    f32 = np.float32
    are = inputs["a_re"][0].reshape(16, 2, 64).transpose(1, 2, 0).reshape(128, 16)
    aim = inputs["a_im"][0].reshape(16, 2, 64).transpose(1, 2, 0).reshape(128, 16)
    ldt = np.repeat(inputs["log_dt"][0].reshape(16, 2).T[:, None, :], 64, axis=1).reshape(128, 16)
    bpr = np.zeros((128, 16, 128), f32)
    bpi = np.zeros((128, 16, 128), f32)
    cpr = np.zeros((128, 16, 128), f32)
    cpi = np.zeros((128, 16, 128), f32)
    for k in range(16):
        for g2 in range(2):
            g = 2 * k + g2
            r0 = (g % 8) * 16
            bpr[r0:r0 + 16, k, g2 * 64:(g2 + 1) * 64] = inputs["b_re"][0][g].T
            bpi[r0:r0 + 16, k, g2 * 64:(g2 + 1) * 64] = inputs["b_im"][0][g].T
            cpr[g2 * 64:(g2 + 1) * 64, k, r0:r0 + 16] = inputs["c_re"][0][g].T
            cpi[g2 * 64:(g2 + 1) * 64, k, r0:r0 + 16] = inputs["c_im"][0][g].T
    dsk = inputs["d_skip"][0].reshape(4, 128).T
    wglu = inputs["w_glu"][0].reshape(4, 128, 512).transpose(1, 0, 2)
    bglu = inputs["b_glu"][0].reshape(4, 128).T
    def kt8(w):
        return w.reshape(8, 128, -1).transpose(1, 0, 2)

    def col8(g):
        return g.reshape(8, 128).T

    post = {"w_out": kt8(inputs["w_out"][0]),
            "g_out": col8(np.concatenate([inputs["g_attn_out"][0], inputs["g_ssm_out"][0]])),
            "g_mem": col8(inputs["g_mem"][0]), "w_xkv": kt8(inputs["w_xkv"][0]),
            "g_xattn": col8(inputs["g_xattn"][0]), "w_xq": kt8(inputs["w_xq"][0]), "w_xo": kt8(inputs["w_xo"][0]),
            "g_ffn": col8(inputs["g_ffn"][0]),
            "gffn_rep": np.broadcast_to(inputs["g_ffn"][0][None, :], (128, D)),
            "gfin_rep": np.broadcast_to(inputs["g_final"][None, :], (128, D)),
            "w_pq": kt8(inputs["w_pq"][0]),
            "skT": inputs["sub_keys"][0].transpose(3, 0, 1, 2).reshape(128, 16, 128),
            "peer_u": inputs["peer_u"][0], "peer_v": inputs["peer_v"][0]}
    common = {"w_in": w_in, "g_mix": g_mix, "a_re": are, "a_im": aim, "log_dt": ldt,
              "bpad_re": bpr, "bpad_im": bpi, "cpad_re": cpr, "cpad_im": cpi,
              "d_skip": dsk, "w_glu": wglu, "b_glu": bglu}
    common.update(post)
    common = {k_: np.ascontiguousarray(v_, dtype=f32) for k_, v_ in common.items()}
    maps = []
    for c in range(8):
        b, j = c // 4, c % 4
        tiles = own_tiles(j)
        segsel = np.zeros((128, NOWN, 16), f32)
        for i_, t_ in enumerate(tiles):
            segsel[:, i_, t_] = 1.0
        x_own = np.concatenate([x[b, t * SEGT:(t + 1) * SEGT] for t in tiles], axis=0)
        qp = np.stack([np.arange(t * SEGT, (t + 1) * SEGT, dtype=np.float32) for t in tiles], axis=0)
        qp = np.ascontiguousarray(np.broadcast_to(qp[None], (128, NOWN, SEGT)))
        x_rev = np.ascontiguousarray(x[b].reshape(NSEG, SEGT, D)[:, ::-1, :].reshape(SEQ, D))
        m = {"x_full": np.ascontiguousarray(x[b]), "x_own": np.ascontiguousarray(x_own), "x_rev": x_rev,
             "qpos": qp, "segsel": segsel, "mem": np.ascontiguousarray(inputs["mem"][b], dtype=f32)}
        m.update(common)
        maps.append(m)
    return maps


def kernel(**inputs):
    nc = build()
    maps = make_in_maps(inputs)
    res = run_bass_kernel_spmd(nc, maps, core_ids=list(range(8)))
    out = np.zeros((2, SEQ, D), np.float32)
    for c in range(8):
        b, j = c // 4, c % 4
        yo = res.results[c]["y"]
        for i, t in enumerate(own_tiles(j)):
            out[b, t * SEGT:(t + 1) * SEGT] = yo[i * SEGT:(i + 1) * SEGT]
    return out
```

```python
from contextlib import ExitStack
import numpy as np
import concourse.bass as bass
import concourse.mybir as mybir
from concourse.bass_utils import run_bass_kernel_spmd

F32 = mybir.dt.float32
BF16 = mybir.dt.bfloat16
U32 = mybir.dt.uint32
I32 = mybir.dt.int32
AF = mybir.ActivationFunctionType
ALU = mybir.AluOpType
AX = mybir.AxisListType

D = 1024
SEQ = 8192
NSEG = 16
SEGT = 512
NOWN = 4
EPS = 1e-6
KB_TOP = [15, 31, 47, 63]
NEG = -1.0e30


def own_tiles(j):
    return [j, 7 - j, 8 + j, 15 - j]


class Tok:
    __slots__ = ("w", "r")

    def __init__(self):
        self.w = None
        self.r = {}


class _Eng:
    def __init__(self, name, h, sem):
        self.name = name
        self.h = h
        self.sem = sem
        self.n = 0
        self.waited = {}


class _Chan:
    def __init__(self, sem):
        self.sem = sem
        self.n = 0


class Sched:
    def __init__(self, nc, es):
        self.nc = nc
        self.es = es
        self.E = {}
        for name, h in (("pe", nc.tensor), ("act", nc.scalar), ("dve", nc.vector),
                        ("pool", nc.gpsimd), ("sp", nc.sync)):
            sem = es.enter_context(nc.semaphore("sem_" + name))
            self.E[name] = _Eng(name, h, sem)
        self.nchan = 0
        self.chans = []

    def chan(self):
        self.nchan += 1
        c = _Chan(self.es.enter_context(self.nc.semaphore("ch%d" % self.nchan)))
        self.chans.append(c)
        return c

    def barrier(self):
        for E in self.E.values():
            for F in self.E.values():
                if F is E or F.n == 0:
                    continue
                if E.waited.get(id(F.sem), 0) < F.n:
                    E.h.wait_ge(F.sem, F.n)
                    E.waited[id(F.sem)] = F.n
            for c in self.chans:
                if c.n > 0 and E.waited.get(id(c.sem), 0) < c.n:
                    E.h.wait_ge(c.sem, c.n)
                    E.waited[id(c.sem)] = c.n

    def _wait(self, E, reads, writes, weak=()):
        deps = {}
        for t in weak:
            for d in [t.w] + list(t.r.values()):
                if d is not None and d[0] is not E.sem:
                    k_ = id(d[0])
                    if k_ not in deps or deps[k_][1] < d[1]:
                        deps[k_] = d

        def add(d):
            if d is None:
                return
            s, v = d
            k = id(s)
            if k not in deps or deps[k][1] < v:
                deps[k] = (s, v)

        for t in reads:
            add(t.w)
        for t in writes:
            add(t.w)
            for d in t.r.values():
                add(d)
        for k, (s, v) in deps.items():
            if E.name == "pe" and s is E.sem:
                continue
            if E.waited.get(k, 0) < v:
                E.h.wait_ge(s, v)
                E.waited[k] = v

    def op(self, eng, fn, reads=(), writes=(), weak=()):
        E = self.E[eng]
        self._wait(E, reads, writes, weak)
        ins = fn(E.h)
        E.n += 1
        ins.then_inc(E.sem, 1)
        me = (E.sem, E.n)
        for t in reads:
            t.r[id(E.sem)] = me
        for t in writes:
            t.w = me
            t.r = {}
        for t in weak:
            t.w = me
            t.r = {}
        return ins

    def dma(self, queue, ch, out, in_, reads=(), writes=(), indirect=None, **kw):
        Q = self.E[queue]
        self._wait(Q, reads, writes)
        if indirect is not None:
            ins = Q.h.indirect_dma_start(out=out, out_offset=None, in_=in_, in_offset=indirect)
        else:
            ins = Q.h.dma_start(out=out, in_=in_, **kw)
        ch.n += 16
        ins.then_inc(ch.sem, 16)
        me = (ch.sem, ch.n)
        for t in reads:
            t.r[id(ch.sem)] = me
        for t in writes:
            t.w = me
            t.r = {}
        return ins

    def wait_tok(self, eng, toks):
        self._wait(self.E[eng], toks, ())


class Ctx:
    pass


_NAME_CNT = [0]


def _sb(nc, es, name, shape, dt):
    _NAME_CNT[0] += 1
    return es.enter_context(nc.sbuf_tensor("%s_%d" % (name, _NAME_CNT[0]), list(shape), dt))


def rms_scale(C, xt, xt_tok, rstd, rstd_tok, junk, junk_tok, ss, ss_tok):
    S = C.S
    S.op("act", lambda e: e.activation(out=junk, in_=xt, func=AF.Square, accum_out=ss),
         reads=[xt_tok], writes=[junk_tok, ss_tok])
    S.op("act", lambda e: e.activation(out=rstd, in_=ss, func=AF.Ln, bias=C.eps_col[:], scale=1.0 / D),
         reads=[ss_tok, C.const_tok], writes=[rstd_tok])
    S.op("act", lambda e: e.activation(out=rstd, in_=rstd, func=AF.Exp, scale=-0.5),
         reads=[rstd_tok], writes=[rstd_tok])


def transpose_to(C, xn, xn_tok, dst_fn, dst_tok, nkt=8):
    S = C.S
    force = getattr(C, "tp_force", None)
    if force is not None:
        pt, pt_tok = C.tp_ps[force], C.tp_tok[force]
    else:
        pt, pt_tok = C.tp_ps[C.tp_i % 2], C.tp_tok[C.tp_i % 2]
        C.tp_i += 1
    ptb = pt[:].bitcast(BF16)
    for kt in range(nkt):
        S.op("pe", lambda e, kt=kt: e.transpose(out=ptb[:, kt * 128:(kt + 1) * 128],
                                                in_=xn[:, kt * 128:(kt + 1) * 128],
                                                identity=C.ident_bf[:]),
             reads=[xn_tok, C.const_tok], writes=[pt_tok])
    return ptb, pt_tok


TWO_PI = 6.283185307179586
CW1 = 6.28125
CW2 = TWO_PI - CW1


def phase_ssm(C, nc, S, ps, ps_tok, I, ssmT, ssm_tok, gmix_sb, gmix_tok):
    with ExitStack() as e2:
        cosT = _sb(nc, e2, "cosT", [128, 16, SEGT], F32)
        sinT = _sb(nc, e2, "sinT", [128, 16, SEGT], F32)
        tab_tok = Tok()
        bre = _sb(nc, e2, "bre", [128, 16, 128], BF16)
        bim = _sb(nc, e2, "bim", [128, 16, 128], BF16)
        cre = _sb(nc, e2, "cre", [128, 16, 128], BF16)
        ncim = _sb(nc, e2, "ncim", [128, 16, 128], BF16)
        bc_tok = Tok()
        wu = _sb(nc, e2, "wu", [128, 8, 512], BF16)
        wglu = _sb(nc, e2, "wglu", [128, 4, 512], BF16)
        diagD = _sb(nc, e2, "diagD", [128, 4, 128], BF16)
        w_tok = Tok()
        P = {}
        for nm in ("are", "aim", "ldt", "dt", "rho", "th", "gre", "gim", "ngim", "pa", "pb", "pc", "pd", "adt", "nadt",
                   "ilr", "ili", "q1", "q2", "q3", "q4"):
            P[nm] = _sb(nc, e2, "p_" + nm, [128, 16], F32)
        p_tok = Tok()
        bglu = _sb(nc, e2, "bglu", [128, 4], F32)
        dsk = _sb(nc, e2, "dsk", [128, 4], F32)
        segsel = _sb(nc, e2, "segsel_sb", [128, 4, 16], F32)
        halfpi = _sb(nc, e2, "halfpi", [128, 1], F32)
        Xst = _sb(nc, e2, "Xst", [128, 2, 16, 17], F32)
        tau1p = _sb(nc, e2, "tau1p", [128, SEGT], F32)
        magt = _sb(nc, e2, "magt", [128, SEGT], F32)
        mag_tok = Tok()
        S.op("pool", lambda e: e.iota(tau1p[:], pattern=[[1, SEGT]], base=1, channel_multiplier=0,
                                      allow_small_or_imprecise_dtypes=True), writes=[mag_tok])
        xst_tok = Tok()
        small_tok = Tok()
        ch0 = S.chan()
        for dst, src in ((P["are"], I["a_re"]), (P["aim"], I["a_im"]), (P["ldt"], I["log_dt"]),
                         (bglu, I["b_glu"]), (dsk, I["d_skip"])):
            S.dma("sp", ch0, dst[:], src[:, :], writes=[small_tok])
        S.dma("sp", ch0, segsel[:], I["segsel"][:, :, :], writes=[small_tok])
        S.op("pool", lambda e: e.memset(halfpi[:], float(np.pi / 2)), writes=[small_tok])
        S.op("pool", lambda e: e.memset(Xst[:], 0.0), writes=[xst_tok])
        S.op("act", lambda e: e.activation(out=P["dt"][:], in_=P["ldt"][:], func=AF.Exp), reads=[small_tok], writes=[p_tok])
        S.op("dve", lambda e: e.tensor_tensor(out=P["pa"][:], in0=P["are"][:], in1=P["dt"][:], op=ALU.mult),
             reads=[small_tok, p_tok], writes=[p_tok])
        S.op("act", lambda e: e.activation(out=P["rho"][:], in_=P["pa"][:], func=AF.Exp), reads=[p_tok], writes=[p_tok])
        S.op("dve", lambda e: e.tensor_copy(out=P["adt"][:], in_=P["pa"][:]), reads=[p_tok], writes=[p_tok])
        S.op("dve", lambda e: e.tensor_scalar(P["nadt"][:], P["pa"][:], -1.0, None, ALU.mult), reads=[p_tok], writes=[p_tok])
        S.op("dve", lambda e: e.tensor_tensor(out=P["th"][:], in0=P["aim"][:], in1=P["dt"][:], op=ALU.mult),
             reads=[small_tok, p_tok], writes=[p_tok])
        with ExitStack() as e3:
            tau1 = _sb(nc, e3, "tau1", [128, SEGT], F32)
            ang = _sb(nc, e3, "ang", [128, 4, SEGT], F32)
            kf = _sb(nc, e3, "kf", [128, 4, SEGT], F32)
            ki = _sb(nc, e3, "ki", [128, 4, SEGT], I32)
            s1 = _sb(nc, e3, "s1", [128, 4, SEGT], F32)
            t_tok = Tok()
            S.op("pool", lambda e: e.iota(tau1[:], pattern=[[1, SEGT]], base=1, channel_multiplier=0,
                                          allow_small_or_imprecise_dtypes=True), writes=[t_tok])
            for gq in range(4):
                for kk in range(4):
                    k = gq * 4 + kk
                    S.op("dve", lambda e, kk=kk, k=k: e.tensor_scalar(ang[:, kk, :], tau1[:], P["th"][:, k:k + 1], None, ALU.mult),
                         reads=[p_tok, t_tok], writes=[t_tok])
                S.op("dve", lambda e: e.tensor_scalar(kf[:], ang[:], 1.0 / TWO_PI, None, ALU.mult), reads=[t_tok], writes=[t_tok])
                S.op("dve", lambda e: e.tensor_copy(out=ki[:], in_=kf[:]), reads=[t_tok], writes=[t_tok])
                S.op("dve", lambda e: e.tensor_copy(out=kf[:], in_=ki[:]), reads=[t_tok], writes=[t_tok])
                S.op("dve", lambda e: e.scalar_tensor_tensor(out=ang[:], in0=kf[:], scalar=-CW1, in1=ang[:], op0=ALU.mult, op1=ALU.add),
                     reads=[t_tok], writes=[t_tok])
                S.op("dve", lambda e: e.scalar_tensor_tensor(out=ang[:], in0=kf[:], scalar=-CW2, in1=ang[:], op0=ALU.mult, op1=ALU.add),
                     reads=[t_tok], writes=[t_tok])
                S.op("act", lambda e: e.activation(out=s1[:], in_=ang[:], func=AF.Sin, scale=0.25), reads=[t_tok], writes=[t_tok])
                S.op("act", lambda e: e.activation(out=kf[:], in_=ang[:], func=AF.Sin, scale=0.25, bias=halfpi[:]),
                     reads=[t_tok, small_tok], writes=[t_tok])
                S.op("dve", lambda e: e.scalar_tensor_tensor(out=ang[:], in0=s1[:], scalar=2.0, in1=kf[:], op0=ALU.mult, op1=ALU.mult),
                     reads=[t_tok], writes=[t_tok])
                S.op("dve", lambda e: e.tensor_tensor(out=s1[:], in0=s1[:], in1=s1[:], op=ALU.mult), reads=[t_tok], writes=[t_tok])
                S.op("dve", lambda e: e.tensor_scalar(s1[:], s1[:], -2.0, 1.0, ALU.mult, ALU.add), reads=[t_tok], writes=[t_tok])
                S.op("dve", lambda e, gq=gq: e.scalar_tensor_tensor(out=sinT[:, gq * 4:(gq + 1) * 4, :], in0=ang[:], scalar=2.0, in1=s1[:],
                                                                    op0=ALU.mult, op1=ALU.mult),
                     reads=[t_tok], writes=[tab_tok])
                S.op("dve", lambda e: e.tensor_tensor(out=ang[:], in0=ang[:], in1=ang[:], op=ALU.mult), reads=[t_tok], writes=[t_tok])
                S.op("dve", lambda e, gq=gq: e.tensor_scalar(cosT[:, gq * 4:(gq + 1) * 4, :], ang[:], -2.0, 1.0, ALU.mult, ALU.add),
                     reads=[t_tok], writes=[tab_tok])
            c0 = cosT[:, :, 0]
            s0 = sinT[:, :, 0]
            tt = lambda o, a, b, op: S.op("dve", lambda e: e.tensor_tensor(out=o, in0=a, in1=b, op=op),
                                          reads=[p_tok, tab_tok, small_tok], writes=[p_tok])
            tt(P["pa"][:], P["rho"][:], c0, ALU.mult)
            S.op("dve", lambda e: e.tensor_scalar(P["pa"][:], P["pa"][:], -1.0, None, ALU.add), reads=[p_tok], writes=[p_tok])
            tt(P["pb"][:], P["rho"][:], s0, ALU.mult)
            tt(P["pc"][:], P["are"][:], P["are"][:], ALU.mult)
            tt(P["pd"][:], P["aim"][:], P["aim"][:], ALU.mult)
            tt(P["pc"][:], P["pc"][:], P["pd"][:], ALU.add)
            S.op("dve", lambda e: e.reciprocal(out=P["pc"][:], in_=P["pc"][:]), reads=[p_tok], writes=[p_tok])
            tt(P["gre"][:], P["pa"][:], P["are"][:], ALU.mult)
            tt(P["pd"][:], P["pb"][:], P["aim"][:], ALU.mult)
            tt(P["gre"][:], P["gre"][:], P["pd"][:], ALU.add)
            tt(P["gre"][:], P["gre"][:], P["pc"][:], ALU.mult)
            tt(P["gim"][:], P["pb"][:], P["are"][:], ALU.mult)
            tt(P["pd"][:], P["pa"][:], P["aim"][:], ALU.mult)
            tt(P["gim"][:], P["gim"][:], P["pd"][:], ALU.subtract)
            tt(P["gim"][:], P["gim"][:], P["pc"][:], ALU.mult)
            S.op("dve", lambda e: e.tensor_scalar(P["ngim"][:], P["gim"][:], -1.0, None, ALU.mult), reads=[p_tok], writes=[p_tok])
            S.barrier()
        with ExitStack() as e3:
            f1 = _sb(nc, e3, "f1", [128, 16, 128], F32)
            f2 = _sb(nc, e3, "f2", [128, 16, 128], F32)
            f3 = _sb(nc, e3, "f3", [128, 128], F32)
            f_tok = Tok()
            chf = S.chan()
            for src, dst in ((I["bpad_re"], bre), (I["bpad_im"], bim)):
                S.dma("sp", chf, f1[:], src[:, :, :], writes=[f_tok])
                S.op("dve", lambda e, dst=dst: e.tensor_copy(out=dst[:], in_=f1[:]), reads=[f_tok], writes=[bc_tok, f_tok])
            S.dma("sp", chf, f1[:], I["cpad_re"][:, :, :], writes=[f_tok])
            S.dma("sp", chf, f2[:], I["cpad_im"][:, :, :], writes=[f_tok])
            for k in range(16):
                S.op("dve", lambda e, k=k: e.tensor_scalar(f3[:], f2[:, k, :], P["gim"][:, k:k + 1], None, ALU.mult),
                     reads=[f_tok, p_tok], writes=[f_tok])
                S.op("dve", lambda e, k=k: e.scalar_tensor_tensor(out=cre[:, k, :], in0=f1[:, k, :], scalar=P["gre"][:, k:k + 1],
                                                                  in1=f3[:], op0=ALU.mult, op1=ALU.subtract),
                     reads=[f_tok, p_tok], writes=[bc_tok])
                S.op("dve", lambda e, k=k: e.tensor_scalar(f3[:], f2[:, k, :], P["gre"][:, k:k + 1], None, ALU.mult),
                     reads=[f_tok, p_tok, bc_tok], writes=[f_tok])
                S.op("dve", lambda e, k=k: e.scalar_tensor_tensor(out=ncim[:, k, :], in0=f1[:, k, :], scalar=P["ngim"][:, k:k + 1],
                                                                  in1=f3[:], op0=ALU.mult, op1=ALU.subtract),
                     reads=[f_tok, p_tok], writes=[bc_tok])
            for kt in range(8):
                for q4 in range(4):
                    S.dma("sp", chf, f3[:], I["w_in"][:, kt, 1536 + q4 * 128:1536 + (q4 + 1) * 128], writes=[f_tok])
                    S.op("dve", lambda e, kt=kt, q4=q4: e.tensor_scalar(wu[:, kt, q4 * 128:(q4 + 1) * 128], f3[:],
                                                                      gmix_sb[:, kt:kt + 1], None, ALU.mult),
                         reads=[f_tok, gmix_tok], writes=[w_tok, f_tok])
            for kt in range(4):
                S.dma("sp", chf, f1[:, 0:4, :], I["w_glu"][:, kt, :].rearrange("p (a b) -> p a b", a=4), writes=[f_tok])
                S.op("dve", lambda e, kt=kt: e.tensor_copy(out=wglu[:, kt, :].rearrange("p (a b) -> p a b", a=4), in_=f1[:, 0:4, :]),
                     reads=[f_tok], writes=[w_tok, f_tok])
            for ct in range(4):
                S.op("dve", lambda e, ct=ct: e.tensor_scalar(diagD[:, ct, :], C.ident_f[:], dsk[:, ct:ct + 1], None, ALU.mult),
                     reads=[small_tok, C.const_tok], writes=[w_tok])
            S.barrier()

        def scale_tables(sign_key):
            for k in range(16):
                S.op("act", lambda e, k=k: e.activation(out=magt[:], in_=tau1p[:], func=AF.Exp, scale=P[sign_key][:, k:k + 1]),
                     reads=[mag_tok, p_tok], writes=[mag_tok])
                S.op("dve", lambda e, k=k: e.tensor_tensor(out=cosT[:, k, :], in0=cosT[:, k, :], in1=magt[:], op=ALU.mult),
                     reads=[mag_tok, tab_tok], writes=[tab_tok])
                S.op("dve", lambda e, k=k: e.tensor_tensor(out=sinT[:, k, :], in0=sinT[:, k, :], in1=magt[:], op=ALU.mult),
                     reads=[mag_tok, tab_tok], writes=[tab_tok])

        scale_tables("adt")
        with ExitStack() as e3:
            xt = [_sb(nc, e3, "axt%d" % i, [128, D], F32) for i in range(2)]
            xt_tok = [Tok(), Tok()]
            xt_ch = [S.chan(), S.chan()]
            xn = [_sb(nc, e3, "axn%d" % i, [128, D], BF16) for i in range(2)]
            xn_tok = [Tok(), Tok()]
            ss = [_sb(nc, e3, "ass%d" % i, [128, 1], F32) for i in range(2)]
            ss_tok = [Tok(), Tok()]
            rstd = [_sb(nc, e3, "arstd%d" % i, [128, 1], F32) for i in range(2)]
            rstd_tok = [Tok(), Tok()]
            xnT = [_sb(nc, e3, "axnT%d" % i, [128, 8, SEGT], BF16) for i in range(2)]
            xnT_tok = [Tok(), Tok()]
            uT = [_sb(nc, e3, "auT%d" % i, [128, 4, SEGT], BF16) for i in range(2)]
            uT_tok = [Tok(), Tok()]
            acc4 = [_sb(nc, e3, "acc4_%d" % i, [128, 4, 16], F32) for i in range(2)]
            acc4_tok = [[[Tok() for _ in range(16)] for _ in range(4)] for _ in range(2)]
            junkS = [_sb(nc, e3, "junkS%d" % i, [128, SEGT], BF16) for i in range(4)]
            junkS_tok = [Tok() for _ in range(4)]
            jc = [0]
            sm_ = {nm: _sb(nc, e3, "sm_" + nm, [128, 16], F32) for nm in ("a", "b", "c", "d", "sr", "si")}
            sm_tok = Tok()
            t0r, t0i = cosT[:, :, 0], sinT[:, :, 0]
            Lr, Li = cosT[:, :, SEGT - 1], sinT[:, :, SEGT - 1]
            tt2 = lambda o, a, b, op: S.op("dve", lambda e: e.tensor_tensor(out=o, in0=a, in1=b, op=op),
                                           reads=[p_tok, tab_tok, sm_tok], writes=[p_tok])
            tt2(P["q1"][:], t0r, t0r, ALU.mult)
            tt2(P["q2"][:], t0i, t0i, ALU.mult)
            tt2(P["q1"][:], P["q1"][:], P["q2"][:], ALU.add)
            S.op("dve", lambda e: e.reciprocal(out=P["q1"][:], in_=P["q1"][:]), reads=[p_tok], writes=[p_tok])
            tt2(P["ilr"][:], t0r, P["q1"][:], ALU.mult)
            tt2(P["ili"][:], t0i, P["q1"][:], ALU.mult)
            S.op("dve", lambda e: e.tensor_scalar(P["ili"][:], P["ili"][:], -1.0, None, ALU.mult), reads=[p_tok], writes=[p_tok])
            ti = 0
            for sg in range(NSEG):
                xb = sg % 2
                for m in range(4):
                    a = ti % 2
                    ti += 1
                    r0 = sg * SEGT + m * 128
                    S.dma("sp", xt_ch[a], xt[a][:], I["x_rev"][r0:r0 + 128, :], writes=[xt_tok[a]])
                    rms_scale(C, xt[a][:], xt_tok[a], rstd[a][:], rstd_tok[a], xn[a][:], xn_tok[a], ss[a][:], ss_tok[a])
                    S.op("dve", lambda e, a=a: e.tensor_scalar(xn[a][:], xt[a][:], rstd[a][:], None, ALU.mult),
                         reads=[xt_tok[a], rstd_tok[a]], writes=[xn_tok[a]])
                    ptb, pt_tok = transpose_to(C, xn[a], xn_tok[a], None, None)
                    S.op("act", lambda e, xb=xb, m=m, ptb=ptb: e.copy(out=xnT[xb][:, :, m * 128:(m + 1) * 128],
                                                                      in_=ptb.rearrange("p (k t) -> p k t", k=8)),
                         reads=[pt_tok], writes=[xnT_tok[xb]])
                for ct in range(4):
                    pb = 4 + ct % 2
                    for kt in range(8):
                        S.op("pe", lambda e, kt=kt, ct=ct, pb=pb, xb=xb: e.matmul(
                            ps[pb][:], lhsT=wu[:, kt, ct * 128:(ct + 1) * 128], rhs=xnT[xb][:, kt, :],
                            start=(kt == 0), stop=(kt == 7)), reads=[w_tok, xnT_tok[xb]], writes=[ps_tok[pb]])
                    S.op("act", lambda e, ct=ct, pb=pb, xb=xb: e.copy(out=uT[xb][:, ct, :], in_=ps[pb][:]),
                         reads=[ps_tok[pb]], writes=[uT_tok[xb]])
                ab = sg % 2
                for k in range(16):
                    ct = k // 4
                    bur, bui = (0, 1) if k % 2 == 0 else (2, 3)
                    S.op("pe", lambda e, k=k, ct=ct, bur=bur: e.matmul(ps[bur][:], lhsT=bre[:, k, :], rhs=uT[xb][:, ct, :], start=True, stop=True),
                         reads=[bc_tok, uT_tok[xb]], writes=[ps_tok[bur]])
                    S.op("pe", lambda e, k=k, ct=ct, bui=bui: e.matmul(ps[bui][:], lhsT=bim[:, k, :], rhs=uT[xb][:, ct, :], start=True, stop=True),
                         reads=[bc_tok, uT_tok[xb]], writes=[ps_tok[bui]])
                    for j, (bk, tab) in enumerate(((bur, cosT), (bui, sinT), (bur, sinT), (bui, cosT))):
                        jb = jc[0] % 4
                        jc[0] += 1
                        S.op("dve", lambda e, j=j, bk=bk, tab=tab, k=k, jb=jb: e.scalar_tensor_tensor(
                            out=junkS[jb][:], in0=ps[bk][:], scalar=1.0, in1=tab[:, k, :], op0=ALU.mult, op1=ALU.mult,
                            accum_out=acc4[ab][:, j, k:k + 1]),
                            reads=[ps_tok[bk], tab_tok], writes=[junkS_tok[jb], acc4_tok[ab][j][k]])
                A_ = acc4[ab]
                rd = [t_ for row_ in acc4_tok[ab] for t_ in row_] + [p_tok, tab_tok, xst_tok, sm_tok]
                tt3 = lambda o, a_, b_, op: S.op("dve", lambda e: e.tensor_tensor(out=o, in0=a_, in1=b_, op=op), reads=rd, writes=[sm_tok])
                tt3(sm_["a"][:], A_[:, 0, :], A_[:, 1, :], ALU.subtract)
                tt3(sm_["b"][:], A_[:, 2, :], A_[:, 3, :], ALU.add)
                tt3(sm_["c"][:], P["ilr"][:], sm_["a"][:], ALU.mult)
                tt3(sm_["d"][:], P["ili"][:], sm_["b"][:], ALU.mult)
                tt3(sm_["sr"][:], sm_["c"][:], sm_["d"][:], ALU.subtract)
                tt3(sm_["c"][:], P["ilr"][:], sm_["b"][:], ALU.mult)
                tt3(sm_["d"][:], P["ili"][:], sm_["a"][:], ALU.mult)
                tt3(sm_["si"][:], sm_["c"][:], sm_["d"][:], ALU.add)
                Xr, Xi = Xst[:, 0, :, sg], Xst[:, 1, :, sg]
                tt3(sm_["a"][:], Lr, Xr, ALU.mult)
                tt3(sm_["b"][:], Li, Xi, ALU.mult)
                tt3(sm_["a"][:], sm_["a"][:], sm_["b"][:], ALU.subtract)
                S.op("dve", lambda e, sg=sg: e.tensor_tensor(out=Xst[:, 0, :, sg + 1], in0=sm_["a"][:], in1=sm_["sr"][:], op=ALU.add),
                     reads=[sm_tok], writes=[xst_tok])
                tt3(sm_["c"][:], Lr, Xi, ALU.mult)
                tt3(sm_["d"][:], Li, Xr, ALU.mult)
                tt3(sm_["c"][:], sm_["c"][:], sm_["d"][:], ALU.add)
                S.op("dve", lambda e, sg=sg: e.tensor_tensor(out=Xst[:, 1, :, sg + 1], in0=sm_["c"][:], in1=sm_["si"][:], op=ALU.add),
                     reads=[sm_tok], writes=[xst_tok])
            S.barrier()
        scale_tables("nadt")
        S.barrier()
        with ExitStack() as e3:
            xt = [_sb(nc, e3, "sxt%d" % i, [128, D], F32) for i in range(2)]
            xt_tok = [Tok(), Tok()]
            xt_ch = [S.chan(), S.chan()]
            xn = [_sb(nc, e3, "sxn%d" % i, [128, D], BF16) for i in range(2)]
            xn_tok = [Tok(), Tok()]
            ss = [_sb(nc, e3, "sss%d" % i, [128, 1], F32) for i in range(2)]
            ss_tok = [Tok(), Tok()]
            rstd = [_sb(nc, e3, "srstd%d" % i, [128, 1], F32) for i in range(2)]
            rstd_tok = [Tok(), Tok()]
            xnT = [_sb(nc, e3, "sxnT", [128, 8, SEGT], BF16)] * 2
            xnT_tok = [Tok()] * 2
            uT = [_sb(nc, e3, "uT%d" % i, [128, 4, SEGT], BF16) for i in range(2)]
            uT_tok = [Tok(), Tok()]
            tmp4 = [_sb(nc, e3, "tm%d" % i, [128, SEGT], F32) for i in range(4)]
            tmp4_tok = [Tok() for _ in range(4)]
            Wr = [_sb(nc, e3, "Wr%d" % i, [128, SEGT], F32) for i in range(2)]
            Wi = [_sb(nc, e3, "Wi%d" % i, [128, SEGT], F32) for i in range(2)]
            Vr = [_sb(nc, e3, "Vr%d" % i, [128, SEGT], F32) for i in range(2)]
            Vi = [_sb(nc, e3, "Vi%d" % i, [128, SEGT], F32) for i in range(2)]
            W_tok = [Tok(), Tok()]
            V_tok = [Tok(), Tok()]
            Xb = [_sb(nc, e3, "Xb%d" % i, [128, 2, SEGT], BF16) for i in range(2)]
            Xb_tok = [Tok(), Tok()]
            xin = _sb(nc, e3, "xin", [128, 32], F32)
            xin_tok = Tok()
            selt = _sb(nc, e3, "selt", [128, 32, 16], F32)
            rot = _sb(nc, e3, "rot", [128, 4], F32)
            rot_tok = Tok()
            yb = _sb(nc, e3, "yb", [128, 4, SEGT], BF16)
            y_tok = [Tok() for _ in range(4)]
            g1 = _sb(nc, e3, "g1", [128, SEGT], F32)
            g2 = _sb(nc, e3, "g2", [128, SEGT], F32)
            g_tok = Tok()
            ti = 0
            pcount = 0
            for sgi in range(NSEG, NSEG + NOWN):
                own = sgi >= NSEG
                sg = sgi - NSEG if own else sgi
                src = I["x_own"] if own else I["x_full"]
                xb = sgi % 2
                for m in range(4):
                    a = ti % 2
                    ti += 1
                    r0 = sg * SEGT + m * 128
                    S.dma("sp", xt_ch[a], xt[a][:], src[r0:r0 + 128, :], writes=[xt_tok[a]])
                    rms_scale(C, xt[a][:], xt_tok[a], rstd[a][:], rstd_tok[a], xn[a][:], xn_tok[a], ss[a][:], ss_tok[a])
                    S.op("dve", lambda e, a=a: e.tensor_scalar(xn[a][:], xt[a][:], rstd[a][:], None, ALU.mult),
                         reads=[xt_tok[a], rstd_tok[a]], writes=[xn_tok[a]])
                    ptb, pt_tok = transpose_to(C, xn[a], xn_tok[a], None, None)
                    S.op("act", lambda e, xb=xb, m=m, ptb=ptb: e.copy(out=xnT[xb][:, :, m * 128:(m + 1) * 128],
                                                                      in_=ptb.rearrange("p (k t) -> p k t", k=8)),
                         reads=[pt_tok], writes=[xnT_tok[xb]])
                for ct in range(4):
                    pb = ct % 2
                    for kt in range(8):
                        S.op("pe", lambda e, kt=kt, ct=ct, pb=pb, xb=xb: e.matmul(
                            ps[pb][:], lhsT=wu[:, kt, ct * 128:(ct + 1) * 128], rhs=xnT[xb][:, kt, :],
                            start=(kt == 0), stop=(kt == 7)), reads=[w_tok, xnT_tok[xb]], writes=[ps_tok[pb]])
                    S.op("act", lambda e, ct=ct, pb=pb, xb=xb: e.copy(out=uT[xb][:, ct, :], in_=ps[pb][:]),
                         reads=[ps_tok[pb]], writes=[uT_tok[xb]])
                if own:
                    S.op("dve", lambda e, sg=sg: e.tensor_tensor(out=selt[:], in0=Xst[:].rearrange("p r k s -> p (r k) s")[:, :, 0:16],
                                                                in1=segsel[:, sg:sg + 1, :].to_broadcast([128, 32, 16]), op=ALU.mult),
                         reads=[xst_tok, small_tok], writes=[xin_tok])
                    S.op("dve", lambda e: e.tensor_reduce(out=xin[:], in_=selt[:], axis=AX.X, op=ALU.add),
                         reads=[xin_tok], writes=[xin_tok])
                for ct in range(4):
                    yps = 4 + ct % 2
                    for kk in range(4):
                        k = ct * 4 + kk
                        pp = pcount % 2
                        pcount += 1
                        bur, bui = 2 * pp, 2 * pp + 1
                        bur, bui = (2, 3) if pp == 0 else (0, 1)
                        S.op("pe", lambda e: e.matmul(ps[bur][:], lhsT=bre[:, k, :], rhs=uT[xb][:, ct, :], start=True, stop=True),
                             reads=[bc_tok, uT_tok[xb]], writes=[ps_tok[bur]])
                        S.op("pe", lambda e: e.matmul(ps[bui][:], lhsT=bim[:, k, :], rhs=uT[xb][:, ct, :], start=True, stop=True),
                             reads=[bc_tok, uT_tok[xb]], writes=[ps_tok[bui]])
                        ck, sk = cosT[:, k, :], sinT[:, k, :]
                        S.op("dve", lambda e: e.tensor_tensor(out=tmp4[0][:], in0=ps[bur][:], in1=ck, op=ALU.mult),
                             reads=[ps_tok[bur], tab_tok], writes=[tmp4_tok[0]])
                        S.op("dve", lambda e: e.tensor_tensor(out=tmp4[1][:], in0=ps[bui][:], in1=sk, op=ALU.mult),
                             reads=[ps_tok[bui], tab_tok], writes=[tmp4_tok[1]])
                        S.op("pool", lambda e: e.tensor_tensor(out=Wr[pp][:], in0=tmp4[0][:], in1=tmp4[1][:], op=ALU.add),
                             reads=[tmp4_tok[0], tmp4_tok[1]], writes=[W_tok[pp]])
                        S.op("dve", lambda e: e.tensor_tensor(out=tmp4[2][:], in0=ps[bui][:], in1=ck, op=ALU.mult),
                             reads=[ps_tok[bui], tab_tok], writes=[tmp4_tok[2]])
                        S.op("dve", lambda e: e.tensor_tensor(out=tmp4[3][:], in0=ps[bur][:], in1=sk, op=ALU.mult),
                             reads=[ps_tok[bur], tab_tok], writes=[tmp4_tok[3]])
                        S.op("pool", lambda e: e.tensor_tensor(out=Wi[pp][:], in0=tmp4[2][:], in1=tmp4[3][:], op=ALU.subtract),
                             reads=[tmp4_tok[2], tmp4_tok[3]], writes=[W_tok[pp]])
                        if own:
                            ir, ii = xin[:, k:k + 1], xin[:, 16 + k:16 + k + 1]
                            itoks = [xin_tok]
                        else:
                            ir, ii = Xst[:, 2 * k, sg:sg + 1], Xst[:, 2 * k + 1, sg:sg + 1]
                            itoks = [xst_tok]
                        rb = P["rho"][:, k:k + 1].to_broadcast([128, SEGT])
                        S.op("dve", lambda e: e.tensor_tensor_scan(out=Vr[pp][:], data0=rb, data1=Wr[pp][:], initial=ir,
                                                                   op0=ALU.mult, op1=ALU.add),
                             reads=[W_tok[pp], p_tok] + itoks, writes=[V_tok[pp]])
                        S.op("dve", lambda e: e.tensor_tensor_scan(out=Vi[pp][:], data0=rb, data1=Wi[pp][:], initial=ii,
                                                                   op0=ALU.mult, op1=ALU.add),
                             reads=[W_tok[pp], p_tok] + itoks, writes=[V_tok[pp]])
                        if not own:
                            cl, sl_ = cosT[:, k, SEGT - 1:SEGT], sinT[:, k, SEGT - 1:SEGT]
                            vr, vi = Vr[pp][:, SEGT - 1:SEGT], Vi[pp][:, SEGT - 1:SEGT]
                            S.op("dve", lambda e: e.tensor_tensor(out=rot[:, 0:1], in0=vi, in1=sl_, op=ALU.mult),
                                 reads=[V_tok[pp], tab_tok], writes=[rot_tok])
                            S.op("dve", lambda e: e.scalar_tensor_tensor(out=Xst[:, 2 * k, sg + 1:sg + 2], in0=vr, scalar=cl, in1=rot[:, 0:1],
                                                                         op0=ALU.mult, op1=ALU.subtract),
                                 reads=[V_tok[pp], tab_tok, rot_tok], writes=[xst_tok])
                            S.op("dve", lambda e: e.tensor_tensor(out=rot[:, 1:2], in0=vr, in1=sl_, op=ALU.mult),
                                 reads=[V_tok[pp], tab_tok], writes=[rot_tok])
                            S.op("dve", lambda e: e.scalar_tensor_tensor(out=Xst[:, 2 * k + 1, sg + 1:sg + 2], in0=vi, scalar=cl, in1=rot[:, 1:2],
                                                                         op0=ALU.mult, op1=ALU.add),
                                 reads=[V_tok[pp], tab_tok, rot_tok], writes=[xst_tok])
                        else:
                            S.op("dve", lambda e: e.tensor_tensor(out=tmp4[0][:], in0=Vr[pp][:], in1=ck, op=ALU.mult),
                                 reads=[V_tok[pp], tab_tok], writes=[tmp4_tok[0]])
                            S.op("pool", lambda e: e.tensor_tensor(out=tmp4[1][:], in0=Vi[pp][:], in1=sk, op=ALU.mult),
                                 reads=[V_tok[pp], tab_tok], writes=[tmp4_tok[1]])
                            S.op("pool", lambda e: e.tensor_tensor(out=Xb[pp][:, 0, :], in0=tmp4[0][:], in1=tmp4[1][:], op=ALU.subtract),
                                 reads=[tmp4_tok[0], tmp4_tok[1]], writes=[Xb_tok[pp]])
                            S.op("dve", lambda e: e.tensor_tensor(out=tmp4[2][:], in0=Vr[pp][:], in1=sk, op=ALU.mult),
                                 reads=[V_tok[pp], tab_tok], writes=[tmp4_tok[2]])
                            S.op("pool", lambda e: e.tensor_tensor(out=tmp4[3][:], in0=Vi[pp][:], in1=ck, op=ALU.mult),
                                 reads=[V_tok[pp], tab_tok], writes=[tmp4_tok[3]])
                            S.op("pool", lambda e: e.tensor_tensor(out=Xb[pp][:, 1, :], in0=tmp4[2][:], in1=tmp4[3][:], op=ALU.add),
                                 reads=[tmp4_tok[2], tmp4_tok[3]], writes=[Xb_tok[pp]])
                            S.op("pe", lambda e: e.matmul(ps[yps][:], lhsT=cre[:, k, :], rhs=Xb[pp][:, 0, :], start=(kk == 0), stop=False),
                                 reads=[bc_tok, Xb_tok[pp]], writes=[ps_tok[yps]])
                            S.op("pe", lambda e: e.matmul(ps[yps][:], lhsT=ncim[:, k, :], rhs=Xb[pp][:, 1, :], start=False, stop=False),
                                 reads=[bc_tok, Xb_tok[pp]], writes=[ps_tok[yps]])
                    if own:
                        S.op("pe", lambda e: e.matmul(ps[yps][:], lhsT=diagD[:, ct, :], rhs=uT[xb][:, ct, :], start=False, stop=True),
                             reads=[w_tok, uT_tok[xb]], writes=[ps_tok[yps]])
                        S.op("act", lambda e: e.activation(out=g1[:], in_=ps[yps][:], func=AF.Square), reads=[ps_tok[yps]], writes=[g_tok])
                        S.op("dve", lambda e: e.tensor_scalar(g1[:], g1[:], 0.044715, 1.0, ALU.mult, ALU.add), reads=[g_tok], writes=[g_tok])
                        S.op("dve", lambda e: e.tensor_tensor(out=g1[:], in0=ps[yps][:], in1=g1[:], op=ALU.mult),
                             reads=[g_tok, ps_tok[yps]], writes=[g_tok])
                        S.op("act", lambda e: e.activation(out=g2[:], in_=g1[:], func=AF.Sigmoid, scale=1.5957691216057308),
                             reads=[g_tok], writes=[g_tok])
                        S.op("dve", lambda e, ct=ct: e.tensor_tensor(out=yb[:, ct, :], in0=ps[yps][:], in1=g2[:], op=ALU.mult),
                             reads=[g_tok, ps_tok[yps]], writes=[y_tok[ct]])
                if own:
                    for c2 in range(4):
                        gp = 4 + c2 % 2
                        for kt in range(4):
                            S.op("pe", lambda e, kt=kt, c2=c2, gp=gp: e.matmul(ps[gp][:], lhsT=wglu[:, kt, c2 * 128:(c2 + 1) * 128],
                                                                              rhs=yb[:, kt, :], start=(kt == 0), stop=(kt == 3)),
                                 reads=[w_tok] + y_tok, writes=[ps_tok[gp]])
                        S.op("act", lambda e, c2=c2, gp=gp: e.activation(out=g2[:], in_=ps[gp][:], func=AF.Sigmoid, bias=bglu[:, c2:c2 + 1]),
                             reads=[ps_tok[gp], small_tok], writes=[g_tok])
                        S.op("dve", lambda e, c2=c2, sg=sg: e.tensor_tensor(out=ssmT[:, c2, sg * SEGT:(sg + 1) * SEGT], in0=yb[:, c2, :],
                                                                           in1=g2[:], op=ALU.mult),
                             reads=[g_tok, y_tok[c2]], writes=[ssm_tok[sg]])
            S.barrier()
        S.barrier()


def _rms_rstd(C, S, src_ap, src_tok, junk_ap, junk_tok, ss, ss_tok, rstd, rstd_tok, n=D):
    S.op("act", lambda e: e.activation(out=junk_ap, in_=src_ap, func=AF.Square, accum_out=ss[:]),
         reads=[src_tok], writes=[junk_tok, ss_tok])
    S.op("act", lambda e: e.activation(out=rstd[:], in_=ss[:], func=AF.Ln, bias=C.eps_col[:], scale=1.0 / n),
         reads=[ss_tok, C.const_tok], writes=[rstd_tok])
    S.op("act", lambda e: e.activation(out=rstd[:], in_=rstd[:], func=AF.Exp, scale=-0.5),
         reads=[rstd_tok], writes=[rstd_tok])


def _load_cast(S, nc, dst, src, gcol, gtok, stg, stg_tok, stg_ch, dst_tok, nkt, ncols, cnt):
    for kt in range(nkt):
        for c0 in range(0, ncols, 512):
            sl = cnt[0] % 2
            cnt[0] += 1
            w = min(512, ncols - c0)
            S.dma("sp", stg_ch[sl], stg[sl][:, 0:w], src[:, kt, c0:c0 + w], writes=[stg_tok[sl]])
            eng = "dve" if sl == 0 else "pool"
            if gcol is not None:
                S.op(eng, lambda e, kt=kt, c0=c0, w=w, sl=sl: e.tensor_scalar(dst[:, kt, c0:c0 + w], stg[sl][:, 0:w],
                                                                            gcol[:, kt:kt + 1], None, ALU.mult),
                     reads=[stg_tok[sl], gtok], writes=[dst_tok])
            else:
                S.op(eng, lambda e, kt=kt, c0=c0, w=w, sl=sl: e.tensor_copy(out=dst[:, kt, c0:c0 + w], in_=stg[sl][:, 0:w]),
                     reads=[stg_tok[sl]], writes=[dst_tok])


def phase_post(C, nc, S, ps, ps_tok, I, attnT, attn_tok, ssmT, ssm_tok, y, dbg):
    NT = NOWN * 4
    h2buf = nc.dram_tensor("h2buf", [NOWN * SEGT, D], F32, kind="Internal").ap()
    h2_tok = [Tok() for _ in range(NT)]
    alla = [t for row in attn_tok for t in row]
    bank = [0]

    def nbk():
        b = bank[0] % 6
        bank[0] += 1
        return b

    eo = ExitStack()
    if dbg not in ("h1", "h2"):
        wpq = _sb(nc, eo, "wpq", [128, 8, 2048], BF16)
        skT = _sb(nc, eo, "skT_sb", [128, 16, 128], BF16)
        pw_tok = Tok()
        gffn = _sb(nc, eo, "gffn", [128, 8], F32)
        gffn_rep = _sb(nc, eo, "gffn_rep_sb", [128, D], F32)
        gfin_rep = _sb(nc, eo, "gfin_rep_sb", [128, D], F32)
        iota256 = _sb(nc, eo, "iota256", [128, 256], F32)
        pg_tok = Tok()
        pchg = S.chan()
        S.dma("sp", pchg, gffn[:], I["g_ffn"][:, :], writes=[pg_tok])
        S.dma("sp", pchg, gffn_rep[:], I["gffn_rep"][:, :], writes=[pg_tok])
        S.dma("sp", pchg, gfin_rep[:], I["gfin_rep"][:, :], writes=[pg_tok])
        S.op("pool", lambda e: e.iota(iota256[:], pattern=[[1, 256]], base=0, channel_multiplier=0,
                                      allow_small_or_imprecise_dtypes=True), writes=[pg_tok])
        pstg = [_sb(nc, eo, "qstg%d" % i, [128, 512], F32) for i in range(2)]
        pstg_tok = [Tok(), Tok()]
        pstg_ch = [S.chan(), S.chan()]
        pcnt = [0]
        _load_cast(S, nc, wpq, I["w_pq"], gffn, pg_tok, pstg, pstg_tok, pstg_ch, pw_tok, 8, 2048, pcnt)
        _load_cast(S, nc, skT, I["skT"], None, None, pstg, pstg_tok, pstg_ch, pw_tok, 16, 128, pcnt)

    uvbf = nc.dram_tensor("uvbf", [16384, 2048], BF16, kind="Internal").ap()
    uv_toks = [Tok() for _ in range(32)]
    if dbg not in ("h1", "h2"):
        with ExitStack() as e2:
            cin = [_sb(nc, e2, "cin%d" % i, [128, 8, D], F32) for i in range(2)]
            cin_tok = [Tok(), Tok()]
            cin_ch = [S.chan(), S.chan()]
            cout = [_sb(nc, e2, "cout%d" % i, [128, 8, D], BF16) for i in range(2)]
            cout_tok = [[Tok() for _ in range(8)] for _ in range(2)]
            cout_ch = [S.chan(), S.chan()]
            ci = 0
            engs = ("act", "dve", "pool", "dve", "act", "dve", "act", "dve")
            for tbl, src in enumerate((I["peer_u"], I["peer_v"])):
                for chk in range(16):
                    sl = ci % 2
                    r0 = chk * 1024
                    S.dma("sp", cin_ch[sl], cin[sl][:], src[r0:r0 + 1024, :].rearrange("(p a) d -> p a d", a=8),
                          writes=[cin_tok[sl]])
                    for a in range(8):
                        if engs[a] == "act":
                            S.op("act", lambda e, a=a, sl=sl: e.copy(out=cout[sl][:, a, :], in_=cin[sl][:, a, :]),
                                 reads=[cin_tok[sl]], writes=[cout_tok[sl][a]])
                        else:
                            S.op(engs[a], lambda e, a=a, sl=sl: e.tensor_copy(out=cout[sl][:, a, :], in_=cin[sl][:, a, :]),
                                 reads=[cin_tok[sl]], writes=[cout_tok[sl][a]])
                    S.dma("sp", cout_ch[sl],
                          uvbf[r0:r0 + 1024, tbl * 1024:(tbl + 1) * 1024].rearrange("(p a) d -> p a d", a=8), cout[sl][:],
                          reads=cout_tok[sl], writes=[uv_toks[ci]])
                    ci += 1
            S.barrier()

    with ExitStack() as e2:
        wo = _sb(nc, e2, "wo", [128, 8, D], BF16)
        wxq = _sb(nc, e2, "wxq", [128, 8, D], BF16)
        wxo = _sb(nc, e2, "wxo", [128, 8, D], BF16)
        KmT = _sb(nc, e2, "KmT", [128, 8, 256], BF16)
        Vm = _sb(nc, e2, "Vm", [128, 2, D], BF16)
        w_tok = Tok()
        kv_tok = Tok()
        gcols = _sb(nc, e2, "gcols", [128, 3, 8], F32)
        g_tok = Tok()
        chg = S.chan()
        S.dma("sp", chg, gcols[:, 0, :], I["g_out"][:, :], writes=[g_tok])
        S.dma("sp", chg, gcols[:, 1, :], I["g_xattn"][:, :], writes=[g_tok])
        S.dma("sp", chg, gcols[:, 2, :], I["g_mem"][:, :], writes=[g_tok])
        stg = [_sb(nc, e2, "pstg%d" % i, [128, 512], F32) for i in range(2)]
        stg_tok = [Tok(), Tok()]
        stg_ch = [S.chan(), S.chan()]
        cnt = [0]
        _load_cast(S, nc, wo, I["w_out"], gcols[:, 0, :], g_tok, stg, stg_tok, stg_ch, w_tok, 8, D, cnt)
        _load_cast(S, nc, wxq, I["w_xq"], gcols[:, 1, :], g_tok, stg, stg_tok, stg_ch, w_tok, 8, D, cnt)
        _load_cast(S, nc, wxo, I["w_xo"], None, None, stg, stg_tok, stg_ch, w_tok, 8, D, cnt)
        h = _sb(nc, e2, "h", [128, D], F32)
        h_tok = Tok()
        h_ch = S.chan()
        hn = _sb(nc, e2, "hn", [128, D], BF16)
        hn_tok = Tok()
        hnT = _sb(nc, e2, "hnT", [128, 8, 128], BF16)
        hnT_tok = Tok()
        ss = _sb(nc, e2, "pss", [128, 1], F32)
        ss_tok = Tok()
        rstd = _sb(nc, e2, "prstd", [128, 1], F32)
        rstd_tok = Tok()
        with ExitStack() as e3:
            memT = _sb(nc, e3, "memT", [128, 8, 256], BF16)
            memT_tok = Tok()
            wch = _sb(nc, e3, "wch", [128, 8, 512], BF16)
            wch_tok = Tok()
            for mt in range(2):
                S.dma("sp", h_ch, h[:], I["mem"][mt * 128:(mt + 1) * 128, :], writes=[h_tok])
                _rms_rstd(C, S, h[:], h_tok, hn[:], hn_tok, ss, ss_tok, rstd, rstd_tok)
                S.op("dve", lambda e: e.tensor_scalar(hn[:], h[:], rstd[:], None, ALU.mult), reads=[h_tok, rstd_tok], writes=[hn_tok])
                ptb, pt_tok = transpose_to(C, hn, hn_tok, None, None)
                S.op("act", lambda e, mt=mt, ptb=ptb: e.copy(out=memT[:, :, mt * 128:(mt + 1) * 128],
                                                           in_=ptb.rearrange("p (k t) -> p k t", k=8)),
                     reads=[pt_tok], writes=[memT_tok])
            for cc in range(4):
                _load_cast(S, nc, wch, I["w_xkv"][:, :, cc * 512:(cc + 1) * 512], gcols[:, 2, :], g_tok, stg, stg_tok, stg_ch,
                           wch_tok, 8, 512, cnt)
                if cc < 2:
                    for j4 in range(4):
                        b_ = nbk()
                        for kt in range(8):
                            S.op("pe", lambda e, kt=kt, j4=j4, b_=b_: e.matmul(ps[b_][:, 0:256], lhsT=wch[:, kt, j4 * 128:(j4 + 1) * 128],
                                                                             rhs=memT[:, kt, :], start=(kt == 0), stop=(kt == 7)),
                                 reads=[wch_tok, memT_tok], writes=[ps_tok[b_]])
                        S.op("act", lambda e, j4=j4, b_=b_, cc=cc: e.copy(out=KmT[:, cc * 4 + j4, :], in_=ps[b_][:, 0:256]),
                             reads=[ps_tok[b_]], writes=[kv_tok])
                else:
                    for mt in range(2):
                        b_ = nbk()
                        for kt in range(8):
                            S.op("pe", lambda e, kt=kt, mt=mt, b_=b_: e.matmul(ps[b_][:], lhsT=memT[:, kt, mt * 128:(mt + 1) * 128],
                                                                             rhs=wch[:, kt, :], start=(kt == 0), stop=(kt == 7)),
                                 reads=[wch_tok, memT_tok], writes=[ps_tok[b_]])
                        S.op("act", lambda e, mt=mt, b_=b_, cc=cc: e.copy(out=Vm[:, mt, (cc - 2) * 512:(cc - 1) * 512], in_=ps[b_][:]),
                             reads=[ps_tok[b_]], writes=[kv_tok])
            S.barrier()
        def mkset(tag):
            B = {}
            for nm, shp, dt_ in (("sq4", [128, 4, 128], BF16), ("rs2", [128, 2], F32), ("qT", [128, 8, 128], BF16),
                                 ("pp", [128, 4, 256], BF16), ("pT", [128, 8, 128], BF16), ("oT", [128, 8, 128], BF16),
                                 ("mx", [128, 4], F32), ("sm", [128, 4], F32), ("h", [128, D], F32), ("hn", [128, D], BF16),
                                 ("hnT", [128, 8, 128], BF16), ("ss", [128, 1], F32), ("rstd", [128, 1], F32)):
                B[nm] = _sb(nc, e2, nm + tag, shp, dt_)
            for nm in ("sq_tok", "rs2_tok", "qT_tok", "pp_tok", "pT_tok", "oT_tok", "sm_tok", "h_tok", "hn_tok", "hnT_tok",
                       "ss_tok", "rstd_tok"):
                B[nm] = Tok()
            B["h_ch"] = S.chan()
            return B

        class _Rec1:
            def __init__(self):
                self.items = []

            def op(self, *a, **k):
                self.items.append(("op", a, k))

            def dma(self, *a, **k):
                self.items.append(("dma", a, k))

        def emit_tile(S_, tile, nbk_, sq4, rs2, qT, pp, pT, oT, mx, sm, h, hn, hnT, ss, rstd, sq_tok, rs2_tok, qT_tok, pp_tok,
                      pT_tok, oT_tok, sm_tok, h_tok, hn_tok, hnT_tok, ss_tok, rstd_tok, h_ch):
            saved = C.S
            C.S = S_
            t0 = tile * 128
            slot = tile // 4
            tsl = slice(t0, t0 + 128)
            for which, (src, toks) in enumerate(((attnT, attn_tok[slot]), (ssmT, [ssm_tok[slot]]))):
                S_.op("act", lambda e, src=src: e.activation(out=sq4[:], in_=src[:, :, tsl], func=AF.Square),
                     reads=toks, writes=[sq_tok])
                b_ = nbk_()
                for hp in range(4):
                    S_.op("pe", lambda e, hp=hp, b_=b_: e.matmul(ps[b_][:, 0:1], lhsT=sq4[:, hp, :], rhs=C.ones_bf[:, 0:1],
                                                                start=(hp == 0), stop=(hp == 3)),
                         reads=[sq_tok, C.const_tok], writes=[ps_tok[b_]])
                S_.op("act", lambda e, which=which, b_=b_: e.activation(out=rs2[:, which:which + 1], in_=ps[b_][:, 0:1], func=AF.Ln,
                                                                       bias=C.eps_col[:], scale=1.0 / 512),
                     reads=[ps_tok[b_], C.const_tok], writes=[rs2_tok])
            S_.op("act", lambda e: e.activation(out=rs2[:], in_=rs2[:], func=AF.Exp, scale=-0.5), reads=[rs2_tok], writes=[rs2_tok])
            S_.dma("sp", h_ch, h[:], I["x_own"][t0:t0 + 128, :], writes=[h_tok])
            for which, (src, toks) in enumerate(((attnT, attn_tok[slot]), (ssmT, [ssm_tok[slot]]))):
                for n2 in range(2):
                    b_ = nbk_()
                    for hp in range(4):
                        S_.op("pe", lambda e, hp=hp, b_=b_, src=src, which=which, n2=n2: e.matmul(
                            ps[b_][:], lhsT=src[:, hp, tsl], rhs=wo[:, which * 4 + hp, n2 * 512:(n2 + 1) * 512],
                            start=(hp == 0), stop=(hp == 3)), reads=toks + [w_tok], writes=[ps_tok[b_]])
                    S_.op("dve", lambda e, b_=b_, which=which, n2=n2: e.scalar_tensor_tensor(
                        out=h[:, n2 * 512:(n2 + 1) * 512], in0=ps[b_][:], scalar=rs2[:, which:which + 1],
                        in1=h[:, n2 * 512:(n2 + 1) * 512], op0=ALU.mult, op1=ALU.add),
                        reads=[ps_tok[b_], rs2_tok], writes=[h_tok])
            if dbg == "h1":
                S_.dma("sp", h_ch, y[t0:t0 + 128, :], h[:], reads=[h_tok], writes=[h2_tok[tile]])
                C.S = saved
                return
            _rms_rstd(C, S_, h[:], h_tok, hn[:], hn_tok, ss, ss_tok, rstd, rstd_tok)
            S_.op("dve", lambda e: e.tensor_scalar(hn[:], h[:], rstd[:], None, ALU.mult), reads=[h_tok, rstd_tok], writes=[hn_tok])
            ptb, pt_tok = transpose_to(C, hn, hn_tok, None, None)
            S_.op("act", lambda e, ptb=ptb: e.copy(out=hnT[:], in_=ptb.rearrange("p (k t) -> p k t", k=8)),
                 reads=[pt_tok], writes=[hnT_tok])
            for half in range(2):
                b_ = nbk_()
                for j4 in range(4):
                    hc = half * 4 + j4
                    for kt in range(8):
                        S_.op("pe", lambda e, kt=kt, hc=hc, j4=j4, b_=b_: e.matmul(
                            ps[b_][:, j4 * 128:(j4 + 1) * 128], lhsT=wxq[:, kt, hc * 128:(hc + 1) * 128], rhs=hnT[:, kt, :],
                            start=(kt == 0), stop=(kt == 7)), reads=[w_tok, hnT_tok], writes=[ps_tok[b_]])
                S_.op("act", lambda e, half=half, b_=b_: e.mul(out=qT[:, half * 4:(half + 1) * 4, :],
                                                             in_=ps[b_][:].rearrange("p (a b) -> p a b", a=4), mul=0.0625),
                     reads=[ps_tok[b_]], writes=[qT_tok])
            sb_ = [nbk_(), nbk_()]
            for hh in range(4):
                b_ = sb_[hh // 2]
                for c2 in range(2):
                    S_.op("pe", lambda e, hh=hh, c2=c2, b_=b_: e.matmul(
                        ps[b_][:, (hh % 2) * 256:(hh % 2 + 1) * 256], lhsT=qT[:, 2 * hh + c2, :], rhs=KmT[:, 2 * hh + c2, :],
                        start=(c2 == 0), stop=(c2 == 1)), reads=[qT_tok, kv_tok], writes=[ps_tok[b_]])
            for i2 in range(2):
                b_ = sb_[i2]
                S_.op("dve", lambda e, i2=i2, b_=b_: e.tensor_reduce(out=mx[:, 2 * i2:2 * i2 + 2],
                                                                    in_=ps[b_][:].rearrange("p (a b) -> p a b", a=2),
                                                                    axis=AX.X, op=ALU.max),
                     reads=[ps_tok[b_]], writes=[sm_tok])
            S_.op("dve", lambda e: e.tensor_scalar(mx[:], mx[:], -1.0, None, ALU.mult), reads=[sm_tok], writes=[sm_tok])
            for hh in range(4):
                b_ = sb_[hh // 2]
                S_.op("act", lambda e, hh=hh, b_=b_: e.activation(out=pp[:, hh, :], in_=ps[b_][:, (hh % 2) * 256:(hh % 2 + 1) * 256],
                                                                 func=AF.Exp, bias=mx[:, hh:hh + 1], accum_out=sm[:, hh:hh + 1]),
                     reads=[ps_tok[b_], sm_tok], writes=[pp_tok, sm_tok])
            S_.op("dve", lambda e: e.reciprocal(out=sm[:], in_=sm[:]), reads=[sm_tok], writes=[sm_tok])
            for hh in range(4):
                S_.op("dve", lambda e, hh=hh: e.tensor_scalar(pp[:, hh, :], pp[:, hh, :], sm[:, hh:hh + 1], None, ALU.mult),
                     reads=[sm_tok, pp_tok], writes=[pp_tok])
            ptb, pt_tok = transpose_to(C, pp[:].rearrange("p a b -> p (a b)"), pp_tok, None, None)
            S_.op("act", lambda e, ptb=ptb: e.copy(out=pT[:], in_=ptb.rearrange("p (k t) -> p k t", k=8)),
                 reads=[pt_tok], writes=[pT_tok])
            for half in range(2):
                b_ = nbk_()
                for j4 in range(4):
                    hc = half * 4 + j4
                    hh, c2 = hc // 2, hc % 2
                    for mt in range(2):
                        S_.op("pe", lambda e, mt=mt, hh=hh, c2=c2, j4=j4, b_=b_: e.matmul(
                            ps[b_][:, j4 * 128:(j4 + 1) * 128], lhsT=Vm[:, mt, hh * 256 + c2 * 128:hh * 256 + (c2 + 1) * 128],
                            rhs=pT[:, 2 * hh + mt, :], start=(mt == 0), stop=(mt == 1)),
                            reads=[kv_tok, pT_tok], writes=[ps_tok[b_]])
                S_.op("act", lambda e, half=half, b_=b_: e.copy(out=oT[:, half * 4:(half + 1) * 4, :],
                                                              in_=ps[b_][:].rearrange("p (a b) -> p a b", a=4)),
                     reads=[ps_tok[b_]], writes=[oT_tok])
            for n2 in range(2):
                b_ = nbk_()
                for hc in range(8):
                    S_.op("pe", lambda e, hc=hc, n2=n2, b_=b_: e.matmul(ps[b_][:], lhsT=oT[:, hc, :], rhs=wxo[:, hc, n2 * 512:(n2 + 1) * 512],
                                                                      start=(hc == 0), stop=(hc == 7)),
                         reads=[oT_tok, w_tok], writes=[ps_tok[b_]])
                S_.op("dve", lambda e, n2=n2, b_=b_: e.tensor_tensor(out=h[:, n2 * 512:(n2 + 1) * 512], in0=ps[b_][:],
                                                                    in1=h[:, n2 * 512:(n2 + 1) * 512], op=ALU.add),
                     reads=[ps_tok[b_]], writes=[h_tok])
            dst = y if dbg == "h2" else h2buf
            S_.dma("sp", h_ch, dst[t0:t0 + 128, :], h[:], reads=[h_tok], writes=[h2_tok[tile]])

            C.S = saved

        sets1 = [mkset("_e"), mkset("_o")]
        bk = [[0], [0]]

        def mk_nbk(par):
            def f():
                b = 3 * par + bk[par][0] % 3
                bk[par][0] += 1
                return b
            return f

        for t2 in range(0, NT, 2):
            recs = []
            for par in range(2):
                r_ = _Rec1()
                C.tp_force = par
                emit_tile(r_, t2 + par, mk_nbk(par), **sets1[par])
                C.tp_force = None
                recs.append(r_.items)
            while recs[0] or recs[1]:
                for par in range(2):
                    for _ in range(6):
                        if recs[par]:
                            kind, a, k = recs[par].pop(0)
                            getattr(S, kind)(*a, **k)
        S.barrier()
    if dbg in ("h1", "h2"):
        S.wait_tok("sp", h2_tok)
        eo.close()
        return

    with ExitStack() as e2:
        h = _sb(nc, e2, "h_b", [128, D], F32)
        h_tok = Tok()
        h_ch = S.chan()
        hn = _sb(nc, e2, "hn_b", [128, D], BF16)
        hn_tok = Tok()
        hnT = _sb(nc, e2, "hnT_b", [128, 8, 128], BF16)
        hnT_tok = Tok()
        hn3 = _sb(nc, e2, "hn3", [128, D], F32)
        hn3_tok = Tok()
        ss = _sb(nc, e2, "qss", [128, 1], F32)
        ss_tok = Tok()
        rstd = _sb(nc, e2, "qrstd", [128, 1], F32)
        rstd_tok = Tok()
        qpT = _sb(nc, e2, "qpT", [128, 16, 128], BF16)
        qpT_tok = Tok()
        sc = _sb(nc, e2, "sc", [128, 16, 128], F32)
        sc_tok = Tok()
        scr = _sb(nc, e2, "scr", [128, 2048], F32)
        scr_tok = Tok()
        hv = _sb(nc, e2, "hv", [128, 16, 16], F32)
        hi = _sb(nc, e2, "hi", [128, 16, 16], U32)
        hif = _sb(nc, e2, "hif", [128, 16, 16], F32)
        hv_tok = Tok()
        cand = _sb(nc, e2, "cand", [128, 8, 256], F32)
        eidx = _sb(nc, e2, "eidx", [128, 8, 256], F32)
        e0 = _sb(nc, e2, "e0", [128, 8, 16], F32)
        cand_tok = Tok()
        bv = _sb(nc, e2, "bv", [128, 8, 16], F32)
        bp = _sb(nc, e2, "bp", [128, 8, 16], U32)
        bpf = _sb(nc, e2, "bpf", [128, 8, 16], F32)
        bv_tok = Tok()
        junk = [_sb(nc, e2, "junk256_%d" % i, [128, 256], F32) for i in range(4)]
        junk_tok = [Tok() for _ in range(4)]
        eidc_tok = [Tok() for _ in range(128)]
        jq = [0]
        eidf = _sb(nc, e2, "eidf", [128, 128], F32)
        eid = _sb(nc, e2, "eid", [128, 128], U32)
        eid_tok = Tok()
        gt = _sb(nc, e2, "gt", [128, 8, 16], F32)
        gs = _sb(nc, e2, "gs", [128, 8], F32)
        nb0 = _sb(nc, e2, "nb0", [128, 8], F32)
        gt_tok = Tok()
        actc = _sb(nc, e2, "actc", [128, 128], F32)
        act_tok = Tok()
        wgt = _sb(nc, e2, "wgt", [128, 128], F32)
        wg2 = _sb(nc, e2, "wg2", [128, 128], F32)
        wgt_tok = Tok()
        NG = 8
        gbuf = [_sb(nc, e2, "gbuf%d" % i, [128, 2 * D], BF16) for i in range(NG)]
        gbuf_tok = [Tok() for _ in range(NG)]
        gbuf_ch = [S.chan() for _ in range(NG)]
        junk2 = [_sb(nc, e2, "junk2_%d" % i, [128, D], BF16) for i in range(2)]
        junk2_tok = [Tok(), Tok()]
        j2 = [0]
        NDG = 4
        dg = [_sb(nc, e2, "dg%d" % i, [128, 128], BF16) for i in range(NDG)]
        dg_tok = [Tok() for _ in range(NDG)]
        slot_tok = [Tok() for _ in range(128)]
        grp_tok = [Tok() for _ in range(32)]
        di = 0
        ytok = Tok()
        gi = 0
        PLAY_N = 15
        bankA = [0]

        def nbkA():
            b = bankA[0] % 4
            bankA[0] += 1
            return b

        class _Rec:
            def __init__(self):
                self.items = []

            def op(self, *a, **k):
                self.items.append(("op", a, k))

            def dma(self, *a, **k):
                self.items.append(("dma", a, k))

        junkF = _sb(nc, e2, "junkF", [128, D], BF16)
        junkF_tok = Tok()
        ss2 = _sb(nc, e2, "ss2", [128, 1], F32)
        ss2_tok = Tok()
        rstd2 = _sb(nc, e2, "rstd2", [128, 1], F32)
        rstd2_tok = Tok()
        obuf = _sb(nc, e2, "obuf", [128, D], F32)
        obuf_tok = Tok()
        o_ch = S.chan()
        h_b2 = _sb(nc, e2, "h_b2", [128, D], F32)
        hn3_b2 = _sb(nc, e2, "hn3_b2", [128, D], F32)
        eid_b2 = _sb(nc, e2, "eid_b2", [128, 128], U32)
        gt_b2 = _sb(nc, e2, "gt_b2", [128, 8, 16], F32)
        sets = [dict(h=h, h_tok=h_tok, h_ch=h_ch, hn3=hn3, hn3_tok=hn3_tok, eid=eid, eid_tok=eid_tok, gt=gt, gt_tok=gt_tok),
                dict(h=h_b2, h_tok=Tok(), h_ch=S.chan(), hn3=hn3_b2, hn3_tok=Tok(), eid=eid_b2, eid_tok=Tok(), gt=gt_b2, gt_tok=Tok())]

        def emitA(S_, tile, h, h_tok, h_ch, hn3, hn3_tok, eid, eid_tok, gt, gt_tok):
            saved = C.S
            C.S = S_
            t0 = tile * 128
            S_.dma("sp", h_ch, h[:], h2buf[t0:t0 + 128, :], reads=[h2_tok[tile]], writes=[h_tok])
            _rms_rstd(C, S_, h[:], h_tok, hn[:], hn_tok, ss, ss_tok, rstd, rstd_tok)
            S_.op("dve", lambda e: e.tensor_scalar(hn[:], h[:], rstd[:], None, ALU.mult), reads=[h_tok, rstd_tok], writes=[hn_tok])
            S_.op("dve", lambda e: e.scalar_tensor_tensor(out=hn3[:], in0=h[:], scalar=rstd[:], in1=gffn_rep[:], op0=ALU.mult, op1=ALU.mult),
                 reads=[h_tok, rstd_tok, pg_tok], writes=[hn3_tok])
            ptb, pt_tok = transpose_to(C, hn, hn_tok, None, None)
            S_.op("act", lambda e, ptb=ptb: e.copy(out=hnT[:], in_=ptb.rearrange("p (k t) -> p k t", k=8)),
                 reads=[pt_tok], writes=[hnT_tok])
            for q4 in range(4):
                b_ = nbkA()
                for j4 in range(4):
                    ch = q4 * 4 + j4
                    for kt in range(8):
                        S_.op("pe", lambda e, kt=kt, ch=ch, j4=j4, b_=b_: e.matmul(
                            ps[b_][:, j4 * 128:(j4 + 1) * 128], lhsT=wpq[:, kt, ch * 128:(ch + 1) * 128], rhs=hnT[:, kt, :],
                            start=(kt == 0), stop=(kt == 7)), reads=[pw_tok, hnT_tok], writes=[ps_tok[b_]])
                S_.op("act", lambda e, q4=q4, b_=b_: e.copy(out=qpT[:, q4 * 4:(q4 + 1) * 4, :],
                                                          in_=ps[b_][:].rearrange("p (a b) -> p a b", a=4)),
                     reads=[ps_tok[b_]], writes=[qpT_tok])
            for q4 in range(4):
                b_ = nbkA()
                for j4 in range(4):
                    ch = q4 * 4 + j4
                    S_.op("pe", lambda e, ch=ch, j4=j4, b_=b_: e.matmul(ps[b_][:, j4 * 128:(j4 + 1) * 128], lhsT=qpT[:, ch, :],
                                                                      rhs=skT[:, ch, :], start=True, stop=True),
                         reads=[pw_tok, qpT_tok], writes=[ps_tok[b_]])
                S_.op("act", lambda e, q4=q4, b_=b_: e.copy(out=sc[:, q4 * 4:(q4 + 1) * 4, :],
                                                          in_=ps[b_][:].rearrange("p (a b) -> p a b", a=4)),
                     reads=[ps_tok[b_]], writes=[sc_tok])
            scr3 = scr[:].rearrange("p (a b) -> p a b", a=16)
            for ch in range(16):
                S_.op("dve", lambda e, ch=ch: e.max(out=hv[:, ch, 0:8], in_=sc[:, ch, :]), reads=[sc_tok], writes=[hv_tok])
                S_.op("dve", lambda e, ch=ch: e.max_index(out=hi[:, ch, 0:8], in_max=hv[:, ch, 0:8], in_values=sc[:, ch, :]),
                     reads=[sc_tok, hv_tok], writes=[hv_tok])
                S_.op("dve", lambda e, ch=ch: e.match_replace(out=scr3[:, ch, :], in_to_replace=hv[:, ch, 0:8], in_values=sc[:, ch, :],
                                                             imm_value=NEG), reads=[sc_tok, hv_tok], writes=[scr_tok])
                S_.op("dve", lambda e, ch=ch: e.max(out=hv[:, ch, 8:16], in_=scr3[:, ch, :]), reads=[scr_tok], writes=[hv_tok])
                S_.op("dve", lambda e, ch=ch: e.max_index(out=hi[:, ch, 8:16], in_max=hv[:, ch, 8:16], in_values=scr3[:, ch, :]),
                     reads=[scr_tok, hv_tok], writes=[hv_tok])
            S_.op("dve", lambda e: e.tensor_copy(out=hif[:], in_=hi[:]), reads=[hv_tok], writes=[hv_tok])
            hv4 = hv[:].rearrange("p (h i) k -> p h i k", i=2)
            hif4 = hif[:].rearrange("p (h i) k -> p h i k", i=2)
            cand4 = cand[:].rearrange("p h (a b) -> p h a b", a=16)
            eidx4 = eidx[:].rearrange("p h (a b) -> p h a b", a=16)
            S_.op("dve", lambda e: e.tensor_tensor(out=cand4, in0=hv4[:, :, 0, :].unsqueeze(3).to_broadcast([128, 8, 16, 16]),
                                                  in1=hv4[:, :, 1, :].unsqueeze(2).to_broadcast([128, 8, 16, 16]), op=ALU.add),
                 reads=[hv_tok], writes=[cand_tok])
            S_.op("dve", lambda e: e.tensor_scalar(e0[:], hif4[:, :, 0, :], 128.0, None, ALU.mult), reads=[hv_tok], writes=[cand_tok])
            S_.op("dve", lambda e: e.tensor_tensor(out=eidx4, in0=e0[:].unsqueeze(3).to_broadcast([128, 8, 16, 16]),
                                                  in1=hif4[:, :, 1, :].unsqueeze(2).to_broadcast([128, 8, 16, 16]), op=ALU.add),
                 reads=[hv_tok, cand_tok], writes=[cand_tok])
            scr8 = scr[:].rearrange("p (a b) -> p a b", a=8)
            for hh in range(8):
                S_.op("dve", lambda e, hh=hh: e.max(out=bv[:, hh, 0:8], in_=cand[:, hh, :]), reads=[cand_tok], writes=[bv_tok])
                S_.op("dve", lambda e, hh=hh: e.max_index(out=bp[:, hh, 0:8], in_max=bv[:, hh, 0:8], in_values=cand[:, hh, :]),
                     reads=[cand_tok, bv_tok], writes=[bv_tok])
                S_.op("dve", lambda e, hh=hh: e.match_replace(out=scr8[:, hh, :], in_to_replace=bv[:, hh, 0:8], in_values=cand[:, hh, :],
                                                             imm_value=NEG), reads=[cand_tok, bv_tok], writes=[scr_tok])
                S_.op("dve", lambda e, hh=hh: e.max(out=bv[:, hh, 8:16], in_=scr8[:, hh, :]), reads=[scr_tok], writes=[bv_tok])
                S_.op("dve", lambda e, hh=hh: e.max_index(out=bp[:, hh, 8:16], in_max=bv[:, hh, 8:16], in_values=scr8[:, hh, :]),
                     reads=[scr_tok, bv_tok], writes=[bv_tok])
            S_.op("dve", lambda e: e.tensor_copy(out=bpf[:], in_=bp[:]), reads=[bv_tok], writes=[bv_tok])
            for hh in range(8):
                for k in range(16):
                    s_ = hh * 16 + k
                    jb = jq[0] % 4
                    jq[0] += 1
                    S_.op("dve", lambda e, hh=hh, k=k, s_=s_, jb=jb: e.scalar_tensor_tensor(
                        out=junk[jb][:], in0=iota256[:], scalar=bpf[:, hh, k:k + 1], in1=eidx[:, hh, :],
                        op0=ALU.is_equal, op1=ALU.mult, accum_out=eidf[:, s_:s_ + 1]),
                        reads=[bv_tok, cand_tok, pg_tok], writes=[junk_tok[jb], eidc_tok[s_]])
            S_.op("dve", lambda e: e.tensor_scalar(eidf[:], eidf[:], 16383.0, 0.0, ALU.min, ALU.max), reads=[eid_tok] + eidc_tok,
                  writes=[eid_tok] + eidc_tok)
            S_.op("dve", lambda e: e.tensor_copy(out=eid[:], in_=eidf[:]), reads=[eid_tok], writes=[eid_tok])
            S_.op("dve", lambda e: e.tensor_scalar(nb0[:], bv[:, :, 0], -1.0, None, ALU.mult), reads=[bv_tok], writes=[gt_tok])
            for hh in range(8):
                S_.op("act", lambda e, hh=hh: e.activation(out=gt[:, hh, :], in_=bv[:, hh, :], func=AF.Exp, bias=nb0[:, hh:hh + 1],
                                                          accum_out=gs[:, hh:hh + 1]), reads=[bv_tok, gt_tok], writes=[gt_tok])
            S_.op("dve", lambda e: e.reciprocal(out=gs[:], in_=gs[:]), reads=[gt_tok], writes=[gt_tok])
            S_.op("dve", lambda e: e.tensor_tensor(out=gt[:], in0=gt[:], in1=gs[:].unsqueeze(2).to_broadcast([128, 8, 16]), op=ALU.mult),
                 reads=[gt_tok], writes=[gt_tok])

            C.S = saved

        def emitB(tile, play, h, h_tok, h_ch, hn3, hn3_tok, eid, eid_tok, gt, gt_tok):
            nonlocal gi, di
            t0 = tile * 128
            pa, pb_ = 4, 5
            gtf = gt[:].rearrange("p a b -> p (a b)")
            for grp in range(32):
                used = []
                for q in range(4):
                    s_ = grp * 4 + q
                    g_ = gi % NG
                    gi += 1
                    used.append(g_)
                    S.dma("pool", gbuf_ch[g_], gbuf[g_][:], uvbf[:, :], reads=[eid_tok] + uv_toks, writes=[gbuf_tok[g_]],
                          indirect=bass.IndirectOffsetOnAxis(ap=eid[:, s_:s_ + 1], axis=0))
                    jb = j2[0] % 2
                    j2[0] += 1
                    S.op("dve", lambda e, g_=g_, s_=s_, jb=jb: e.scalar_tensor_tensor(out=junk2[jb][:], in0=gbuf[g_][:, 0:D], scalar=1.0, in1=hn3[:],
                                                                                     op0=ALU.mult, op1=ALU.mult, accum_out=actc[:, s_:s_ + 1]),
                         reads=[gbuf_tok[g_], hn3_tok], writes=[junk2_tok[jb], slot_tok[s_]])
                cs = slice(grp * 4, grp * 4 + 4)
                gk = grp_tok[grp]
                S.op("dve", lambda e: e.tensor_tensor(out=wg2[:, cs], in0=actc[:, cs], in1=actc[:, cs], op=ALU.mult),
                     reads=[slot_tok[grp * 4 + q] for q in range(4)], writes=[gk])
                S.op("dve", lambda e: e.tensor_scalar(wg2[:, cs], wg2[:, cs], 0.044715, 1.0, ALU.mult, ALU.add), reads=[gk], writes=[gk])
                S.op("dve", lambda e: e.tensor_tensor(out=wg2[:, cs], in0=wg2[:, cs], in1=actc[:, cs], op=ALU.mult),
                     reads=[gk] + [slot_tok[grp * 4 + q] for q in range(4)], writes=[gk])
                S.op("act", lambda e: e.activation(out=wg2[:, cs], in_=wg2[:, cs], func=AF.Sigmoid, scale=1.5957691216057308),
                     reads=[gk], writes=[gk])
                S.op("dve", lambda e: e.tensor_tensor(out=wgt[:, cs], in0=wg2[:, cs], in1=actc[:, cs], op=ALU.mult),
                     reads=[gk] + [slot_tok[grp * 4 + q] for q in range(4)], writes=[gk])
                S.op("dve", lambda e: e.tensor_tensor(out=wgt[:, cs], in0=wgt[:, cs], in1=gtf[:, cs], op=ALU.mult),
                     reads=[gk, gt_tok], writes=[gk])
                for q in range(4):
                    s_ = grp * 4 + q
                    g_ = used[q]
                    d_ = di % NDG
                    di += 1
                    S.op("act", lambda e, d_=d_, s_=s_: e.activation(out=dg[d_][:], in_=C.ident_bf[:], func=AF.Copy,
                                                                    scale=wgt[:, s_:s_ + 1]),
                         reads=[gk, C.const_tok], writes=[dg_tok[d_]])
                    for n2, pbk in enumerate((pa, pb_)):
                        S.op("pe", lambda e, d_=d_, g_=g_, n2=n2, pbk=pbk, s_=s_: e.matmul(
                            ps[pbk][:], lhsT=dg[d_][:], rhs=gbuf[g_][:, D + n2 * 512:D + (n2 + 1) * 512],
                            start=(s_ == 0), stop=(s_ == 127)), reads=[dg_tok[d_], gbuf_tok[g_]], writes=[ps_tok[pbk]])
                play(PLAY_N)
            for n2, pbk in enumerate((pa, pb_)):
                S.op("dve", lambda e, n2=n2, pbk=pbk: e.tensor_tensor(out=h[:, n2 * 512:(n2 + 1) * 512], in0=ps[pbk][:],
                                                                      in1=h[:, n2 * 512:(n2 + 1) * 512], op=ALU.add),
                     reads=[ps_tok[pbk]], writes=[h_tok])
            if dbg == "h3":
                S.dma("sp", h_ch, y[t0:t0 + 128, :], h[:], reads=[h_tok], writes=[ytok])
                play(10 ** 9)
                return
            _rms_rstd(C, S, h[:], h_tok, junkF[:], junkF_tok, ss2, ss2_tok, rstd2, rstd2_tok)
            S.op("dve", lambda e: e.scalar_tensor_tensor(out=obuf[:], in0=h[:], scalar=rstd2[:], in1=gfin_rep[:], op0=ALU.mult, op1=ALU.mult),
                 reads=[h_tok, rstd2_tok, pg_tok], writes=[obuf_tok])
            S.dma("sp", o_ch, y[t0:t0 + 128, :], obuf[:], reads=[obuf_tok], writes=[ytok])
            play(10 ** 9)

        rec = _Rec()
        emitA(rec, 0, **sets[0])
        for kind, a, k in rec.items:
            getattr(S, kind)(*a, **k)
        for tile in range(NT):
            rec = _Rec()
            if tile + 1 < NT:
                emitA(rec, tile + 1, **sets[(tile + 1) % 2])
            items = rec.items

            def play(n, items=items):
                while n > 0 and items:
                    kind, a, k = items.pop(0)
                    getattr(S, kind)(*a, **k)
                    n -= 1

            emitB(tile, play, **sets[tile % 2])
            play(10 ** 9)
        S.wait_tok("sp", [ytok])
        S.barrier()
    eo.close()


def build(dbg=None):
    nc = bass.Bass("TRN2", target_bir_lowering=False)
    C = Ctx()
    C.nc = nc
    C.dbg = dbg

    def din(name, shape, dt=F32):
        return nc.dram_tensor(name, list(shape), dt, kind="ExternalInput").ap()

    x_full = din("x_full", [SEQ, D])
    x_own = din("x_own", [NOWN * SEGT, D])
    qpos = din("qpos", [128, NOWN, SEGT])
    w_in = din("w_in", [128, 8, 2048])
    g_mix = din("g_mix", [128, 8])
    x_rev = din("x_rev", [SEQ, D])
    I = {"x_full": x_full, "x_own": x_own, "w_in": w_in, "x_rev": x_rev}
    for nm, shp in (("a_re", [128, 16]), ("a_im", [128, 16]), ("log_dt", [128, 16]),
                    ("bpad_re", [128, 16, 128]), ("bpad_im", [128, 16, 128]),
                    ("cpad_re", [128, 16, 128]), ("cpad_im", [128, 16, 128]),
                    ("d_skip", [128, 4]), ("w_glu", [128, 4, 512]), ("b_glu", [128, 4]),
                    ("segsel", [128, 4, 16]),
                    ("w_out", [128, 8, D]), ("g_out", [128, 8]), ("mem", [256, D]), ("g_mem", [128, 8]),
                    ("w_xkv", [128, 8, 2048]), ("g_xattn", [128, 8]), ("w_xq", [128, 8, D]), ("w_xo", [128, 8, D]),
                    ("g_ffn", [128, 8]), ("gffn_rep", [128, D]), ("gfin_rep", [128, D]), ("w_pq", [128, 8, 2048]),
                    ("skT", [128, 16, 128]), ("peer_u", [16384, D]), ("peer_v", [16384, D])):
        I[nm] = din(nm, shp)
    y = nc.dram_tensor("y", [NOWN * SEGT, D], F32, kind="ExternalOutput").ap()
    if dbg == "attn":
        dbg_out = nc.dram_tensor("dbg_attn", [128, 4, NOWN * SEGT], F32, kind="ExternalOutput").ap()

    with ExitStack() as es:
        S = Sched(nc, es)
        C.S = S
        C.const_tok = Tok()
        ident_f = _sb(nc, es, "ident_f", [128, 128], F32)
        C.ident_f = ident_f
        C.ident_bf = _sb(nc, es, "ident_bf", [128, 128], BF16)
        ones_f = _sb(nc, es, "ones_f", [128, 128], F32)
        C.ones_bf = _sb(nc, es, "ones_bf", [128, 128], BF16)
        C.tri_bf = _sb(nc, es, "tri_bf", [128, 128], BF16)
        C.atri_bf = _sb(nc, es, "atri_bf", [128, 128], BF16)
        C.eps_col = _sb(nc, es, "eps_col", [128, 1], F32)
        C.kpos = _sb(nc, es, "kpos", [128, 64], F32)
        S.op("pool", lambda e: e.memset(ones_f[:], 1.0), writes=[C.const_tok])
        S.op("pool", lambda e: e.memset(C.eps_col[:], EPS), writes=[C.const_tok])
        S.op("pool", lambda e: e.affine_select(out=ident_f[:], in_=ones_f[:], pattern=[[-1, 128]],
                                               compare_op=ALU.is_equal, fill=0.0, base=0,
                                               channel_multiplier=1),
             reads=[C.const_tok], writes=[C.const_tok])
        S.op("pool", lambda e: e.tensor_copy(out=C.ident_bf[:], in_=ident_f[:]),
             reads=[C.const_tok], writes=[C.const_tok])
        S.op("pool", lambda e: e.tensor_copy(out=C.ones_bf[:], in_=ones_f[:]),
             reads=[C.const_tok], writes=[C.const_tok])
        S.op("pool", lambda e: e.affine_select(out=C.tri_bf[:], in_=ones_f[:], pattern=[[-1, 128]],
                                               compare_op=ALU.is_ge, fill=0.0, base=0,
                                               channel_multiplier=1),
             reads=[C.const_tok], writes=[C.const_tok])
        S.op("pool", lambda e: e.affine_select(out=C.atri_bf[:], in_=ones_f[:], pattern=[[1, 128]],
                                               compare_op=ALU.is_gt, fill=0.0, base=0,
                                               channel_multiplier=-1),
             reads=[C.const_tok], writes=[C.const_tok])
        S.op("pool", lambda e: e.iota(C.kpos[:], pattern=[[128, 64]], base=0, channel_multiplier=1,
                                      allow_small_or_imprecise_dtypes=True),
             writes=[C.const_tok])

        ps = [es.enter_context(nc.psum_tensor("ps%d" % i, [128, 512], F32)) for i in range(8)]
        ps_tok = [Tok() for _ in range(8)]
        C.tp_ps = [ps[6], ps[7]]
        C.tp_tok = [ps_tok[6], ps_tok[7]]
        C.tp_i = 0

        attnT = _sb(nc, es, "attnT", [128, 4, NOWN * SEGT], BF16)
        attn_tok = [[Tok() for _ in range(8)] for _ in range(NOWN)]

        gmix_sb = _sb(nc, es, "gmix", [128, 8], F32)
        gmix_tok = Tok()
        S.dma("sp", S.chan(), gmix_sb[:], g_mix[:, :], writes=[gmix_tok])

        with ExitStack() as e2:
          if dbg != "ssm":
              KT = _sb(nc, e2, "KT", [128, 4, SEQ], BF16)
              V = _sb(nc, e2, "V", [128, 64, 512], BF16)
              QT = _sb(nc, e2, "QT", [128, 4, NOWN * SEGT], BF16)
              kt_tok = [Tok() for _ in range(NSEG)]
              v_tok = [Tok() for _ in range(NSEG)]
              q_tok = [Tok() for _ in range(NOWN)]
              with ExitStack() as e3:
                  wk = _sb(nc, e3, "wk", [128, 8, 512], BF16)
                  wq = wk
                  wv = _sb(nc, e3, "wv", [128, 8, 512], BF16)
                  w_tok = Tok()
                  wk_tok = Tok()
                  wv_tok = Tok()
                  xt = [_sb(nc, e3, "xt%d" % i, [128, D], F32) for i in range(2)]
                  xt_tok = [Tok() for _ in range(2)]
                  xt_ch = [S.chan() for _ in range(2)]
                  wi_box = [0]

                  def load_w(wdst, c0, wtok):
                      for k2 in range(4):
                          sl = wi_box[0] % 2
                          wi_box[0] += 1
                          S.dma("sp", xt_ch[sl], xt[sl][:].rearrange("p (a b) -> p a b", a=2),
                                w_in[:, 2 * k2:2 * k2 + 2, c0:c0 + 512], writes=[xt_tok[sl]])
                          for a2 in range(2):
                              kt = 2 * k2 + a2
                              eng = "dve" if a2 == 0 else "pool"
                              S.op(eng, lambda e, kt=kt, sl=sl, wdst=wdst, a2=a2: e.tensor_scalar(
                                  wdst[:, kt, :], xt[sl][:, a2 * 512:(a2 + 1) * 512], gmix_sb[:, kt:kt + 1], None, ALU.mult),
                                  reads=[xt_tok[sl], gmix_tok], writes=[wtok])

                  load_w(wk, 512, wk_tok)
                  load_w(wv, 1024, wv_tok)

                  xn = [_sb(nc, e3, "xn%d" % i, [128, D], BF16) for i in range(2)]
                  xn_tok = [Tok(), Tok()]
                  ss = [_sb(nc, e3, "ss%d" % i, [128, 1], F32) for i in range(2)]
                  ss_tok = [Tok(), Tok()]
                  rstd = [_sb(nc, e3, "rstd%d" % i, [128, 1], F32) for i in range(2)]
                  rstd_tok = [Tok(), Tok()]
                  xnT = [_sb(nc, e3, "xnT%d" % i, [128, 8, SEGT], BF16) for i in range(2)]
                  xnT_tok = [Tok(), Tok()]
                  ti = 0
                  for sgi in range(NSEG + NOWN):
                      own = sgi >= NSEG
                      sg = sgi - NSEG if own else sgi
                      if sgi == NSEG:
                          load_w(wq, 0, wk_tok)
                      src = x_own if own else x_full
                      xb = sgi % 2
                      for m in range(4):
                          a = ti % 2
                          b = ti % 2
                          ti += 1
                          r0 = sg * SEGT + m * 128
                          S.dma("sp", xt_ch[a], xt[a][:], src[r0:r0 + 128, :], writes=[xt_tok[a]])
                          rms_scale(C, xt[a][:], xt_tok[a], rstd[b][:], rstd_tok[b], xn[b][:], xn_tok[b],
                                    ss[b][:], ss_tok[b])
                          S.op("dve", lambda e, a=a, b=b: e.tensor_scalar(xn[b][:], xt[a][:], rstd[b][:], None, ALU.mult),
                               reads=[xt_tok[a], rstd_tok[b]], writes=[xn_tok[b]])
                          ptb, pt_tok = transpose_to(C, xn[b], xn_tok[b], None, None)
                          S.op("dve", lambda e, xb=xb, m=m, ptb=ptb: e.tensor_copy(
                              out=xnT[xb][:, :, m * 128:(m + 1) * 128],
                              in_=ptb.rearrange("p (k t) -> p k t", k=8)),
                              reads=[pt_tok], writes=[xnT_tok[xb]])
                      if not own:
                          for hp in range(4):
                              pb = hp % 2
                              for kt in range(8):
                                  S.op("pe", lambda e, kt=kt, hp=hp, pb=pb, xb=xb: e.matmul(
                                      ps[pb][:], lhsT=wk[:, kt, hp * 128:(hp + 1) * 128], rhs=xnT[xb][:, kt, :],
                                      start=(kt == 0), stop=(kt == 7)),
                                      reads=[wk_tok, xnT_tok[xb]], writes=[ps_tok[pb]])
                              S.op("act", lambda e, hp=hp, pb=pb, sg=sg: e.copy(
                                  out=KT[:, hp, sg * SEGT:(sg + 1) * SEGT], in_=ps[pb][:]),
                                  reads=[ps_tok[pb]], writes=[kt_tok[sg]])
                          for m in range(4):
                              pb = 2 + m % 2
                              for kt in range(8):
                                  S.op("pe", lambda e, kt=kt, m=m, pb=pb, xb=xb: e.matmul(
                                      ps[pb][:], lhsT=xnT[xb][:, kt, m * 128:(m + 1) * 128], rhs=wv[:, kt, :],
                                      start=(kt == 0), stop=(kt == 7)),
                                      reads=[wv_tok, xnT_tok[xb]], writes=[ps_tok[pb]])
                              S.op("act", lambda e, m=m, pb=pb, sg=sg: e.copy(
                                  out=V[:, sg * 4 + m, :], in_=ps[pb][:]),
                                  reads=[ps_tok[pb]], writes=[v_tok[sg]])
                      else:
                          for hp in range(4):
                              pb = hp % 2
                              for kt in range(8):
                                  S.op("pe", lambda e, kt=kt, hp=hp, pb=pb, xb=xb: e.matmul(
                                      ps[pb][:], lhsT=wq[:, kt, hp * 128:(hp + 1) * 128], rhs=xnT[xb][:, kt, :],
                                      start=(kt == 0), stop=(kt == 7)),
                                      reads=[wk_tok, xnT_tok[xb]], writes=[ps_tok[pb]])
                              S.op("act", lambda e, hp=hp, pb=pb, sg=sg: e.mul(
                                  out=QT[:, hp, sg * SEGT:(sg + 1) * SEGT], in_=ps[pb][:], mul=0.125),
                                  reads=[ps_tok[pb]], writes=[q_tok[sg]])

              S.barrier()
              if dbg == "kv":
                  dch = S.chan()
                  dk = nc.dram_tensor("dbg_kt", [128, 4, SEQ], BF16, kind="ExternalOutput").ap()
                  dv = nc.dram_tensor("dbg_v", [128, 64, 512], BF16, kind="ExternalOutput").ap()
                  dq = nc.dram_tensor("dbg_q", [128, 4, NOWN * SEGT], BF16, kind="ExternalOutput").ap()
                  dtok = Tok()
                  S.dma("sp", dch, dk[:, :, :], KT[:], reads=kt_tok, writes=[dtok])
                  S.dma("sp", dch, dv[:, :, :], V[:], reads=v_tok, writes=[dtok])
                  S.dma("sp", dch, dq[:, :, :], QT[:], reads=q_tok, writes=[dtok])
                  S.wait_tok("sp", [dtok])
              with ExitStack() as e3:
                if dbg != "kv":
                    NE = 3
                    e_sb = [_sb(nc, e3, "e%d" % i, [128, 512], F32) for i in range(NE)]
                    e_tok = [Tok() for _ in range(NE)]
                    sp_sb = [_sb(nc, e3, "sp%d" % i, [128, 512], BF16) for i in range(3)]
                    sp_tok = [Tok(), Tok(), Tok()]
                    ex_sb = [_sb(nc, e3, "ex%d" % i, [128, 512], BF16) for i in range(2)]
                    ex_tok = [Tok(), Tok()]
                    w_sb = [_sb(nc, e3, "w%d" % i, [128, 512], BF16) for i in range(2)]
                    w_tok2 = [Tok(), Tok()]
                    masks = _sb(nc, e3, "masks", [128, 16, 512], BF16)
                    mask_tok = Tok()
                    qp = _sb(nc, e3, "qp", [128, NOWN, SEGT], F32)
                    qp_tok = Tok()
                    S.dma("sp", S.chan(), qp[:], qpos[:, :, :], writes=[qp_tok])
                    zps = [ps[0], ps[1]]
                    zps_tok = [ps_tok[0], ps_tok[1]]
                    cps = [ps[2], ps[3]]
                    cps_tok = [ps_tok[2], ps_tok[3]]
                    ops_ = [ps[4], ps[5]]
                    ops_tok = [ps_tok[4], ps_tok[5]]

                    for slot in range(NOWN):
                        top = KB_TOP[slot]
                        nb = top + 1
                        for r in range(16):
                            kb = top - r
                            S.op("dve", lambda e, r=r, kb=kb, slot=slot: e.tensor_scalar(
                                masks[:, r, :], qp[:, slot, :], C.kpos[:, kb:kb + 1], None, ALU.is_gt),
                                reads=[qp_tok, C.const_tok], writes=[mask_tok])
                        blocks = [(h, top - r, r) for h in range(8) for r in range(nb)]
                        nblk = len(blocks)

                        def s1(i):
                            h, kb, r = blocks[i]
                            hp, hh = h // 2, h % 2
                            zb = i % 2
                            S.op("pe", lambda e: e.matmul(
                                zps[zb][:], lhsT=KT[hh * 64:(hh + 1) * 64, hp, kb * 128:(kb + 1) * 128],
                                rhs=QT[hh * 64:(hh + 1) * 64, hp, slot * SEGT:(slot + 1) * SEGT],
                                start=True, stop=True),
                                reads=[kt_tok[kb // 4], q_tok[slot]], writes=[zps_tok[zb]])

                        def s2(i):
                            h, kb, r = blocks[i]
                            zb, eb, sb = i % 2, i % NE, i % 3
                            S.op("act", lambda e: e.activation(out=e_sb[eb][:], in_=zps[zb][:], func=AF.Exp),
                                 reads=[zps_tok[zb]], writes=[e_tok[eb]])
                            if r < 16:
                                S.op("dve", lambda e: e.tensor_tensor(out=e_sb[eb][:], in0=e_sb[eb][:],
                                                                       in1=masks[:, r, :], op=ALU.mult),
                                     reads=[mask_tok], writes=[e_tok[eb]])
                            S.op("act", lambda e: e.activation(out=sp_sb[sb][:], in_=e_sb[eb][:], func=AF.Ln, bias=1.0),
                                 reads=[e_tok[eb]], writes=[sp_tok[sb]])

                        def s3(i):
                            h, kb, r = blocks[i]
                            sb = i % 3
                            cb = h % 2
                            S.op("pe", lambda e: e.matmul(cps[cb][:], lhsT=C.tri_bf[:], rhs=sp_sb[sb][:],
                                                          start=(r == 0), stop=True, skip_group_check=(r != 0)),
                                 reads=[sp_tok[sb], C.const_tok], writes=[cps_tok[cb]])

                        def s3b(i):
                            h, kb, r = blocks[i]
                            if r == nb - 1:
                                return
                            sb = i % 3
                            cb = h % 2
                            S.op("pe", lambda e: e.matmul(cps[cb][:], lhsT=C.atri_bf[:], rhs=sp_sb[sb][:],
                                                          start=False, stop=True, skip_group_check=True),
                                 reads=[sp_tok[sb], C.const_tok], writes=[cps_tok[cb]])

                        def s4(i):
                            h, kb, r = blocks[i]
                            cb, xb_, eb, wb = h % 2, i % 2, i % NE, i % 2
                            S.op("act", lambda e: e.activation(out=ex_sb[xb_][:], in_=cps[cb][:], func=AF.Exp, scale=-1.0),
                                 reads=[cps_tok[cb]], writes=[ex_tok[xb_]])
                            S.op("dve" if i % 2 == 0 else "pool", lambda e: e.tensor_tensor(out=w_sb[wb][:], in0=e_sb[eb][:], in1=ex_sb[xb_][:],
                                                                   op=ALU.mult),
                                 reads=[e_tok[eb], ex_tok[xb_]], writes=[w_tok2[wb]])

                        def s5(i):
                            h, kb, r = blocks[i]
                            wb = i % 2
                            ob = h % 2
                            hp, hh = h // 2, h % 2
                            S.op("pe", lambda e: e.matmul(ops_[ob][hh * 64:(hh + 1) * 64, :], lhsT=V[:, kb, h * 64:(h + 1) * 64],
                                                          rhs=w_sb[wb][:], start=(r == 0), stop=(r == nb - 1)),
                                 reads=[w_tok2[wb], v_tok[kb // 4]], writes=[ops_tok[ob]])
                            if r == nb - 1:
                                S.op("act", lambda e: e.copy(out=attnT[hh * 64:(hh + 1) * 64, hp, slot * SEGT:(slot + 1) * SEGT],
                                                             in_=ops_[ob][hh * 64:(hh + 1) * 64, :]),
                                     reads=[ops_tok[ob]], writes=[attn_tok[slot][h]])

                        for t in range(nblk + 2):
                            if t < nblk:
                                s1(t)
                                s2(t)
                            if 0 <= t - 2 < nblk:
                                s3b(t - 2)
                            if 0 <= t - 1 < nblk:
                                s3(t - 1)
                                s4(t - 1)
                            if 0 <= t - 2 < nblk:
                                s5(t - 2)

        S.barrier()
        ssmT = _sb(nc, es, "ssmT", [128, 4, NOWN * SEGT], BF16)
        ssm_tok = [Tok() for _ in range(NOWN)]
        if dbg in ("ssm", None, "h1", "h2", "h3"):
            phase_ssm(C, nc, S, ps, ps_tok, I, ssmT, ssm_tok, gmix_sb, gmix_tok)
        S.barrier()
        och = S.chan()
        if dbg == "ssm":
            dbg_ssm = nc.dram_tensor("dbg_ssm", [128, 4, NOWN * SEGT], BF16, kind="ExternalOutput").ap()
            ytok = Tok()
            S.dma("sp", och, dbg_ssm[:, :, :], ssmT[:], reads=ssm_tok, writes=[ytok])
            S.wait_tok("sp", [ytok])
        if dbg == "attn":
            with ExitStack() as e2:
                tmp = _sb(nc, e2, "dbgtmp", [128, 4, NOWN * SEGT], F32)
                tmp_tok = Tok()
                alltoks = [t for row in attn_tok for t in row]
                S.op("dve", lambda e: e.tensor_copy(out=tmp[:], in_=attnT[:]), reads=alltoks, writes=[tmp_tok])
                ytok = Tok()
                S.dma("sp", och, dbg_out[:, :, :], tmp[:], reads=[tmp_tok], writes=[ytok])
                S.wait_tok("sp", [ytok])
        S.barrier()
        if dbg in (None, "h1", "h2", "h3"):
            phase_post(C, nc, S, ps, ps_tok, I, attnT, attn_tok, ssmT, ssm_tok, y, dbg)
            return nc
        with ExitStack() as e2:
            zt = _sb(nc, e2, "zt", [128, D], F32)
            zt_tok = Tok()
            S.op("pool", lambda e: e.memset(zt[:], 0.0), writes=[zt_tok])
            ytok = Tok()
            for i in range(NOWN * 4):
                S.dma("sp", och, y[i * 128:(i + 1) * 128, :], zt[:], reads=[zt_tok], writes=[ytok])
            S.wait_tok("sp", [ytok])
    return nc


def make_in_maps(inputs):
    x = np.ascontiguousarray(inputs["x"], dtype=np.float32)
    w_in = np.ascontiguousarray(inputs["w_in"][0].reshape(8, 128, 2048).transpose(1, 0, 2))
    g_mix = np.ascontiguousarray(inputs["g_mix"][0].reshape(8, 128).T)
    f32 = np.float32
    are = inputs["a_re"][0].reshape(16, 2, 64).transpose(1, 2, 0).reshape(128, 16)
    aim = inputs["a_im"][0].reshape(16, 2, 64).transpose(1, 2, 0).reshape(128, 16)
    ldt = np.repeat(inputs["log_dt"][0].reshape(16, 2).T[:, None, :], 64, axis=1).reshape(128, 16)
    bpr = np.zeros((128, 16, 128), f32)
    bpi = np.zeros((128, 16, 128), f32)
    cpr = np.zeros((128, 16, 128), f32)
    cpi = np.zeros((128, 16, 128), f32)
    for k in range(16):
        for g2 in range(2):
            g = 2 * k + g2
            r0 = (g % 8) * 16
            bpr[r0:r0 + 16, k, g2 * 64:(g2 + 1) * 64] = inputs["b_re"][0][g].T
            bpi[r0:r0 + 16, k, g2 * 64:(g2 + 1) * 64] = inputs["b_im"][0][g].T
            cpr[g2 * 64:(g2 + 1) * 64, k, r0:r0 + 16] = inputs["c_re"][0][g].T
            cpi[g2 * 64:(g2 + 1) * 64, k, r0:r0 + 16] = inputs["c_im"][0][g].T
    dsk = inputs["d_skip"][0].reshape(4, 128).T
    wglu = inputs["w_glu"][0].reshape(4, 128, 512).transpose(1, 0, 2)
    bglu = inputs["b_glu"][0].reshape(4, 128).T
    def kt8(w):
        return w.reshape(8, 128, -1).transpose(1, 0, 2)

    def col8(g):
        return g.reshape(8, 128).T

    post = {"w_out": kt8(inputs["w_out"][0]),
            "g_out": col8(np.concatenate([inputs["g_attn_out"][0], inputs["g_ssm_out"][0]])),
            "g_mem": col8(inputs["g_mem"][0]), "w_xkv": kt8(inputs["w_xkv"][0]),
            "g_xattn": col8(inputs["g_xattn"][0]), "w_xq": kt8(inputs["w_xq"][0]), "w_xo": kt8(inputs["w_xo"][0]),
            "g_ffn": col8(inputs["g_ffn"][0]),
            "gffn_rep": np.broadcast_to(inputs["g_ffn"][0][None, :], (128, D)),
            "gfin_rep": np.broadcast_to(inputs["g_final"][None, :], (128, D)),
            "w_pq": kt8(inputs["w_pq"][0]),
            "skT": inputs["sub_keys"][0].transpose(3, 0, 1, 2).reshape(128, 16, 128),
            "peer_u": inputs["peer_u"][0], "peer_v": inputs["peer_v"][0]}
    common = {"w_in": w_in, "g_mix": g_mix, "a_re": are, "a_im": aim, "log_dt": ldt,
              "bpad_re": bpr, "bpad_im": bpi, "cpad_re": cpr, "cpad_im": cpi,
              "d_skip": dsk, "w_glu": wglu, "b_glu": bglu}
    common.update(post)
    common = {k_: np.ascontiguousarray(v_, dtype=f32) for k_, v_ in common.items()}
    maps = []
    for c in range(8):
        b, j = c // 4, c % 4
        tiles = own_tiles(j)
        segsel = np.zeros((128, NOWN, 16), f32)
        for i_, t_ in enumerate(tiles):
            segsel[:, i_, t_] = 1.0
        x_own = np.concatenate([x[b, t * SEGT:(t + 1) * SEGT] for t in tiles], axis=0)
        qp = np.stack([np.arange(t * SEGT, (t + 1) * SEGT, dtype=np.float32) for t in tiles], axis=0)
        qp = np.ascontiguousarray(np.broadcast_to(qp[None], (128, NOWN, SEGT)))
        x_rev = np.ascontiguousarray(x[b].reshape(NSEG, SEGT, D)[:, ::-1, :].reshape(SEQ, D))
        m = {"x_full": np.ascontiguousarray(x[b]), "x_own": np.ascontiguousarray(x_own), "x_rev": x_rev,
             "qpos": qp, "segsel": segsel, "mem": np.ascontiguousarray(inputs["mem"][b], dtype=f32)}
        m.update(common)
        maps.append(m)
    return maps


def kernel(**inputs):
    nc = build()
    maps = make_in_maps(inputs)
    res = run_bass_kernel_spmd(nc, maps, core_ids=list(range(8)))
    out = np.zeros((2, SEQ, D), np.float32)
    for c in range(8):
        b, j = c // 4, c % 4
        yo = res.results[c]["y"]
        for i, t in enumerate(own_tiles(j)):
            out[b, t * SEGT:(t + 1) * SEGT] = yo[i * SEGT:(i + 1) * SEGT]
    return out
```

```python
from contextlib import ExitStack
import numpy as np
import concourse.bass as bass
import concourse.mybir as mybir
from concourse.bass_utils import run_bass_kernel_spmd

F32 = mybir.dt.float32
BF16 = mybir.dt.bfloat16
U32 = mybir.dt.uint32
I32 = mybir.dt.int32
AF = mybir.ActivationFunctionType
ALU = mybir.AluOpType
AX = mybir.AxisListType

D = 1024
SEQ = 8192
NSEG = 16
SEGT = 512
NOWN = 4
EPS = 1e-6
KB_TOP = [15, 31, 47, 63]
NEG = -1.0e30


def own_tiles(j):
    return [j, 7 - j, 8 + j, 15 - j]


class Tok:
    __slots__ = ("w", "r")

    def __init__(self):
        self.w = None
        self.r = {}


class _Eng:
    def __init__(self, name, h, sem):
        self.name = name
        self.h = h
        self.sem = sem
        self.n = 0
        self.waited = {}


class _Chan:
    def __init__(self, sem):
        self.sem = sem
        self.n = 0


class Sched:
    def __init__(self, nc, es):
        self.nc = nc
        self.es = es
        self.E = {}
        for name, h in (("pe", nc.tensor), ("act", nc.scalar), ("dve", nc.vector),
                        ("pool", nc.gpsimd), ("sp", nc.sync)):
            sem = es.enter_context(nc.semaphore("sem_" + name))
            self.E[name] = _Eng(name, h, sem)
        self.nchan = 0
        self.chans = []

    def chan(self):
        self.nchan += 1
        c = _Chan(self.es.enter_context(self.nc.semaphore("ch%d" % self.nchan)))
        self.chans.append(c)
        return c

    def barrier(self):
        for E in self.E.values():
            for F in self.E.values():
                if F is E or F.n == 0:
                    continue
                if E.waited.get(id(F.sem), 0) < F.n:
                    E.h.wait_ge(F.sem, F.n)
                    E.waited[id(F.sem)] = F.n
            for c in self.chans:
                if c.n > 0 and E.waited.get(id(c.sem), 0) < c.n:
                    E.h.wait_ge(c.sem, c.n)
                    E.waited[id(c.sem)] = c.n

    def _wait(self, E, reads, writes, weak=()):
        deps = {}
        for t in weak:
            for d in [t.w] + list(t.r.values()):
                if d is not None and d[0] is not E.sem:
                    k_ = id(d[0])
                    if k_ not in deps or deps[k_][1] < d[1]:
                        deps[k_] = d

        def add(d):
            if d is None:
                return
            s, v = d
            k = id(s)
            if k not in deps or deps[k][1] < v:
                deps[k] = (s, v)

        for t in reads:
            add(t.w)
        for t in writes:
            add(t.w)
            for d in t.r.values():
                add(d)
        for k, (s, v) in deps.items():
            if E.name == "pe" and s is E.sem:
                continue
            if E.waited.get(k, 0) < v:
                E.h.wait_ge(s, v)
                E.waited[k] = v

    def op(self, eng, fn, reads=(), writes=(), weak=()):
        E = self.E[eng]
        self._wait(E, reads, writes, weak)
        ins = fn(E.h)
        E.n += 1
        ins.then_inc(E.sem, 1)
        me = (E.sem, E.n)
        for t in reads:
            t.r[id(E.sem)] = me
        for t in writes:
            t.w = me
            t.r = {}
        for t in weak:
            t.w = me
            t.r = {}
        return ins

    def dma(self, queue, ch, out, in_, reads=(), writes=(), indirect=None, **kw):
        Q = self.E[queue]
        self._wait(Q, reads, writes)
        if indirect is not None:
            ins = Q.h.indirect_dma_start(out=out, out_offset=None, in_=in_, in_offset=indirect)
        else:
            ins = Q.h.dma_start(out=out, in_=in_, **kw)
        ch.n += 16
        ins.then_inc(ch.sem, 16)
        me = (ch.sem, ch.n)
        for t in reads:
            t.r[id(ch.sem)] = me
        for t in writes:
            t.w = me
            t.r = {}
        return ins

    def wait_tok(self, eng, toks):
        self._wait(self.E[eng], toks, ())


class Ctx:
    pass


_NAME_CNT = [0]


def _sb(nc, es, name, shape, dt):
    _NAME_CNT[0] += 1
    return es.enter_context(nc.sbuf_tensor("%s_%d" % (name, _NAME_CNT[0]), list(shape), dt))


def rms_scale(C, xt, xt_tok, rstd, rstd_tok, junk, junk_tok, ss, ss_tok):
    S = C.S
    S.op("act", lambda e: e.activation(out=junk, in_=xt, func=AF.Square, accum_out=ss),
         reads=[xt_tok], writes=[junk_tok, ss_tok])
    S.op("act", lambda e: e.activation(out=rstd, in_=ss, func=AF.Ln, bias=C.eps_col[:], scale=1.0 / D),
         reads=[ss_tok, C.const_tok], writes=[rstd_tok])
    S.op("act", lambda e: e.activation(out=rstd, in_=rstd, func=AF.Exp, scale=-0.5),
         reads=[rstd_tok], writes=[rstd_tok])


def transpose_to(C, xn, xn_tok, dst_fn, dst_tok, nkt=8):
    S = C.S
    force = getattr(C, "tp_force", None)
    if force is not None:
        pt, pt_tok = C.tp_ps[force], C.tp_tok[force]
    else:
        pt, pt_tok = C.tp_ps[C.tp_i % 2], C.tp_tok[C.tp_i % 2]
        C.tp_i += 1
    ptb = pt[:].bitcast(BF16)
    for kt in range(nkt):
        S.op("pe", lambda e, kt=kt: e.transpose(out=ptb[:, kt * 128:(kt + 1) * 128],
                                                in_=xn[:, kt * 128:(kt + 1) * 128],
                                                identity=C.ident_bf[:]),
             reads=[xn_tok, C.const_tok], writes=[pt_tok])
    return ptb, pt_tok


TWO_PI = 6.283185307179586
CW1 = 6.28125
CW2 = TWO_PI - CW1


def phase_ssm(C, nc, S, ps, ps_tok, I, ssmT, ssm_tok, gmix_sb, gmix_tok):
    with ExitStack() as e2:
        cosT = _sb(nc, e2, "cosT", [128, 16, SEGT], F32)
        sinT = _sb(nc, e2, "sinT", [128, 16, SEGT], F32)
        tab_tok = Tok()
        bre = _sb(nc, e2, "bre", [128, 16, 128], BF16)
        bim = _sb(nc, e2, "bim", [128, 16, 128], BF16)
        cre = _sb(nc, e2, "cre", [128, 16, 128], BF16)
        ncim = _sb(nc, e2, "ncim", [128, 16, 128], BF16)
        bc_tok = Tok()
        wu = _sb(nc, e2, "wu", [128, 8, 512], BF16)
        wglu = _sb(nc, e2, "wglu", [128, 4, 512], BF16)
        diagD = _sb(nc, e2, "diagD", [128, 4, 128], BF16)
        w_tok = Tok()
        P = {}
        for nm in ("are", "aim", "ldt", "dt", "rho", "th", "gre", "gim", "ngim", "pa", "pb", "pc", "pd", "adt", "nadt",
                   "ilr", "ili", "q1", "q2", "q3", "q4"):
            P[nm] = _sb(nc, e2, "p_" + nm, [128, 16], F32)
        p_tok = Tok()
        bglu = _sb(nc, e2, "bglu", [128, 4], F32)
        dsk = _sb(nc, e2, "dsk", [128, 4], F32)
        segsel = _sb(nc, e2, "segsel_sb", [128, 4, 16], F32)
        halfpi = _sb(nc, e2, "halfpi", [128, 1], F32)
        Xst = _sb(nc, e2, "Xst", [128, 2, 16, 17], F32)
        tau1p = _sb(nc, e2, "tau1p", [128, SEGT], F32)
        magt = _sb(nc, e2, "magt", [128, SEGT], F32)
        mag_tok = Tok()
        S.op("pool", lambda e: e.iota(tau1p[:], pattern=[[1, SEGT]], base=1, channel_multiplier=0,
                                      allow_small_or_imprecise_dtypes=True), writes=[mag_tok])
        xst_tok = Tok()
        small_tok = Tok()
        ch0 = S.chan()
        for dst, src in ((P["are"], I["a_re"]), (P["aim"], I["a_im"]), (P["ldt"], I["log_dt"]),
                         (bglu, I["b_glu"]), (dsk, I["d_skip"])):
            S.dma("sp", ch0, dst[:], src[:, :], writes=[small_tok])
        S.dma("sp", ch0, segsel[:], I["segsel"][:, :, :], writes=[small_tok])
        S.op("pool", lambda e: e.memset(halfpi[:], float(np.pi / 2)), writes=[small_tok])
        S.op("pool", lambda e: e.memset(Xst[:], 0.0), writes=[xst_tok])
        S.op("act", lambda e: e.activation(out=P["dt"][:], in_=P["ldt"][:], func=AF.Exp), reads=[small_tok], writes=[p_tok])
        S.op("dve", lambda e: e.tensor_tensor(out=P["pa"][:], in0=P["are"][:], in1=P["dt"][:], op=ALU.mult),
             reads=[small_tok, p_tok], writes=[p_tok])
        S.op("act", lambda e: e.activation(out=P["rho"][:], in_=P["pa"][:], func=AF.Exp), reads=[p_tok], writes=[p_tok])
        S.op("dve", lambda e: e.tensor_copy(out=P["adt"][:], in_=P["pa"][:]), reads=[p_tok], writes=[p_tok])
        S.op("dve", lambda e: e.tensor_scalar(P["nadt"][:], P["pa"][:], -1.0, None, ALU.mult), reads=[p_tok], writes=[p_tok])
        S.op("dve", lambda e: e.tensor_tensor(out=P["th"][:], in0=P["aim"][:], in1=P["dt"][:], op=ALU.mult),
             reads=[small_tok, p_tok], writes=[p_tok])
        with ExitStack() as e3:
            tau1 = _sb(nc, e3, "tau1", [128, SEGT], F32)
            ang = _sb(nc, e3, "ang", [128, 4, SEGT], F32)
            kf = _sb(nc, e3, "kf", [128, 4, SEGT], F32)
            ki = _sb(nc, e3, "ki", [128, 4, SEGT], I32)
            s1 = _sb(nc, e3, "s1", [128, 4, SEGT], F32)
            t_tok = Tok()
            S.op("pool", lambda e: e.iota(tau1[:], pattern=[[1, SEGT]], base=1, channel_multiplier=0,
                                          allow_small_or_imprecise_dtypes=True), writes=[t_tok])
            for gq in range(4):
                for kk in range(4):
                    k = gq * 4 + kk
                    S.op("dve", lambda e, kk=kk, k=k: e.tensor_scalar(ang[:, kk, :], tau1[:], P["th"][:, k:k + 1], None, ALU.mult),
                         reads=[p_tok, t_tok], writes=[t_tok])
                S.op("dve", lambda e: e.tensor_scalar(kf[:], ang[:], 1.0 / TWO_PI, None, ALU.mult), reads=[t_tok], writes=[t_tok])
                S.op("dve", lambda e: e.tensor_copy(out=ki[:], in_=kf[:]), reads=[t_tok], writes=[t_tok])
                S.op("dve", lambda e: e.tensor_copy(out=kf[:], in_=ki[:]), reads=[t_tok], writes=[t_tok])
                S.op("dve", lambda e: e.scalar_tensor_tensor(out=ang[:], in0=kf[:], scalar=-CW1, in1=ang[:], op0=ALU.mult, op1=ALU.add),
                     reads=[t_tok], writes=[t_tok])
                S.op("dve", lambda e: e.scalar_tensor_tensor(out=ang[:], in0=kf[:], scalar=-CW2, in1=ang[:], op0=ALU.mult, op1=ALU.add),
                     reads=[t_tok], writes=[t_tok])
                S.op("act", lambda e: e.activation(out=s1[:], in_=ang[:], func=AF.Sin, scale=0.25), reads=[t_tok], writes=[t_tok])
                S.op("act", lambda e: e.activation(out=kf[:], in_=ang[:], func=AF.Sin, scale=0.25, bias=halfpi[:]),
                     reads=[t_tok, small_tok], writes=[t_tok])
                S.op("dve", lambda e: e.scalar_tensor_tensor(out=ang[:], in0=s1[:], scalar=2.0, in1=kf[:], op0=ALU.mult, op1=ALU.mult),
                     reads=[t_tok], writes=[t_tok])
                S.op("dve", lambda e: e.tensor_tensor(out=s1[:], in0=s1[:], in1=s1[:], op=ALU.mult), reads=[t_tok], writes=[t_tok])
                S.op("dve", lambda e: e.tensor_scalar(s1[:], s1[:], -2.0, 1.0, ALU.mult, ALU.add), reads=[t_tok], writes=[t_tok])
                S.op("dve", lambda e, gq=gq: e.scalar_tensor_tensor(out=sinT[:, gq * 4:(gq + 1) * 4, :], in0=ang[:], scalar=2.0, in1=s1[:],
                                                                    op0=ALU.mult, op1=ALU.mult),
                     reads=[t_tok], writes=[tab_tok])
                S.op("dve", lambda e: e.tensor_tensor(out=ang[:], in0=ang[:], in1=ang[:], op=ALU.mult), reads=[t_tok], writes=[t_tok])
                S.op("dve", lambda e, gq=gq: e.tensor_scalar(cosT[:, gq * 4:(gq + 1) * 4, :], ang[:], -2.0, 1.0, ALU.mult, ALU.add),
                     reads=[t_tok], writes=[tab_tok])
            c0 = cosT[:, :, 0]
            s0 = sinT[:, :, 0]
            tt = lambda o, a, b, op: S.op("dve", lambda e: e.tensor_tensor(out=o, in0=a, in1=b, op=op),
                                          reads=[p_tok, tab_tok, small_tok], writes=[p_tok])
            tt(P["pa"][:], P["rho"][:], c0, ALU.mult)
            S.op("dve", lambda e: e.tensor_scalar(P["pa"][:], P["pa"][:], -1.0, None, ALU.add), reads=[p_tok], writes=[p_tok])
            tt(P["pb"][:], P["rho"][:], s0, ALU.mult)
            tt(P["pc"][:], P["are"][:], P["are"][:], ALU.mult)
            tt(P["pd"][:], P["aim"][:], P["aim"][:], ALU.mult)
            tt(P["pc"][:], P["pc"][:], P["pd"][:], ALU.add)
            S.op("dve", lambda e: e.reciprocal(out=P["pc"][:], in_=P["pc"][:]), reads=[p_tok], writes=[p_tok])
            tt(P["gre"][:], P["pa"][:], P["are"][:], ALU.mult)
            tt(P["pd"][:], P["pb"][:], P["aim"][:], ALU.mult)
            tt(P["gre"][:], P["gre"][:], P["pd"][:], ALU.add)
            tt(P["gre"][:], P["gre"][:], P["pc"][:], ALU.mult)
            tt(P["gim"][:], P["pb"][:], P["are"][:], ALU.mult)
            tt(P["pd"][:], P["pa"][:], P["aim"][:], ALU.mult)
            tt(P["gim"][:], P["gim"][:], P["pd"][:], ALU.subtract)
            tt(P["gim"][:], P["gim"][:], P["pc"][:], ALU.mult)
            S.op("dve", lambda e: e.tensor_scalar(P["ngim"][:], P["gim"][:], -1.0, None, ALU.mult), reads=[p_tok], writes=[p_tok])
            S.barrier()
        with ExitStack() as e3:
            f1 = _sb(nc, e3, "f1", [128, 16, 128], F32)
            f2 = _sb(nc, e3, "f2", [128, 16, 128], F32)
            f3 = _sb(nc, e3, "f3", [128, 128], F32)
            f_tok = Tok()
            chf = S.chan()
            for src, dst in ((I["bpad_re"], bre), (I["bpad_im"], bim)):
                S.dma("sp", chf, f1[:], src[:, :, :], writes=[f_tok])
                S.op("dve", lambda e, dst=dst: e.tensor_copy(out=dst[:], in_=f1[:]), reads=[f_tok], writes=[bc_tok, f_tok])
            S.dma("sp", chf, f1[:], I["cpad_re"][:, :, :], writes=[f_tok])
            S.dma("sp", chf, f2[:], I["cpad_im"][:, :, :], writes=[f_tok])
            for k in range(16):
                S.op("dve", lambda e, k=k: e.tensor_scalar(f3[:], f2[:, k, :], P["gim"][:, k:k + 1], None, ALU.mult),
                     reads=[f_tok, p_tok], writes=[f_tok])
                S.op("dve", lambda e, k=k: e.scalar_tensor_tensor(out=cre[:, k, :], in0=f1[:, k, :], scalar=P["gre"][:, k:k + 1],
                                                                  in1=f3[:], op0=ALU.mult, op1=ALU.subtract),
                     reads=[f_tok, p_tok], writes=[bc_tok])
                S.op("dve", lambda e, k=k: e.tensor_scalar(f3[:], f2[:, k, :], P["gre"][:, k:k + 1], None, ALU.mult),
                     reads=[f_tok, p_tok, bc_tok], writes=[f_tok])
                S.op("dve", lambda e, k=k: e.scalar_tensor_tensor(out=ncim[:, k, :], in0=f1[:, k, :], scalar=P["ngim"][:, k:k + 1],
                                                                  in1=f3[:], op0=ALU.mult, op1=ALU.subtract),
                     reads=[f_tok, p_tok], writes=[bc_tok])
            f1v = f1[:].rearrange("p a b -> p (a b)").rearrange("p (k c) -> p k c", k=4)
            for half in range(2):
                S.dma("sp", chf, f1v, I["w_in"][:, 4 * half:4 * half + 4, 1536:2048], writes=[f_tok])
                for k4 in range(4):
                    kt = 4 * half + k4
                    S.op("dve" if k4 % 2 == 0 else "pool", lambda e, kt=kt, k4=k4: e.tensor_scalar(
                        wu[:, kt, :], f1v[:, k4, :], gmix_sb[:, kt:kt + 1], None, ALU.mult),
                        reads=[f_tok, gmix_tok], writes=[w_tok])
            for kt in range(4):
                S.dma("sp", chf, f1[:, 0:4, :], I["w_glu"][:, kt, :].rearrange("p (a b) -> p a b", a=4), writes=[f_tok])
                S.op("dve", lambda e, kt=kt: e.tensor_copy(out=wglu[:, kt, :].rearrange("p (a b) -> p a b", a=4), in_=f1[:, 0:4, :]),
                     reads=[f_tok], writes=[w_tok, f_tok])
            for ct in range(4):
                S.op("dve", lambda e, ct=ct: e.tensor_scalar(diagD[:, ct, :], C.ident_f[:], dsk[:, ct:ct + 1], None, ALU.mult),
                     reads=[small_tok, C.const_tok], writes=[w_tok])
            S.barrier()

        def scale_tables(sign_key):
            for k in range(16):
                S.op("act", lambda e, k=k: e.activation(out=magt[:], in_=tau1p[:], func=AF.Exp, scale=P[sign_key][:, k:k + 1]),
                     reads=[mag_tok, p_tok], writes=[mag_tok])
                S.op("dve", lambda e, k=k: e.tensor_tensor(out=cosT[:, k, :], in0=cosT[:, k, :], in1=magt[:], op=ALU.mult),
                     reads=[mag_tok, tab_tok], writes=[tab_tok])
                S.op("dve", lambda e, k=k: e.tensor_tensor(out=sinT[:, k, :], in0=sinT[:, k, :], in1=magt[:], op=ALU.mult),
                     reads=[mag_tok, tab_tok], writes=[tab_tok])

        scale_tables("adt")
        with ExitStack() as e3:
            xt = [_sb(nc, e3, "axt%d" % i, [128, D], F32) for i in range(2)]
            xt_tok = [Tok(), Tok()]
            xt_ch = [S.chan(), S.chan()]
            xn = [_sb(nc, e3, "axn%d" % i, [128, D], BF16) for i in range(2)]
            xn_tok = [Tok(), Tok()]
            ss = [_sb(nc, e3, "ass%d" % i, [128, 1], F32) for i in range(2)]
            ss_tok = [Tok(), Tok()]
            rstd = [_sb(nc, e3, "arstd%d" % i, [128, 1], F32) for i in range(2)]
            rstd_tok = [Tok(), Tok()]
            xnT = [_sb(nc, e3, "axnT%d" % i, [128, 8, SEGT], BF16) for i in range(2)]
            xnT_tok = [Tok(), Tok()]
            uT = [_sb(nc, e3, "auT%d" % i, [128, 4, SEGT], BF16) for i in range(2)]
            uT_tok = [Tok(), Tok()]
            acc4 = [_sb(nc, e3, "acc4_%d" % i, [128, 4, 16], F32) for i in range(2)]
            acc4_tok = [[[Tok() for _ in range(16)] for _ in range(4)] for _ in range(2)]
            junkS = [_sb(nc, e3, "junkS%d" % i, [128, SEGT], BF16) for i in range(4)]
            junkS_tok = [Tok() for _ in range(4)]
            jc = [0]
            sm_ = {nm: _sb(nc, e3, "sm_" + nm, [128, 16], F32) for nm in ("a", "b", "c", "d", "sr", "si")}
            sm_tok = Tok()
            t0r, t0i = cosT[:, :, 0], sinT[:, :, 0]
            Lr, Li = cosT[:, :, SEGT - 1], sinT[:, :, SEGT - 1]
            tt2 = lambda o, a, b, op: S.op("dve", lambda e: e.tensor_tensor(out=o, in0=a, in1=b, op=op),
                                           reads=[p_tok, tab_tok, sm_tok], writes=[p_tok])
            tt2(P["q1"][:], t0r, t0r, ALU.mult)
            tt2(P["q2"][:], t0i, t0i, ALU.mult)
            tt2(P["q1"][:], P["q1"][:], P["q2"][:], ALU.add)
            S.op("dve", lambda e: e.reciprocal(out=P["q1"][:], in_=P["q1"][:]), reads=[p_tok], writes=[p_tok])
            tt2(P["ilr"][:], t0r, P["q1"][:], ALU.mult)
            tt2(P["ili"][:], t0i, P["q1"][:], ALU.mult)
            S.op("dve", lambda e: e.tensor_scalar(P["ili"][:], P["ili"][:], -1.0, None, ALU.mult), reads=[p_tok], writes=[p_tok])
            ti = 0
            for sg in range(NSEG):
                xb = sg % 2
                for m in range(4):
                    a = ti % 2
                    ti += 1
                    r0 = sg * SEGT + m * 128
                    S.dma("sp", xt_ch[a], xt[a][:], I["x_rev"][r0:r0 + 128, :], writes=[xt_tok[a]])
                    rms_scale(C, xt[a][:], xt_tok[a], rstd[a][:], rstd_tok[a], xn[a][:], xn_tok[a], ss[a][:], ss_tok[a])
                    S.op("dve", lambda e, a=a: e.tensor_scalar(xn[a][:], xt[a][:], rstd[a][:], None, ALU.mult),
                         reads=[xt_tok[a], rstd_tok[a]], writes=[xn_tok[a]])
                    ptb, pt_tok = transpose_to(C, xn[a], xn_tok[a], None, None)
                    S.op("act", lambda e, xb=xb, m=m, ptb=ptb: e.copy(out=xnT[xb][:, :, m * 128:(m + 1) * 128],
                                                                      in_=ptb.rearrange("p (k t) -> p k t", k=8)),
                         reads=[pt_tok], writes=[xnT_tok[xb]])
                for ct in range(4):
                    pb = 4 + ct % 2
                    for kt in range(8):
                        S.op("pe", lambda e, kt=kt, ct=ct, pb=pb, xb=xb: e.matmul(
                            ps[pb][:], lhsT=wu[:, kt, ct * 128:(ct + 1) * 128], rhs=xnT[xb][:, kt, :],
                            start=(kt == 0), stop=(kt == 7)), reads=[w_tok, xnT_tok[xb]], writes=[ps_tok[pb]])
                    S.op("act", lambda e, ct=ct, pb=pb, xb=xb: e.copy(out=uT[xb][:, ct, :], in_=ps[pb][:]),
                         reads=[ps_tok[pb]], writes=[uT_tok[xb]])
                ab = sg % 2
                for k in range(16):
                    ct = k // 4
                    bur, bui = (0, 1) if k % 2 == 0 else (2, 3)
                    S.op("pe", lambda e, k=k, ct=ct, bur=bur: e.matmul(ps[bur][:], lhsT=bre[:, k, :], rhs=uT[xb][:, ct, :], start=True, stop=True),
                         reads=[bc_tok, uT_tok[xb]], writes=[ps_tok[bur]])
                    S.op("pe", lambda e, k=k, ct=ct, bui=bui: e.matmul(ps[bui][:], lhsT=bim[:, k, :], rhs=uT[xb][:, ct, :], start=True, stop=True),
                         reads=[bc_tok, uT_tok[xb]], writes=[ps_tok[bui]])
                    for j, (bk, tab) in enumerate(((bur, cosT), (bui, sinT), (bur, sinT), (bui, cosT))):
                        jb = jc[0] % 4
                        jc[0] += 1
                        S.op("dve", lambda e, j=j, bk=bk, tab=tab, k=k, jb=jb: e.scalar_tensor_tensor(
                            out=junkS[jb][:], in0=ps[bk][:], scalar=1.0, in1=tab[:, k, :], op0=ALU.mult, op1=ALU.mult,
                            accum_out=acc4[ab][:, j, k:k + 1]),
                            reads=[ps_tok[bk], tab_tok], writes=[junkS_tok[jb], acc4_tok[ab][j][k]])
                A_ = acc4[ab]
                rd = [t_ for row_ in acc4_tok[ab] for t_ in row_] + [p_tok, tab_tok, xst_tok, sm_tok]
                tt3 = lambda o, a_, b_, op: S.op("dve", lambda e: e.tensor_tensor(out=o, in0=a_, in1=b_, op=op), reads=rd, writes=[sm_tok])
                tt3(sm_["a"][:], A_[:, 0, :], A_[:, 1, :], ALU.subtract)
                tt3(sm_["b"][:], A_[:, 2, :], A_[:, 3, :], ALU.add)
                tt3(sm_["c"][:], P["ilr"][:], sm_["a"][:], ALU.mult)
                tt3(sm_["d"][:], P["ili"][:], sm_["b"][:], ALU.mult)
                tt3(sm_["sr"][:], sm_["c"][:], sm_["d"][:], ALU.subtract)
                tt3(sm_["c"][:], P["ilr"][:], sm_["b"][:], ALU.mult)
                tt3(sm_["d"][:], P["ili"][:], sm_["a"][:], ALU.mult)
                tt3(sm_["si"][:], sm_["c"][:], sm_["d"][:], ALU.add)
                Xr, Xi = Xst[:, 0, :, sg], Xst[:, 1, :, sg]
                tt3(sm_["a"][:], Lr, Xr, ALU.mult)
                tt3(sm_["b"][:], Li, Xi, ALU.mult)
                tt3(sm_["a"][:], sm_["a"][:], sm_["b"][:], ALU.subtract)
                S.op("dve", lambda e, sg=sg: e.tensor_tensor(out=Xst[:, 0, :, sg + 1], in0=sm_["a"][:], in1=sm_["sr"][:], op=ALU.add),
                     reads=[sm_tok], writes=[xst_tok])
                tt3(sm_["c"][:], Lr, Xi, ALU.mult)
                tt3(sm_["d"][:], Li, Xr, ALU.mult)
                tt3(sm_["c"][:], sm_["c"][:], sm_["d"][:], ALU.add)
                S.op("dve", lambda e, sg=sg: e.tensor_tensor(out=Xst[:, 1, :, sg + 1], in0=sm_["c"][:], in1=sm_["si"][:], op=ALU.add),
                     reads=[sm_tok], writes=[xst_tok])
            S.barrier()
        scale_tables("nadt")
        S.barrier()
        with ExitStack() as e3:
            xt = [_sb(nc, e3, "sxt%d" % i, [128, D], F32) for i in range(2)]
            xt_tok = [Tok(), Tok()]
            xt_ch = [S.chan(), S.chan()]
            xn = [_sb(nc, e3, "sxn%d" % i, [128, D], BF16) for i in range(2)]
            xn_tok = [Tok(), Tok()]
            ss = [_sb(nc, e3, "sss%d" % i, [128, 1], F32) for i in range(2)]
            ss_tok = [Tok(), Tok()]
            rstd = [_sb(nc, e3, "srstd%d" % i, [128, 1], F32) for i in range(2)]
            rstd_tok = [Tok(), Tok()]
            xnT = [_sb(nc, e3, "sxnT", [128, 8, SEGT], BF16)] * 2
            xnT_tok = [Tok()] * 2
            uT = [_sb(nc, e3, "uT%d" % i, [128, 4, SEGT], BF16) for i in range(2)]
            uT_tok = [Tok(), Tok()]
            tmp4 = [_sb(nc, e3, "tm%d" % i, [128, SEGT], F32) for i in range(4)]
            tmp4_tok = [Tok() for _ in range(4)]
            Wr = [_sb(nc, e3, "Wr%d" % i, [128, SEGT], F32) for i in range(2)]
            Wi = [_sb(nc, e3, "Wi%d" % i, [128, SEGT], F32) for i in range(2)]
            Vr = [_sb(nc, e3, "Vr%d" % i, [128, SEGT], F32) for i in range(2)]
            Vi = [_sb(nc, e3, "Vi%d" % i, [128, SEGT], F32) for i in range(2)]
            W_tok = [Tok(), Tok()]
            V_tok = [Tok(), Tok()]
            Xb = [_sb(nc, e3, "Xb%d" % i, [128, 2, SEGT], BF16) for i in range(2)]
            Xb_tok = [Tok(), Tok()]
            xin = _sb(nc, e3, "xin", [128, 32], F32)
            xin_tok = Tok()
            selt = _sb(nc, e3, "selt", [128, 32, 16], F32)
            rot = _sb(nc, e3, "rot", [128, 4], F32)
            rot_tok = Tok()
            yb = _sb(nc, e3, "yb", [128, 4, SEGT], BF16)
            y_tok = [Tok() for _ in range(4)]
            g1 = _sb(nc, e3, "g1", [128, SEGT], F32)
            g2 = _sb(nc, e3, "g2", [128, SEGT], F32)
            g_tok = Tok()
            ti = 0
            pcount = 0
            for sgi in range(NSEG, NSEG + NOWN):
                own = sgi >= NSEG
                sg = sgi - NSEG if own else sgi
                src = I["x_own"] if own else I["x_full"]
                xb = sgi % 2
                for m in range(4):
                    a = ti % 2
                    ti += 1
                    r0 = sg * SEGT + m * 128
                    S.dma("sp", xt_ch[a], xt[a][:], src[r0:r0 + 128, :], writes=[xt_tok[a]])
                    rms_scale(C, xt[a][:], xt_tok[a], rstd[a][:], rstd_tok[a], xn[a][:], xn_tok[a], ss[a][:], ss_tok[a])
                    S.op("dve", lambda e, a=a: e.tensor_scalar(xn[a][:], xt[a][:], rstd[a][:], None, ALU.mult),
                         reads=[xt_tok[a], rstd_tok[a]], writes=[xn_tok[a]])
                    ptb, pt_tok = transpose_to(C, xn[a], xn_tok[a], None, None)
                    S.op("act", lambda e, xb=xb, m=m, ptb=ptb: e.copy(out=xnT[xb][:, :, m * 128:(m + 1) * 128],
                                                                      in_=ptb.rearrange("p (k t) -> p k t", k=8)),
                         reads=[pt_tok], writes=[xnT_tok[xb]])
                for ct in range(4):
                    pb = ct % 2
                    for kt in range(8):
                        S.op("pe", lambda e, kt=kt, ct=ct, pb=pb, xb=xb: e.matmul(
                            ps[pb][:], lhsT=wu[:, kt, ct * 128:(ct + 1) * 128], rhs=xnT[xb][:, kt, :],
                            start=(kt == 0), stop=(kt == 7)), reads=[w_tok, xnT_tok[xb]], writes=[ps_tok[pb]])
                    S.op("act", lambda e, ct=ct, pb=pb, xb=xb: e.copy(out=uT[xb][:, ct, :], in_=ps[pb][:]),
                         reads=[ps_tok[pb]], writes=[uT_tok[xb]])
                if own:
                    S.op("dve", lambda e, sg=sg: e.tensor_tensor(out=selt[:], in0=Xst[:].rearrange("p r k s -> p (r k) s")[:, :, 0:16],
                                                                in1=segsel[:, sg:sg + 1, :].to_broadcast([128, 32, 16]), op=ALU.mult),
                         reads=[xst_tok, small_tok], writes=[xin_tok])
                    S.op("dve", lambda e: e.tensor_reduce(out=xin[:], in_=selt[:], axis=AX.X, op=ALU.add),
                         reads=[xin_tok], writes=[xin_tok])
                for ct in range(4):
                    yps = 4 + ct % 2
                    for kk in range(4):
                        k = ct * 4 + kk
                        pp = pcount % 2
                        pcount += 1
                        bur, bui = 2 * pp, 2 * pp + 1
                        bur, bui = (2, 3) if pp == 0 else (0, 1)
                        S.op("pe", lambda e: e.matmul(ps[bur][:], lhsT=bre[:, k, :], rhs=uT[xb][:, ct, :], start=True, stop=True),
                             reads=[bc_tok, uT_tok[xb]], writes=[ps_tok[bur]])
                        S.op("pe", lambda e: e.matmul(ps[bui][:], lhsT=bim[:, k, :], rhs=uT[xb][:, ct, :], start=True, stop=True),
                             reads=[bc_tok, uT_tok[xb]], writes=[ps_tok[bui]])
                        ck, sk = cosT[:, k, :], sinT[:, k, :]
                        S.op("dve", lambda e: e.tensor_tensor(out=tmp4[0][:], in0=ps[bur][:], in1=ck, op=ALU.mult),
                             reads=[ps_tok[bur], tab_tok], writes=[tmp4_tok[0]])
                        S.op("dve", lambda e: e.tensor_tensor(out=tmp4[1][:], in0=ps[bui][:], in1=sk, op=ALU.mult),
                             reads=[ps_tok[bui], tab_tok], writes=[tmp4_tok[1]])
                        S.op("pool", lambda e: e.tensor_tensor(out=Wr[pp][:], in0=tmp4[0][:], in1=tmp4[1][:], op=ALU.add),
                             reads=[tmp4_tok[0], tmp4_tok[1]], writes=[W_tok[pp]])
                        S.op("dve", lambda e: e.tensor_tensor(out=tmp4[2][:], in0=ps[bui][:], in1=ck, op=ALU.mult),
                             reads=[ps_tok[bui], tab_tok], writes=[tmp4_tok[2]])
                        S.op("dve", lambda e: e.tensor_tensor(out=tmp4[3][:], in0=ps[bur][:], in1=sk, op=ALU.mult),
                             reads=[ps_tok[bur], tab_tok], writes=[tmp4_tok[3]])
                        S.op("pool", lambda e: e.tensor_tensor(out=Wi[pp][:], in0=tmp4[2][:], in1=tmp4[3][:], op=ALU.subtract),
                             reads=[tmp4_tok[2], tmp4_tok[3]], writes=[W_tok[pp]])
                        if own:
                            ir, ii = xin[:, k:k + 1], xin[:, 16 + k:16 + k + 1]
                            itoks = [xin_tok]
                        else:
                            ir, ii = Xst[:, 2 * k, sg:sg + 1], Xst[:, 2 * k + 1, sg:sg + 1]
                            itoks = [xst_tok]
                        rb = P["rho"][:, k:k + 1].to_broadcast([128, SEGT])
                        S.op("dve", lambda e: e.tensor_tensor_scan(out=Vr[pp][:], data0=rb, data1=Wr[pp][:], initial=ir,
                                                                   op0=ALU.mult, op1=ALU.add),
                             reads=[W_tok[pp], p_tok] + itoks, writes=[V_tok[pp]])
                        S.op("dve", lambda e: e.tensor_tensor_scan(out=Vi[pp][:], data0=rb, data1=Wi[pp][:], initial=ii,
                                                                   op0=ALU.mult, op1=ALU.add),
                             reads=[W_tok[pp], p_tok] + itoks, writes=[V_tok[pp]])
                        if not own:
                            cl, sl_ = cosT[:, k, SEGT - 1:SEGT], sinT[:, k, SEGT - 1:SEGT]
                            vr, vi = Vr[pp][:, SEGT - 1:SEGT], Vi[pp][:, SEGT - 1:SEGT]
                            S.op("dve", lambda e: e.tensor_tensor(out=rot[:, 0:1], in0=vi, in1=sl_, op=ALU.mult),
                                 reads=[V_tok[pp], tab_tok], writes=[rot_tok])
                            S.op("dve", lambda e: e.scalar_tensor_tensor(out=Xst[:, 2 * k, sg + 1:sg + 2], in0=vr, scalar=cl, in1=rot[:, 0:1],
                                                                         op0=ALU.mult, op1=ALU.subtract),
                                 reads=[V_tok[pp], tab_tok, rot_tok], writes=[xst_tok])
                            S.op("dve", lambda e: e.tensor_tensor(out=rot[:, 1:2], in0=vr, in1=sl_, op=ALU.mult),
                                 reads=[V_tok[pp], tab_tok], writes=[rot_tok])
                            S.op("dve", lambda e: e.scalar_tensor_tensor(out=Xst[:, 2 * k + 1, sg + 1:sg + 2], in0=vi, scalar=cl, in1=rot[:, 1:2],
                                                                         op0=ALU.mult, op1=ALU.add),
                                 reads=[V_tok[pp], tab_tok, rot_tok], writes=[xst_tok])
                        else:
                            S.op("dve", lambda e: e.tensor_tensor(out=tmp4[0][:], in0=Vr[pp][:], in1=ck, op=ALU.mult),
                                 reads=[V_tok[pp], tab_tok], writes=[tmp4_tok[0]])
                            S.op("pool", lambda e: e.tensor_tensor(out=tmp4[1][:], in0=Vi[pp][:], in1=sk, op=ALU.mult),
                                 reads=[V_tok[pp], tab_tok], writes=[tmp4_tok[1]])
                            S.op("pool", lambda e: e.tensor_tensor(out=Xb[pp][:, 0, :], in0=tmp4[0][:], in1=tmp4[1][:], op=ALU.subtract),
                                 reads=[tmp4_tok[0], tmp4_tok[1]], writes=[Xb_tok[pp]])
                            S.op("dve", lambda e: e.tensor_tensor(out=tmp4[2][:], in0=Vr[pp][:], in1=sk, op=ALU.mult),
                                 reads=[V_tok[pp], tab_tok], writes=[tmp4_tok[2]])
                            S.op("pool", lambda e: e.tensor_tensor(out=tmp4[3][:], in0=Vi[pp][:], in1=ck, op=ALU.mult),
                                 reads=[V_tok[pp], tab_tok], writes=[tmp4_tok[3]])
                            S.op("pool", lambda e: e.tensor_tensor(out=Xb[pp][:, 1, :], in0=tmp4[2][:], in1=tmp4[3][:], op=ALU.add),
                                 reads=[tmp4_tok[2], tmp4_tok[3]], writes=[Xb_tok[pp]])
                            S.op("pe", lambda e: e.matmul(ps[yps][:], lhsT=cre[:, k, :], rhs=Xb[pp][:, 0, :], start=(kk == 0), stop=False),
                                 reads=[bc_tok, Xb_tok[pp]], writes=[ps_tok[yps]])
                            S.op("pe", lambda e: e.matmul(ps[yps][:], lhsT=ncim[:, k, :], rhs=Xb[pp][:, 1, :], start=False, stop=False),
                                 reads=[bc_tok, Xb_tok[pp]], writes=[ps_tok[yps]])
                    if own:
                        S.op("pe", lambda e: e.matmul(ps[yps][:], lhsT=diagD[:, ct, :], rhs=uT[xb][:, ct, :], start=False, stop=True),
                             reads=[w_tok, uT_tok[xb]], writes=[ps_tok[yps]])
                        S.op("act", lambda e: e.activation(out=g1[:], in_=ps[yps][:], func=AF.Square), reads=[ps_tok[yps]], writes=[g_tok])
                        S.op("dve", lambda e: e.tensor_scalar(g1[:], g1[:], 0.044715, 1.0, ALU.mult, ALU.add), reads=[g_tok], writes=[g_tok])
                        S.op("dve", lambda e: e.tensor_tensor(out=g1[:], in0=ps[yps][:], in1=g1[:], op=ALU.mult),
                             reads=[g_tok, ps_tok[yps]], writes=[g_tok])
                        S.op("act", lambda e: e.activation(out=g2[:], in_=g1[:], func=AF.Sigmoid, scale=1.5957691216057308),
                             reads=[g_tok], writes=[g_tok])
                        S.op("dve", lambda e, ct=ct: e.tensor_tensor(out=yb[:, ct, :], in0=ps[yps][:], in1=g2[:], op=ALU.mult),
                             reads=[g_tok, ps_tok[yps]], writes=[y_tok[ct]])
                if own:
                    for c2 in range(4):
                        gp = 4 + c2 % 2
                        for kt in range(4):
                            S.op("pe", lambda e, kt=kt, c2=c2, gp=gp: e.matmul(ps[gp][:], lhsT=wglu[:, kt, c2 * 128:(c2 + 1) * 128],
                                                                              rhs=yb[:, kt, :], start=(kt == 0), stop=(kt == 3)),
                                 reads=[w_tok] + y_tok, writes=[ps_tok[gp]])
                        S.op("act", lambda e, c2=c2, gp=gp: e.activation(out=g2[:], in_=ps[gp][:], func=AF.Sigmoid, bias=bglu[:, c2:c2 + 1]),
                             reads=[ps_tok[gp], small_tok], writes=[g_tok])
                        S.op("dve", lambda e, c2=c2, sg=sg: e.tensor_tensor(out=ssmT[:, c2, sg * SEGT:(sg + 1) * SEGT], in0=yb[:, c2, :],
                                                                           in1=g2[:], op=ALU.mult),
                             reads=[g_tok, y_tok[c2]], writes=[ssm_tok[sg]])
            S.barrier()
        S.barrier()


def _rms_rstd(C, S, src_ap, src_tok, junk_ap, junk_tok, ss, ss_tok, rstd, rstd_tok, n=D):
    S.op("act", lambda e: e.activation(out=junk_ap, in_=src_ap, func=AF.Square, accum_out=ss[:]),
         reads=[src_tok], writes=[junk_tok, ss_tok])
    S.op("act", lambda e: e.activation(out=rstd[:], in_=ss[:], func=AF.Ln, bias=C.eps_col[:], scale=1.0 / n),
         reads=[ss_tok, C.const_tok], writes=[rstd_tok])
    S.op("act", lambda e: e.activation(out=rstd[:], in_=rstd[:], func=AF.Exp, scale=-0.5),
         reads=[rstd_tok], writes=[rstd_tok])


def _load_cast(S, nc, dst, src, gcol, gtok, stg, stg_tok, stg_ch, dst_tok, nkt, ncols, cnt):
    for kt in range(nkt):
        for c0 in range(0, ncols, 512):
            sl = cnt[0] % 2
            cnt[0] += 1
            w = min(512, ncols - c0)
            S.dma("sp", stg_ch[sl], stg[sl][:, 0:w], src[:, kt, c0:c0 + w], writes=[stg_tok[sl]])
            eng = "dve" if sl == 0 else "pool"
            if gcol is not None:
                S.op(eng, lambda e, kt=kt, c0=c0, w=w, sl=sl: e.tensor_scalar(dst[:, kt, c0:c0 + w], stg[sl][:, 0:w],
                                                                            gcol[:, kt:kt + 1], None, ALU.mult),
                     reads=[stg_tok[sl], gtok], writes=[dst_tok])
            else:
                S.op(eng, lambda e, kt=kt, c0=c0, w=w, sl=sl: e.tensor_copy(out=dst[:, kt, c0:c0 + w], in_=stg[sl][:, 0:w]),
                     reads=[stg_tok[sl]], writes=[dst_tok])


def phase_post(C, nc, S, ps, ps_tok, I, attnT, attn_tok, ssmT, ssm_tok, y, dbg):
    NT = NOWN * 4
    h2buf = nc.dram_tensor("h2buf", [NOWN * SEGT, D], F32, kind="Internal").ap()
    h2_tok = [Tok() for _ in range(NT)]
    alla = [t for row in attn_tok for t in row]
    bank = [0]

    def nbk():
        b = bank[0] % 6
        bank[0] += 1
        return b

    eo = ExitStack()
    if dbg not in ("h1", "h2"):
        wpq = _sb(nc, eo, "wpq", [128, 8, 2048], BF16)
        skT = _sb(nc, eo, "skT_sb", [128, 16, 128], BF16)
        pw_tok = Tok()
        gffn = _sb(nc, eo, "gffn", [128, 8], F32)
        gffn_rep = _sb(nc, eo, "gffn_rep_sb", [128, D], F32)
        gfin_rep = _sb(nc, eo, "gfin_rep_sb", [128, D], F32)
        iota256 = _sb(nc, eo, "iota256", [128, 256], F32)
        pg_tok = Tok()
        pchg = S.chan()
        S.dma("sp", pchg, gffn[:], I["g_ffn"][:, :], writes=[pg_tok])
        S.dma("sp", pchg, gffn_rep[:], I["gffn_rep"][:, :], writes=[pg_tok])
        S.dma("sp", pchg, gfin_rep[:], I["gfin_rep"][:, :], writes=[pg_tok])
        S.op("pool", lambda e: e.iota(iota256[:], pattern=[[1, 256]], base=0, channel_multiplier=0,
                                      allow_small_or_imprecise_dtypes=True), writes=[pg_tok])
        pstg = [_sb(nc, eo, "qstg%d" % i, [128, 512], F32) for i in range(2)]
        pstg_tok = [Tok(), Tok()]
        pstg_ch = [S.chan(), S.chan()]
        pcnt = [0]
        _load_cast(S, nc, wpq, I["w_pq"], gffn, pg_tok, pstg, pstg_tok, pstg_ch, pw_tok, 8, 2048, pcnt)
        _load_cast(S, nc, skT, I["skT"], None, None, pstg, pstg_tok, pstg_ch, pw_tok, 16, 128, pcnt)

    uvbf = nc.dram_tensor("uvbf", [16384, 2048], BF16, kind="Internal").ap()
    uv_toks = [Tok() for _ in range(32)]
    if dbg not in ("h1", "h2"):
        with ExitStack() as e2:
            cin = [_sb(nc, e2, "cin%d" % i, [128, 8, D], F32) for i in range(2)]
            cin_tok = [Tok(), Tok()]
            cin_ch = [S.chan(), S.chan()]
            cout = [_sb(nc, e2, "cout%d" % i, [128, 8, D], BF16) for i in range(2)]
            cout_tok = [[Tok() for _ in range(8)] for _ in range(2)]
            cout_ch = [S.chan(), S.chan()]
            ci = 0
            engs = ("act", "dve", "pool", "dve", "act", "dve", "act", "dve")
            for tbl, src in enumerate((I["peer_u"], I["peer_v"])):
                for chk in range(16):
                    sl = ci % 2
                    r0 = chk * 1024
                    S.dma("sp", cin_ch[sl], cin[sl][:], src[r0:r0 + 1024, :].rearrange("(p a) d -> p a d", a=8),
                          writes=[cin_tok[sl]])
                    for a in range(8):
                        if engs[a] == "act":
                            S.op("act", lambda e, a=a, sl=sl: e.copy(out=cout[sl][:, a, :], in_=cin[sl][:, a, :]),
                                 reads=[cin_tok[sl]], writes=[cout_tok[sl][a]])
                        else:
                            S.op(engs[a], lambda e, a=a, sl=sl: e.tensor_copy(out=cout[sl][:, a, :], in_=cin[sl][:, a, :]),
                                 reads=[cin_tok[sl]], writes=[cout_tok[sl][a]])
                    S.dma("sp", cout_ch[sl],
                          uvbf[r0:r0 + 1024, tbl * 1024:(tbl + 1) * 1024].rearrange("(p a) d -> p a d", a=8), cout[sl][:],
                          reads=cout_tok[sl], writes=[uv_toks[ci]])
                    ci += 1
            S.barrier()

    with ExitStack() as e2:
        wo = _sb(nc, e2, "wo", [128, 8, D], BF16)
        wxq = _sb(nc, e2, "wxq", [128, 8, D], BF16)
        wxo = _sb(nc, e2, "wxo", [128, 8, D], BF16)
        KmT = _sb(nc, e2, "KmT", [128, 8, 256], BF16)
        Vm = _sb(nc, e2, "Vm", [128, 2, D], BF16)
        w_tok = Tok()
        kv_tok = Tok()
        gcols = _sb(nc, e2, "gcols", [128, 3, 8], F32)
        g_tok = Tok()
        chg = S.chan()
        S.dma("sp", chg, gcols[:, 0, :], I["g_out"][:, :], writes=[g_tok])
        S.dma("sp", chg, gcols[:, 1, :], I["g_xattn"][:, :], writes=[g_tok])
        S.dma("sp", chg, gcols[:, 2, :], I["g_mem"][:, :], writes=[g_tok])
        stg = [_sb(nc, e2, "pstg%d" % i, [128, 512], F32) for i in range(2)]
        stg_tok = [Tok(), Tok()]
        stg_ch = [S.chan(), S.chan()]
        cnt = [0]
        wo_tok, wxq_tok, wxo_tok = Tok(), Tok(), Tok()
        _load_cast(S, nc, wo, I["w_out"], gcols[:, 0, :], g_tok, stg, stg_tok, stg_ch, wo_tok, 8, D, cnt)
        _load_cast(S, nc, wxq, I["w_xq"], gcols[:, 1, :], g_tok, stg, stg_tok, stg_ch, wxq_tok, 8, D, cnt)
        _load_cast(S, nc, wxo, I["w_xo"], None, None, stg, stg_tok, stg_ch, wxo_tok, 8, D, cnt)
        h = _sb(nc, e2, "h", [128, D], F32)
        h_tok = Tok()
        h_ch = S.chan()
        hn = _sb(nc, e2, "hn", [128, D], BF16)
        hn_tok = Tok()
        hnT = _sb(nc, e2, "hnT", [128, 8, 128], BF16)
        hnT_tok = Tok()
        ss = _sb(nc, e2, "pss", [128, 1], F32)
        ss_tok = Tok()
        rstd = _sb(nc, e2, "prstd", [128, 1], F32)
        rstd_tok = Tok()
        with ExitStack() as e3:
            memT = _sb(nc, e3, "memT", [128, 8, 256], BF16)
            memT_tok = Tok()
            wch = _sb(nc, e3, "wch", [128, 8, 512], BF16)
            wch_tok = Tok()
            for mt in range(2):
                S.dma("sp", h_ch, h[:], I["mem"][mt * 128:(mt + 1) * 128, :], writes=[h_tok])
                _rms_rstd(C, S, h[:], h_tok, hn[:], hn_tok, ss, ss_tok, rstd, rstd_tok)
                S.op("dve", lambda e: e.tensor_scalar(hn[:], h[:], rstd[:], None, ALU.mult), reads=[h_tok, rstd_tok], writes=[hn_tok])
                ptb, pt_tok = transpose_to(C, hn, hn_tok, None, None)
                S.op("act", lambda e, mt=mt, ptb=ptb: e.copy(out=memT[:, :, mt * 128:(mt + 1) * 128],
                                                           in_=ptb.rearrange("p (k t) -> p k t", k=8)),
                     reads=[pt_tok], writes=[memT_tok])
            for cc in range(4):
                _load_cast(S, nc, wch, I["w_xkv"][:, :, cc * 512:(cc + 1) * 512], gcols[:, 2, :], g_tok, stg, stg_tok, stg_ch,
                           wch_tok, 8, 512, cnt)
                if cc < 2:
                    for j4 in range(4):
                        b_ = nbk()
                        for kt in range(8):
                            S.op("pe", lambda e, kt=kt, j4=j4, b_=b_: e.matmul(ps[b_][:, 0:256], lhsT=wch[:, kt, j4 * 128:(j4 + 1) * 128],
                                                                             rhs=memT[:, kt, :], start=(kt == 0), stop=(kt == 7)),
                                 reads=[wch_tok, memT_tok], writes=[ps_tok[b_]])
                        S.op("act", lambda e, j4=j4, b_=b_, cc=cc: e.copy(out=KmT[:, cc * 4 + j4, :], in_=ps[b_][:, 0:256]),
                             reads=[ps_tok[b_]], writes=[kv_tok])
                else:
                    for mt in range(2):
                        b_ = nbk()
                        for kt in range(8):
                            S.op("pe", lambda e, kt=kt, mt=mt, b_=b_: e.matmul(ps[b_][:], lhsT=memT[:, kt, mt * 128:(mt + 1) * 128],
                                                                             rhs=wch[:, kt, :], start=(kt == 0), stop=(kt == 7)),
                                 reads=[wch_tok, memT_tok], writes=[ps_tok[b_]])
                        S.op("act", lambda e, mt=mt, b_=b_, cc=cc: e.copy(out=Vm[:, mt, (cc - 2) * 512:(cc - 1) * 512], in_=ps[b_][:]),
                             reads=[ps_tok[b_]], writes=[kv_tok])
            S.barrier()
        def mkset(tag):
            B = {}
            for nm, shp, dt_ in (("sq4", [128, 4, 128], BF16), ("rs2", [128, 2], F32), ("qT", [128, 8, 128], BF16),
                                 ("pp", [128, 4, 256], BF16), ("pT", [128, 8, 128], BF16), ("oT", [128, 8, 128], BF16),
                                 ("mx", [128, 4], F32), ("sm", [128, 4], F32), ("h", [128, D], F32), ("hn", [128, D], BF16),
                                 ("hnT", [128, 8, 128], BF16), ("ss", [128, 1], F32), ("rstd", [128, 1], F32)):
                B[nm] = _sb(nc, e2, nm + tag, shp, dt_)
            for nm in ("sq_tok", "rs2_tok", "qT_tok", "pp_tok", "pT_tok", "oT_tok", "sm_tok", "h_tok", "hn_tok", "hnT_tok",
                       "ss_tok", "rstd_tok"):
                B[nm] = Tok()
            B["h_ch"] = S.chan()
            return B

        class _Rec1:
            def __init__(self):
                self.items = []

            def op(self, *a, **k):
                self.items.append(("op", a, k))

            def dma(self, *a, **k):
                self.items.append(("dma", a, k))

        def emit_tile(S_, tile, nbk_, sq4, rs2, qT, pp, pT, oT, mx, sm, h, hn, hnT, ss, rstd, sq_tok, rs2_tok, qT_tok, pp_tok,
                      pT_tok, oT_tok, sm_tok, h_tok, hn_tok, hnT_tok, ss_tok, rstd_tok, h_ch):
            saved = C.S
            C.S = S_
            t0 = tile * 128
            slot = tile // 4
            tsl = slice(t0, t0 + 128)
            for which, (src, toks) in enumerate(((attnT, attn_tok[slot]), (ssmT, [ssm_tok[slot]]))):
                S_.op("act", lambda e, src=src: e.activation(out=sq4[:], in_=src[:, :, tsl], func=AF.Square),
                     reads=toks, writes=[sq_tok])
                b_ = nbk_()
                for hp in range(4):
                    S_.op("pe", lambda e, hp=hp, b_=b_: e.matmul(ps[b_][:, 0:1], lhsT=sq4[:, hp, :], rhs=C.ones_bf[:, 0:1],
                                                                start=(hp == 0), stop=(hp == 3)),
                         reads=[sq_tok, C.const_tok], writes=[ps_tok[b_]])
                S_.op("act", lambda e, which=which, b_=b_: e.activation(out=rs2[:, which:which + 1], in_=ps[b_][:, 0:1], func=AF.Ln,
                                                                       bias=C.eps_col[:], scale=1.0 / 512),
                     reads=[ps_tok[b_], C.const_tok], writes=[rs2_tok])
            S_.op("act", lambda e: e.activation(out=rs2[:], in_=rs2[:], func=AF.Exp, scale=-0.5), reads=[rs2_tok], writes=[rs2_tok])
            S_.dma("sp", h_ch, h[:], I["x_own"][t0:t0 + 128, :], writes=[h_tok])
            for which, (src, toks) in enumerate(((attnT, attn_tok[slot]), (ssmT, [ssm_tok[slot]]))):
                for n2 in range(2):
                    b_ = nbk_()
                    for hp in range(4):
                        S_.op("pe", lambda e, hp=hp, b_=b_, src=src, which=which, n2=n2: e.matmul(
                            ps[b_][:], lhsT=src[:, hp, tsl], rhs=wo[:, which * 4 + hp, n2 * 512:(n2 + 1) * 512],
                            start=(hp == 0), stop=(hp == 3)), reads=toks + [wo_tok], writes=[ps_tok[b_]])
                    S_.op("dve", lambda e, b_=b_, which=which, n2=n2: e.scalar_tensor_tensor(
                        out=h[:, n2 * 512:(n2 + 1) * 512], in0=ps[b_][:], scalar=rs2[:, which:which + 1],
                        in1=h[:, n2 * 512:(n2 + 1) * 512], op0=ALU.mult, op1=ALU.add),
                        reads=[ps_tok[b_], rs2_tok], writes=[h_tok])
            if dbg == "h1":
                S_.dma("sp", h_ch, y[t0:t0 + 128, :], h[:], reads=[h_tok], writes=[h2_tok[tile]])
                C.S = saved
                return
            _rms_rstd(C, S_, h[:], h_tok, hn[:], hn_tok, ss, ss_tok, rstd, rstd_tok)
            S_.op("dve", lambda e: e.tensor_scalar(hn[:], h[:], rstd[:], None, ALU.mult), reads=[h_tok, rstd_tok], writes=[hn_tok])
            ptb, pt_tok = transpose_to(C, hn, hn_tok, None, None)
            S_.op("act", lambda e, ptb=ptb: e.copy(out=hnT[:], in_=ptb.rearrange("p (k t) -> p k t", k=8)),
                 reads=[pt_tok], writes=[hnT_tok])
            for half in range(2):
                b_ = nbk_()
                for j4 in range(4):
                    hc = half * 4 + j4
                    for kt in range(8):
                        S_.op("pe", lambda e, kt=kt, hc=hc, j4=j4, b_=b_: e.matmul(
                            ps[b_][:, j4 * 128:(j4 + 1) * 128], lhsT=wxq[:, kt, hc * 128:(hc + 1) * 128], rhs=hnT[:, kt, :],
                            start=(kt == 0), stop=(kt == 7)), reads=[wxq_tok, hnT_tok], writes=[ps_tok[b_]])
                S_.op("act", lambda e, half=half, b_=b_: e.mul(out=qT[:, half * 4:(half + 1) * 4, :],
                                                             in_=ps[b_][:].rearrange("p (a b) -> p a b", a=4), mul=0.0625),
                     reads=[ps_tok[b_]], writes=[qT_tok])
            sb_ = [nbk_(), nbk_()]
            for hh in range(4):
                b_ = sb_[hh // 2]
                for c2 in range(2):
                    S_.op("pe", lambda e, hh=hh, c2=c2, b_=b_: e.matmul(
                        ps[b_][:, (hh % 2) * 256:(hh % 2 + 1) * 256], lhsT=qT[:, 2 * hh + c2, :], rhs=KmT[:, 2 * hh + c2, :],
                        start=(c2 == 0), stop=(c2 == 1)), reads=[qT_tok, kv_tok], writes=[ps_tok[b_]])
            for i2 in range(2):
                b_ = sb_[i2]
                S_.op("dve", lambda e, i2=i2, b_=b_: e.tensor_reduce(out=mx[:, 2 * i2:2 * i2 + 2],
                                                                    in_=ps[b_][:].rearrange("p (a b) -> p a b", a=2),
                                                                    axis=AX.X, op=ALU.max),
                     reads=[ps_tok[b_]], writes=[sm_tok])
            S_.op("dve", lambda e: e.tensor_scalar(mx[:], mx[:], -1.0, None, ALU.mult), reads=[sm_tok], writes=[sm_tok])
            for hh in range(4):
                b_ = sb_[hh // 2]
                S_.op("act", lambda e, hh=hh, b_=b_: e.activation(out=pp[:, hh, :], in_=ps[b_][:, (hh % 2) * 256:(hh % 2 + 1) * 256],
                                                                 func=AF.Exp, bias=mx[:, hh:hh + 1], accum_out=sm[:, hh:hh + 1]),
                     reads=[ps_tok[b_], sm_tok], writes=[pp_tok, sm_tok])
            S_.op("dve", lambda e: e.reciprocal(out=sm[:], in_=sm[:]), reads=[sm_tok], writes=[sm_tok])
            for hh in range(4):
                S_.op("dve", lambda e, hh=hh: e.tensor_scalar(pp[:, hh, :], pp[:, hh, :], sm[:, hh:hh + 1], None, ALU.mult),
                     reads=[sm_tok, pp_tok], writes=[pp_tok])
            ptb, pt_tok = transpose_to(C, pp[:].rearrange("p a b -> p (a b)"), pp_tok, None, None)
            S_.op("act", lambda e, ptb=ptb: e.copy(out=pT[:], in_=ptb.rearrange("p (k t) -> p k t", k=8)),
                 reads=[pt_tok], writes=[pT_tok])
            for half in range(2):
                b_ = nbk_()
                for j4 in range(4):
                    hc = half * 4 + j4
                    hh, c2 = hc // 2, hc % 2
                    for mt in range(2):
                        S_.op("pe", lambda e, mt=mt, hh=hh, c2=c2, j4=j4, b_=b_: e.matmul(
                            ps[b_][:, j4 * 128:(j4 + 1) * 128], lhsT=Vm[:, mt, hh * 256 + c2 * 128:hh * 256 + (c2 + 1) * 128],
                            rhs=pT[:, 2 * hh + mt, :], start=(mt == 0), stop=(mt == 1)),
                            reads=[kv_tok, pT_tok], writes=[ps_tok[b_]])
                S_.op("act", lambda e, half=half, b_=b_: e.copy(out=oT[:, half * 4:(half + 1) * 4, :],
                                                              in_=ps[b_][:].rearrange("p (a b) -> p a b", a=4)),
                     reads=[ps_tok[b_]], writes=[oT_tok])
            for n2 in range(2):
                b_ = nbk_()
                for hc in range(8):
                    S_.op("pe", lambda e, hc=hc, n2=n2, b_=b_: e.matmul(ps[b_][:], lhsT=oT[:, hc, :], rhs=wxo[:, hc, n2 * 512:(n2 + 1) * 512],
                                                                      start=(hc == 0), stop=(hc == 7)),
                         reads=[oT_tok, wxo_tok], writes=[ps_tok[b_]])
                S_.op("dve", lambda e, n2=n2, b_=b_: e.tensor_tensor(out=h[:, n2 * 512:(n2 + 1) * 512], in0=ps[b_][:],
                                                                    in1=h[:, n2 * 512:(n2 + 1) * 512], op=ALU.add),
                     reads=[ps_tok[b_]], writes=[h_tok])
            dst = y if dbg == "h2" else h2buf
            S_.dma("sp", h_ch, dst[t0:t0 + 128, :], h[:], reads=[h_tok], writes=[h2_tok[tile]])

            C.S = saved

        sets1 = [mkset("_e"), mkset("_o")]
        bk = [[0], [0]]

        def mk_nbk(par):
            def f():
                b = 3 * par + bk[par][0] % 3
                bk[par][0] += 1
                return b
            return f

        for t2 in range(0, NT, 2):
            recs = []
            for par in range(2):
                r_ = _Rec1()
                C.tp_force = par
                emit_tile(r_, t2 + par, mk_nbk(par), **sets1[par])
                C.tp_force = None
                recs.append(r_.items)
            while recs[0] or recs[1]:
                for par in range(2):
                    for _ in range(6):
                        if recs[par]:
                            kind, a, k = recs[par].pop(0)
                            getattr(S, kind)(*a, **k)
        S.barrier()
    if dbg in ("h1", "h2"):
        S.wait_tok("sp", h2_tok)
        eo.close()
        return

    with ExitStack() as e2:
        h = _sb(nc, e2, "h_b", [128, D], F32)
        h_tok = Tok()
        h_ch = S.chan()
        hn = _sb(nc, e2, "hn_b", [128, D], BF16)
        hn_tok = Tok()
        hnT = _sb(nc, e2, "hnT_b", [128, 8, 128], BF16)
        hnT_tok = Tok()
        hn3 = _sb(nc, e2, "hn3", [128, D], F32)
        hn3_tok = Tok()
        ss = _sb(nc, e2, "qss", [128, 1], F32)
        ss_tok = Tok()
        rstd = _sb(nc, e2, "qrstd", [128, 1], F32)
        rstd_tok = Tok()
        qpT = _sb(nc, e2, "qpT", [128, 16, 128], BF16)
        qpT_tok = Tok()
        sc = _sb(nc, e2, "sc", [128, 16, 128], F32)
        sc_tok = Tok()
        scr = _sb(nc, e2, "scr", [128, 2048], F32)
        scr_tok = Tok()
        hv = _sb(nc, e2, "hv", [128, 16, 16], F32)
        hi = _sb(nc, e2, "hi", [128, 16, 16], U32)
        hif = _sb(nc, e2, "hif", [128, 16, 16], F32)
        hv_tok = Tok()
        cand = _sb(nc, e2, "cand", [128, 8, 256], F32)
        eidx = _sb(nc, e2, "eidx", [128, 8, 256], F32)
        e0 = _sb(nc, e2, "e0", [128, 8, 16], F32)
        cand_tok = Tok()
        bv = _sb(nc, e2, "bv", [128, 8, 16], F32)
        bp = _sb(nc, e2, "bp", [128, 8, 16], U32)
        bpf = _sb(nc, e2, "bpf", [128, 8, 16], F32)
        bv_tok = Tok()
        junk = [_sb(nc, e2, "junk256_%d" % i, [128, 256], F32) for i in range(4)]
        junk_tok = [Tok() for _ in range(4)]
        eidc_tok = [Tok() for _ in range(128)]
        jq = [0]
        eidf = _sb(nc, e2, "eidf", [128, 128], F32)
        eid = _sb(nc, e2, "eid", [128, 128], U32)
        eid_tok = Tok()
        gt = _sb(nc, e2, "gt", [128, 8, 16], F32)
        gs = _sb(nc, e2, "gs", [128, 8], F32)
        nb0 = _sb(nc, e2, "nb0", [128, 8], F32)
        gt_tok = Tok()
        actc = _sb(nc, e2, "actc", [128, 128], F32)
        act_tok = Tok()
        wgt = _sb(nc, e2, "wgt", [128, 128], F32)
        wg2 = _sb(nc, e2, "wg2", [128, 128], F32)
        wgt_tok = Tok()
        NG = 8
        gbuf = [_sb(nc, e2, "gbuf%d" % i, [128, 2 * D], BF16) for i in range(NG)]
        gbuf_tok = [Tok() for _ in range(NG)]
        gbuf_ch = [S.chan() for _ in range(NG)]
        junk2 = [_sb(nc, e2, "junk2_%d" % i, [128, D], BF16) for i in range(2)]
        junk2_tok = [Tok(), Tok()]
        j2 = [0]
        NDG = 4
        dg = [_sb(nc, e2, "dg%d" % i, [128, 128], BF16) for i in range(NDG)]
        dg_tok = [Tok() for _ in range(NDG)]
        slot_tok = [Tok() for _ in range(128)]
        grp_tok = [Tok() for _ in range(32)]
        di = 0
        ytok = Tok()
        gi = 0
        PLAY_N = 15
        bankA = [0]

        def nbkA():
            b = bankA[0] % 4
            bankA[0] += 1
            return b

        class _Rec:
            def __init__(self):
                self.items = []

            def op(self, *a, **k):
                self.items.append(("op", a, k))

            def dma(self, *a, **k):
                self.items.append(("dma", a, k))

        junkF = _sb(nc, e2, "junkF", [128, D], BF16)
        junkF_tok = Tok()
        ss2 = _sb(nc, e2, "ss2", [128, 1], F32)
        ss2_tok = Tok()
        rstd2 = _sb(nc, e2, "rstd2", [128, 1], F32)
        rstd2_tok = Tok()
        obuf = _sb(nc, e2, "obuf", [128, D], F32)
        obuf_tok = Tok()
        o_ch = S.chan()
        h_b2 = _sb(nc, e2, "h_b2", [128, D], F32)
        hn3_b2 = _sb(nc, e2, "hn3_b2", [128, D], F32)
        eid_b2 = _sb(nc, e2, "eid_b2", [128, 128], U32)
        gt_b2 = _sb(nc, e2, "gt_b2", [128, 8, 16], F32)
        sets = [dict(h=h, h_tok=h_tok, h_ch=h_ch, hn3=hn3, hn3_tok=hn3_tok, eid=eid, eid_tok=eid_tok, gt=gt, gt_tok=gt_tok),
                dict(h=h_b2, h_tok=Tok(), h_ch=S.chan(), hn3=hn3_b2, hn3_tok=Tok(), eid=eid_b2, eid_tok=Tok(), gt=gt_b2, gt_tok=Tok())]

        def emitA(S_, tile, h, h_tok, h_ch, hn3, hn3_tok, eid, eid_tok, gt, gt_tok):
            saved = C.S
            C.S = S_
            t0 = tile * 128
            S_.dma("sp", h_ch, h[:], h2buf[t0:t0 + 128, :], reads=[h2_tok[tile]], writes=[h_tok])
            _rms_rstd(C, S_, h[:], h_tok, hn[:], hn_tok, ss, ss_tok, rstd, rstd_tok)
            S_.op("dve", lambda e: e.tensor_scalar(hn[:], h[:], rstd[:], None, ALU.mult), reads=[h_tok, rstd_tok], writes=[hn_tok])
            S_.op("dve", lambda e: e.scalar_tensor_tensor(out=hn3[:], in0=h[:], scalar=rstd[:], in1=gffn_rep[:], op0=ALU.mult, op1=ALU.mult),
                 reads=[h_tok, rstd_tok, pg_tok], writes=[hn3_tok])
            ptb, pt_tok = transpose_to(C, hn, hn_tok, None, None)
            S_.op("act", lambda e, ptb=ptb: e.copy(out=hnT[:], in_=ptb.rearrange("p (k t) -> p k t", k=8)),
                 reads=[pt_tok], writes=[hnT_tok])
            for q4 in range(4):
                b_ = nbkA()
                for j4 in range(4):
                    ch = q4 * 4 + j4
                    for kt in range(8):
                        S_.op("pe", lambda e, kt=kt, ch=ch, j4=j4, b_=b_: e.matmul(
                            ps[b_][:, j4 * 128:(j4 + 1) * 128], lhsT=wpq[:, kt, ch * 128:(ch + 1) * 128], rhs=hnT[:, kt, :],
                            start=(kt == 0), stop=(kt == 7)), reads=[pw_tok, hnT_tok], writes=[ps_tok[b_]])
                S_.op("act", lambda e, q4=q4, b_=b_: e.copy(out=qpT[:, q4 * 4:(q4 + 1) * 4, :],
                                                          in_=ps[b_][:].rearrange("p (a b) -> p a b", a=4)),
                     reads=[ps_tok[b_]], writes=[qpT_tok])
            for q4 in range(4):
                b_ = nbkA()
                for j4 in range(4):
                    ch = q4 * 4 + j4
                    S_.op("pe", lambda e, ch=ch, j4=j4, b_=b_: e.matmul(ps[b_][:, j4 * 128:(j4 + 1) * 128], lhsT=qpT[:, ch, :],
                                                                      rhs=skT[:, ch, :], start=True, stop=True),
                         reads=[pw_tok, qpT_tok], writes=[ps_tok[b_]])
                S_.op("act", lambda e, q4=q4, b_=b_: e.copy(out=sc[:, q4 * 4:(q4 + 1) * 4, :],
                                                          in_=ps[b_][:].rearrange("p (a b) -> p a b", a=4)),
                     reads=[ps_tok[b_]], writes=[sc_tok])
            scr3 = scr[:].rearrange("p (a b) -> p a b", a=16)
            for ch in range(16):
                S_.op("dve", lambda e, ch=ch: e.max(out=hv[:, ch, 0:8], in_=sc[:, ch, :]), reads=[sc_tok], writes=[hv_tok])
                S_.op("dve", lambda e, ch=ch: e.max_index(out=hi[:, ch, 0:8], in_max=hv[:, ch, 0:8], in_values=sc[:, ch, :]),
                     reads=[sc_tok, hv_tok], writes=[hv_tok])
                S_.op("dve", lambda e, ch=ch: e.match_replace(out=scr3[:, ch, :], in_to_replace=hv[:, ch, 0:8], in_values=sc[:, ch, :],
                                                             imm_value=NEG), reads=[sc_tok, hv_tok], writes=[scr_tok])
                S_.op("dve", lambda e, ch=ch: e.max(out=hv[:, ch, 8:16], in_=scr3[:, ch, :]), reads=[scr_tok], writes=[hv_tok])
                S_.op("dve", lambda e, ch=ch: e.max_index(out=hi[:, ch, 8:16], in_max=hv[:, ch, 8:16], in_values=scr3[:, ch, :]),
                     reads=[scr_tok, hv_tok], writes=[hv_tok])
            S_.op("dve", lambda e: e.tensor_copy(out=hif[:], in_=hi[:]), reads=[hv_tok], writes=[hv_tok])
            hv4 = hv[:].rearrange("p (h i) k -> p h i k", i=2)
            hif4 = hif[:].rearrange("p (h i) k -> p h i k", i=2)
            cand4 = cand[:].rearrange("p h (a b) -> p h a b", a=16)
            eidx4 = eidx[:].rearrange("p h (a b) -> p h a b", a=16)
            S_.op("dve", lambda e: e.tensor_tensor(out=cand4, in0=hv4[:, :, 0, :].unsqueeze(3).to_broadcast([128, 8, 16, 16]),
                                                  in1=hv4[:, :, 1, :].unsqueeze(2).to_broadcast([128, 8, 16, 16]), op=ALU.add),
                 reads=[hv_tok], writes=[cand_tok])
            S_.op("dve", lambda e: e.tensor_scalar(e0[:], hif4[:, :, 0, :], 128.0, None, ALU.mult), reads=[hv_tok], writes=[cand_tok])
            S_.op("dve", lambda e: e.tensor_tensor(out=eidx4, in0=e0[:].unsqueeze(3).to_broadcast([128, 8, 16, 16]),
                                                  in1=hif4[:, :, 1, :].unsqueeze(2).to_broadcast([128, 8, 16, 16]), op=ALU.add),
                 reads=[hv_tok, cand_tok], writes=[cand_tok])
            scr8 = scr[:].rearrange("p (a b) -> p a b", a=8)
            for hh in range(8):
                S_.op("dve", lambda e, hh=hh: e.max(out=bv[:, hh, 0:8], in_=cand[:, hh, :]), reads=[cand_tok], writes=[bv_tok])
                S_.op("dve", lambda e, hh=hh: e.max_index(out=bp[:, hh, 0:8], in_max=bv[:, hh, 0:8], in_values=cand[:, hh, :]),
                     reads=[cand_tok, bv_tok], writes=[bv_tok])
                S_.op("dve", lambda e, hh=hh: e.match_replace(out=scr8[:, hh, :], in_to_replace=bv[:, hh, 0:8], in_values=cand[:, hh, :],
                                                             imm_value=NEG), reads=[cand_tok, bv_tok], writes=[scr_tok])
                S_.op("dve", lambda e, hh=hh: e.max(out=bv[:, hh, 8:16], in_=scr8[:, hh, :]), reads=[scr_tok], writes=[bv_tok])
                S_.op("dve", lambda e, hh=hh: e.max_index(out=bp[:, hh, 8:16], in_max=bv[:, hh, 8:16], in_values=scr8[:, hh, :]),
                     reads=[scr_tok, bv_tok], writes=[bv_tok])
            S_.op("dve", lambda e: e.tensor_copy(out=bpf[:], in_=bp[:]), reads=[bv_tok], writes=[bv_tok])
            for hh in range(8):
                for k in range(16):
                    s_ = hh * 16 + k
                    jb = jq[0] % 4
                    jq[0] += 1
                    S_.op("dve", lambda e, hh=hh, k=k, s_=s_, jb=jb: e.scalar_tensor_tensor(
                        out=junk[jb][:], in0=iota256[:], scalar=bpf[:, hh, k:k + 1], in1=eidx[:, hh, :],
                        op0=ALU.is_equal, op1=ALU.mult, accum_out=eidf[:, s_:s_ + 1]),
                        reads=[bv_tok, cand_tok, pg_tok], writes=[junk_tok[jb], eidc_tok[s_]])
            S_.op("dve", lambda e: e.tensor_scalar(eidf[:], eidf[:], 16383.0, 0.0, ALU.min, ALU.max), reads=[eid_tok] + eidc_tok,
                  writes=[eid_tok] + eidc_tok)
            S_.op("dve", lambda e: e.tensor_copy(out=eid[:], in_=eidf[:]), reads=[eid_tok], writes=[eid_tok])
            S_.op("dve", lambda e: e.tensor_scalar(nb0[:], bv[:, :, 0], -1.0, None, ALU.mult), reads=[bv_tok], writes=[gt_tok])
            for hh in range(8):
                S_.op("act", lambda e, hh=hh: e.activation(out=gt[:, hh, :], in_=bv[:, hh, :], func=AF.Exp, bias=nb0[:, hh:hh + 1],
                                                          accum_out=gs[:, hh:hh + 1]), reads=[bv_tok, gt_tok], writes=[gt_tok])
            S_.op("dve", lambda e: e.reciprocal(out=gs[:], in_=gs[:]), reads=[gt_tok], writes=[gt_tok])
            S_.op("dve", lambda e: e.tensor_tensor(out=gt[:], in0=gt[:], in1=gs[:].unsqueeze(2).to_broadcast([128, 8, 16]), op=ALU.mult),
                 reads=[gt_tok], writes=[gt_tok])

            C.S = saved

        def emitB(tile, play, h, h_tok, h_ch, hn3, hn3_tok, eid, eid_tok, gt, gt_tok):
            nonlocal gi, di
            t0 = tile * 128
            pa, pb_ = 4, 5
            gtf = gt[:].rearrange("p a b -> p (a b)")
            for grp in range(32):
                used = []
                for q in range(4):
                    s_ = grp * 4 + q
                    g_ = gi % NG
                    gi += 1
                    used.append(g_)
                    S.dma("pool", gbuf_ch[g_], gbuf[g_][:], uvbf[:, :], reads=[eid_tok] + uv_toks, writes=[gbuf_tok[g_]],
                          indirect=bass.IndirectOffsetOnAxis(ap=eid[:, s_:s_ + 1], axis=0))
                    jb = j2[0] % 2
                    j2[0] += 1
                    S.op("dve", lambda e, g_=g_, s_=s_, jb=jb: e.scalar_tensor_tensor(out=junk2[jb][:], in0=gbuf[g_][:, 0:D], scalar=1.0, in1=hn3[:],
                                                                                     op0=ALU.mult, op1=ALU.mult, accum_out=actc[:, s_:s_ + 1]),
                         reads=[gbuf_tok[g_], hn3_tok], writes=[junk2_tok[jb], slot_tok[s_]])
                cs = slice(grp * 4, grp * 4 + 4)
                gk = grp_tok[grp]
                S.op("dve", lambda e: e.tensor_tensor(out=wg2[:, cs], in0=actc[:, cs], in1=actc[:, cs], op=ALU.mult),
                     reads=[slot_tok[grp * 4 + q] for q in range(4)], writes=[gk])
                S.op("dve", lambda e: e.tensor_scalar(wg2[:, cs], wg2[:, cs], 0.044715, 1.0, ALU.mult, ALU.add), reads=[gk], writes=[gk])
                S.op("dve", lambda e: e.tensor_tensor(out=wg2[:, cs], in0=wg2[:, cs], in1=actc[:, cs], op=ALU.mult),
                     reads=[gk] + [slot_tok[grp * 4 + q] for q in range(4)], writes=[gk])
                S.op("act", lambda e: e.activation(out=wg2[:, cs], in_=wg2[:, cs], func=AF.Sigmoid, scale=1.5957691216057308),
                     reads=[gk], writes=[gk])
                S.op("dve", lambda e: e.tensor_tensor(out=wgt[:, cs], in0=wg2[:, cs], in1=actc[:, cs], op=ALU.mult),
                     reads=[gk] + [slot_tok[grp * 4 + q] for q in range(4)], writes=[gk])
                S.op("dve", lambda e: e.tensor_tensor(out=wgt[:, cs], in0=wgt[:, cs], in1=gtf[:, cs], op=ALU.mult),
                     reads=[gk, gt_tok], writes=[gk])
                for q in range(4):
                    s_ = grp * 4 + q
                    g_ = used[q]
                    d_ = di % NDG
                    di += 1
                    S.op("act", lambda e, d_=d_, s_=s_: e.activation(out=dg[d_][:], in_=C.ident_bf[:], func=AF.Copy,
                                                                    scale=wgt[:, s_:s_ + 1]),
                         reads=[gk, C.const_tok], writes=[dg_tok[d_]])
                    for n2, pbk in enumerate((pa, pb_)):
                        S.op("pe", lambda e, d_=d_, g_=g_, n2=n2, pbk=pbk, s_=s_: e.matmul(
                            ps[pbk][:], lhsT=dg[d_][:], rhs=gbuf[g_][:, D + n2 * 512:D + (n2 + 1) * 512],
                            start=(s_ == 0), stop=(s_ == 127)), reads=[dg_tok[d_], gbuf_tok[g_]], writes=[ps_tok[pbk]])
                play(PLAY_N)
            for n2, pbk in enumerate((pa, pb_)):
                S.op("dve", lambda e, n2=n2, pbk=pbk: e.tensor_tensor(out=h[:, n2 * 512:(n2 + 1) * 512], in0=ps[pbk][:],
                                                                      in1=h[:, n2 * 512:(n2 + 1) * 512], op=ALU.add),
                     reads=[ps_tok[pbk]], writes=[h_tok])
            if dbg == "h3":
                S.dma("sp", h_ch, y[t0:t0 + 128, :], h[:], reads=[h_tok], writes=[ytok])
                play(10 ** 9)
                return
            _rms_rstd(C, S, h[:], h_tok, junkF[:], junkF_tok, ss2, ss2_tok, rstd2, rstd2_tok)
            S.op("dve", lambda e: e.scalar_tensor_tensor(out=obuf[:], in0=h[:], scalar=rstd2[:], in1=gfin_rep[:], op0=ALU.mult, op1=ALU.mult),
                 reads=[h_tok, rstd2_tok, pg_tok], writes=[obuf_tok])
            S.dma("sp", o_ch, y[t0:t0 + 128, :], obuf[:], reads=[obuf_tok], writes=[ytok])
            play(10 ** 9)

        rec = _Rec()
        emitA(rec, 0, **sets[0])
        for kind, a, k in rec.items:
            getattr(S, kind)(*a, **k)
        for tile in range(NT):
            rec = _Rec()
            if tile + 1 < NT:
                emitA(rec, tile + 1, **sets[(tile + 1) % 2])
            items = rec.items

            def play(n, items=items):
                while n > 0 and items:
                    kind, a, k = items.pop(0)
                    getattr(S, kind)(*a, **k)
                    n -= 1

            emitB(tile, play, **sets[tile % 2])
            play(10 ** 9)
        S.wait_tok("sp", [ytok])
        S.barrier()
    eo.close()


def build(dbg=None):
    nc = bass.Bass("TRN2", target_bir_lowering=False)
    C = Ctx()
    C.nc = nc
    C.dbg = dbg

    def din(name, shape, dt=F32):
        return nc.dram_tensor(name, list(shape), dt, kind="ExternalInput").ap()

    x_full = din("x_full", [SEQ, D])
    x_own = din("x_own", [NOWN * SEGT, D])
    qpos = din("qpos", [128, NOWN, SEGT])
    w_in = din("w_in", [128, 8, 2048])
    g_mix = din("g_mix", [128, 8])
    x_rev = din("x_rev", [SEQ, D])
    I = {"x_full": x_full, "x_own": x_own, "w_in": w_in, "x_rev": x_rev}
    for nm, shp in (("a_re", [128, 16]), ("a_im", [128, 16]), ("log_dt", [128, 16]),
                    ("bpad_re", [128, 16, 128]), ("bpad_im", [128, 16, 128]),
                    ("cpad_re", [128, 16, 128]), ("cpad_im", [128, 16, 128]),
                    ("d_skip", [128, 4]), ("w_glu", [128, 4, 512]), ("b_glu", [128, 4]),
                    ("segsel", [128, 4, 16]),
                    ("w_out", [128, 8, D]), ("g_out", [128, 8]), ("mem", [256, D]), ("g_mem", [128, 8]),
                    ("w_xkv", [128, 8, 2048]), ("g_xattn", [128, 8]), ("w_xq", [128, 8, D]), ("w_xo", [128, 8, D]),
                    ("g_ffn", [128, 8]), ("gffn_rep", [128, D]), ("gfin_rep", [128, D]), ("w_pq", [128, 8, 2048]),
                    ("skT", [128, 16, 128]), ("peer_u", [16384, D]), ("peer_v", [16384, D])):
        I[nm] = din(nm, shp)
    y = nc.dram_tensor("y", [NOWN * SEGT, D], F32, kind="ExternalOutput").ap()
    if dbg == "attn":
        dbg_out = nc.dram_tensor("dbg_attn", [128, 4, NOWN * SEGT], F32, kind="ExternalOutput").ap()

    with ExitStack() as es:
        S = Sched(nc, es)
        C.S = S
        C.const_tok = Tok()
        ident_f = _sb(nc, es, "ident_f", [128, 128], F32)
        C.ident_f = ident_f
        C.ident_bf = _sb(nc, es, "ident_bf", [128, 128], BF16)
        ones_f = _sb(nc, es, "ones_f", [128, 128], F32)
        C.ones_bf = _sb(nc, es, "ones_bf", [128, 128], BF16)
        C.tri_bf = _sb(nc, es, "tri_bf", [128, 128], BF16)
        C.atri_bf = _sb(nc, es, "atri_bf", [128, 128], BF16)
        C.eps_col = _sb(nc, es, "eps_col", [128, 1], F32)
        C.kpos = _sb(nc, es, "kpos", [128, 64], F32)
        S.op("pool", lambda e: e.memset(ones_f[:], 1.0), writes=[C.const_tok])
        S.op("pool", lambda e: e.memset(C.eps_col[:], EPS), writes=[C.const_tok])
        S.op("pool", lambda e: e.affine_select(out=ident_f[:], in_=ones_f[:], pattern=[[-1, 128]],
                                               compare_op=ALU.is_equal, fill=0.0, base=0,
                                               channel_multiplier=1),
             reads=[C.const_tok], writes=[C.const_tok])
        S.op("pool", lambda e: e.tensor_copy(out=C.ident_bf[:], in_=ident_f[:]),
             reads=[C.const_tok], writes=[C.const_tok])
        S.op("pool", lambda e: e.tensor_copy(out=C.ones_bf[:], in_=ones_f[:]),
             reads=[C.const_tok], writes=[C.const_tok])
        S.op("pool", lambda e: e.affine_select(out=C.tri_bf[:], in_=ones_f[:], pattern=[[-1, 128]],
                                               compare_op=ALU.is_ge, fill=0.0, base=0,
                                               channel_multiplier=1),
             reads=[C.const_tok], writes=[C.const_tok])
        S.op("pool", lambda e: e.affine_select(out=C.atri_bf[:], in_=ones_f[:], pattern=[[1, 128]],
                                               compare_op=ALU.is_gt, fill=0.0, base=0,
                                               channel_multiplier=-1),
             reads=[C.const_tok], writes=[C.const_tok])
        S.op("pool", lambda e: e.iota(C.kpos[:], pattern=[[128, 64]], base=0, channel_multiplier=1,
                                      allow_small_or_imprecise_dtypes=True),
             writes=[C.const_tok])

        ps = [es.enter_context(nc.psum_tensor("ps%d" % i, [128, 512], F32)) for i in range(8)]
        ps_tok = [Tok() for _ in range(8)]
        C.tp_ps = [ps[6], ps[7]]
        C.tp_tok = [ps_tok[6], ps_tok[7]]
        C.tp_i = 0

        attnT = _sb(nc, es, "attnT", [128, 4, NOWN * SEGT], BF16)
        attn_tok = [[Tok() for _ in range(8)] for _ in range(NOWN)]

        gmix_sb = _sb(nc, es, "gmix", [128, 8], F32)
        gmix_tok = Tok()
        S.dma("sp", S.chan(), gmix_sb[:], g_mix[:, :], writes=[gmix_tok])

        with ExitStack() as e2:
          if dbg != "ssm":
              KT = _sb(nc, e2, "KT", [128, 4, SEQ], BF16)
              V = _sb(nc, e2, "V", [128, 64, 512], BF16)
              QT = _sb(nc, e2, "QT", [128, 4, NOWN * SEGT], BF16)
              kt_tok = [Tok() for _ in range(NSEG)]
              v_tok = [Tok() for _ in range(NSEG)]
              q_tok = [Tok() for _ in range(NOWN)]
              with ExitStack() as e3:
                  wk = _sb(nc, e3, "wk", [128, 8, 512], BF16)
                  wq = wk
                  wv = _sb(nc, e3, "wv", [128, 8, 512], BF16)
                  w_tok = Tok()
                  wk_tok = Tok()
                  wv_tok = Tok()
                  xt = [_sb(nc, e3, "xt%d" % i, [128, D], F32) for i in range(2)]
                  xt_tok = [Tok() for _ in range(2)]
                  xt_ch = [S.chan() for _ in range(2)]
                  wi_box = [0]

                  def load_w(wdst, c0, wtok):
                      for k2 in range(4):
                          sl = wi_box[0] % 2
                          wi_box[0] += 1
                          S.dma("sp", xt_ch[sl], xt[sl][:].rearrange("p (a b) -> p a b", a=2),
                                w_in[:, 2 * k2:2 * k2 + 2, c0:c0 + 512], writes=[xt_tok[sl]])
                          for a2 in range(2):
                              kt = 2 * k2 + a2
                              eng = "dve" if a2 == 0 else "pool"
                              S.op(eng, lambda e, kt=kt, sl=sl, wdst=wdst, a2=a2: e.tensor_scalar(
                                  wdst[:, kt, :], xt[sl][:, a2 * 512:(a2 + 1) * 512], gmix_sb[:, kt:kt + 1], None, ALU.mult),
                                  reads=[xt_tok[sl], gmix_tok], writes=[wtok])

                  load_w(wk, 512, wk_tok)
                  load_w(wv, 1024, wv_tok)

                  xn = [_sb(nc, e3, "xn%d" % i, [128, D], BF16) for i in range(2)]
                  xn_tok = [Tok(), Tok()]
                  ss = [_sb(nc, e3, "ss%d" % i, [128, 1], F32) for i in range(2)]
                  ss_tok = [Tok(), Tok()]
                  rstd = [_sb(nc, e3, "rstd%d" % i, [128, 1], F32) for i in range(2)]
                  rstd_tok = [Tok(), Tok()]
                  xnT = [_sb(nc, e3, "xnT%d" % i, [128, 8, SEGT], BF16) for i in range(2)]
                  xnT_tok = [Tok(), Tok()]
                  ti = 0
                  for sgi in range(NSEG + NOWN):
                      own = sgi >= NSEG
                      sg = sgi - NSEG if own else sgi
                      if sgi == NSEG:
                          load_w(wq, 0, wk_tok)
                      src = x_own if own else x_full
                      xb = sgi % 2
                      for m in range(4):
                          a = ti % 2
                          b = ti % 2
                          ti += 1
                          r0 = sg * SEGT + m * 128
                          S.dma("sp", xt_ch[a], xt[a][:], src[r0:r0 + 128, :], writes=[xt_tok[a]])
                          rms_scale(C, xt[a][:], xt_tok[a], rstd[b][:], rstd_tok[b], xn[b][:], xn_tok[b],
                                    ss[b][:], ss_tok[b])
                          S.op("dve", lambda e, a=a, b=b: e.tensor_scalar(xn[b][:], xt[a][:], rstd[b][:], None, ALU.mult),
                               reads=[xt_tok[a], rstd_tok[b]], writes=[xn_tok[b]])
                          ptb, pt_tok = transpose_to(C, xn[b], xn_tok[b], None, None)
                          S.op("dve", lambda e, xb=xb, m=m, ptb=ptb: e.tensor_copy(
                              out=xnT[xb][:, :, m * 128:(m + 1) * 128],
                              in_=ptb.rearrange("p (k t) -> p k t", k=8)),
                              reads=[pt_tok], writes=[xnT_tok[xb]])
                      if not own:
                          for hp in range(4):
                              pb = hp % 2
                              for kt in range(8):
                                  S.op("pe", lambda e, kt=kt, hp=hp, pb=pb, xb=xb: e.matmul(
                                      ps[pb][:], lhsT=wk[:, kt, hp * 128:(hp + 1) * 128], rhs=xnT[xb][:, kt, :],
                                      start=(kt == 0), stop=(kt == 7)),
                                      reads=[wk_tok, xnT_tok[xb]], writes=[ps_tok[pb]])
                              S.op("act", lambda e, hp=hp, pb=pb, sg=sg: e.copy(
                                  out=KT[:, hp, sg * SEGT:(sg + 1) * SEGT], in_=ps[pb][:]),
                                  reads=[ps_tok[pb]], writes=[kt_tok[sg]])
                          for m in range(4):
                              pb = 2 + m % 2
                              for kt in range(8):
                                  S.op("pe", lambda e, kt=kt, m=m, pb=pb, xb=xb: e.matmul(
                                      ps[pb][:], lhsT=xnT[xb][:, kt, m * 128:(m + 1) * 128], rhs=wv[:, kt, :],
                                      start=(kt == 0), stop=(kt == 7)),
                                      reads=[wv_tok, xnT_tok[xb]], writes=[ps_tok[pb]])
                              S.op("act", lambda e, m=m, pb=pb, sg=sg: e.copy(
                                  out=V[:, sg * 4 + m, :], in_=ps[pb][:]),
                                  reads=[ps_tok[pb]], writes=[v_tok[sg]])
                      else:
                          for hp in range(4):
                              pb = hp % 2
                              for kt in range(8):
                                  S.op("pe", lambda e, kt=kt, hp=hp, pb=pb, xb=xb: e.matmul(
                                      ps[pb][:], lhsT=wq[:, kt, hp * 128:(hp + 1) * 128], rhs=xnT[xb][:, kt, :],
                                      start=(kt == 0), stop=(kt == 7)),
                                      reads=[wk_tok, xnT_tok[xb]], writes=[ps_tok[pb]])
                              S.op("act", lambda e, hp=hp, pb=pb, sg=sg: e.mul(
                                  out=QT[:, hp, sg * SEGT:(sg + 1) * SEGT], in_=ps[pb][:], mul=0.125),
                                  reads=[ps_tok[pb]], writes=[q_tok[sg]])

              S.barrier()
              if dbg == "kv":
                  dch = S.chan()
                  dk = nc.dram_tensor("dbg_kt", [128, 4, SEQ], BF16, kind="ExternalOutput").ap()
                  dv = nc.dram_tensor("dbg_v", [128, 64, 512], BF16, kind="ExternalOutput").ap()
                  dq = nc.dram_tensor("dbg_q", [128, 4, NOWN * SEGT], BF16, kind="ExternalOutput").ap()
                  dtok = Tok()
                  S.dma("sp", dch, dk[:, :, :], KT[:], reads=kt_tok, writes=[dtok])
                  S.dma("sp", dch, dv[:, :, :], V[:], reads=v_tok, writes=[dtok])
                  S.dma("sp", dch, dq[:, :, :], QT[:], reads=q_tok, writes=[dtok])
                  S.wait_tok("sp", [dtok])
              with ExitStack() as e3:
                if dbg != "kv":
                    NE = 3
                    e_sb = [_sb(nc, e3, "e%d" % i, [128, 512], F32) for i in range(NE)]
                    e_tok = [Tok() for _ in range(NE)]
                    sp_sb = [_sb(nc, e3, "sp%d" % i, [128, 512], BF16) for i in range(3)]
                    sp_tok = [Tok(), Tok(), Tok()]
                    ex_sb = [_sb(nc, e3, "ex%d" % i, [128, 512], BF16) for i in range(2)]
                    ex_tok = [Tok(), Tok()]
                    w_sb = [_sb(nc, e3, "w%d" % i, [128, 512], BF16) for i in range(2)]
                    w_tok2 = [Tok(), Tok()]
                    masks = _sb(nc, e3, "masks", [128, 16, 512], BF16)
                    mask_tok = Tok()
                    qp = _sb(nc, e3, "qp", [128, NOWN, SEGT], F32)
                    qp_tok = Tok()
                    S.dma("sp", S.chan(), qp[:], qpos[:, :, :], writes=[qp_tok])
                    zps = [ps[0], ps[1]]
                    zps_tok = [ps_tok[0], ps_tok[1]]
                    cps = [ps[2], ps[3]]
                    cps_tok = [ps_tok[2], ps_tok[3]]
                    ops_ = [ps[4], ps[5]]
                    ops_tok = [ps_tok[4], ps_tok[5]]

                    for slot in range(NOWN):
                        top = KB_TOP[slot]
                        nb = top + 1
                        for r in range(16):
                            kb = top - r
                            S.op("dve", lambda e, r=r, kb=kb, slot=slot: e.tensor_scalar(
                                masks[:, r, :], qp[:, slot, :], C.kpos[:, kb:kb + 1], None, ALU.is_gt),
                                reads=[qp_tok, C.const_tok], writes=[mask_tok])
                        blocks = [(h, top - r, r) for h in range(8) for r in range(nb)]
                        nblk = len(blocks)

                        def s1(i):
                            h, kb, r = blocks[i]
                            hp, hh = h // 2, h % 2
                            zb = i % 2
                            S.op("pe", lambda e: e.matmul(
                                zps[zb][:], lhsT=KT[hh * 64:(hh + 1) * 64, hp, kb * 128:(kb + 1) * 128],
                                rhs=QT[hh * 64:(hh + 1) * 64, hp, slot * SEGT:(slot + 1) * SEGT],
                                start=True, stop=True),
                                reads=[kt_tok[kb // 4], q_tok[slot]], writes=[zps_tok[zb]])

                        def s2(i):
                            h, kb, r = blocks[i]
                            zb, eb, sb = i % 2, i % NE, i % 3
                            S.op("act", lambda e: e.activation(out=e_sb[eb][:], in_=zps[zb][:], func=AF.Exp),
                                 reads=[zps_tok[zb]], writes=[e_tok[eb]])
                            if r < 16:
                                S.op("dve", lambda e: e.tensor_tensor(out=e_sb[eb][:], in0=e_sb[eb][:],
                                                                       in1=masks[:, r, :], op=ALU.mult),
                                     reads=[mask_tok], writes=[e_tok[eb]])
                            S.op("act", lambda e: e.activation(out=sp_sb[sb][:], in_=e_sb[eb][:], func=AF.Ln, bias=1.0),
                                 reads=[e_tok[eb]], writes=[sp_tok[sb]])

                        def s3(i):
                            h, kb, r = blocks[i]
                            sb = i % 3
                            cb = h % 2
                            S.op("pe", lambda e: e.matmul(cps[cb][:], lhsT=C.tri_bf[:], rhs=sp_sb[sb][:],
                                                          start=(r == 0), stop=True, skip_group_check=(r != 0)),
                                 reads=[sp_tok[sb], C.const_tok], writes=[cps_tok[cb]])

                        def s3b(i):
                            h, kb, r = blocks[i]
                            if r == nb - 1:
                                return
                            sb = i % 3
                            cb = h % 2
                            S.op("pe", lambda e: e.matmul(cps[cb][:], lhsT=C.atri_bf[:], rhs=sp_sb[sb][:],
                                                          start=False, stop=True, skip_group_check=True),
                                 reads=[sp_tok[sb], C.const_tok], writes=[cps_tok[cb]])

                        def s4(i):
                            h, kb, r = blocks[i]
                            cb, xb_, eb, wb = h % 2, i % 2, i % NE, i % 2
                            S.op("act", lambda e: e.activation(out=ex_sb[xb_][:], in_=cps[cb][:], func=AF.Exp, scale=-1.0),
                                 reads=[cps_tok[cb]], writes=[ex_tok[xb_]])
                            S.op("dve" if i % 2 == 0 else "pool", lambda e: e.tensor_tensor(out=w_sb[wb][:], in0=e_sb[eb][:], in1=ex_sb[xb_][:],
                                                                   op=ALU.mult),
                                 reads=[e_tok[eb], ex_tok[xb_]], writes=[w_tok2[wb]])

                        def s5(i):
                            h, kb, r = blocks[i]
                            wb = i % 2
                            ob = h % 2
                            hp, hh = h // 2, h % 2
                            S.op("pe", lambda e: e.matmul(ops_[ob][hh * 64:(hh + 1) * 64, :], lhsT=V[:, kb, h * 64:(h + 1) * 64],
                                                          rhs=w_sb[wb][:], start=(r == 0), stop=(r == nb - 1)),
                                 reads=[w_tok2[wb], v_tok[kb // 4]], writes=[ops_tok[ob]])
                            if r == nb - 1:
                                S.op("act", lambda e: e.copy(out=attnT[hh * 64:(hh + 1) * 64, hp, slot * SEGT:(slot + 1) * SEGT],
                                                             in_=ops_[ob][hh * 64:(hh + 1) * 64, :]),
                                     reads=[ops_tok[ob]], writes=[attn_tok[slot][h]])

                        for t in range(nblk + 2):
                            if t < nblk:
                                s1(t)
                                s2(t)
                            if 0 <= t - 2 < nblk:
                                s3b(t - 2)
                            if 0 <= t - 1 < nblk:
                                s3(t - 1)
                                s4(t - 1)
                            if 0 <= t - 2 < nblk:
                                s5(t - 2)

        S.barrier()
        ssmT = _sb(nc, es, "ssmT", [128, 4, NOWN * SEGT], BF16)
        ssm_tok = [Tok() for _ in range(NOWN)]
        if dbg in ("ssm", None, "h1", "h2", "h3"):
            phase_ssm(C, nc, S, ps, ps_tok, I, ssmT, ssm_tok, gmix_sb, gmix_tok)
        S.barrier()
        och = S.chan()
        if dbg == "ssm":
            dbg_ssm = nc.dram_tensor("dbg_ssm", [128, 4, NOWN * SEGT], BF16, kind="ExternalOutput").ap()
            ytok = Tok()
            S.dma("sp", och, dbg_ssm[:, :, :], ssmT[:], reads=ssm_tok, writes=[ytok])
            S.wait_tok("sp", [ytok])
        if dbg == "attn":
            with ExitStack() as e2:
                tmp = _sb(nc, e2, "dbgtmp", [128, 4, NOWN * SEGT], F32)
                tmp_tok = Tok()
                alltoks = [t for row in attn_tok for t in row]
                S.op("dve", lambda e: e.tensor_copy(out=tmp[:], in_=attnT[:]), reads=alltoks, writes=[tmp_tok])
                ytok = Tok()
                S.dma("sp", och, dbg_out[:, :, :], tmp[:], reads=[tmp_tok], writes=[ytok])
                S.wait_tok("sp", [ytok])
        S.barrier()
        if dbg in (None, "h1", "h2", "h3"):
            phase_post(C, nc, S, ps, ps_tok, I, attnT, attn_tok, ssmT, ssm_tok, y, dbg)
            return nc
        with ExitStack() as e2:
            zt = _sb(nc, e2, "zt", [128, D], F32)
            zt_tok = Tok()
            S.op("pool", lambda e: e.memset(zt[:], 0.0), writes=[zt_tok])
            ytok = Tok()
            for i in range(NOWN * 4):
                S.dma("sp", och, y[i * 128:(i + 1) * 128, :], zt[:], reads=[zt_tok], writes=[ytok])
            S.wait_tok("sp", [ytok])
    return nc


def make_in_maps(inputs):
    x = np.ascontiguousarray(inputs["x"], dtype=np.float32)
    w_in = np.ascontiguousarray(inputs["w_in"][0].reshape(8, 128, 2048).transpose(1, 0, 2))
    g_mix = np.ascontiguousarray(inputs["g_mix"][0].reshape(8, 128).T)
    f32 = np.float32
    are = inputs["a_re"][0].reshape(16, 2, 64).transpose(1, 2, 0).reshape(128, 16)
    aim = inputs["a_im"][0].reshape(16, 2, 64).transpose(1, 2, 0).reshape(128, 16)
    ldt = np.repeat(inputs["log_dt"][0].reshape(16, 2).T[:, None, :], 64, axis=1).reshape(128, 16)
    bpr = np.zeros((128, 16, 128), f32)
    bpi = np.zeros((128, 16, 128), f32)
    cpr = np.zeros((128, 16, 128), f32)
    cpi = np.zeros((128, 16, 128), f32)
    for k in range(16):
        for g2 in range(2):
            g = 2 * k + g2
            r0 = (g % 8) * 16
            bpr[r0:r0 + 16, k, g2 * 64:(g2 + 1) * 64] = inputs["b_re"][0][g].T
            bpi[r0:r0 + 16, k, g2 * 64:(g2 + 1) * 64] = inputs["b_im"][0][g].T
            cpr[g2 * 64:(g2 + 1) * 64, k, r0:r0 + 16] = inputs["c_re"][0][g].T
            cpi[g2 * 64:(g2 + 1) * 64, k, r0:r0 + 16] = inputs["c_im"][0][g].T
    dsk = inputs["d_skip"][0].reshape(4, 128).T
    wglu = inputs["w_glu"][0].reshape(4, 128, 512).transpose(1, 0, 2)
    bglu = inputs["b_glu"][0].reshape(4, 128).T
    def kt8(w):
        return w.reshape(8, 128, -1).transpose(1, 0, 2)

    def col8(g):
        return g.reshape(8, 128).T

    post = {"w_out": kt8(inputs["w_out"][0]),
            "g_out": col8(np.concatenate([inputs["g_attn_out"][0], inputs["g_ssm_out"][0]])),
            "g_mem": col8(inputs["g_mem"][0]), "w_xkv": kt8(inputs["w_xkv"][0]),
            "g_xattn": col8(inputs["g_xattn"][0]), "w_xq": kt8(inputs["w_xq"][0]), "w_xo": kt8(inputs["w_xo"][0]),
            "g_ffn": col8(inputs["g_ffn"][0]),
            "gffn_rep": np.broadcast_to(inputs["g_ffn"][0][None, :], (128, D)),
            "gfin_rep": np.broadcast_to(inputs["g_final"][None, :], (128, D)),
            "w_pq": kt8(inputs["w_pq"][0]),
            "skT": inputs["sub_keys"][0].transpose(3, 0, 1, 2).reshape(128, 16, 128),
            "peer_u": inputs["peer_u"][0], "peer_v": inputs["peer_v"][0]}
    common = {"w_in": w_in, "g_mix": g_mix, "a_re": are, "a_im": aim, "log_dt": ldt,
              "bpad_re": bpr, "bpad_im": bpi, "cpad_re": cpr, "cpad_im": cpi,
              "d_skip": dsk, "w_glu": wglu, "b_glu": bglu}
    common.update(post)
    common = {k_: np.ascontiguousarray(v_, dtype=f32) for k_, v_ in common.items()}
    maps = []
    for c in range(8):
        b, j = c // 4, c % 4
        tiles = own_tiles(j)
        segsel = np.zeros((128, NOWN, 16), f32)
        for i_, t_ in enumerate(tiles):
            segsel[:, i_, t_] = 1.0
        x_own = np.concatenate([x[b, t * SEGT:(t + 1) * SEGT] for t in tiles], axis=0)
        qp = np.stack([np.arange(t * SEGT, (t + 1) * SEGT, dtype=np.float32) for t in tiles], axis=0)
        qp = np.ascontiguousarray(np.broadcast_to(qp[None], (128, NOWN, SEGT)))
        x_rev = np.ascontiguousarray(x[b].reshape(NSEG, SEGT, D)[:, ::-1, :].reshape(SEQ, D))
        m = {"x_full": np.ascontiguousarray(x[b]), "x_own": np.ascontiguousarray(x_own), "x_rev": x_rev,
             "qpos": qp, "segsel": segsel, "mem": np.ascontiguousarray(inputs["mem"][b], dtype=f32)}
        m.update(common)
        maps.append(m)
    return maps


def kernel(**inputs):
    nc = build()
    maps = make_in_maps(inputs)
    res = run_bass_kernel_spmd(nc, maps, core_ids=list(range(8)))
    out = np.zeros((2, SEQ, D), np.float32)
    for c in range(8):
        b, j = c // 4, c % 4
        yo = res.results[c]["y"]
        for i, t in enumerate(own_tiles(j)):
            out[b, t * SEGT:(t + 1) * SEGT] = yo[i * SEGT:(i + 1) * SEGT]
    return out
```

```python
from contextlib import ExitStack
import numpy as np
import concourse.bass as bass
import concourse.mybir as mybir
from concourse.bass_utils import run_bass_kernel_spmd

F32 = mybir.dt.float32
BF16 = mybir.dt.bfloat16
U32 = mybir.dt.uint32
I32 = mybir.dt.int32
AF = mybir.ActivationFunctionType
ALU = mybir.AluOpType
AX = mybir.AxisListType

D = 1024
SEQ = 8192
NSEG = 16
SEGT = 512
NOWN = 4
EPS = 1e-6
KB_TOP = [15, 31, 47, 63]
NEG = -1.0e30


def own_tiles(j):
    return [j, 7 - j, 8 + j, 15 - j]


class Tok:
    __slots__ = ("w", "r")

    def __init__(self):
        self.w = None
        self.r = {}


class _Eng:
    def __init__(self, name, h, sem):
        self.name = name
        self.h = h
        self.sem = sem
        self.n = 0
        self.waited = {}


class _Chan:
    def __init__(self, sem):
        self.sem = sem
        self.n = 0


class Sched:
    def __init__(self, nc, es):
        self.nc = nc
        self.es = es
        self.E = {}
        for name, h in (("pe", nc.tensor), ("act", nc.scalar), ("dve", nc.vector),
                        ("pool", nc.gpsimd), ("sp", nc.sync)):
            sem = es.enter_context(nc.semaphore("sem_" + name))
            self.E[name] = _Eng(name, h, sem)
        self.nchan = 0
        self.chans = []

    def chan(self):
        self.nchan += 1
        c = _Chan(self.es.enter_context(self.nc.semaphore("ch%d" % self.nchan)))
        self.chans.append(c)
        return c

    def barrier(self):
        for E in self.E.values():
            for F in self.E.values():
                if F is E or F.n == 0:
                    continue
                if E.waited.get(id(F.sem), 0) < F.n:
                    E.h.wait_ge(F.sem, F.n)
                    E.waited[id(F.sem)] = F.n
            for c in self.chans:
                if c.n > 0 and E.waited.get(id(c.sem), 0) < c.n:
                    E.h.wait_ge(c.sem, c.n)
                    E.waited[id(c.sem)] = c.n

    def _wait(self, E, reads, writes, weak=()):
        deps = {}
        for t in weak:
            for d in [t.w] + list(t.r.values()):
                if d is not None and d[0] is not E.sem:
                    k_ = id(d[0])
                    if k_ not in deps or deps[k_][1] < d[1]:
                        deps[k_] = d

        def add(d):
            if d is None:
                return
            s, v = d
            k = id(s)
            if k not in deps or deps[k][1] < v:
                deps[k] = (s, v)

        for t in reads:
            add(t.w)
        for t in writes:
            add(t.w)
            for d in t.r.values():
                add(d)
        for k, (s, v) in deps.items():
            if E.name == "pe" and s is E.sem:
                continue
            if E.waited.get(k, 0) < v:
                E.h.wait_ge(s, v)
                E.waited[k] = v

    def op(self, eng, fn, reads=(), writes=(), weak=()):
        E = self.E[eng]
        self._wait(E, reads, writes, weak)
        ins = fn(E.h)
        E.n += 1
        ins.then_inc(E.sem, 1)
        me = (E.sem, E.n)
        for t in reads:
            t.r[id(E.sem)] = me
        for t in writes:
            t.w = me
            t.r = {}
        for t in weak:
            t.w = me
            t.r = {}
        return ins

    def dma(self, queue, ch, out, in_, reads=(), writes=(), indirect=None, **kw):
        Q = self.E[queue]
        self._wait(Q, reads, writes)
        if indirect is not None:
            ins = Q.h.indirect_dma_start(out=out, out_offset=None, in_=in_, in_offset=indirect)
        else:
            ins = Q.h.dma_start(out=out, in_=in_, **kw)
        ch.n += 16
        ins.then_inc(ch.sem, 16)
        me = (ch.sem, ch.n)
        for t in reads:
            t.r[id(ch.sem)] = me
        for t in writes:
            t.w = me
            t.r = {}
        return ins

    def wait_tok(self, eng, toks):
        self._wait(self.E[eng], toks, ())


class Ctx:
    pass


_NAME_CNT = [0]


def _sb(nc, es, name, shape, dt):
    _NAME_CNT[0] += 1
    return es.enter_context(nc.sbuf_tensor("%s_%d" % (name, _NAME_CNT[0]), list(shape), dt))


def rms_scale(C, xt, xt_tok, rstd, rstd_tok, junk, junk_tok, ss, ss_tok):
    S = C.S
    S.op("act", lambda e: e.activation(out=junk, in_=xt, func=AF.Square, accum_out=ss),
         reads=[xt_tok], writes=[junk_tok, ss_tok])
    S.op("act", lambda e: e.activation(out=rstd, in_=ss, func=AF.Ln, bias=C.eps_col[:], scale=1.0 / D),
         reads=[ss_tok, C.const_tok], writes=[rstd_tok])
    S.op("act", lambda e: e.activation(out=rstd, in_=rstd, func=AF.Exp, scale=-0.5),
         reads=[rstd_tok], writes=[rstd_tok])


def transpose_to(C, xn, xn_tok, dst_fn, dst_tok, nkt=8):
    S = C.S
    force = getattr(C, "tp_force", None)
    if force is not None:
        pt, pt_tok = C.tp_ps[force], C.tp_tok[force]
    else:
        pt, pt_tok = C.tp_ps[C.tp_i % 2], C.tp_tok[C.tp_i % 2]
        C.tp_i += 1
    ptb = pt[:].bitcast(BF16)
    for kt in range(nkt):
        S.op("pe", lambda e, kt=kt: e.transpose(out=ptb[:, kt * 128:(kt + 1) * 128],
                                                in_=xn[:, kt * 128:(kt + 1) * 128],
                                                identity=C.ident_bf[:]),
             reads=[xn_tok, C.const_tok], writes=[pt_tok])
    return ptb, pt_tok


TWO_PI = 6.283185307179586
CW1 = 6.28125
CW2 = TWO_PI - CW1


def phase_ssm(C, nc, S, ps, ps_tok, I, ssmT, ssm_tok, gmix_sb, gmix_tok):
    with ExitStack() as e2:
        cosT = _sb(nc, e2, "cosT", [128, 16, SEGT], F32)
        sinT = _sb(nc, e2, "sinT", [128, 16, SEGT], F32)
        tab_tok = Tok()
        bre = _sb(nc, e2, "bre", [128, 16, 128], BF16)
        bim = _sb(nc, e2, "bim", [128, 16, 128], BF16)
        cre = _sb(nc, e2, "cre", [128, 16, 128], BF16)
        ncim = _sb(nc, e2, "ncim", [128, 16, 128], BF16)
        bc_tok = Tok()
        wu = _sb(nc, e2, "wu", [128, 8, 512], BF16)
        wglu = _sb(nc, e2, "wglu", [128, 4, 512], BF16)
        diagD = _sb(nc, e2, "diagD", [128, 4, 128], BF16)
        w_tok = Tok()
        P = {}
        for nm in ("are", "aim", "ldt", "dt", "rho", "th", "gre", "gim", "ngim", "pa", "pb", "pc", "pd", "adt", "nadt",
                   "ilr", "ili", "q1", "q2", "q3", "q4"):
            P[nm] = _sb(nc, e2, "p_" + nm, [128, 16], F32)
        p_tok = Tok()
        bglu = _sb(nc, e2, "bglu", [128, 4], F32)
        dsk = _sb(nc, e2, "dsk", [128, 4], F32)
        segsel = _sb(nc, e2, "segsel_sb", [128, 4, 16], F32)
        halfpi = _sb(nc, e2, "halfpi", [128, 1], F32)
        Xst = _sb(nc, e2, "Xst", [128, 2, 16, 17], F32)
        tau1p = _sb(nc, e2, "tau1p", [128, SEGT], F32)
        magt = _sb(nc, e2, "magt", [128, SEGT], F32)
        mag_tok = Tok()
        S.op("pool", lambda e: e.iota(tau1p[:], pattern=[[1, SEGT]], base=1, channel_multiplier=0,
                                      allow_small_or_imprecise_dtypes=True), writes=[mag_tok])
        xst_tok = Tok()
        small_tok = Tok()
        ch0 = S.chan()
        for dst, src in ((P["are"], I["a_re"]), (P["aim"], I["a_im"]), (P["ldt"], I["log_dt"]),
                         (bglu, I["b_glu"]), (dsk, I["d_skip"])):
            S.dma("sp", ch0, dst[:], src[:, :], writes=[small_tok])
        S.dma("sp", ch0, segsel[:], I["segsel"][:, :, :], writes=[small_tok])
        S.op("pool", lambda e: e.memset(halfpi[:], float(np.pi / 2)), writes=[small_tok])
        S.op("pool", lambda e: e.memset(Xst[:], 0.0), writes=[xst_tok])
        S.op("act", lambda e: e.activation(out=P["dt"][:], in_=P["ldt"][:], func=AF.Exp), reads=[small_tok], writes=[p_tok])
        S.op("dve", lambda e: e.tensor_tensor(out=P["pa"][:], in0=P["are"][:], in1=P["dt"][:], op=ALU.mult),
             reads=[small_tok, p_tok], writes=[p_tok])
        S.op("act", lambda e: e.activation(out=P["rho"][:], in_=P["pa"][:], func=AF.Exp), reads=[p_tok], writes=[p_tok])
        S.op("dve", lambda e: e.tensor_copy(out=P["adt"][:], in_=P["pa"][:]), reads=[p_tok], writes=[p_tok])
        S.op("dve", lambda e: e.tensor_scalar(P["nadt"][:], P["pa"][:], -1.0, None, ALU.mult), reads=[p_tok], writes=[p_tok])
        S.op("dve", lambda e: e.tensor_tensor(out=P["th"][:], in0=P["aim"][:], in1=P["dt"][:], op=ALU.mult),
             reads=[small_tok, p_tok], writes=[p_tok])
        with ExitStack() as e3:
            tau1 = _sb(nc, e3, "tau1", [128, SEGT], F32)
            ang = _sb(nc, e3, "ang", [128, 4, SEGT], F32)
            kf = _sb(nc, e3, "kf", [128, 4, SEGT], F32)
            ki = _sb(nc, e3, "ki", [128, 4, SEGT], I32)
            s1 = _sb(nc, e3, "s1", [128, 4, SEGT], F32)
            t_tok = Tok()
            S.op("pool", lambda e: e.iota(tau1[:], pattern=[[1, SEGT]], base=1, channel_multiplier=0,
                                          allow_small_or_imprecise_dtypes=True), writes=[t_tok])
            for gq in range(4):
                for kk in range(4):
                    k = gq * 4 + kk
                    S.op("dve", lambda e, kk=kk, k=k: e.tensor_scalar(ang[:, kk, :], tau1[:], P["th"][:, k:k + 1], None, ALU.mult),
                         reads=[p_tok, t_tok], writes=[t_tok])
                S.op("dve", lambda e: e.tensor_scalar(kf[:], ang[:], 1.0 / TWO_PI, None, ALU.mult), reads=[t_tok], writes=[t_tok])
                S.op("dve", lambda e: e.tensor_copy(out=ki[:], in_=kf[:]), reads=[t_tok], writes=[t_tok])
                S.op("dve", lambda e: e.tensor_copy(out=kf[:], in_=ki[:]), reads=[t_tok], writes=[t_tok])
                S.op("dve", lambda e: e.scalar_tensor_tensor(out=ang[:], in0=kf[:], scalar=-CW1, in1=ang[:], op0=ALU.mult, op1=ALU.add),
                     reads=[t_tok], writes=[t_tok])
                S.op("dve", lambda e: e.scalar_tensor_tensor(out=ang[:], in0=kf[:], scalar=-CW2, in1=ang[:], op0=ALU.mult, op1=ALU.add),
                     reads=[t_tok], writes=[t_tok])
                S.op("act", lambda e: e.activation(out=s1[:], in_=ang[:], func=AF.Sin, scale=0.25), reads=[t_tok], writes=[t_tok])
                S.op("act", lambda e: e.activation(out=kf[:], in_=ang[:], func=AF.Sin, scale=0.25, bias=halfpi[:]),
                     reads=[t_tok, small_tok], writes=[t_tok])
                S.op("dve", lambda e: e.scalar_tensor_tensor(out=ang[:], in0=s1[:], scalar=2.0, in1=kf[:], op0=ALU.mult, op1=ALU.mult),
                     reads=[t_tok], writes=[t_tok])
                S.op("dve", lambda e: e.tensor_tensor(out=s1[:], in0=s1[:], in1=s1[:], op=ALU.mult), reads=[t_tok], writes=[t_tok])
                S.op("dve", lambda e: e.tensor_scalar(s1[:], s1[:], -2.0, 1.0, ALU.mult, ALU.add), reads=[t_tok], writes=[t_tok])
                S.op("dve", lambda e, gq=gq: e.scalar_tensor_tensor(out=sinT[:, gq * 4:(gq + 1) * 4, :], in0=ang[:], scalar=2.0, in1=s1[:],
                                                                    op0=ALU.mult, op1=ALU.mult),
                     reads=[t_tok], writes=[tab_tok])
                S.op("dve", lambda e: e.tensor_tensor(out=ang[:], in0=ang[:], in1=ang[:], op=ALU.mult), reads=[t_tok], writes=[t_tok])
                S.op("dve", lambda e, gq=gq: e.tensor_scalar(cosT[:, gq * 4:(gq + 1) * 4, :], ang[:], -2.0, 1.0, ALU.mult, ALU.add),
                     reads=[t_tok], writes=[tab_tok])
            c0 = cosT[:, :, 0]
            s0 = sinT[:, :, 0]
            tt = lambda o, a, b, op: S.op("dve", lambda e: e.tensor_tensor(out=o, in0=a, in1=b, op=op),
                                          reads=[p_tok, tab_tok, small_tok], writes=[p_tok])
            tt(P["pa"][:], P["rho"][:], c0, ALU.mult)
            S.op("dve", lambda e: e.tensor_scalar(P["pa"][:], P["pa"][:], -1.0, None, ALU.add), reads=[p_tok], writes=[p_tok])
            tt(P["pb"][:], P["rho"][:], s0, ALU.mult)
            tt(P["pc"][:], P["are"][:], P["are"][:], ALU.mult)
            tt(P["pd"][:], P["aim"][:], P["aim"][:], ALU.mult)
            tt(P["pc"][:], P["pc"][:], P["pd"][:], ALU.add)
            S.op("dve", lambda e: e.reciprocal(out=P["pc"][:], in_=P["pc"][:]), reads=[p_tok], writes=[p_tok])
            tt(P["gre"][:], P["pa"][:], P["are"][:], ALU.mult)
            tt(P["pd"][:], P["pb"][:], P["aim"][:], ALU.mult)
            tt(P["gre"][:], P["gre"][:], P["pd"][:], ALU.add)
            tt(P["gre"][:], P["gre"][:], P["pc"][:], ALU.mult)
            tt(P["gim"][:], P["pb"][:], P["are"][:], ALU.mult)
            tt(P["pd"][:], P["pa"][:], P["aim"][:], ALU.mult)
            tt(P["gim"][:], P["gim"][:], P["pd"][:], ALU.subtract)
            tt(P["gim"][:], P["gim"][:], P["pc"][:], ALU.mult)
            S.op("dve", lambda e: e.tensor_scalar(P["ngim"][:], P["gim"][:], -1.0, None, ALU.mult), reads=[p_tok], writes=[p_tok])
            S.barrier()
        with ExitStack() as e3:
            f1 = _sb(nc, e3, "f1", [128, 16, 128], F32)
            f2 = _sb(nc, e3, "f2", [128, 16, 128], F32)
            f3 = _sb(nc, e3, "f3", [128, 128], F32)
            f_tok = Tok()
            chf = S.chan()
            for src, dst in ((I["bpad_re"], bre), (I["bpad_im"], bim)):
                S.dma("sp", chf, f1[:], src[:, :, :], writes=[f_tok])
                S.op("dve", lambda e, dst=dst: e.tensor_copy(out=dst[:], in_=f1[:]), reads=[f_tok], writes=[bc_tok, f_tok])
            S.dma("sp", chf, f1[:], I["cpad_re"][:, :, :], writes=[f_tok])
            S.dma("sp", chf, f2[:], I["cpad_im"][:, :, :], writes=[f_tok])
            for k in range(16):
                S.op("dve", lambda e, k=k: e.tensor_scalar(f3[:], f2[:, k, :], P["gim"][:, k:k + 1], None, ALU.mult),
                     reads=[f_tok, p_tok], writes=[f_tok])
                S.op("dve", lambda e, k=k: e.scalar_tensor_tensor(out=cre[:, k, :], in0=f1[:, k, :], scalar=P["gre"][:, k:k + 1],
                                                                  in1=f3[:], op0=ALU.mult, op1=ALU.subtract),
                     reads=[f_tok, p_tok], writes=[bc_tok])
                S.op("dve", lambda e, k=k: e.tensor_scalar(f3[:], f2[:, k, :], P["gre"][:, k:k + 1], None, ALU.mult),
                     reads=[f_tok, p_tok, bc_tok], writes=[f_tok])
                S.op("dve", lambda e, k=k: e.scalar_tensor_tensor(out=ncim[:, k, :], in0=f1[:, k, :], scalar=P["ngim"][:, k:k + 1],
                                                                  in1=f3[:], op0=ALU.mult, op1=ALU.subtract),
                     reads=[f_tok, p_tok], writes=[bc_tok])
            f1v = f1[:].rearrange("p a b -> p (a b)").rearrange("p (k c) -> p k c", k=4)
            for half in range(2):
                S.dma("sp", chf, f1v, I["w_in"][:, 4 * half:4 * half + 4, 1536:2048], writes=[f_tok])
                for k4 in range(4):
                    kt = 4 * half + k4
                    S.op("dve" if k4 % 2 == 0 else "pool", lambda e, kt=kt, k4=k4: e.tensor_scalar(
                        wu[:, kt, :], f1v[:, k4, :], gmix_sb[:, kt:kt + 1], None, ALU.mult),
                        reads=[f_tok, gmix_tok], writes=[w_tok])
            for kt in range(4):
                S.dma("sp", chf, f1[:, 0:4, :], I["w_glu"][:, kt, :].rearrange("p (a b) -> p a b", a=4), writes=[f_tok])
                S.op("dve", lambda e, kt=kt: e.tensor_copy(out=wglu[:, kt, :].rearrange("p (a b) -> p a b", a=4), in_=f1[:, 0:4, :]),
                     reads=[f_tok], writes=[w_tok, f_tok])
            for ct in range(4):
                S.op("dve", lambda e, ct=ct: e.tensor_scalar(diagD[:, ct, :], C.ident_f[:], dsk[:, ct:ct + 1], None, ALU.mult),
                     reads=[small_tok, C.const_tok], writes=[w_tok])
            S.barrier()

        def scale_tables(sign_key):
            for k in range(16):
                S.op("act", lambda e, k=k: e.activation(out=magt[:], in_=tau1p[:], func=AF.Exp, scale=P[sign_key][:, k:k + 1]),
                     reads=[mag_tok, p_tok], writes=[mag_tok])
                S.op("dve", lambda e, k=k: e.tensor_tensor(out=cosT[:, k, :], in0=cosT[:, k, :], in1=magt[:], op=ALU.mult),
                     reads=[mag_tok, tab_tok], writes=[tab_tok])
                S.op("dve", lambda e, k=k: e.tensor_tensor(out=sinT[:, k, :], in0=sinT[:, k, :], in1=magt[:], op=ALU.mult),
                     reads=[mag_tok, tab_tok], writes=[tab_tok])

        scale_tables("adt")
        with ExitStack() as e3:
            xt = [_sb(nc, e3, "axt%d" % i, [128, D], F32) for i in range(2)]
            xt_tok = [Tok(), Tok()]
            xt_ch = [S.chan(), S.chan()]
            xn = [_sb(nc, e3, "axn%d" % i, [128, D], BF16) for i in range(2)]
            xn_tok = [Tok(), Tok()]
            ss = [_sb(nc, e3, "ass%d" % i, [128, 1], F32) for i in range(2)]
            ss_tok = [Tok(), Tok()]
            rstd = [_sb(nc, e3, "arstd%d" % i, [128, 1], F32) for i in range(2)]
            rstd_tok = [Tok(), Tok()]
            xnT = [_sb(nc, e3, "axnT%d" % i, [128, 8, SEGT], BF16) for i in range(2)]
            xnT_tok = [Tok(), Tok()]
            uT = [_sb(nc, e3, "auT%d" % i, [128, 4, SEGT], BF16) for i in range(2)]
            uT_tok = [Tok(), Tok()]
            acc4 = [_sb(nc, e3, "acc4_%d" % i, [128, 4, 16], F32) for i in range(2)]
            acc4_tok = [[[Tok() for _ in range(16)] for _ in range(4)] for _ in range(2)]
            junkS = [_sb(nc, e3, "junkS%d" % i, [128, SEGT], BF16) for i in range(4)]
            junkS_tok = [Tok() for _ in range(4)]
            jc = [0]
            sm_ = {nm: _sb(nc, e3, "sm_" + nm, [128, 16], F32) for nm in ("a", "b", "c", "d", "sr", "si")}
            sm_tok = Tok()
            t0r, t0i = cosT[:, :, 0], sinT[:, :, 0]
            Lr, Li = cosT[:, :, SEGT - 1], sinT[:, :, SEGT - 1]
            tt2 = lambda o, a, b, op: S.op("dve", lambda e: e.tensor_tensor(out=o, in0=a, in1=b, op=op),
                                           reads=[p_tok, tab_tok, sm_tok], writes=[p_tok])
            tt2(P["q1"][:], t0r, t0r, ALU.mult)
            tt2(P["q2"][:], t0i, t0i, ALU.mult)
            tt2(P["q1"][:], P["q1"][:], P["q2"][:], ALU.add)
            S.op("dve", lambda e: e.reciprocal(out=P["q1"][:], in_=P["q1"][:]), reads=[p_tok], writes=[p_tok])
            tt2(P["ilr"][:], t0r, P["q1"][:], ALU.mult)
            tt2(P["ili"][:], t0i, P["q1"][:], ALU.mult)
            S.op("dve", lambda e: e.tensor_scalar(P["ili"][:], P["ili"][:], -1.0, None, ALU.mult), reads=[p_tok], writes=[p_tok])
            ti = 0
            for sg in range(NSEG):
                xb = sg % 2
                for m in range(4):
                    a = ti % 2
                    ti += 1
                    r0 = sg * SEGT + m * 128
                    S.dma("sp", xt_ch[a], xt[a][:], I["x_rev"][r0:r0 + 128, :], writes=[xt_tok[a]])
                    rms_scale(C, xt[a][:], xt_tok[a], rstd[a][:], rstd_tok[a], xn[a][:], xn_tok[a], ss[a][:], ss_tok[a])
                    S.op("dve", lambda e, a=a: e.tensor_scalar(xn[a][:], xt[a][:], rstd[a][:], None, ALU.mult),
                         reads=[xt_tok[a], rstd_tok[a]], writes=[xn_tok[a]])
                    ptb, pt_tok = transpose_to(C, xn[a], xn_tok[a], None, None)
                    S.op("act", lambda e, xb=xb, m=m, ptb=ptb: e.copy(out=xnT[xb][:, :, m * 128:(m + 1) * 128],
                                                                      in_=ptb.rearrange("p (k t) -> p k t", k=8)),
                         reads=[pt_tok], writes=[xnT_tok[xb]])
                for ct in range(4):
                    pb = 4 + ct % 2
                    for kt in range(8):
                        S.op("pe", lambda e, kt=kt, ct=ct, pb=pb, xb=xb: e.matmul(
                            ps[pb][:], lhsT=wu[:, kt, ct * 128:(ct + 1) * 128], rhs=xnT[xb][:, kt, :],
                            start=(kt == 0), stop=(kt == 7)), reads=[w_tok, xnT_tok[xb]], writes=[ps_tok[pb]])
                    S.op("act", lambda e, ct=ct, pb=pb, xb=xb: e.copy(out=uT[xb][:, ct, :], in_=ps[pb][:]),
                         reads=[ps_tok[pb]], writes=[uT_tok[xb]])
                ab = sg % 2
                for k in range(16):
                    ct = k // 4
                    bur, bui = (0, 1) if k % 2 == 0 else (2, 3)
                    S.op("pe", lambda e, k=k, ct=ct, bur=bur: e.matmul(ps[bur][:], lhsT=bre[:, k, :], rhs=uT[xb][:, ct, :], start=True, stop=True),
                         reads=[bc_tok, uT_tok[xb]], writes=[ps_tok[bur]])
                    S.op("pe", lambda e, k=k, ct=ct, bui=bui: e.matmul(ps[bui][:], lhsT=bim[:, k, :], rhs=uT[xb][:, ct, :], start=True, stop=True),
                         reads=[bc_tok, uT_tok[xb]], writes=[ps_tok[bui]])
                    for j, (bk, tab) in enumerate(((bur, cosT), (bui, sinT), (bur, sinT), (bui, cosT))):
                        jb = jc[0] % 4
                        jc[0] += 1
                        S.op("dve", lambda e, j=j, bk=bk, tab=tab, k=k, jb=jb: e.scalar_tensor_tensor(
                            out=junkS[jb][:], in0=ps[bk][:], scalar=1.0, in1=tab[:, k, :], op0=ALU.mult, op1=ALU.mult,
                            accum_out=acc4[ab][:, j, k:k + 1]),
                            reads=[ps_tok[bk], tab_tok], writes=[junkS_tok[jb], acc4_tok[ab][j][k]])
                A_ = acc4[ab]
                rd = [t_ for row_ in acc4_tok[ab] for t_ in row_] + [p_tok, tab_tok, xst_tok, sm_tok]
                tt3 = lambda o, a_, b_, op: S.op("dve", lambda e: e.tensor_tensor(out=o, in0=a_, in1=b_, op=op), reads=rd, writes=[sm_tok])
                tt3(sm_["a"][:], A_[:, 0, :], A_[:, 1, :], ALU.subtract)
                tt3(sm_["b"][:], A_[:, 2, :], A_[:, 3, :], ALU.add)
                tt3(sm_["c"][:], P["ilr"][:], sm_["a"][:], ALU.mult)
                tt3(sm_["d"][:], P["ili"][:], sm_["b"][:], ALU.mult)
                tt3(sm_["sr"][:], sm_["c"][:], sm_["d"][:], ALU.subtract)
                tt3(sm_["c"][:], P["ilr"][:], sm_["b"][:], ALU.mult)
                tt3(sm_["d"][:], P["ili"][:], sm_["a"][:], ALU.mult)
                tt3(sm_["si"][:], sm_["c"][:], sm_["d"][:], ALU.add)
                Xr, Xi = Xst[:, 0, :, sg], Xst[:, 1, :, sg]
                tt3(sm_["a"][:], Lr, Xr, ALU.mult)
                tt3(sm_["b"][:], Li, Xi, ALU.mult)
                tt3(sm_["a"][:], sm_["a"][:], sm_["b"][:], ALU.subtract)
                S.op("dve", lambda e, sg=sg: e.tensor_tensor(out=Xst[:, 0, :, sg + 1], in0=sm_["a"][:], in1=sm_["sr"][:], op=ALU.add),
                     reads=[sm_tok], writes=[xst_tok])
                tt3(sm_["c"][:], Lr, Xi, ALU.mult)
                tt3(sm_["d"][:], Li, Xr, ALU.mult)
                tt3(sm_["c"][:], sm_["c"][:], sm_["d"][:], ALU.add)
                S.op("dve", lambda e, sg=sg: e.tensor_tensor(out=Xst[:, 1, :, sg + 1], in0=sm_["c"][:], in1=sm_["si"][:], op=ALU.add),
                     reads=[sm_tok], writes=[xst_tok])
            S.barrier()
        scale_tables("nadt")
        S.barrier()
        with ExitStack() as e3:
            xt = [_sb(nc, e3, "sxt%d" % i, [128, D], F32) for i in range(2)]
            xt_tok = [Tok(), Tok()]
            xt_ch = [S.chan(), S.chan()]
            xn = [_sb(nc, e3, "sxn%d" % i, [128, D], BF16) for i in range(2)]
            xn_tok = [Tok(), Tok()]
            ss = [_sb(nc, e3, "sss%d" % i, [128, 1], F32) for i in range(2)]
            ss_tok = [Tok(), Tok()]
            rstd = [_sb(nc, e3, "srstd%d" % i, [128, 1], F32) for i in range(2)]
            rstd_tok = [Tok(), Tok()]
            xnT = [_sb(nc, e3, "sxnT", [128, 8, SEGT], BF16)] * 2
            xnT_tok = [Tok()] * 2
            uT = [_sb(nc, e3, "uT%d" % i, [128, 4, SEGT], BF16) for i in range(2)]
            uT_tok = [Tok(), Tok()]
            tmp4 = [_sb(nc, e3, "tm%d" % i, [128, SEGT], F32) for i in range(4)]
            tmp4_tok = [Tok() for _ in range(4)]
            Wr = [_sb(nc, e3, "Wr%d" % i, [128, SEGT], F32) for i in range(2)]
            Wi = [_sb(nc, e3, "Wi%d" % i, [128, SEGT], F32) for i in range(2)]
            Vr = [_sb(nc, e3, "Vr%d" % i, [128, SEGT], F32) for i in range(2)]
            Vi = [_sb(nc, e3, "Vi%d" % i, [128, SEGT], F32) for i in range(2)]
            W_tok = [Tok(), Tok()]
            V_tok = [Tok(), Tok()]
            Xb = [_sb(nc, e3, "Xb%d" % i, [128, 2, SEGT], BF16) for i in range(2)]
            Xb_tok = [Tok(), Tok()]
            xin = _sb(nc, e3, "xin", [128, 32], F32)
            xin_tok = Tok()
            selt = _sb(nc, e3, "selt", [128, 32, 16], F32)
            rot = _sb(nc, e3, "rot", [128, 4], F32)
            rot_tok = Tok()
            yb = _sb(nc, e3, "yb", [128, 4, SEGT], BF16)
            y_tok = [Tok() for _ in range(4)]
            g1 = _sb(nc, e3, "g1", [128, SEGT], F32)
            g2 = _sb(nc, e3, "g2", [128, SEGT], F32)
            g_tok = Tok()
            ti = 0
            pcount = 0
            for sgi in range(NSEG, NSEG + NOWN):
                own = sgi >= NSEG
                sg = sgi - NSEG if own else sgi
                src = I["x_own"] if own else I["x_full"]
                xb = sgi % 2
                for m in range(4):
                    a = ti % 2
                    ti += 1
                    r0 = sg * SEGT + m * 128
                    S.dma("sp", xt_ch[a], xt[a][:], src[r0:r0 + 128, :], writes=[xt_tok[a]])
                    rms_scale(C, xt[a][:], xt_tok[a], rstd[a][:], rstd_tok[a], xn[a][:], xn_tok[a], ss[a][:], ss_tok[a])
                    S.op("dve", lambda e, a=a: e.tensor_scalar(xn[a][:], xt[a][:], rstd[a][:], None, ALU.mult),
                         reads=[xt_tok[a], rstd_tok[a]], writes=[xn_tok[a]])
                    ptb, pt_tok = transpose_to(C, xn[a], xn_tok[a], None, None)
                    S.op("act", lambda e, xb=xb, m=m, ptb=ptb: e.copy(out=xnT[xb][:, :, m * 128:(m + 1) * 128],
                                                                      in_=ptb.rearrange("p (k t) -> p k t", k=8)),
                         reads=[pt_tok], writes=[xnT_tok[xb]])
                for ct in range(4):
                    pb = ct % 2
                    for kt in range(8):
                        S.op("pe", lambda e, kt=kt, ct=ct, pb=pb, xb=xb: e.matmul(
                            ps[pb][:], lhsT=wu[:, kt, ct * 128:(ct + 1) * 128], rhs=xnT[xb][:, kt, :],
                            start=(kt == 0), stop=(kt == 7)), reads=[w_tok, xnT_tok[xb]], writes=[ps_tok[pb]])
                    S.op("act", lambda e, ct=ct, pb=pb, xb=xb: e.copy(out=uT[xb][:, ct, :], in_=ps[pb][:]),
                         reads=[ps_tok[pb]], writes=[uT_tok[xb]])
                if own:
                    S.op("dve", lambda e, sg=sg: e.tensor_tensor(out=selt[:], in0=Xst[:].rearrange("p r k s -> p (r k) s")[:, :, 0:16],
                                                                in1=segsel[:, sg:sg + 1, :].to_broadcast([128, 32, 16]), op=ALU.mult),
                         reads=[xst_tok, small_tok], writes=[xin_tok])
                    S.op("dve", lambda e: e.tensor_reduce(out=xin[:], in_=selt[:], axis=AX.X, op=ALU.add),
                         reads=[xin_tok], writes=[xin_tok])
                for ct in range(4):
                    yps = 4 + ct % 2
                    for kk in range(4):
                        k = ct * 4 + kk
                        pp = pcount % 2
                        pcount += 1
                        bur, bui = 2 * pp, 2 * pp + 1
                        bur, bui = (2, 3) if pp == 0 else (0, 1)
                        S.op("pe", lambda e: e.matmul(ps[bur][:], lhsT=bre[:, k, :], rhs=uT[xb][:, ct, :], start=True, stop=True),
                             reads=[bc_tok, uT_tok[xb]], writes=[ps_tok[bur]])
                        S.op("pe", lambda e: e.matmul(ps[bui][:], lhsT=bim[:, k, :], rhs=uT[xb][:, ct, :], start=True, stop=True),
                             reads=[bc_tok, uT_tok[xb]], writes=[ps_tok[bui]])
                        ck, sk = cosT[:, k, :], sinT[:, k, :]
                        S.op("dve", lambda e: e.tensor_tensor(out=tmp4[0][:], in0=ps[bur][:], in1=ck, op=ALU.mult),
                             reads=[ps_tok[bur], tab_tok], writes=[tmp4_tok[0]])
                        S.op("dve", lambda e: e.tensor_tensor(out=tmp4[1][:], in0=ps[bui][:], in1=sk, op=ALU.mult),
                             reads=[ps_tok[bui], tab_tok], writes=[tmp4_tok[1]])
                        S.op("dve", lambda e: e.tensor_tensor(out=Wr[pp][:], in0=tmp4[0][:], in1=tmp4[1][:], op=ALU.add),
                             reads=[tmp4_tok[0], tmp4_tok[1]], writes=[W_tok[pp]])
                        S.op("dve", lambda e: e.tensor_tensor(out=tmp4[2][:], in0=ps[bui][:], in1=ck, op=ALU.mult),
                             reads=[ps_tok[bui], tab_tok], writes=[tmp4_tok[2]])
                        S.op("dve", lambda e: e.tensor_tensor(out=tmp4[3][:], in0=ps[bur][:], in1=sk, op=ALU.mult),
                             reads=[ps_tok[bur], tab_tok], writes=[tmp4_tok[3]])
                        S.op("dve", lambda e: e.tensor_tensor(out=Wi[pp][:], in0=tmp4[2][:], in1=tmp4[3][:], op=ALU.subtract),
                             reads=[tmp4_tok[2], tmp4_tok[3]], writes=[W_tok[pp]])
                        if own:
                            ir, ii = xin[:, k:k + 1], xin[:, 16 + k:16 + k + 1]
                            itoks = [xin_tok]
                        else:
                            ir, ii = Xst[:, 2 * k, sg:sg + 1], Xst[:, 2 * k + 1, sg:sg + 1]
                            itoks = [xst_tok]
                        rb = P["rho"][:, k:k + 1].to_broadcast([128, SEGT])
                        S.op("dve", lambda e: e.tensor_tensor_scan(out=Vr[pp][:], data0=rb, data1=Wr[pp][:], initial=ir,
                                                                   op0=ALU.mult, op1=ALU.add),
                             reads=[W_tok[pp], p_tok] + itoks, writes=[V_tok[pp]])
                        S.op("dve", lambda e: e.tensor_tensor_scan(out=Vi[pp][:], data0=rb, data1=Wi[pp][:], initial=ii,
                                                                   op0=ALU.mult, op1=ALU.add),
                             reads=[W_tok[pp], p_tok] + itoks, writes=[V_tok[pp]])
                        if not own:
                            cl, sl_ = cosT[:, k, SEGT - 1:SEGT], sinT[:, k, SEGT - 1:SEGT]
                            vr, vi = Vr[pp][:, SEGT - 1:SEGT], Vi[pp][:, SEGT - 1:SEGT]
                            S.op("dve", lambda e: e.tensor_tensor(out=rot[:, 0:1], in0=vi, in1=sl_, op=ALU.mult),
                                 reads=[V_tok[pp], tab_tok], writes=[rot_tok])
                            S.op("dve", lambda e: e.scalar_tensor_tensor(out=Xst[:, 2 * k, sg + 1:sg + 2], in0=vr, scalar=cl, in1=rot[:, 0:1],
                                                                         op0=ALU.mult, op1=ALU.subtract),
                                 reads=[V_tok[pp], tab_tok, rot_tok], writes=[xst_tok])
                            S.op("dve", lambda e: e.tensor_tensor(out=rot[:, 1:2], in0=vr, in1=sl_, op=ALU.mult),
                                 reads=[V_tok[pp], tab_tok], writes=[rot_tok])
                            S.op("dve", lambda e: e.scalar_tensor_tensor(out=Xst[:, 2 * k + 1, sg + 1:sg + 2], in0=vi, scalar=cl, in1=rot[:, 1:2],
                                                                         op0=ALU.mult, op1=ALU.add),
                                 reads=[V_tok[pp], tab_tok, rot_tok], writes=[xst_tok])
                        else:
                            S.op("dve", lambda e: e.tensor_tensor(out=tmp4[0][:], in0=Vr[pp][:], in1=ck, op=ALU.mult),
                                 reads=[V_tok[pp], tab_tok], writes=[tmp4_tok[0]])
                            S.op("dve", lambda e: e.tensor_tensor(out=tmp4[1][:], in0=Vi[pp][:], in1=sk, op=ALU.mult),
                                 reads=[V_tok[pp], tab_tok], writes=[tmp4_tok[1]])
                            S.op("dve", lambda e: e.tensor_tensor(out=Xb[pp][:, 0, :], in0=tmp4[0][:], in1=tmp4[1][:], op=ALU.subtract),
                                 reads=[tmp4_tok[0], tmp4_tok[1]], writes=[Xb_tok[pp]])
                            S.op("dve", lambda e: e.tensor_tensor(out=tmp4[2][:], in0=Vr[pp][:], in1=sk, op=ALU.mult),
                                 reads=[V_tok[pp], tab_tok], writes=[tmp4_tok[2]])
                            S.op("dve", lambda e: e.tensor_tensor(out=tmp4[3][:], in0=Vi[pp][:], in1=ck, op=ALU.mult),
                                 reads=[V_tok[pp], tab_tok], writes=[tmp4_tok[3]])
                            S.op("dve", lambda e: e.tensor_tensor(out=Xb[pp][:, 1, :], in0=tmp4[2][:], in1=tmp4[3][:], op=ALU.add),
                                 reads=[tmp4_tok[2], tmp4_tok[3]], writes=[Xb_tok[pp]])
                            S.op("pe", lambda e: e.matmul(ps[yps][:], lhsT=cre[:, k, :], rhs=Xb[pp][:, 0, :], start=(kk == 0), stop=False),
                                 reads=[bc_tok, Xb_tok[pp]], writes=[ps_tok[yps]])
                            S.op("pe", lambda e: e.matmul(ps[yps][:], lhsT=ncim[:, k, :], rhs=Xb[pp][:, 1, :], start=False, stop=False),
                                 reads=[bc_tok, Xb_tok[pp]], writes=[ps_tok[yps]])
                    if own:
                        S.op("pe", lambda e: e.matmul(ps[yps][:], lhsT=diagD[:, ct, :], rhs=uT[xb][:, ct, :], start=False, stop=True),
                             reads=[w_tok, uT_tok[xb]], writes=[ps_tok[yps]])
                        S.op("act", lambda e: e.activation(out=g1[:], in_=ps[yps][:], func=AF.Square), reads=[ps_tok[yps]], writes=[g_tok])
                        S.op("dve", lambda e: e.tensor_scalar(g1[:], g1[:], 0.044715, 1.0, ALU.mult, ALU.add), reads=[g_tok], writes=[g_tok])
                        S.op("dve", lambda e: e.tensor_tensor(out=g1[:], in0=ps[yps][:], in1=g1[:], op=ALU.mult),
                             reads=[g_tok, ps_tok[yps]], writes=[g_tok])
                        S.op("act", lambda e: e.activation(out=g2[:], in_=g1[:], func=AF.Sigmoid, scale=1.5957691216057308),
                             reads=[g_tok], writes=[g_tok])
                        S.op("dve", lambda e, ct=ct: e.tensor_tensor(out=yb[:, ct, :], in0=ps[yps][:], in1=g2[:], op=ALU.mult),
                             reads=[g_tok, ps_tok[yps]], writes=[y_tok[ct]])
                if own:
                    for c2 in range(4):
                        gp = 4 + c2 % 2
                        for kt in range(4):
                            S.op("pe", lambda e, kt=kt, c2=c2, gp=gp: e.matmul(ps[gp][:], lhsT=wglu[:, kt, c2 * 128:(c2 + 1) * 128],
                                                                              rhs=yb[:, kt, :], start=(kt == 0), stop=(kt == 3)),
                                 reads=[w_tok] + y_tok, writes=[ps_tok[gp]])
                        S.op("act", lambda e, c2=c2, gp=gp: e.activation(out=g2[:], in_=ps[gp][:], func=AF.Sigmoid, bias=bglu[:, c2:c2 + 1]),
                             reads=[ps_tok[gp], small_tok], writes=[g_tok])
                        S.op("dve", lambda e, c2=c2, sg=sg: e.tensor_tensor(out=ssmT[:, c2, sg * SEGT:(sg + 1) * SEGT], in0=yb[:, c2, :],
                                                                           in1=g2[:], op=ALU.mult),
                             reads=[g_tok, y_tok[c2]], writes=[ssm_tok[sg]])
            S.barrier()
        S.barrier()


def _rms_rstd(C, S, src_ap, src_tok, junk_ap, junk_tok, ss, ss_tok, rstd, rstd_tok, n=D):
    S.op("act", lambda e: e.activation(out=junk_ap, in_=src_ap, func=AF.Square, accum_out=ss[:]),
         reads=[src_tok], writes=[junk_tok, ss_tok])
    S.op("act", lambda e: e.activation(out=rstd[:], in_=ss[:], func=AF.Ln, bias=C.eps_col[:], scale=1.0 / n),
         reads=[ss_tok, C.const_tok], writes=[rstd_tok])
    S.op("act", lambda e: e.activation(out=rstd[:], in_=rstd[:], func=AF.Exp, scale=-0.5),
         reads=[rstd_tok], writes=[rstd_tok])


def _load_cast(S, nc, dst, src, gcol, gtok, stg, stg_tok, stg_ch, dst_tok, nkt, ncols, cnt):
    for kt in range(nkt):
        for c0 in range(0, ncols, 512):
            sl = cnt[0] % 2
            cnt[0] += 1
            w = min(512, ncols - c0)
            S.dma("sp", stg_ch[sl], stg[sl][:, 0:w], src[:, kt, c0:c0 + w], writes=[stg_tok[sl]])
            eng = "dve" if sl == 0 else "pool"
            if gcol is not None:
                S.op(eng, lambda e, kt=kt, c0=c0, w=w, sl=sl: e.tensor_scalar(dst[:, kt, c0:c0 + w], stg[sl][:, 0:w],
                                                                            gcol[:, kt:kt + 1], None, ALU.mult),
                     reads=[stg_tok[sl], gtok], writes=[dst_tok])
            else:
                S.op(eng, lambda e, kt=kt, c0=c0, w=w, sl=sl: e.tensor_copy(out=dst[:, kt, c0:c0 + w], in_=stg[sl][:, 0:w]),
                     reads=[stg_tok[sl]], writes=[dst_tok])


def phase_post(C, nc, S, ps, ps_tok, I, attnT, attn_tok, ssmT, ssm_tok, y, dbg):
    NT = NOWN * 4
    h2buf = nc.dram_tensor("h2buf", [NOWN * SEGT, D], F32, kind="Internal").ap()
    h2_tok = [Tok() for _ in range(NT)]
    alla = [t for row in attn_tok for t in row]
    bank = [0]

    def nbk():
        b = bank[0] % 6
        bank[0] += 1
        return b

    eo = ExitStack()
    if dbg not in ("h1", "h2"):
        wpq = _sb(nc, eo, "wpq", [128, 8, 2048], BF16)
        skT = _sb(nc, eo, "skT_sb", [128, 16, 128], BF16)
        pw_tok = Tok()
        gffn = _sb(nc, eo, "gffn", [128, 8], F32)
        gffn_rep = _sb(nc, eo, "gffn_rep_sb", [128, D], F32)
        gfin_rep = _sb(nc, eo, "gfin_rep_sb", [128, D], F32)
        iota256 = _sb(nc, eo, "iota256", [128, 256], F32)
        pg_tok = Tok()
        pchg = S.chan()
        S.dma("sp", pchg, gffn[:], I["g_ffn"][:, :], writes=[pg_tok])
        S.dma("sp", pchg, gffn_rep[:], I["gffn_rep"][:, :], writes=[pg_tok])
        S.dma("sp", pchg, gfin_rep[:], I["gfin_rep"][:, :], writes=[pg_tok])
        S.op("pool", lambda e: e.iota(iota256[:], pattern=[[1, 256]], base=0, channel_multiplier=0,
                                      allow_small_or_imprecise_dtypes=True), writes=[pg_tok])
        pstg = [_sb(nc, eo, "qstg%d" % i, [128, 512], F32) for i in range(2)]
        pstg_tok = [Tok(), Tok()]
        pstg_ch = [S.chan(), S.chan()]
        pcnt = [0]
        _load_cast(S, nc, wpq, I["w_pq"], gffn, pg_tok, pstg, pstg_tok, pstg_ch, pw_tok, 8, 2048, pcnt)
        _load_cast(S, nc, skT, I["skT"], None, None, pstg, pstg_tok, pstg_ch, pw_tok, 16, 128, pcnt)

    uvbf = nc.dram_tensor("uvbf", [16384, 2048], BF16, kind="Internal").ap()
    uv_toks = [Tok() for _ in range(32)]
    if dbg not in ("h1", "h2"):
        with ExitStack() as e2:
            cin = [_sb(nc, e2, "cin%d" % i, [128, 8, D], F32) for i in range(2)]
            cin_tok = [Tok(), Tok()]
            cin_ch = [S.chan(), S.chan()]
            cout = [_sb(nc, e2, "cout%d" % i, [128, 8, D], BF16) for i in range(2)]
            cout_tok = [[Tok() for _ in range(8)] for _ in range(2)]
            cout_ch = [S.chan(), S.chan()]
            ci = 0
            engs = ("act", "dve", "pool", "dve", "act", "dve", "act", "dve")
            for tbl, src in enumerate((I["peer_u"], I["peer_v"])):
                for chk in range(16):
                    sl = ci % 2
                    r0 = chk * 1024
                    S.dma("sp", cin_ch[sl], cin[sl][:], src[r0:r0 + 1024, :].rearrange("(p a) d -> p a d", a=8),
                          writes=[cin_tok[sl]])
                    for a in range(8):
                        if engs[a] == "act":
                            S.op("act", lambda e, a=a, sl=sl: e.copy(out=cout[sl][:, a, :], in_=cin[sl][:, a, :]),
                                 reads=[cin_tok[sl]], writes=[cout_tok[sl][a]])
                        else:
                            S.op(engs[a], lambda e, a=a, sl=sl: e.tensor_copy(out=cout[sl][:, a, :], in_=cin[sl][:, a, :]),
                                 reads=[cin_tok[sl]], writes=[cout_tok[sl][a]])
                    S.dma("sp", cout_ch[sl],
                          uvbf[r0:r0 + 1024, tbl * 1024:(tbl + 1) * 1024].rearrange("(p a) d -> p a d", a=8), cout[sl][:],
                          reads=cout_tok[sl], writes=[uv_toks[ci]])
                    ci += 1
            S.barrier()

    with ExitStack() as e2:
        wo = _sb(nc, e2, "wo", [128, 8, D], BF16)
        wxq = _sb(nc, e2, "wxq", [128, 8, D], BF16)
        wxo = _sb(nc, e2, "wxo", [128, 8, D], BF16)
        KmT = _sb(nc, e2, "KmT", [128, 8, 256], BF16)
        Vm = _sb(nc, e2, "Vm", [128, 2, D], BF16)
        w_tok = Tok()
        kv_tok = Tok()
        gcols = _sb(nc, e2, "gcols", [128, 3, 8], F32)
        g_tok = Tok()
        chg = S.chan()
        S.dma("sp", chg, gcols[:, 0, :], I["g_out"][:, :], writes=[g_tok])
        S.dma("sp", chg, gcols[:, 1, :], I["g_xattn"][:, :], writes=[g_tok])
        S.dma("sp", chg, gcols[:, 2, :], I["g_mem"][:, :], writes=[g_tok])
        stg = [_sb(nc, e2, "pstg%d" % i, [128, 512], F32) for i in range(2)]
        stg_tok = [Tok(), Tok()]
        stg_ch = [S.chan(), S.chan()]
        cnt = [0]
        wo_tok, wxq_tok, wxo_tok = Tok(), Tok(), Tok()
        _load_cast(S, nc, wo, I["w_out"], gcols[:, 0, :], g_tok, stg, stg_tok, stg_ch, wo_tok, 8, D, cnt)
        _load_cast(S, nc, wxq, I["w_xq"], gcols[:, 1, :], g_tok, stg, stg_tok, stg_ch, wxq_tok, 8, D, cnt)
        _load_cast(S, nc, wxo, I["w_xo"], None, None, stg, stg_tok, stg_ch, wxo_tok, 8, D, cnt)
        h = _sb(nc, e2, "h", [128, D], F32)
        h_tok = Tok()
        h_ch = S.chan()
        hn = _sb(nc, e2, "hn", [128, D], BF16)
        hn_tok = Tok()
        hnT = _sb(nc, e2, "hnT", [128, 8, 128], BF16)
        hnT_tok = Tok()
        ss = _sb(nc, e2, "pss", [128, 1], F32)
        ss_tok = Tok()
        rstd = _sb(nc, e2, "prstd", [128, 1], F32)
        rstd_tok = Tok()
        with ExitStack() as e3:
            memT = _sb(nc, e3, "memT", [128, 8, 256], BF16)
            memT_tok = Tok()
            wch = _sb(nc, e3, "wch", [128, 8, 512], BF16)
            wch_tok = Tok()
            for mt in range(2):
                S.dma("sp", h_ch, h[:], I["mem"][mt * 128:(mt + 1) * 128, :], writes=[h_tok])
                _rms_rstd(C, S, h[:], h_tok, hn[:], hn_tok, ss, ss_tok, rstd, rstd_tok)
                S.op("dve", lambda e: e.tensor_scalar(hn[:], h[:], rstd[:], None, ALU.mult), reads=[h_tok, rstd_tok], writes=[hn_tok])
                ptb, pt_tok = transpose_to(C, hn, hn_tok, None, None)
                S.op("act", lambda e, mt=mt, ptb=ptb: e.copy(out=memT[:, :, mt * 128:(mt + 1) * 128],
                                                           in_=ptb.rearrange("p (k t) -> p k t", k=8)),
                     reads=[pt_tok], writes=[memT_tok])
            for cc in range(4):
                _load_cast(S, nc, wch, I["w_xkv"][:, :, cc * 512:(cc + 1) * 512], gcols[:, 2, :], g_tok, stg, stg_tok, stg_ch,
                           wch_tok, 8, 512, cnt)
                if cc < 2:
                    for j4 in range(4):
                        b_ = nbk()
                        for kt in range(8):
                            S.op("pe", lambda e, kt=kt, j4=j4, b_=b_: e.matmul(ps[b_][:, 0:256], lhsT=wch[:, kt, j4 * 128:(j4 + 1) * 128],
                                                                             rhs=memT[:, kt, :], start=(kt == 0), stop=(kt == 7)),
                                 reads=[wch_tok, memT_tok], writes=[ps_tok[b_]])
                        S.op("act", lambda e, j4=j4, b_=b_, cc=cc: e.copy(out=KmT[:, cc * 4 + j4, :], in_=ps[b_][:, 0:256]),
                             reads=[ps_tok[b_]], writes=[kv_tok])
                else:
                    for mt in range(2):
                        b_ = nbk()
                        for kt in range(8):
                            S.op("pe", lambda e, kt=kt, mt=mt, b_=b_: e.matmul(ps[b_][:], lhsT=memT[:, kt, mt * 128:(mt + 1) * 128],
                                                                             rhs=wch[:, kt, :], start=(kt == 0), stop=(kt == 7)),
                                 reads=[wch_tok, memT_tok], writes=[ps_tok[b_]])
                        S.op("act", lambda e, mt=mt, b_=b_, cc=cc: e.copy(out=Vm[:, mt, (cc - 2) * 512:(cc - 1) * 512], in_=ps[b_][:]),
                             reads=[ps_tok[b_]], writes=[kv_tok])
            S.barrier()
        def mkset(tag):
            B = {}
            for nm, shp, dt_ in (("sq4", [128, 4, 128], BF16), ("rs2", [128, 2], F32), ("qT", [128, 8, 128], BF16),
                                 ("pp", [128, 4, 256], BF16), ("pT", [128, 8, 128], BF16), ("oT", [128, 8, 128], BF16),
                                 ("mx", [128, 4], F32), ("sm", [128, 4], F32), ("h", [128, D], F32), ("hn", [128, D], BF16),
                                 ("hnT", [128, 8, 128], BF16), ("ss", [128, 1], F32), ("rstd", [128, 1], F32)):
                B[nm] = _sb(nc, e2, nm + tag, shp, dt_)
            for nm in ("sq_tok", "rs2_tok", "qT_tok", "pp_tok", "pT_tok", "oT_tok", "sm_tok", "h_tok", "hn_tok", "hnT_tok",
                       "ss_tok", "rstd_tok"):
                B[nm] = Tok()
            B["h_ch"] = S.chan()
            return B

        class _Rec1:
            def __init__(self):
                self.items = []

            def op(self, *a, **k):
                self.items.append(("op", a, k))

            def dma(self, *a, **k):
                self.items.append(("dma", a, k))

        def emit_tile(S_, tile, nbk_, sq4, rs2, qT, pp, pT, oT, mx, sm, h, hn, hnT, ss, rstd, sq_tok, rs2_tok, qT_tok, pp_tok,
                      pT_tok, oT_tok, sm_tok, h_tok, hn_tok, hnT_tok, ss_tok, rstd_tok, h_ch):
            saved = C.S
            C.S = S_
            t0 = tile * 128
            slot = tile // 4
            tsl = slice(t0, t0 + 128)
            for which, (src, toks) in enumerate(((attnT, attn_tok[slot]), (ssmT, [ssm_tok[slot]]))):
                S_.op("act", lambda e, src=src: e.activation(out=sq4[:], in_=src[:, :, tsl], func=AF.Square),
                     reads=toks, writes=[sq_tok])
                b_ = nbk_()
                for hp in range(4):
                    S_.op("pe", lambda e, hp=hp, b_=b_: e.matmul(ps[b_][:, 0:1], lhsT=sq4[:, hp, :], rhs=C.ones_bf[:, 0:1],
                                                                start=(hp == 0), stop=(hp == 3)),
                         reads=[sq_tok, C.const_tok], writes=[ps_tok[b_]])
                S_.op("act", lambda e, which=which, b_=b_: e.activation(out=rs2[:, which:which + 1], in_=ps[b_][:, 0:1], func=AF.Ln,
                                                                       bias=C.eps_col[:], scale=1.0 / 512),
                     reads=[ps_tok[b_], C.const_tok], writes=[rs2_tok])
            S_.op("act", lambda e: e.activation(out=rs2[:], in_=rs2[:], func=AF.Exp, scale=-0.5), reads=[rs2_tok], writes=[rs2_tok])
            S_.dma("sp", h_ch, h[:], I["x_own"][t0:t0 + 128, :], writes=[h_tok])
            for which, (src, toks) in enumerate(((attnT, attn_tok[slot]), (ssmT, [ssm_tok[slot]]))):
                for n2 in range(2):
                    b_ = nbk_()
                    for hp in range(4):
                        S_.op("pe", lambda e, hp=hp, b_=b_, src=src, which=which, n2=n2: e.matmul(
                            ps[b_][:], lhsT=src[:, hp, tsl], rhs=wo[:, which * 4 + hp, n2 * 512:(n2 + 1) * 512],
                            start=(hp == 0), stop=(hp == 3)), reads=toks + [wo_tok], writes=[ps_tok[b_]])
                    S_.op("dve", lambda e, b_=b_, which=which, n2=n2: e.scalar_tensor_tensor(
                        out=h[:, n2 * 512:(n2 + 1) * 512], in0=ps[b_][:], scalar=rs2[:, which:which + 1],
                        in1=h[:, n2 * 512:(n2 + 1) * 512], op0=ALU.mult, op1=ALU.add),
                        reads=[ps_tok[b_], rs2_tok], writes=[h_tok])
            if dbg == "h1":
                S_.dma("sp", h_ch, y[t0:t0 + 128, :], h[:], reads=[h_tok], writes=[h2_tok[tile]])
                C.S = saved
                return
            _rms_rstd(C, S_, h[:], h_tok, hn[:], hn_tok, ss, ss_tok, rstd, rstd_tok)
            S_.op("dve", lambda e: e.tensor_scalar(hn[:], h[:], rstd[:], None, ALU.mult), reads=[h_tok, rstd_tok], writes=[hn_tok])
            ptb, pt_tok = transpose_to(C, hn, hn_tok, None, None)
            S_.op("act", lambda e, ptb=ptb: e.copy(out=hnT[:], in_=ptb.rearrange("p (k t) -> p k t", k=8)),
                 reads=[pt_tok], writes=[hnT_tok])
            for half in range(2):
                b_ = nbk_()
                for j4 in range(4):
                    hc = half * 4 + j4
                    for kt in range(8):
                        S_.op("pe", lambda e, kt=kt, hc=hc, j4=j4, b_=b_: e.matmul(
                            ps[b_][:, j4 * 128:(j4 + 1) * 128], lhsT=wxq[:, kt, hc * 128:(hc + 1) * 128], rhs=hnT[:, kt, :],
                            start=(kt == 0), stop=(kt == 7)), reads=[wxq_tok, hnT_tok], writes=[ps_tok[b_]])
                S_.op("act", lambda e, half=half, b_=b_: e.mul(out=qT[:, half * 4:(half + 1) * 4, :],
                                                             in_=ps[b_][:].rearrange("p (a b) -> p a b", a=4), mul=0.0625),
                     reads=[ps_tok[b_]], writes=[qT_tok])
            sb_ = [nbk_(), nbk_()]
            for hh in range(4):
                b_ = sb_[hh // 2]
                for c2 in range(2):
                    S_.op("pe", lambda e, hh=hh, c2=c2, b_=b_: e.matmul(
                        ps[b_][:, (hh % 2) * 256:(hh % 2 + 1) * 256], lhsT=qT[:, 2 * hh + c2, :], rhs=KmT[:, 2 * hh + c2, :],
                        start=(c2 == 0), stop=(c2 == 1)), reads=[qT_tok, kv_tok], writes=[ps_tok[b_]])
            for i2 in range(2):
                b_ = sb_[i2]
                S_.op("dve", lambda e, i2=i2, b_=b_: e.tensor_reduce(out=mx[:, 2 * i2:2 * i2 + 2],
                                                                    in_=ps[b_][:].rearrange("p (a b) -> p a b", a=2),
                                                                    axis=AX.X, op=ALU.max),
                     reads=[ps_tok[b_]], writes=[sm_tok])
            S_.op("dve", lambda e: e.tensor_scalar(mx[:], mx[:], -1.0, None, ALU.mult), reads=[sm_tok], writes=[sm_tok])
            for hh in range(4):
                b_ = sb_[hh // 2]
                S_.op("act", lambda e, hh=hh, b_=b_: e.activation(out=pp[:, hh, :], in_=ps[b_][:, (hh % 2) * 256:(hh % 2 + 1) * 256],
                                                                 func=AF.Exp, bias=mx[:, hh:hh + 1], accum_out=sm[:, hh:hh + 1]),
                     reads=[ps_tok[b_], sm_tok], writes=[pp_tok, sm_tok])
            S_.op("dve", lambda e: e.reciprocal(out=sm[:], in_=sm[:]), reads=[sm_tok], writes=[sm_tok])
            for hh in range(4):
                S_.op("dve", lambda e, hh=hh: e.tensor_scalar(pp[:, hh, :], pp[:, hh, :], sm[:, hh:hh + 1], None, ALU.mult),
                     reads=[sm_tok, pp_tok], writes=[pp_tok])
            ptb, pt_tok = transpose_to(C, pp[:].rearrange("p a b -> p (a b)"), pp_tok, None, None)
            S_.op("act", lambda e, ptb=ptb: e.copy(out=pT[:], in_=ptb.rearrange("p (k t) -> p k t", k=8)),
                 reads=[pt_tok], writes=[pT_tok])
            for half in range(2):
                b_ = nbk_()
                for j4 in range(4):
                    hc = half * 4 + j4
                    hh, c2 = hc // 2, hc % 2
                    for mt in range(2):
                        S_.op("pe", lambda e, mt=mt, hh=hh, c2=c2, j4=j4, b_=b_: e.matmul(
                            ps[b_][:, j4 * 128:(j4 + 1) * 128], lhsT=Vm[:, mt, hh * 256 + c2 * 128:hh * 256 + (c2 + 1) * 128],
                            rhs=pT[:, 2 * hh + mt, :], start=(mt == 0), stop=(mt == 1)),
                            reads=[kv_tok, pT_tok], writes=[ps_tok[b_]])
                S_.op("act", lambda e, half=half, b_=b_: e.copy(out=oT[:, half * 4:(half + 1) * 4, :],
                                                              in_=ps[b_][:].rearrange("p (a b) -> p a b", a=4)),
                     reads=[ps_tok[b_]], writes=[oT_tok])
            for n2 in range(2):
                b_ = nbk_()
                for hc in range(8):
                    S_.op("pe", lambda e, hc=hc, n2=n2, b_=b_: e.matmul(ps[b_][:], lhsT=oT[:, hc, :], rhs=wxo[:, hc, n2 * 512:(n2 + 1) * 512],
                                                                      start=(hc == 0), stop=(hc == 7)),
                         reads=[oT_tok, wxo_tok], writes=[ps_tok[b_]])
                S_.op("dve", lambda e, n2=n2, b_=b_: e.tensor_tensor(out=h[:, n2 * 512:(n2 + 1) * 512], in0=ps[b_][:],
                                                                    in1=h[:, n2 * 512:(n2 + 1) * 512], op=ALU.add),
                     reads=[ps_tok[b_]], writes=[h_tok])
            dst = y if dbg == "h2" else h2buf
            S_.dma("sp", h_ch, dst[t0:t0 + 128, :], h[:], reads=[h_tok], writes=[h2_tok[tile]])

            C.S = saved

        sets1 = [mkset("_e"), mkset("_o")]
        bk = [[0], [0]]

        def mk_nbk(par):
            def f():
                b = 3 * par + bk[par][0] % 3
                bk[par][0] += 1
                return b
            return f

        for t2 in range(0, NT, 2):
            recs = []
            for par in range(2):
                r_ = _Rec1()
                C.tp_force = par
                emit_tile(r_, t2 + par, mk_nbk(par), **sets1[par])
                C.tp_force = None
                recs.append(r_.items)
            while recs[0] or recs[1]:
                for par in range(2):
                    for _ in range(6):
                        if recs[par]:
                            kind, a, k = recs[par].pop(0)
                            getattr(S, kind)(*a, **k)
        S.barrier()
    if dbg in ("h1", "h2"):
        S.wait_tok("sp", h2_tok)
        eo.close()
        return

    with ExitStack() as e2:
        h = _sb(nc, e2, "h_b", [128, D], F32)
        h_tok = Tok()
        h_ch = S.chan()
        hn = _sb(nc, e2, "hn_b", [128, D], BF16)
        hn_tok = Tok()
        hnT = _sb(nc, e2, "hnT_b", [128, 8, 128], BF16)
        hnT_tok = Tok()
        hn3 = _sb(nc, e2, "hn3", [128, D], F32)
        hn3_tok = Tok()
        ss = _sb(nc, e2, "qss", [128, 1], F32)
        ss_tok = Tok()
        rstd = _sb(nc, e2, "qrstd", [128, 1], F32)
        rstd_tok = Tok()
        qpT = _sb(nc, e2, "qpT", [128, 16, 128], BF16)
        qpT_tok = Tok()
        sc = _sb(nc, e2, "sc", [128, 16, 128], F32)
        sc_tok = Tok()
        scr = _sb(nc, e2, "scr", [128, 2048], F32)
        scr_tok = Tok()
        hv = _sb(nc, e2, "hv", [128, 16, 16], F32)
        hi = _sb(nc, e2, "hi", [128, 16, 16], U32)
        hif = _sb(nc, e2, "hif", [128, 16, 16], F32)
        hv_tok = Tok()
        cand = _sb(nc, e2, "cand", [128, 8, 256], F32)
        eidx = _sb(nc, e2, "eidx", [128, 8, 256], F32)
        e0 = _sb(nc, e2, "e0", [128, 8, 16], F32)
        cand_tok = Tok()
        bv = _sb(nc, e2, "bv", [128, 8, 16], F32)
        bp = _sb(nc, e2, "bp", [128, 8, 16], U32)
        bpf = _sb(nc, e2, "bpf", [128, 8, 16], F32)
        bv_tok = Tok()
        junk = [_sb(nc, e2, "junk256_%d" % i, [128, 256], F32) for i in range(4)]
        junk_tok = [Tok() for _ in range(4)]
        eidc_tok = [Tok() for _ in range(128)]
        jq = [0]
        eidf = _sb(nc, e2, "eidf", [128, 128], F32)
        eid = _sb(nc, e2, "eid", [128, 128], U32)
        eid_tok = Tok()
        gt = _sb(nc, e2, "gt", [128, 8, 16], F32)
        gs = _sb(nc, e2, "gs", [128, 8], F32)
        nb0 = _sb(nc, e2, "nb0", [128, 8], F32)
        gt_tok = Tok()
        actc = _sb(nc, e2, "actc", [128, 128], F32)
        act_tok = Tok()
        wgt = _sb(nc, e2, "wgt", [128, 128], F32)
        wg2 = _sb(nc, e2, "wg2", [128, 128], F32)
        wgt_tok = Tok()
        NG = 8
        gbuf = [_sb(nc, e2, "gbuf%d" % i, [128, 2 * D], BF16) for i in range(NG)]
        gbuf_tok = [Tok() for _ in range(NG)]
        gbuf_ch = [S.chan() for _ in range(NG)]
        junk2 = [_sb(nc, e2, "junk2_%d" % i, [128, D], BF16) for i in range(2)]
        junk2_tok = [Tok(), Tok()]
        j2 = [0]
        NDG = 4
        dg = [_sb(nc, e2, "dg%d" % i, [128, 128], BF16) for i in range(NDG)]
        dg_tok = [Tok() for _ in range(NDG)]
        slot_tok = [Tok() for _ in range(128)]
        grp_tok = [Tok() for _ in range(32)]
        di = 0
        ytok = Tok()
        gi = 0
        PLAY_N = 15
        bankA = [0]

        def nbkA():
            b = bankA[0] % 4
            bankA[0] += 1
            return b

        class _Rec:
            def __init__(self):
                self.items = []

            def op(self, *a, **k):
                self.items.append(("op", a, k))

            def dma(self, *a, **k):
                self.items.append(("dma", a, k))

        junkF = _sb(nc, e2, "junkF", [128, D], BF16)
        junkF_tok = Tok()
        ss2 = _sb(nc, e2, "ss2", [128, 1], F32)
        ss2_tok = Tok()
        rstd2 = _sb(nc, e2, "rstd2", [128, 1], F32)
        rstd2_tok = Tok()
        obuf = _sb(nc, e2, "obuf", [128, D], F32)
        obuf_tok = Tok()
        o_ch = S.chan()
        h_b2 = _sb(nc, e2, "h_b2", [128, D], F32)
        hn3_b2 = _sb(nc, e2, "hn3_b2", [128, D], F32)
        eid_b2 = _sb(nc, e2, "eid_b2", [128, 128], U32)
        gt_b2 = _sb(nc, e2, "gt_b2", [128, 8, 16], F32)
        sets = [dict(h=h, h_tok=h_tok, h_ch=h_ch, hn3=hn3, hn3_tok=hn3_tok, eid=eid, eid_tok=eid_tok, gt=gt, gt_tok=gt_tok),
                dict(h=h_b2, h_tok=Tok(), h_ch=S.chan(), hn3=hn3_b2, hn3_tok=Tok(), eid=eid_b2, eid_tok=Tok(), gt=gt_b2, gt_tok=Tok())]

        def emitA(S_, tile, h, h_tok, h_ch, hn3, hn3_tok, eid, eid_tok, gt, gt_tok):
            saved = C.S
            C.S = S_
            t0 = tile * 128
            S_.dma("sp", h_ch, h[:], h2buf[t0:t0 + 128, :], reads=[h2_tok[tile]], writes=[h_tok])
            _rms_rstd(C, S_, h[:], h_tok, hn[:], hn_tok, ss, ss_tok, rstd, rstd_tok)
            S_.op("dve", lambda e: e.tensor_scalar(hn[:], h[:], rstd[:], None, ALU.mult), reads=[h_tok, rstd_tok], writes=[hn_tok])
            S_.op("dve", lambda e: e.scalar_tensor_tensor(out=hn3[:], in0=h[:], scalar=rstd[:], in1=gffn_rep[:], op0=ALU.mult, op1=ALU.mult),
                 reads=[h_tok, rstd_tok, pg_tok], writes=[hn3_tok])
            ptb, pt_tok = transpose_to(C, hn, hn_tok, None, None)
            S_.op("act", lambda e, ptb=ptb: e.copy(out=hnT[:], in_=ptb.rearrange("p (k t) -> p k t", k=8)),
                 reads=[pt_tok], writes=[hnT_tok])
            for q4 in range(4):
                b_ = nbkA()
                for j4 in range(4):
                    ch = q4 * 4 + j4
                    for kt in range(8):
                        S_.op("pe", lambda e, kt=kt, ch=ch, j4=j4, b_=b_: e.matmul(
                            ps[b_][:, j4 * 128:(j4 + 1) * 128], lhsT=wpq[:, kt, ch * 128:(ch + 1) * 128], rhs=hnT[:, kt, :],
                            start=(kt == 0), stop=(kt == 7)), reads=[pw_tok, hnT_tok], writes=[ps_tok[b_]])
                S_.op("act", lambda e, q4=q4, b_=b_: e.copy(out=qpT[:, q4 * 4:(q4 + 1) * 4, :],
                                                          in_=ps[b_][:].rearrange("p (a b) -> p a b", a=4)),
                     reads=[ps_tok[b_]], writes=[qpT_tok])
            for q4 in range(4):
                b_ = nbkA()
                for j4 in range(4):
                    ch = q4 * 4 + j4
                    S_.op("pe", lambda e, ch=ch, j4=j4, b_=b_: e.matmul(ps[b_][:, j4 * 128:(j4 + 1) * 128], lhsT=qpT[:, ch, :],
                                                                      rhs=skT[:, ch, :], start=True, stop=True),
                         reads=[pw_tok, qpT_tok], writes=[ps_tok[b_]])
                S_.op("act", lambda e, q4=q4, b_=b_: e.copy(out=sc[:, q4 * 4:(q4 + 1) * 4, :],
                                                          in_=ps[b_][:].rearrange("p (a b) -> p a b", a=4)),
                     reads=[ps_tok[b_]], writes=[sc_tok])
            scr3 = scr[:].rearrange("p (a b) -> p a b", a=16)
            for ch in range(16):
                S_.op("dve", lambda e, ch=ch: e.max(out=hv[:, ch, 0:8], in_=sc[:, ch, :]), reads=[sc_tok], writes=[hv_tok])
                S_.op("dve", lambda e, ch=ch: e.max_index(out=hi[:, ch, 0:8], in_max=hv[:, ch, 0:8], in_values=sc[:, ch, :]),
                     reads=[sc_tok, hv_tok], writes=[hv_tok])
                S_.op("dve", lambda e, ch=ch: e.match_replace(out=scr3[:, ch, :], in_to_replace=hv[:, ch, 0:8], in_values=sc[:, ch, :],
                                                             imm_value=NEG), reads=[sc_tok, hv_tok], writes=[scr_tok])
                S_.op("dve", lambda e, ch=ch: e.max(out=hv[:, ch, 8:16], in_=scr3[:, ch, :]), reads=[scr_tok], writes=[hv_tok])
                S_.op("dve", lambda e, ch=ch: e.max_index(out=hi[:, ch, 8:16], in_max=hv[:, ch, 8:16], in_values=scr3[:, ch, :]),
                     reads=[scr_tok, hv_tok], writes=[hv_tok])
            S_.op("dve", lambda e: e.tensor_copy(out=hif[:], in_=hi[:]), reads=[hv_tok], writes=[hv_tok])
            hv4 = hv[:].rearrange("p (h i) k -> p h i k", i=2)
            hif4 = hif[:].rearrange("p (h i) k -> p h i k", i=2)
            cand4 = cand[:].rearrange("p h (a b) -> p h a b", a=16)
            eidx4 = eidx[:].rearrange("p h (a b) -> p h a b", a=16)
            S_.op("dve", lambda e: e.tensor_tensor(out=cand4, in0=hv4[:, :, 0, :].unsqueeze(3).to_broadcast([128, 8, 16, 16]),
                                                  in1=hv4[:, :, 1, :].unsqueeze(2).to_broadcast([128, 8, 16, 16]), op=ALU.add),
                 reads=[hv_tok], writes=[cand_tok])
            S_.op("dve", lambda e: e.tensor_scalar(e0[:], hif4[:, :, 0, :], 128.0, None, ALU.mult), reads=[hv_tok], writes=[cand_tok])
            S_.op("dve", lambda e: e.tensor_tensor(out=eidx4, in0=e0[:].unsqueeze(3).to_broadcast([128, 8, 16, 16]),
                                                  in1=hif4[:, :, 1, :].unsqueeze(2).to_broadcast([128, 8, 16, 16]), op=ALU.add),
                 reads=[hv_tok, cand_tok], writes=[cand_tok])
            scr8 = scr[:].rearrange("p (a b) -> p a b", a=8)
            for hh in range(8):
                S_.op("dve", lambda e, hh=hh: e.max(out=bv[:, hh, 0:8], in_=cand[:, hh, :]), reads=[cand_tok], writes=[bv_tok])
                S_.op("dve", lambda e, hh=hh: e.max_index(out=bp[:, hh, 0:8], in_max=bv[:, hh, 0:8], in_values=cand[:, hh, :]),
                     reads=[cand_tok, bv_tok], writes=[bv_tok])
                S_.op("dve", lambda e, hh=hh: e.match_replace(out=scr8[:, hh, :], in_to_replace=bv[:, hh, 0:8], in_values=cand[:, hh, :],
                                                             imm_value=NEG), reads=[cand_tok, bv_tok], writes=[scr_tok])
                S_.op("dve", lambda e, hh=hh: e.max(out=bv[:, hh, 8:16], in_=scr8[:, hh, :]), reads=[scr_tok], writes=[bv_tok])
                S_.op("dve", lambda e, hh=hh: e.max_index(out=bp[:, hh, 8:16], in_max=bv[:, hh, 8:16], in_values=scr8[:, hh, :]),
                     reads=[scr_tok, bv_tok], writes=[bv_tok])
            S_.op("dve", lambda e: e.tensor_copy(out=bpf[:], in_=bp[:]), reads=[bv_tok], writes=[bv_tok])
            for hh in range(8):
                for k in range(16):
                    s_ = hh * 16 + k
                    jb = jq[0] % 4
                    jq[0] += 1
                    S_.op("dve", lambda e, hh=hh, k=k, s_=s_, jb=jb: e.scalar_tensor_tensor(
                        out=junk[jb][:], in0=iota256[:], scalar=bpf[:, hh, k:k + 1], in1=eidx[:, hh, :],
                        op0=ALU.is_equal, op1=ALU.mult, accum_out=eidf[:, s_:s_ + 1]),
                        reads=[bv_tok, cand_tok, pg_tok], writes=[junk_tok[jb], eidc_tok[s_]])
            S_.op("dve", lambda e: e.tensor_scalar(eidf[:], eidf[:], 16383.0, 0.0, ALU.min, ALU.max), reads=[eid_tok] + eidc_tok,
                  writes=[eid_tok] + eidc_tok)
            S_.op("dve", lambda e: e.tensor_copy(out=eid[:], in_=eidf[:]), reads=[eid_tok], writes=[eid_tok])
            S_.op("dve", lambda e: e.tensor_scalar(nb0[:], bv[:, :, 0], -1.0, None, ALU.mult), reads=[bv_tok], writes=[gt_tok])
            for hh in range(8):
                S_.op("act", lambda e, hh=hh: e.activation(out=gt[:, hh, :], in_=bv[:, hh, :], func=AF.Exp, bias=nb0[:, hh:hh + 1],
                                                          accum_out=gs[:, hh:hh + 1]), reads=[bv_tok, gt_tok], writes=[gt_tok])
            S_.op("dve", lambda e: e.reciprocal(out=gs[:], in_=gs[:]), reads=[gt_tok], writes=[gt_tok])
            S_.op("dve", lambda e: e.tensor_tensor(out=gt[:], in0=gt[:], in1=gs[:].unsqueeze(2).to_broadcast([128, 8, 16]), op=ALU.mult),
                 reads=[gt_tok], writes=[gt_tok])

            C.S = saved

        def emitB(tile, play, h, h_tok, h_ch, hn3, hn3_tok, eid, eid_tok, gt, gt_tok):
            nonlocal gi, di
            t0 = tile * 128
            pa, pb_ = 4, 5
            gtf = gt[:].rearrange("p a b -> p (a b)")
            for grp in range(32):
                used = []
                for q in range(4):
                    s_ = grp * 4 + q
                    g_ = gi % NG
                    gi += 1
                    used.append(g_)
                    S.dma("pool", gbuf_ch[g_], gbuf[g_][:], uvbf[:, :], reads=[eid_tok] + uv_toks, writes=[gbuf_tok[g_]],
                          indirect=bass.IndirectOffsetOnAxis(ap=eid[:, s_:s_ + 1], axis=0))
                    jb = j2[0] % 2
                    j2[0] += 1
                    S.op("dve", lambda e, g_=g_, s_=s_, jb=jb: e.scalar_tensor_tensor(out=junk2[jb][:], in0=gbuf[g_][:, 0:D], scalar=1.0, in1=hn3[:],
                                                                                     op0=ALU.mult, op1=ALU.mult, accum_out=actc[:, s_:s_ + 1]),
                         reads=[gbuf_tok[g_], hn3_tok], writes=[junk2_tok[jb], slot_tok[s_]])
                cs = slice(grp * 4, grp * 4 + 4)
                gk = grp_tok[grp]
                S.op("dve", lambda e: e.tensor_tensor(out=wg2[:, cs], in0=actc[:, cs], in1=actc[:, cs], op=ALU.mult),
                     reads=[slot_tok[grp * 4 + q] for q in range(4)], writes=[gk])
                S.op("dve", lambda e: e.tensor_scalar(wg2[:, cs], wg2[:, cs], 0.044715, 1.0, ALU.mult, ALU.add), reads=[gk], writes=[gk])
                S.op("dve", lambda e: e.tensor_tensor(out=wg2[:, cs], in0=wg2[:, cs], in1=actc[:, cs], op=ALU.mult),
                     reads=[gk] + [slot_tok[grp * 4 + q] for q in range(4)], writes=[gk])
                S.op("act", lambda e: e.activation(out=wg2[:, cs], in_=wg2[:, cs], func=AF.Sigmoid, scale=1.5957691216057308),
                     reads=[gk], writes=[gk])
                S.op("dve", lambda e: e.tensor_tensor(out=wgt[:, cs], in0=wg2[:, cs], in1=actc[:, cs], op=ALU.mult),
                     reads=[gk] + [slot_tok[grp * 4 + q] for q in range(4)], writes=[gk])
                S.op("dve", lambda e: e.tensor_tensor(out=wgt[:, cs], in0=wgt[:, cs], in1=gtf[:, cs], op=ALU.mult),
                     reads=[gk, gt_tok], writes=[gk])
                for q in range(4):
                    s_ = grp * 4 + q
                    g_ = used[q]
                    d_ = di % NDG
                    di += 1
                    S.op("act", lambda e, d_=d_, s_=s_: e.activation(out=dg[d_][:], in_=C.ident_bf[:], func=AF.Copy,
                                                                    scale=wgt[:, s_:s_ + 1]),
                         reads=[gk, C.const_tok], writes=[dg_tok[d_]])
                    for n2, pbk in enumerate((pa, pb_)):
                        S.op("pe", lambda e, d_=d_, g_=g_, n2=n2, pbk=pbk, s_=s_: e.matmul(
                            ps[pbk][:], lhsT=dg[d_][:], rhs=gbuf[g_][:, D + n2 * 512:D + (n2 + 1) * 512],
                            start=(s_ == 0), stop=(s_ == 127)), reads=[dg_tok[d_], gbuf_tok[g_]], writes=[ps_tok[pbk]])
                play(PLAY_N)
            for n2, pbk in enumerate((pa, pb_)):
                S.op("dve", lambda e, n2=n2, pbk=pbk: e.tensor_tensor(out=h[:, n2 * 512:(n2 + 1) * 512], in0=ps[pbk][:],
                                                                      in1=h[:, n2 * 512:(n2 + 1) * 512], op=ALU.add),
                     reads=[ps_tok[pbk]], writes=[h_tok])
            if dbg == "h3":
                S.dma("sp", h_ch, y[t0:t0 + 128, :], h[:], reads=[h_tok], writes=[ytok])
                play(10 ** 9)
                return
            _rms_rstd(C, S, h[:], h_tok, junkF[:], junkF_tok, ss2, ss2_tok, rstd2, rstd2_tok)
            S.op("dve", lambda e: e.scalar_tensor_tensor(out=obuf[:], in0=h[:], scalar=rstd2[:], in1=gfin_rep[:], op0=ALU.mult, op1=ALU.mult),
                 reads=[h_tok, rstd2_tok, pg_tok], writes=[obuf_tok])
            S.dma("sp", o_ch, y[t0:t0 + 128, :], obuf[:], reads=[obuf_tok], writes=[ytok])
            play(10 ** 9)

        rec = _Rec()
        emitA(rec, 0, **sets[0])
        for kind, a, k in rec.items:
            getattr(S, kind)(*a, **k)
        for tile in range(NT):
            rec = _Rec()
            if tile + 1 < NT:
                emitA(rec, tile + 1, **sets[(tile + 1) % 2])
            items = rec.items

            def play(n, items=items):
                while n > 0 and items:
                    kind, a, k = items.pop(0)
                    getattr(S, kind)(*a, **k)
                    n -= 1

            emitB(tile, play, **sets[tile % 2])
            play(10 ** 9)
        S.wait_tok("sp", [ytok])
        S.barrier()
    eo.close()


def build(dbg=None):
    nc = bass.Bass("TRN2", target_bir_lowering=False)
    C = Ctx()
    C.nc = nc
    C.dbg = dbg

    def din(name, shape, dt=F32):
        return nc.dram_tensor(name, list(shape), dt, kind="ExternalInput").ap()

    x_full = din("x_full", [SEQ, D])
    x_own = din("x_own", [NOWN * SEGT, D])
    qpos = din("qpos", [128, NOWN, SEGT])
    w_in = din("w_in", [128, 8, 2048])
    g_mix = din("g_mix", [128, 8])
    x_rev = din("x_rev", [SEQ, D])
    I = {"x_full": x_full, "x_own": x_own, "w_in": w_in, "x_rev": x_rev}
    for nm, shp in (("a_re", [128, 16]), ("a_im", [128, 16]), ("log_dt", [128, 16]),
                    ("bpad_re", [128, 16, 128]), ("bpad_im", [128, 16, 128]),
                    ("cpad_re", [128, 16, 128]), ("cpad_im", [128, 16, 128]),
                    ("d_skip", [128, 4]), ("w_glu", [128, 4, 512]), ("b_glu", [128, 4]),
                    ("segsel", [128, 4, 16]),
                    ("w_out", [128, 8, D]), ("g_out", [128, 8]), ("mem", [256, D]), ("g_mem", [128, 8]),
                    ("w_xkv", [128, 8, 2048]), ("g_xattn", [128, 8]), ("w_xq", [128, 8, D]), ("w_xo", [128, 8, D]),
                    ("g_ffn", [128, 8]), ("gffn_rep", [128, D]), ("gfin_rep", [128, D]), ("w_pq", [128, 8, 2048]),
                    ("skT", [128, 16, 128]), ("peer_u", [16384, D]), ("peer_v", [16384, D])):
        I[nm] = din(nm, shp)
    y = nc.dram_tensor("y", [NOWN * SEGT, D], F32, kind="ExternalOutput").ap()
    if dbg == "attn":
        dbg_out = nc.dram_tensor("dbg_attn", [128, 4, NOWN * SEGT], F32, kind="ExternalOutput").ap()

    with ExitStack() as es:
        S = Sched(nc, es)
        C.S = S
        C.const_tok = Tok()
        ident_f = _sb(nc, es, "ident_f", [128, 128], F32)
        C.ident_f = ident_f
        C.ident_bf = _sb(nc, es, "ident_bf", [128, 128], BF16)
        ones_f = _sb(nc, es, "ones_f", [128, 128], F32)
        C.ones_bf = _sb(nc, es, "ones_bf", [128, 128], BF16)
        C.tri_bf = _sb(nc, es, "tri_bf", [128, 128], BF16)
        C.atri_bf = _sb(nc, es, "atri_bf", [128, 128], BF16)
        C.eps_col = _sb(nc, es, "eps_col", [128, 1], F32)
        C.kpos = _sb(nc, es, "kpos", [128, 64], F32)
        S.op("pool", lambda e: e.memset(ones_f[:], 1.0), writes=[C.const_tok])
        S.op("pool", lambda e: e.memset(C.eps_col[:], EPS), writes=[C.const_tok])
        S.op("pool", lambda e: e.affine_select(out=ident_f[:], in_=ones_f[:], pattern=[[-1, 128]],
                                               compare_op=ALU.is_equal, fill=0.0, base=0,
                                               channel_multiplier=1),
             reads=[C.const_tok], writes=[C.const_tok])
        S.op("pool", lambda e: e.tensor_copy(out=C.ident_bf[:], in_=ident_f[:]),
             reads=[C.const_tok], writes=[C.const_tok])
        S.op("pool", lambda e: e.tensor_copy(out=C.ones_bf[:], in_=ones_f[:]),
             reads=[C.const_tok], writes=[C.const_tok])
        S.op("pool", lambda e: e.affine_select(out=C.tri_bf[:], in_=ones_f[:], pattern=[[-1, 128]],
                                               compare_op=ALU.is_ge, fill=0.0, base=0,
                                               channel_multiplier=1),
             reads=[C.const_tok], writes=[C.const_tok])
        S.op("pool", lambda e: e.affine_select(out=C.atri_bf[:], in_=ones_f[:], pattern=[[1, 128]],
                                               compare_op=ALU.is_gt, fill=0.0, base=0,
                                               channel_multiplier=-1),
             reads=[C.const_tok], writes=[C.const_tok])
        S.op("pool", lambda e: e.iota(C.kpos[:], pattern=[[128, 64]], base=0, channel_multiplier=1,
                                      allow_small_or_imprecise_dtypes=True),
             writes=[C.const_tok])

        ps = [es.enter_context(nc.psum_tensor("ps%d" % i, [128, 512], F32)) for i in range(8)]
        ps_tok = [Tok() for _ in range(8)]
        C.tp_ps = [ps[6], ps[7]]
        C.tp_tok = [ps_tok[6], ps_tok[7]]
        C.tp_i = 0

        attnT = _sb(nc, es, "attnT", [128, 4, NOWN * SEGT], BF16)
        attn_tok = [[Tok() for _ in range(8)] for _ in range(NOWN)]

        gmix_sb = _sb(nc, es, "gmix", [128, 8], F32)
        gmix_tok = Tok()
        S.dma("sp", S.chan(), gmix_sb[:], g_mix[:, :], writes=[gmix_tok])

        with ExitStack() as e2:
          if dbg != "ssm":
              KT = _sb(nc, e2, "KT", [128, 4, SEQ], BF16)
              V = _sb(nc, e2, "V", [128, 64, 512], BF16)
              QT = _sb(nc, e2, "QT", [128, 4, NOWN * SEGT], BF16)
              kt_tok = [Tok() for _ in range(NSEG)]
              v_tok = [Tok() for _ in range(NSEG)]
              q_tok = [Tok() for _ in range(NOWN)]
              with ExitStack() as e3:
                  wk = _sb(nc, e3, "wk", [128, 8, 512], BF16)
                  wq = wk
                  wv = _sb(nc, e3, "wv", [128, 8, 512], BF16)
                  w_tok = Tok()
                  wk_tok = Tok()
                  wv_tok = Tok()
                  xt = [_sb(nc, e3, "xt%d" % i, [128, D], F32) for i in range(2)]
                  xt_tok = [Tok() for _ in range(2)]
                  xt_ch = [S.chan() for _ in range(2)]
                  wi_box = [0]

                  def load_w(wdst, c0, wtok):
                      for k2 in range(4):
                          sl = wi_box[0] % 2
                          wi_box[0] += 1
                          S.dma("sp", xt_ch[sl], xt[sl][:].rearrange("p (a b) -> p a b", a=2),
                                w_in[:, 2 * k2:2 * k2 + 2, c0:c0 + 512], writes=[xt_tok[sl]])
                          for a2 in range(2):
                              kt = 2 * k2 + a2
                              eng = "dve" if a2 == 0 else "pool"
                              S.op(eng, lambda e, kt=kt, sl=sl, wdst=wdst, a2=a2: e.tensor_scalar(
                                  wdst[:, kt, :], xt[sl][:, a2 * 512:(a2 + 1) * 512], gmix_sb[:, kt:kt + 1], None, ALU.mult),
                                  reads=[xt_tok[sl], gmix_tok], writes=[wtok])

                  load_w(wk, 512, wk_tok)
                  load_w(wv, 1024, wv_tok)

                  xn = [_sb(nc, e3, "xn%d" % i, [128, D], BF16) for i in range(2)]
                  xn_tok = [Tok(), Tok()]
                  ss = [_sb(nc, e3, "ss%d" % i, [128, 1], F32) for i in range(2)]
                  ss_tok = [Tok(), Tok()]
                  rstd = [_sb(nc, e3, "rstd%d" % i, [128, 1], F32) for i in range(2)]
                  rstd_tok = [Tok(), Tok()]
                  xnT = [_sb(nc, e3, "xnT%d" % i, [128, 8, SEGT], BF16) for i in range(2)]
                  xnT_tok = [Tok(), Tok()]
                  ti = 0
                  for sgi in range(NSEG + NOWN):
                      own = sgi >= NSEG
                      sg = sgi - NSEG if own else sgi
                      if sgi == NSEG:
                          load_w(wq, 0, wk_tok)
                      src = x_own if own else x_full
                      xb = sgi % 2
                      for m in range(4):
                          a = ti % 2
                          b = ti % 2
                          ti += 1
                          r0 = sg * SEGT + m * 128
                          S.dma("sp", xt_ch[a], xt[a][:], src[r0:r0 + 128, :], writes=[xt_tok[a]])
                          rms_scale(C, xt[a][:], xt_tok[a], rstd[b][:], rstd_tok[b], xn[b][:], xn_tok[b],
                                    ss[b][:], ss_tok[b])
                          S.op("dve", lambda e, a=a, b=b: e.tensor_scalar(xn[b][:], xt[a][:], rstd[b][:], None, ALU.mult),
                               reads=[xt_tok[a], rstd_tok[b]], writes=[xn_tok[b]])
                          ptb, pt_tok = transpose_to(C, xn[b], xn_tok[b], None, None)
                          S.op("dve", lambda e, xb=xb, m=m, ptb=ptb: e.tensor_copy(
                              out=xnT[xb][:, :, m * 128:(m + 1) * 128],
                              in_=ptb.rearrange("p (k t) -> p k t", k=8)),
                              reads=[pt_tok], writes=[xnT_tok[xb]])
                      if not own:
                          for hp in range(4):
                              pb = hp % 2
                              for kt in range(8):
                                  S.op("pe", lambda e, kt=kt, hp=hp, pb=pb, xb=xb: e.matmul(
                                      ps[pb][:], lhsT=wk[:, kt, hp * 128:(hp + 1) * 128], rhs=xnT[xb][:, kt, :],
                                      start=(kt == 0), stop=(kt == 7)),
                                      reads=[wk_tok, xnT_tok[xb]], writes=[ps_tok[pb]])
                              S.op("act", lambda e, hp=hp, pb=pb, sg=sg: e.copy(
                                  out=KT[:, hp, sg * SEGT:(sg + 1) * SEGT], in_=ps[pb][:]),
                                  reads=[ps_tok[pb]], writes=[kt_tok[sg]])
                          for m in range(4):
                              pb = 2 + m % 2
                              for kt in range(8):
                                  S.op("pe", lambda e, kt=kt, m=m, pb=pb, xb=xb: e.matmul(
                                      ps[pb][:], lhsT=xnT[xb][:, kt, m * 128:(m + 1) * 128], rhs=wv[:, kt, :],
                                      start=(kt == 0), stop=(kt == 7)),
                                      reads=[wv_tok, xnT_tok[xb]], writes=[ps_tok[pb]])
                              S.op("act", lambda e, m=m, pb=pb, sg=sg: e.copy(
                                  out=V[:, sg * 4 + m, :], in_=ps[pb][:]),
                                  reads=[ps_tok[pb]], writes=[v_tok[sg]])
                      else:
                          for hp in range(4):
                              pb = hp % 2
                              for kt in range(8):
                                  S.op("pe", lambda e, kt=kt, hp=hp, pb=pb, xb=xb: e.matmul(
                                      ps[pb][:], lhsT=wq[:, kt, hp * 128:(hp + 1) * 128], rhs=xnT[xb][:, kt, :],
                                      start=(kt == 0), stop=(kt == 7)),
                                      reads=[wk_tok, xnT_tok[xb]], writes=[ps_tok[pb]])
                              S.op("act", lambda e, hp=hp, pb=pb, sg=sg: e.mul(
                                  out=QT[:, hp, sg * SEGT:(sg + 1) * SEGT], in_=ps[pb][:], mul=0.125),
                                  reads=[ps_tok[pb]], writes=[q_tok[sg]])

              S.barrier()
              if dbg == "kv":
                  dch = S.chan()
                  dk = nc.dram_tensor("dbg_kt", [128, 4, SEQ], BF16, kind="ExternalOutput").ap()
                  dv = nc.dram_tensor("dbg_v", [128, 64, 512], BF16, kind="ExternalOutput").ap()
                  dq = nc.dram_tensor("dbg_q", [128, 4, NOWN * SEGT], BF16, kind="ExternalOutput").ap()
                  dtok = Tok()
                  S.dma("sp", dch, dk[:, :, :], KT[:], reads=kt_tok, writes=[dtok])
                  S.dma("sp", dch, dv[:, :, :], V[:], reads=v_tok, writes=[dtok])
                  S.dma("sp", dch, dq[:, :, :], QT[:], reads=q_tok, writes=[dtok])
                  S.wait_tok("sp", [dtok])
              with ExitStack() as e3:
                if dbg != "kv":
                    NE = 3
                    e_sb = [_sb(nc, e3, "e%d" % i, [128, 512], F32) for i in range(NE)]
                    e_tok = [Tok() for _ in range(NE)]
                    sp_sb = [_sb(nc, e3, "sp%d" % i, [128, 512], BF16) for i in range(3)]
                    sp_tok = [Tok(), Tok(), Tok()]
                    ex_sb = [_sb(nc, e3, "ex%d" % i, [128, 512], BF16) for i in range(2)]
                    ex_tok = [Tok(), Tok()]
                    w_sb = [_sb(nc, e3, "w%d" % i, [128, 512], BF16) for i in range(2)]
                    w_tok2 = [Tok(), Tok()]
                    masks = _sb(nc, e3, "masks", [128, 16, 512], BF16)
                    mask_tok = Tok()
                    qp = _sb(nc, e3, "qp", [128, NOWN, SEGT], F32)
                    qp_tok = Tok()
                    S.dma("sp", S.chan(), qp[:], qpos[:, :, :], writes=[qp_tok])
                    zps = [ps[0], ps[1]]
                    zps_tok = [ps_tok[0], ps_tok[1]]
                    cps = [ps[2], ps[3]]
                    cps_tok = [ps_tok[2], ps_tok[3]]
                    ops_ = [ps[4], ps[5]]
                    ops_tok = [ps_tok[4], ps_tok[5]]

                    for slot in range(NOWN):
                        top = KB_TOP[slot]
                        nb = top + 1
                        for r in range(16):
                            kb = top - r
                            S.op("dve", lambda e, r=r, kb=kb, slot=slot: e.tensor_scalar(
                                masks[:, r, :], qp[:, slot, :], C.kpos[:, kb:kb + 1], None, ALU.is_gt),
                                reads=[qp_tok, C.const_tok], writes=[mask_tok])
                        blocks = [(h, top - r, r) for h in range(8) for r in range(nb)]
                        nblk = len(blocks)

                        def s1(i):
                            h, kb, r = blocks[i]
                            hp, hh = h // 2, h % 2
                            zb = i % 2
                            S.op("pe", lambda e: e.matmul(
                                zps[zb][:], lhsT=KT[hh * 64:(hh + 1) * 64, hp, kb * 128:(kb + 1) * 128],
                                rhs=QT[hh * 64:(hh + 1) * 64, hp, slot * SEGT:(slot + 1) * SEGT],
                                start=True, stop=True),
                                reads=[kt_tok[kb // 4], q_tok[slot]], writes=[zps_tok[zb]])

                        def s2(i):
                            h, kb, r = blocks[i]
                            zb, eb, sb = i % 2, i % NE, i % 3
                            S.op("act", lambda e: e.activation(out=e_sb[eb][:], in_=zps[zb][:], func=AF.Exp),
                                 reads=[zps_tok[zb]], writes=[e_tok[eb]])
                            if r < 16:
                                S.op("dve", lambda e: e.tensor_tensor(out=e_sb[eb][:], in0=e_sb[eb][:],
                                                                       in1=masks[:, r, :], op=ALU.mult),
                                     reads=[mask_tok], writes=[e_tok[eb]])
                            S.op("act", lambda e: e.activation(out=sp_sb[sb][:], in_=e_sb[eb][:], func=AF.Ln, bias=1.0),
                                 reads=[e_tok[eb]], writes=[sp_tok[sb]])

                        def s3(i):
                            h, kb, r = blocks[i]
                            sb = i % 3
                            cb = h % 2
                            S.op("pe", lambda e: e.matmul(cps[cb][:], lhsT=C.tri_bf[:], rhs=sp_sb[sb][:],
                                                          start=(r == 0), stop=True, skip_group_check=(r != 0)),
                                 reads=[sp_tok[sb], C.const_tok], writes=[cps_tok[cb]])

                        def s3b(i):
                            h, kb, r = blocks[i]
                            if r == nb - 1:
                                return
                            sb = i % 3
                            cb = h % 2
                            S.op("pe", lambda e: e.matmul(cps[cb][:], lhsT=C.atri_bf[:], rhs=sp_sb[sb][:],
                                                          start=False, stop=True, skip_group_check=True),
                                 reads=[sp_tok[sb], C.const_tok], writes=[cps_tok[cb]])

                        def s4(i):
                            h, kb, r = blocks[i]
                            cb, xb_, eb, wb = h % 2, i % 2, i % NE, i % 2
                            S.op("act", lambda e: e.activation(out=ex_sb[xb_][:], in_=cps[cb][:], func=AF.Exp, scale=-1.0),
                                 reads=[cps_tok[cb]], writes=[ex_tok[xb_]])
                            S.op("dve" if i % 2 == 0 else "pool", lambda e: e.tensor_tensor(out=w_sb[wb][:], in0=e_sb[eb][:], in1=ex_sb[xb_][:],
                                                                   op=ALU.mult),
                                 reads=[e_tok[eb], ex_tok[xb_]], writes=[w_tok2[wb]])

                        def s5(i):
                            h, kb, r = blocks[i]
                            wb = i % 2
                            ob = h % 2
                            hp, hh = h // 2, h % 2
                            S.op("pe", lambda e: e.matmul(ops_[ob][hh * 64:(hh + 1) * 64, :], lhsT=V[:, kb, h * 64:(h + 1) * 64],
                                                          rhs=w_sb[wb][:], start=(r == 0), stop=(r == nb - 1)),
                                 reads=[w_tok2[wb], v_tok[kb // 4]], writes=[ops_tok[ob]])
                            if r == nb - 1:
                                S.op("act", lambda e: e.copy(out=attnT[hh * 64:(hh + 1) * 64, hp, slot * SEGT:(slot + 1) * SEGT],
                                                             in_=ops_[ob][hh * 64:(hh + 1) * 64, :]),
                                     reads=[ops_tok[ob]], writes=[attn_tok[slot][h]])

                        for t in range(nblk + 2):
                            if t < nblk:
                                s1(t)
                                s2(t)
                            if 0 <= t - 2 < nblk:
                                s3b(t - 2)
                            if 0 <= t - 1 < nblk:
                                s3(t - 1)
                                s4(t - 1)
                            if 0 <= t - 2 < nblk:
                                s5(t - 2)

        S.barrier()
        ssmT = _sb(nc, es, "ssmT", [128, 4, NOWN * SEGT], BF16)
        ssm_tok = [Tok() for _ in range(NOWN)]
        if dbg in ("ssm", None, "h1", "h2", "h3"):
            phase_ssm(C, nc, S, ps, ps_tok, I, ssmT, ssm_tok, gmix_sb, gmix_tok)
        S.barrier()
        och = S.chan()
        if dbg == "ssm":
            dbg_ssm = nc.dram_tensor("dbg_ssm", [128, 4, NOWN * SEGT], BF16, kind="ExternalOutput").ap()
            ytok = Tok()
            S.dma("sp", och, dbg_ssm[:, :, :], ssmT[:], reads=ssm_tok, writes=[ytok])
            S.wait_tok("sp", [ytok])
        if dbg == "attn":
            with ExitStack() as e2:
                tmp = _sb(nc, e2, "dbgtmp", [128, 4, NOWN * SEGT], F32)
                tmp_tok = Tok()
                alltoks = [t for row in attn_tok for t in row]
                S.op("dve", lambda e: e.tensor_copy(out=tmp[:], in_=attnT[:]), reads=alltoks, writes=[tmp_tok])
                ytok = Tok()
                S.dma("sp", och, dbg_out[:, :, :], tmp[:], reads=[tmp_tok], writes=[ytok])
                S.wait_tok("sp", [ytok])
        S.barrier()
        if dbg in (None, "h1", "h2", "h3"):
            phase_post(C, nc, S, ps, ps_tok, I, attnT, attn_tok, ssmT, ssm_tok, y, dbg)
            return nc
        with ExitStack() as e2:
            zt = _sb(nc, e2, "zt", [128, D], F32)
            zt_tok = Tok()
            S.op("pool", lambda e: e.memset(zt[:], 0.0), writes=[zt_tok])
            ytok = Tok()
            for i in range(NOWN * 4):
                S.dma("sp", och, y[i * 128:(i + 1) * 128, :], zt[:], reads=[zt_tok], writes=[ytok])
            S.wait_tok("sp", [ytok])
    return nc


def make_in_maps(inputs):
    x = np.ascontiguousarray(inputs["x"], dtype=np.float32)
    w_in = np.ascontiguousarray(inputs["w_in"][0].reshape(8, 128, 2048).transpose(1, 0, 2))
    g_mix = np.ascontiguousarray(inputs["g_mix"][0].reshape(8, 128).T)
    f32 = np.float32
    are = inputs["a_re"][0].reshape(16, 2, 64).transpose(1, 2, 0).reshape(128, 16)
    aim = inputs["a_im"][0].reshape(16, 2, 64).transpose(1, 2, 0).reshape(128, 16)
    ldt = np.repeat(inputs["log_dt"][0].reshape(16, 2).T[:, None, :], 64, axis=1).reshape(128, 16)
    bpr = np.zeros((128, 16, 128), f32)
    bpi = np.zeros((128, 16, 128), f32)
    cpr = np.zeros((128, 16, 128), f32)
    cpi = np.zeros((128, 16, 128), f32)
    for k in range(16):
        for g2 in range(2):
            g = 2 * k + g2
            r0 = (g % 8) * 16
            bpr[r0:r0 + 16, k, g2 * 64:(g2 + 1) * 64] = inputs["b_re"][0][g].T
            bpi[r0:r0 + 16, k, g2 * 64:(g2 + 1) * 64] = inputs["b_im"][0][g].T
            cpr[g2 * 64:(g2 + 1) * 64, k, r0:r0 + 16] = inputs["c_re"][0][g].T
            cpi[g2 * 64:(g2 + 1) * 64, k, r0:r0 + 16] = inputs["c_im"][0][g].T
    dsk = inputs["d_skip"][0].reshape(4, 128).T
    wglu = inputs["w_glu"][0].reshape(4, 128, 512).transpose(1, 0, 2)
    bglu = inputs["b_glu"][0].reshape(4, 128).T
    def kt8(w):
        return w.reshape(8, 128, -1).transpose(1, 0, 2)

    def col8(g):
        return g.reshape(8, 128).T

    post = {"w_out": kt8(inputs["w_out"][0]),
            "g_out": col8(np.concatenate([inputs["g_attn_out"][0], inputs["g_ssm_out"][0]])),
            "g_mem": col8(inputs["g_mem"][0]), "w_xkv": kt8(inputs["w_xkv"][0]),
            "g_xattn": col8(inputs["g_xattn"][0]), "w_xq": kt8(inputs["w_xq"][0]), "w_xo": kt8(inputs["w_xo"][0]),
            "g_ffn": col8(inputs["g_ffn"][0]),
            "gffn_rep": np.broadcast_to(inputs["g_ffn"][0][None, :], (128, D)),
            "gfin_rep": np.broadcast_to(inputs["g_final"][None, :], (128, D)),
            "w_pq": kt8(inputs["w_pq"][0]),
            "skT": inputs["sub_keys"][0].transpose(3, 0, 1, 2).reshape(128, 16, 128),
            "peer_u": inputs["peer_u"][0], "peer_v": inputs["peer_v"][0]}
    common = {"w_in": w_in, "g_mix": g_mix, "a_re": are, "a_im": aim, "log_dt": ldt,
              "bpad_re": bpr, "bpad_im": bpi, "cpad_re": cpr, "cpad_im": cpi,
              "d_skip": dsk, "w_glu": wglu, "b_glu": bglu}
    common.update(post)
    common = {k_: np.ascontiguousarray(v_, dtype=f32) for k_, v_ in common.items()}
    maps = []
    for c in range(8):
        b, j = c // 4, c % 4
        tiles = own_tiles(j)
        segsel = np.zeros((128, NOWN, 16), f32)
        for i_, t_ in enumerate(tiles):
            segsel[:, i_, t_] = 1.0
        x_own = np.concatenate([x[b, t * SEGT:(t + 1) * SEGT] for t in tiles], axis=0)
        qp = np.stack([np.arange(t * SEGT, (t + 1) * SEGT, dtype=np.float32) for t in tiles], axis=0)
        qp = np.ascontiguousarray(np.broadcast_to(qp[None], (128, NOWN, SEGT)))
        x_rev = np.ascontiguousarray(x[b].reshape(NSEG, SEGT, D)[:, ::-1, :].reshape(SEQ, D))
        m = {"x_full": np.ascontiguousarray(x[b]), "x_own": np.ascontiguousarray(x_own), "x_rev": x_rev,
             "qpos": qp, "segsel": segsel, "mem": np.ascontiguousarray(inputs["mem"][b], dtype=f32)}
        m.update(common)
        maps.append(m)
    return maps


def kernel(**inputs):
    nc = build()
    maps = make_in_maps(inputs)
    res = run_bass_kernel_spmd(nc, maps, core_ids=list(range(8)))
    out = np.zeros((2, SEQ, D), np.float32)
    for c in range(8):
        b, j = c // 4, c % 4
        yo = res.results[c]["y"]
        for i, t in enumerate(own_tiles(j)):
            out[b, t * SEGT:(t + 1) * SEGT] = yo[i * SEGT:(i + 1) * SEGT]
    return out
```

```python
from contextlib import ExitStack
import numpy as np
import concourse.bass as bass
import concourse.mybir as mybir
from concourse.bass_utils import run_bass_kernel_spmd

F32 = mybir.dt.float32
BF16 = mybir.dt.bfloat16
U32 = mybir.dt.uint32
I32 = mybir.dt.int32
AF = mybir.ActivationFunctionType
ALU = mybir.AluOpType
AX = mybir.AxisListType

D = 1024
SEQ = 8192
NSEG = 16
SEGT = 512
NOWN = 4
EPS = 1e-6
KB_TOP = [15, 31, 47, 63]
NEG = -1.0e30


def own_tiles(j):
    return [j, 7 - j, 8 + j, 15 - j]


class Tok:
    __slots__ = ("w", "r")

    def __init__(self):
        self.w = None
        self.r = {}


class _Eng:
    def __init__(self, name, h, sem):
        self.name = name
        self.h = h
        self.sem = sem
        self.n = 0
        self.waited = {}


class _Chan:
    def __init__(self, sem):
        self.sem = sem
        self.n = 0


class Sched:
    def __init__(self, nc, es):
        self.nc = nc
        self.es = es
        self.E = {}
        for name, h in (("pe", nc.tensor), ("act", nc.scalar), ("dve", nc.vector),
                        ("pool", nc.gpsimd), ("sp", nc.sync)):
            sem = es.enter_context(nc.semaphore("sem_" + name))
            self.E[name] = _Eng(name, h, sem)
        self.nchan = 0
        self.chans = []

    def chan(self):
        self.nchan += 1
        c = _Chan(self.es.enter_context(self.nc.semaphore("ch%d" % self.nchan)))
        self.chans.append(c)
        return c

    def barrier(self):
        for E in self.E.values():
            for F in self.E.values():
                if F is E or F.n == 0:
                    continue
                if E.waited.get(id(F.sem), 0) < F.n:
                    E.h.wait_ge(F.sem, F.n)
                    E.waited[id(F.sem)] = F.n
            for c in self.chans:
                if c.n > 0 and E.waited.get(id(c.sem), 0) < c.n:
                    E.h.wait_ge(c.sem, c.n)
                    E.waited[id(c.sem)] = c.n

    def _wait(self, E, reads, writes, weak=()):
        deps = {}
        for t in weak:
            for d in [t.w] + list(t.r.values()):
                if d is not None and d[0] is not E.sem:
                    k_ = id(d[0])
                    if k_ not in deps or deps[k_][1] < d[1]:
                        deps[k_] = d

        def add(d):
            if d is None:
                return
            s, v = d
            k = id(s)
            if k not in deps or deps[k][1] < v:
                deps[k] = (s, v)

        for t in reads:
            add(t.w)
        for t in writes:
            add(t.w)
            for d in t.r.values():
                add(d)
        for k, (s, v) in deps.items():
            if E.name == "pe" and s is E.sem:
                continue
            if E.waited.get(k, 0) < v:
                E.h.wait_ge(s, v)
                E.waited[k] = v

    def op(self, eng, fn, reads=(), writes=(), weak=()):
        E = self.E[eng]
        self._wait(E, reads, writes, weak)
        ins = fn(E.h)
        E.n += 1
        ins.then_inc(E.sem, 1)
        me = (E.sem, E.n)
        for t in reads:
            t.r[id(E.sem)] = me
        for t in writes:
            t.w = me
            t.r = {}
        for t in weak:
            t.w = me
            t.r = {}
        return ins

    def dma(self, queue, ch, out, in_, reads=(), writes=(), indirect=None, **kw):
        Q = self.E[queue]
        self._wait(Q, reads, writes)
        if indirect is not None:
            ins = Q.h.indirect_dma_start(out=out, out_offset=None, in_=in_, in_offset=indirect)
        else:
            ins = Q.h.dma_start(out=out, in_=in_, **kw)
        ch.n += 16
        ins.then_inc(ch.sem, 16)
        me = (ch.sem, ch.n)
        for t in reads:
            t.r[id(ch.sem)] = me
        for t in writes:
            t.w = me
            t.r = {}
        return ins

    def wait_tok(self, eng, toks):
        self._wait(self.E[eng], toks, ())


class Ctx:
    pass


_NAME_CNT = [0]


def _sb(nc, es, name, shape, dt):
    _NAME_CNT[0] += 1
    return es.enter_context(nc.sbuf_tensor("%s_%d" % (name, _NAME_CNT[0]), list(shape), dt))


def rms_scale(C, xt, xt_tok, rstd, rstd_tok, junk, junk_tok, ss, ss_tok):
    S = C.S
    S.op("act", lambda e: e.activation(out=junk, in_=xt, func=AF.Square, accum_out=ss),
         reads=[xt_tok], writes=[junk_tok, ss_tok])
    S.op("act", lambda e: e.activation(out=rstd, in_=ss, func=AF.Ln, bias=C.eps_col[:], scale=1.0 / D),
         reads=[ss_tok, C.const_tok], writes=[rstd_tok])
    S.op("act", lambda e: e.activation(out=rstd, in_=rstd, func=AF.Exp, scale=-0.5),
         reads=[rstd_tok], writes=[rstd_tok])


def transpose_to(C, xn, xn_tok, dst_fn, dst_tok, nkt=8):
    S = C.S
    force = getattr(C, "tp_force", None)
    if force is not None:
        pt, pt_tok = C.tp_ps[force], C.tp_tok[force]
    else:
        pt, pt_tok = C.tp_ps[C.tp_i % 2], C.tp_tok[C.tp_i % 2]
        C.tp_i += 1
    ptb = pt[:].bitcast(BF16)
    for kt in range(nkt):
        S.op("pe", lambda e, kt=kt: e.transpose(out=ptb[:, kt * 128:(kt + 1) * 128],
                                                in_=xn[:, kt * 128:(kt + 1) * 128],
                                                identity=C.ident_bf[:]),
             reads=[xn_tok, C.const_tok], writes=[pt_tok])
    return ptb, pt_tok


TWO_PI = 6.283185307179586
CW1 = 6.28125
CW2 = TWO_PI - CW1


def phase_ssm(C, nc, S, ps, ps_tok, I, ssmT, ssm_tok, gmix_sb, gmix_tok):
    with ExitStack() as e2:
        cosT = _sb(nc, e2, "cosT", [128, 16, SEGT], F32)
        sinT = _sb(nc, e2, "sinT", [128, 16, SEGT], F32)
        tab_tok = Tok()
        bre = _sb(nc, e2, "bre", [128, 16, 128], BF16)
        bim = _sb(nc, e2, "bim", [128, 16, 128], BF16)
        cre = _sb(nc, e2, "cre", [128, 16, 128], BF16)
        ncim = _sb(nc, e2, "ncim", [128, 16, 128], BF16)
        bc_tok = Tok()
        wu = _sb(nc, e2, "wu", [128, 8, 512], BF16)
        wglu = _sb(nc, e2, "wglu", [128, 4, 512], BF16)
        diagD = _sb(nc, e2, "diagD", [128, 4, 128], BF16)
        w_tok = Tok()
        P = {}
        for nm in ("are", "aim", "ldt", "dt", "rho", "th", "gre", "gim", "ngim", "pa", "pb", "pc", "pd", "adt", "nadt",
                   "ilr", "ili", "q1", "q2", "q3", "q4"):
            P[nm] = _sb(nc, e2, "p_" + nm, [128, 16], F32)
        p_tok = Tok()
        bglu = _sb(nc, e2, "bglu", [128, 4], F32)
        dsk = _sb(nc, e2, "dsk", [128, 4], F32)
        segsel = _sb(nc, e2, "segsel_sb", [128, 4, 16], F32)
        halfpi = _sb(nc, e2, "halfpi", [128, 1], F32)
        Xst = _sb(nc, e2, "Xst", [128, 2, 16, 17], F32)
        tau1p = _sb(nc, e2, "tau1p", [128, SEGT], F32)
        magt = _sb(nc, e2, "magt", [128, SEGT], F32)
        mag_tok = Tok()
        S.op("pool", lambda e: e.iota(tau1p[:], pattern=[[1, SEGT]], base=1, channel_multiplier=0,
                                      allow_small_or_imprecise_dtypes=True), writes=[mag_tok])
        xst_tok = Tok()
        small_tok = Tok()
        ch0 = S.chan()
        for dst, src in ((P["are"], I["a_re"]), (P["aim"], I["a_im"]), (P["ldt"], I["log_dt"]),
                         (bglu, I["b_glu"]), (dsk, I["d_skip"])):
            S.dma("sp", ch0, dst[:], src[:, :], writes=[small_tok])
        S.dma("sp", ch0, segsel[:], I["segsel"][:, :, :], writes=[small_tok])
        S.op("pool", lambda e: e.memset(halfpi[:], float(np.pi / 2)), writes=[small_tok])
        S.op("pool", lambda e: e.memset(Xst[:], 0.0), writes=[xst_tok])
        S.op("act", lambda e: e.activation(out=P["dt"][:], in_=P["ldt"][:], func=AF.Exp), reads=[small_tok], writes=[p_tok])
        S.op("dve", lambda e: e.tensor_tensor(out=P["pa"][:], in0=P["are"][:], in1=P["dt"][:], op=ALU.mult),
             reads=[small_tok, p_tok], writes=[p_tok])
        S.op("act", lambda e: e.activation(out=P["rho"][:], in_=P["pa"][:], func=AF.Exp), reads=[p_tok], writes=[p_tok])
        S.op("dve", lambda e: e.tensor_copy(out=P["adt"][:], in_=P["pa"][:]), reads=[p_tok], writes=[p_tok])
        S.op("dve", lambda e: e.tensor_scalar(P["nadt"][:], P["pa"][:], -1.0, None, ALU.mult), reads=[p_tok], writes=[p_tok])
        S.op("dve", lambda e: e.tensor_tensor(out=P["th"][:], in0=P["aim"][:], in1=P["dt"][:], op=ALU.mult),
             reads=[small_tok, p_tok], writes=[p_tok])
        with ExitStack() as e3:
            tau1 = _sb(nc, e3, "tau1", [128, SEGT], F32)
            ang = _sb(nc, e3, "ang", [128, 4, SEGT], F32)
            kf = _sb(nc, e3, "kf", [128, 4, SEGT], F32)
            ki = _sb(nc, e3, "ki", [128, 4, SEGT], I32)
            s1 = _sb(nc, e3, "s1", [128, 4, SEGT], F32)
            t_tok = Tok()
            S.op("pool", lambda e: e.iota(tau1[:], pattern=[[1, SEGT]], base=1, channel_multiplier=0,
                                          allow_small_or_imprecise_dtypes=True), writes=[t_tok])
            for gq in range(4):
                for kk in range(4):
                    k = gq * 4 + kk
                    S.op("dve", lambda e, kk=kk, k=k: e.tensor_scalar(ang[:, kk, :], tau1[:], P["th"][:, k:k + 1], None, ALU.mult),
                         reads=[p_tok, t_tok], writes=[t_tok])
                S.op("dve", lambda e: e.tensor_scalar(kf[:], ang[:], 1.0 / TWO_PI, None, ALU.mult), reads=[t_tok], writes=[t_tok])
                S.op("dve", lambda e: e.tensor_copy(out=ki[:], in_=kf[:]), reads=[t_tok], writes=[t_tok])
                S.op("dve", lambda e: e.tensor_copy(out=kf[:], in_=ki[:]), reads=[t_tok], writes=[t_tok])
                S.op("dve", lambda e: e.scalar_tensor_tensor(out=ang[:], in0=kf[:], scalar=-CW1, in1=ang[:], op0=ALU.mult, op1=ALU.add),
                     reads=[t_tok], writes=[t_tok])
                S.op("dve", lambda e: e.scalar_tensor_tensor(out=ang[:], in0=kf[:], scalar=-CW2, in1=ang[:], op0=ALU.mult, op1=ALU.add),
                     reads=[t_tok], writes=[t_tok])
                S.op("act", lambda e: e.activation(out=s1[:], in_=ang[:], func=AF.Sin, scale=0.25), reads=[t_tok], writes=[t_tok])
                S.op("act", lambda e: e.activation(out=kf[:], in_=ang[:], func=AF.Sin, scale=0.25, bias=halfpi[:]),
                     reads=[t_tok, small_tok], writes=[t_tok])
                S.op("dve", lambda e: e.scalar_tensor_tensor(out=ang[:], in0=s1[:], scalar=2.0, in1=kf[:], op0=ALU.mult, op1=ALU.mult),
                     reads=[t_tok], writes=[t_tok])
                S.op("dve", lambda e: e.tensor_tensor(out=s1[:], in0=s1[:], in1=s1[:], op=ALU.mult), reads=[t_tok], writes=[t_tok])
                S.op("dve", lambda e: e.tensor_scalar(s1[:], s1[:], -2.0, 1.0, ALU.mult, ALU.add), reads=[t_tok], writes=[t_tok])
                S.op("dve", lambda e, gq=gq: e.scalar_tensor_tensor(out=sinT[:, gq * 4:(gq + 1) * 4, :], in0=ang[:], scalar=2.0, in1=s1[:],
                                                                    op0=ALU.mult, op1=ALU.mult),
                     reads=[t_tok], writes=[tab_tok])
                S.op("dve", lambda e: e.tensor_tensor(out=ang[:], in0=ang[:], in1=ang[:], op=ALU.mult), reads=[t_tok], writes=[t_tok])
                S.op("dve", lambda e, gq=gq: e.tensor_scalar(cosT[:, gq * 4:(gq + 1) * 4, :], ang[:], -2.0, 1.0, ALU.mult, ALU.add),
                     reads=[t_tok], writes=[tab_tok])
            c0 = cosT[:, :, 0]
            s0 = sinT[:, :, 0]
            tt = lambda o, a, b, op: S.op("dve", lambda e: e.tensor_tensor(out=o, in0=a, in1=b, op=op),
                                          reads=[p_tok, tab_tok, small_tok], writes=[p_tok])
            tt(P["pa"][:], P["rho"][:], c0, ALU.mult)
            S.op("dve", lambda e: e.tensor_scalar(P["pa"][:], P["pa"][:], -1.0, None, ALU.add), reads=[p_tok], writes=[p_tok])
            tt(P["pb"][:], P["rho"][:], s0, ALU.mult)
            tt(P["pc"][:], P["are"][:], P["are"][:], ALU.mult)
            tt(P["pd"][:], P["aim"][:], P["aim"][:], ALU.mult)
            tt(P["pc"][:], P["pc"][:], P["pd"][:], ALU.add)
            S.op("dve", lambda e: e.reciprocal(out=P["pc"][:], in_=P["pc"][:]), reads=[p_tok], writes=[p_tok])
            tt(P["gre"][:], P["pa"][:], P["are"][:], ALU.mult)
            tt(P["pd"][:], P["pb"][:], P["aim"][:], ALU.mult)
            tt(P["gre"][:], P["gre"][:], P["pd"][:], ALU.add)
            tt(P["gre"][:], P["gre"][:], P["pc"][:], ALU.mult)
            tt(P["gim"][:], P["pb"][:], P["are"][:], ALU.mult)
            tt(P["pd"][:], P["pa"][:], P["aim"][:], ALU.mult)
            tt(P["gim"][:], P["gim"][:], P["pd"][:], ALU.subtract)
            tt(P["gim"][:], P["gim"][:], P["pc"][:], ALU.mult)
            S.op("dve", lambda e: e.tensor_scalar(P["ngim"][:], P["gim"][:], -1.0, None, ALU.mult), reads=[p_tok], writes=[p_tok])
            S.barrier()
        with ExitStack() as e3:
            f1 = _sb(nc, e3, "f1", [128, 16, 128], F32)
            f2 = _sb(nc, e3, "f2", [128, 16, 128], F32)
            f3 = _sb(nc, e3, "f3", [128, 128], F32)
            f_tok = Tok()
            chf = S.chan()
            for src, dst in ((I["bpad_re"], bre), (I["bpad_im"], bim)):
                S.dma("sp", chf, f1[:], src[:, :, :], writes=[f_tok])
                S.op("dve", lambda e, dst=dst: e.tensor_copy(out=dst[:], in_=f1[:]), reads=[f_tok], writes=[bc_tok, f_tok])
            S.dma("sp", chf, f1[:], I["cpad_re"][:, :, :], writes=[f_tok])
            S.dma("sp", chf, f2[:], I["cpad_im"][:, :, :], writes=[f_tok])
            for k in range(16):
                S.op("dve", lambda e, k=k: e.tensor_scalar(f3[:], f2[:, k, :], P["gim"][:, k:k + 1], None, ALU.mult),
                     reads=[f_tok, p_tok], writes=[f_tok])
                S.op("dve", lambda e, k=k: e.scalar_tensor_tensor(out=cre[:, k, :], in0=f1[:, k, :], scalar=P["gre"][:, k:k + 1],
                                                                  in1=f3[:], op0=ALU.mult, op1=ALU.subtract),
                     reads=[f_tok, p_tok], writes=[bc_tok])
                S.op("dve", lambda e, k=k: e.tensor_scalar(f3[:], f2[:, k, :], P["gre"][:, k:k + 1], None, ALU.mult),
                     reads=[f_tok, p_tok, bc_tok], writes=[f_tok])
                S.op("dve", lambda e, k=k: e.scalar_tensor_tensor(out=ncim[:, k, :], in0=f1[:, k, :], scalar=P["ngim"][:, k:k + 1],
                                                                  in1=f3[:], op0=ALU.mult, op1=ALU.subtract),
                     reads=[f_tok, p_tok], writes=[bc_tok])
            f1v = f1[:].rearrange("p a b -> p (a b)").rearrange("p (k c) -> p k c", k=4)
            for half in range(2):
                S.dma("sp", chf, f1v, I["w_in"][:, 4 * half:4 * half + 4, 1536:2048], writes=[f_tok])
                for k4 in range(4):
                    kt = 4 * half + k4
                    S.op("dve" if k4 % 2 == 0 else "pool", lambda e, kt=kt, k4=k4: e.tensor_scalar(
                        wu[:, kt, :], f1v[:, k4, :], gmix_sb[:, kt:kt + 1], None, ALU.mult),
                        reads=[f_tok, gmix_tok], writes=[w_tok])
            for kt in range(4):
                S.dma("sp", chf, f1[:, 0:4, :], I["w_glu"][:, kt, :].rearrange("p (a b) -> p a b", a=4), writes=[f_tok])
                S.op("dve", lambda e, kt=kt: e.tensor_copy(out=wglu[:, kt, :].rearrange("p (a b) -> p a b", a=4), in_=f1[:, 0:4, :]),
                     reads=[f_tok], writes=[w_tok, f_tok])
            for ct in range(4):
                S.op("dve", lambda e, ct=ct: e.tensor_scalar(diagD[:, ct, :], C.ident_f[:], dsk[:, ct:ct + 1], None, ALU.mult),
                     reads=[small_tok, C.const_tok], writes=[w_tok])
            S.barrier()

        def scale_tables(sign_key):
            for k in range(16):
                S.op("act", lambda e, k=k: e.activation(out=magt[:], in_=tau1p[:], func=AF.Exp, scale=P[sign_key][:, k:k + 1]),
                     reads=[mag_tok, p_tok], writes=[mag_tok])
                S.op("dve", lambda e, k=k: e.tensor_tensor(out=cosT[:, k, :], in0=cosT[:, k, :], in1=magt[:], op=ALU.mult),
                     reads=[mag_tok, tab_tok], writes=[tab_tok])
                S.op("dve", lambda e, k=k: e.tensor_tensor(out=sinT[:, k, :], in0=sinT[:, k, :], in1=magt[:], op=ALU.mult),
                     reads=[mag_tok, tab_tok], writes=[tab_tok])

        scale_tables("adt")
        with ExitStack() as e3:
            xt = [_sb(nc, e3, "axt%d" % i, [128, D], F32) for i in range(2)]
            xt_tok = [Tok(), Tok()]
            xt_ch = [S.chan(), S.chan()]
            xn = [_sb(nc, e3, "axn%d" % i, [128, D], BF16) for i in range(2)]
            xn_tok = [Tok(), Tok()]
            ss = [_sb(nc, e3, "ass%d" % i, [128, 1], F32) for i in range(2)]
            ss_tok = [Tok(), Tok()]
            rstd = [_sb(nc, e3, "arstd%d" % i, [128, 1], F32) for i in range(2)]
            rstd_tok = [Tok(), Tok()]
            xnT = [_sb(nc, e3, "axnT%d" % i, [128, 8, SEGT], BF16) for i in range(2)]
            xnT_tok = [Tok(), Tok()]
            uT = [_sb(nc, e3, "auT%d" % i, [128, 4, SEGT], BF16) for i in range(2)]
            uT_tok = [Tok(), Tok()]
            acc4 = [_sb(nc, e3, "acc4_%d" % i, [128, 4, 16], F32) for i in range(2)]
            acc4_tok = [[[Tok() for _ in range(16)] for _ in range(4)] for _ in range(2)]
            junkS = [_sb(nc, e3, "junkS%d" % i, [128, SEGT], BF16) for i in range(4)]
            junkS_tok = [Tok() for _ in range(4)]
            jc = [0]
            sm_ = {nm: _sb(nc, e3, "sm_" + nm, [128, 16], F32) for nm in ("a", "b", "c", "d", "sr", "si")}
            sm_tok = Tok()
            t0r, t0i = cosT[:, :, 0], sinT[:, :, 0]
            Lr, Li = cosT[:, :, SEGT - 1], sinT[:, :, SEGT - 1]
            tt2 = lambda o, a, b, op: S.op("dve", lambda e: e.tensor_tensor(out=o, in0=a, in1=b, op=op),
                                           reads=[p_tok, tab_tok, sm_tok], writes=[p_tok])
            tt2(P["q1"][:], t0r, t0r, ALU.mult)
            tt2(P["q2"][:], t0i, t0i, ALU.mult)
            tt2(P["q1"][:], P["q1"][:], P["q2"][:], ALU.add)
            S.op("dve", lambda e: e.reciprocal(out=P["q1"][:], in_=P["q1"][:]), reads=[p_tok], writes=[p_tok])
            tt2(P["ilr"][:], t0r, P["q1"][:], ALU.mult)
            tt2(P["ili"][:], t0i, P["q1"][:], ALU.mult)
            S.op("dve", lambda e: e.tensor_scalar(P["ili"][:], P["ili"][:], -1.0, None, ALU.mult), reads=[p_tok], writes=[p_tok])
            ti = 0
            for sg in range(NSEG):
                xb = sg % 2
                for m in range(4):
                    a = ti % 2
                    ti += 1
                    r0 = sg * SEGT + m * 128
                    S.dma("sp", xt_ch[a], xt[a][:], I["x_rev"][r0:r0 + 128, :], writes=[xt_tok[a]])
                    rms_scale(C, xt[a][:], xt_tok[a], rstd[a][:], rstd_tok[a], xn[a][:], xn_tok[a], ss[a][:], ss_tok[a])
                    S.op("dve", lambda e, a=a: e.tensor_scalar(xn[a][:], xt[a][:], rstd[a][:], None, ALU.mult),
                         reads=[xt_tok[a], rstd_tok[a]], writes=[xn_tok[a]])
                    ptb, pt_tok = transpose_to(C, xn[a], xn_tok[a], None, None)
                    S.op("act", lambda e, xb=xb, m=m, ptb=ptb: e.copy(out=xnT[xb][:, :, m * 128:(m + 1) * 128],
                                                                      in_=ptb.rearrange("p (k t) -> p k t", k=8)),
                         reads=[pt_tok], writes=[xnT_tok[xb]])
                for ct in range(4):
                    pb = 4 + ct % 2
                    for kt in range(8):
                        S.op("pe", lambda e, kt=kt, ct=ct, pb=pb, xb=xb: e.matmul(
                            ps[pb][:], lhsT=wu[:, kt, ct * 128:(ct + 1) * 128], rhs=xnT[xb][:, kt, :],
                            start=(kt == 0), stop=(kt == 7)), reads=[w_tok, xnT_tok[xb]], writes=[ps_tok[pb]])
                    S.op("act", lambda e, ct=ct, pb=pb, xb=xb: e.copy(out=uT[xb][:, ct, :], in_=ps[pb][:]),
                         reads=[ps_tok[pb]], writes=[uT_tok[xb]])
                ab = sg % 2
                for k in range(16):
                    ct = k // 4
                    bur, bui = (0, 1) if k % 2 == 0 else (2, 3)
                    S.op("pe", lambda e, k=k, ct=ct, bur=bur: e.matmul(ps[bur][:], lhsT=bre[:, k, :], rhs=uT[xb][:, ct, :], start=True, stop=True),
                         reads=[bc_tok, uT_tok[xb]], writes=[ps_tok[bur]])
                    S.op("pe", lambda e, k=k, ct=ct, bui=bui: e.matmul(ps[bui][:], lhsT=bim[:, k, :], rhs=uT[xb][:, ct, :], start=True, stop=True),
                         reads=[bc_tok, uT_tok[xb]], writes=[ps_tok[bui]])
                    for j, (bk, tab) in enumerate(((bur, cosT), (bui, sinT), (bur, sinT), (bui, cosT))):
                        jb = jc[0] % 4
                        jc[0] += 1
                        S.op("dve", lambda e, j=j, bk=bk, tab=tab, k=k, jb=jb: e.scalar_tensor_tensor(
                            out=junkS[jb][:], in0=ps[bk][:], scalar=1.0, in1=tab[:, k, :], op0=ALU.mult, op1=ALU.mult,
                            accum_out=acc4[ab][:, j, k:k + 1]),
                            reads=[ps_tok[bk], tab_tok], writes=[junkS_tok[jb], acc4_tok[ab][j][k]])
                A_ = acc4[ab]
                rd = [t_ for row_ in acc4_tok[ab] for t_ in row_] + [p_tok, tab_tok, xst_tok, sm_tok]
                tt3 = lambda o, a_, b_, op: S.op("dve", lambda e: e.tensor_tensor(out=o, in0=a_, in1=b_, op=op), reads=rd, writes=[sm_tok])
                tt3(sm_["a"][:], A_[:, 0, :], A_[:, 1, :], ALU.subtract)
                tt3(sm_["b"][:], A_[:, 2, :], A_[:, 3, :], ALU.add)
                tt3(sm_["c"][:], P["ilr"][:], sm_["a"][:], ALU.mult)
                tt3(sm_["d"][:], P["ili"][:], sm_["b"][:], ALU.mult)
                tt3(sm_["sr"][:], sm_["c"][:], sm_["d"][:], ALU.subtract)
                tt3(sm_["c"][:], P["ilr"][:], sm_["b"][:], ALU.mult)
                tt3(sm_["d"][:], P["ili"][:], sm_["a"][:], ALU.mult)
                tt3(sm_["si"][:], sm_["c"][:], sm_["d"][:], ALU.add)
                Xr, Xi = Xst[:, 0, :, sg], Xst[:, 1, :, sg]
                tt3(sm_["a"][:], Lr, Xr, ALU.mult)
                tt3(sm_["b"][:], Li, Xi, ALU.mult)
                tt3(sm_["a"][:], sm_["a"][:], sm_["b"][:], ALU.subtract)
                S.op("dve", lambda e, sg=sg: e.tensor_tensor(out=Xst[:, 0, :, sg + 1], in0=sm_["a"][:], in1=sm_["sr"][:], op=ALU.add),
                     reads=[sm_tok], writes=[xst_tok])
                tt3(sm_["c"][:], Lr, Xi, ALU.mult)
                tt3(sm_["d"][:], Li, Xr, ALU.mult)
                tt3(sm_["c"][:], sm_["c"][:], sm_["d"][:], ALU.add)
                S.op("dve", lambda e, sg=sg: e.tensor_tensor(out=Xst[:, 1, :, sg + 1], in0=sm_["c"][:], in1=sm_["si"][:], op=ALU.add),
                     reads=[sm_tok], writes=[xst_tok])
            S.barrier()
        scale_tables("nadt")
        S.barrier()
        with ExitStack() as e3:
            xt = [_sb(nc, e3, "sxt%d" % i, [128, D], F32) for i in range(2)]
            xt_tok = [Tok(), Tok()]
            xt_ch = [S.chan(), S.chan()]
            xn = [_sb(nc, e3, "sxn%d" % i, [128, D], BF16) for i in range(2)]
            xn_tok = [Tok(), Tok()]
            ss = [_sb(nc, e3, "sss%d" % i, [128, 1], F32) for i in range(2)]
            ss_tok = [Tok(), Tok()]
            rstd = [_sb(nc, e3, "srstd%d" % i, [128, 1], F32) for i in range(2)]
            rstd_tok = [Tok(), Tok()]
            xnT = [_sb(nc, e3, "sxnT", [128, 8, SEGT], BF16)] * 2
            xnT_tok = [Tok()] * 2
            uT = [_sb(nc, e3, "uT%d" % i, [128, 4, SEGT], BF16) for i in range(2)]
            uT_tok = [Tok(), Tok()]
            tmp4 = [_sb(nc, e3, "tm%d" % i, [128, SEGT], F32) for i in range(4)]
            tmp4_tok = [Tok() for _ in range(4)]
            Wr = [_sb(nc, e3, "Wr%d" % i, [128, SEGT], F32) for i in range(2)]
            Wi = [_sb(nc, e3, "Wi%d" % i, [128, SEGT], F32) for i in range(2)]
            Vr = [_sb(nc, e3, "Vr%d" % i, [128, SEGT], F32) for i in range(2)]
            Vi = [_sb(nc, e3, "Vi%d" % i, [128, SEGT], F32) for i in range(2)]
            W_tok = [Tok(), Tok()]
            V_tok = [Tok(), Tok()]
            Xb = [_sb(nc, e3, "Xb%d" % i, [128, 2, SEGT], BF16) for i in range(2)]
            Xb_tok = [Tok(), Tok()]
            xin = _sb(nc, e3, "xin", [128, 32], F32)
            xin_tok = Tok()
            selt = _sb(nc, e3, "selt", [128, 32, 16], F32)
            rot = _sb(nc, e3, "rot", [128, 4], F32)
            rot_tok = Tok()
            yb = _sb(nc, e3, "yb", [128, 4, SEGT], BF16)
            y_tok = [Tok() for _ in range(4)]
            g1 = _sb(nc, e3, "g1", [128, SEGT], F32)
            g2 = _sb(nc, e3, "g2", [128, SEGT], F32)
            g_tok = Tok()
            ti = 0
            pcount = 0
            for sgi in range(NSEG, NSEG + NOWN):
                own = sgi >= NSEG
                sg = sgi - NSEG if own else sgi
                src = I["x_own"] if own else I["x_full"]
                xb = sgi % 2
                for m in range(4):
                    a = ti % 2
                    ti += 1
                    r0 = sg * SEGT + m * 128
                    S.dma("sp", xt_ch[a], xt[a][:], src[r0:r0 + 128, :], writes=[xt_tok[a]])
                    rms_scale(C, xt[a][:], xt_tok[a], rstd[a][:], rstd_tok[a], xn[a][:], xn_tok[a], ss[a][:], ss_tok[a])
                    S.op("dve", lambda e, a=a: e.tensor_scalar(xn[a][:], xt[a][:], rstd[a][:], None, ALU.mult),
                         reads=[xt_tok[a], rstd_tok[a]], writes=[xn_tok[a]])
                    ptb, pt_tok = transpose_to(C, xn[a], xn_tok[a], None, None)
                    S.op("act", lambda e, xb=xb, m=m, ptb=ptb: e.copy(out=xnT[xb][:, :, m * 128:(m + 1) * 128],
                                                                      in_=ptb.rearrange("p (k t) -> p k t", k=8)),
                         reads=[pt_tok], writes=[xnT_tok[xb]])
                for ct in range(4):
                    pb = ct % 2
                    for kt in range(8):
                        S.op("pe", lambda e, kt=kt, ct=ct, pb=pb, xb=xb: e.matmul(
                            ps[pb][:], lhsT=wu[:, kt, ct * 128:(ct + 1) * 128], rhs=xnT[xb][:, kt, :],
                            start=(kt == 0), stop=(kt == 7)), reads=[w_tok, xnT_tok[xb]], writes=[ps_tok[pb]])
                    S.op("act", lambda e, ct=ct, pb=pb, xb=xb: e.copy(out=uT[xb][:, ct, :], in_=ps[pb][:]),
                         reads=[ps_tok[pb]], writes=[uT_tok[xb]])
                if own:
                    S.op("dve", lambda e, sg=sg: e.tensor_tensor(out=selt[:], in0=Xst[:].rearrange("p r k s -> p (r k) s")[:, :, 0:16],
                                                                in1=segsel[:, sg:sg + 1, :].to_broadcast([128, 32, 16]), op=ALU.mult),
                         reads=[xst_tok, small_tok], writes=[xin_tok])
                    S.op("dve", lambda e: e.tensor_reduce(out=xin[:], in_=selt[:], axis=AX.X, op=ALU.add),
                         reads=[xin_tok], writes=[xin_tok])
                for ct in range(4):
                    yps = 4 + ct % 2
                    for kk in range(4):
                        k = ct * 4 + kk
                        pp = pcount % 2
                        pcount += 1
                        bur, bui = 2 * pp, 2 * pp + 1
                        bur, bui = (2, 3) if pp == 0 else (0, 1)
                        S.op("pe", lambda e: e.matmul(ps[bur][:], lhsT=bre[:, k, :], rhs=uT[xb][:, ct, :], start=True, stop=True),
                             reads=[bc_tok, uT_tok[xb]], writes=[ps_tok[bur]])
                        S.op("pe", lambda e: e.matmul(ps[bui][:], lhsT=bim[:, k, :], rhs=uT[xb][:, ct, :], start=True, stop=True),
                             reads=[bc_tok, uT_tok[xb]], writes=[ps_tok[bui]])
                        ck, sk = cosT[:, k, :], sinT[:, k, :]
                        S.op("dve", lambda e: e.tensor_tensor(out=tmp4[0][:], in0=ps[bur][:], in1=ck, op=ALU.mult),
                             reads=[ps_tok[bur], tab_tok], writes=[tmp4_tok[0]])
                        S.op("dve", lambda e: e.tensor_tensor(out=tmp4[1][:], in0=ps[bui][:], in1=sk, op=ALU.mult),
                             reads=[ps_tok[bui], tab_tok], writes=[tmp4_tok[1]])
                        S.op("dve", lambda e: e.tensor_tensor(out=Wr[pp][:], in0=tmp4[0][:], in1=tmp4[1][:], op=ALU.add),
                             reads=[tmp4_tok[0], tmp4_tok[1]], writes=[W_tok[pp]])
                        S.op("dve", lambda e: e.tensor_tensor(out=tmp4[2][:], in0=ps[bui][:], in1=ck, op=ALU.mult),
                             reads=[ps_tok[bui], tab_tok], writes=[tmp4_tok[2]])
                        S.op("dve", lambda e: e.tensor_tensor(out=tmp4[3][:], in0=ps[bur][:], in1=sk, op=ALU.mult),
                             reads=[ps_tok[bur], tab_tok], writes=[tmp4_tok[3]])
                        S.op("dve", lambda e: e.tensor_tensor(out=Wi[pp][:], in0=tmp4[2][:], in1=tmp4[3][:], op=ALU.subtract),
                             reads=[tmp4_tok[2], tmp4_tok[3]], writes=[W_tok[pp]])
                        if own:
                            ir, ii = xin[:, k:k + 1], xin[:, 16 + k:16 + k + 1]
                            itoks = [xin_tok]
                        else:
                            ir, ii = Xst[:, 2 * k, sg:sg + 1], Xst[:, 2 * k + 1, sg:sg + 1]
                            itoks = [xst_tok]
                        rb = P["rho"][:, k:k + 1].to_broadcast([128, SEGT])
                        S.op("dve", lambda e: e.tensor_tensor_scan(out=Vr[pp][:], data0=rb, data1=Wr[pp][:], initial=ir,
                                                                   op0=ALU.mult, op1=ALU.add),
                             reads=[W_tok[pp], p_tok] + itoks, writes=[V_tok[pp]])
                        S.op("dve", lambda e: e.tensor_tensor_scan(out=Vi[pp][:], data0=rb, data1=Wi[pp][:], initial=ii,
                                                                   op0=ALU.mult, op1=ALU.add),
                             reads=[W_tok[pp], p_tok] + itoks, writes=[V_tok[pp]])
                        if not own:
                            cl, sl_ = cosT[:, k, SEGT - 1:SEGT], sinT[:, k, SEGT - 1:SEGT]
                            vr, vi = Vr[pp][:, SEGT - 1:SEGT], Vi[pp][:, SEGT - 1:SEGT]
                            S.op("dve", lambda e: e.tensor_tensor(out=rot[:, 0:1], in0=vi, in1=sl_, op=ALU.mult),
                                 reads=[V_tok[pp], tab_tok], writes=[rot_tok])
                            S.op("dve", lambda e: e.scalar_tensor_tensor(out=Xst[:, 2 * k, sg + 1:sg + 2], in0=vr, scalar=cl, in1=rot[:, 0:1],
                                                                         op0=ALU.mult, op1=ALU.subtract),
                                 reads=[V_tok[pp], tab_tok, rot_tok], writes=[xst_tok])
                            S.op("dve", lambda e: e.tensor_tensor(out=rot[:, 1:2], in0=vr, in1=sl_, op=ALU.mult),
                                 reads=[V_tok[pp], tab_tok], writes=[rot_tok])
                            S.op("dve", lambda e: e.scalar_tensor_tensor(out=Xst[:, 2 * k + 1, sg + 1:sg + 2], in0=vi, scalar=cl, in1=rot[:, 1:2],
                                                                         op0=ALU.mult, op1=ALU.add),
                                 reads=[V_tok[pp], tab_tok, rot_tok], writes=[xst_tok])
                        else:
                            S.op("dve", lambda e: e.tensor_tensor(out=tmp4[0][:], in0=Vr[pp][:], in1=ck, op=ALU.mult),
                                 reads=[V_tok[pp], tab_tok], writes=[tmp4_tok[0]])
                            S.op("dve", lambda e: e.tensor_tensor(out=tmp4[1][:], in0=Vi[pp][:], in1=sk, op=ALU.mult),
                                 reads=[V_tok[pp], tab_tok], writes=[tmp4_tok[1]])
                            S.op("dve", lambda e: e.tensor_tensor(out=Xb[pp][:, 0, :], in0=tmp4[0][:], in1=tmp4[1][:], op=ALU.subtract),
                                 reads=[tmp4_tok[0], tmp4_tok[1]], writes=[Xb_tok[pp]])
                            S.op("dve", lambda e: e.tensor_tensor(out=tmp4[2][:], in0=Vr[pp][:], in1=sk, op=ALU.mult),
                                 reads=[V_tok[pp], tab_tok], writes=[tmp4_tok[2]])
                            S.op("dve", lambda e: e.tensor_tensor(out=tmp4[3][:], in0=Vi[pp][:], in1=ck, op=ALU.mult),
                                 reads=[V_tok[pp], tab_tok], writes=[tmp4_tok[3]])
                            S.op("dve", lambda e: e.tensor_tensor(out=Xb[pp][:, 1, :], in0=tmp4[2][:], in1=tmp4[3][:], op=ALU.add),
                                 reads=[tmp4_tok[2], tmp4_tok[3]], writes=[Xb_tok[pp]])
                            S.op("pe", lambda e: e.matmul(ps[yps][:], lhsT=cre[:, k, :], rhs=Xb[pp][:, 0, :], start=(kk == 0), stop=False),
                                 reads=[bc_tok, Xb_tok[pp]], writes=[ps_tok[yps]])
                            S.op("pe", lambda e: e.matmul(ps[yps][:], lhsT=ncim[:, k, :], rhs=Xb[pp][:, 1, :], start=False, stop=False),
                                 reads=[bc_tok, Xb_tok[pp]], writes=[ps_tok[yps]])
                    if own:
                        S.op("pe", lambda e: e.matmul(ps[yps][:], lhsT=diagD[:, ct, :], rhs=uT[xb][:, ct, :], start=False, stop=True),
                             reads=[w_tok, uT_tok[xb]], writes=[ps_tok[yps]])
                        S.op("act", lambda e: e.activation(out=g1[:], in_=ps[yps][:], func=AF.Square), reads=[ps_tok[yps]], writes=[g_tok])
                        S.op("dve", lambda e: e.tensor_scalar(g1[:], g1[:], 0.044715, 1.0, ALU.mult, ALU.add), reads=[g_tok], writes=[g_tok])
                        S.op("dve", lambda e: e.tensor_tensor(out=g1[:], in0=ps[yps][:], in1=g1[:], op=ALU.mult),
                             reads=[g_tok, ps_tok[yps]], writes=[g_tok])
                        S.op("act", lambda e: e.activation(out=g2[:], in_=g1[:], func=AF.Sigmoid, scale=1.5957691216057308),
                             reads=[g_tok], writes=[g_tok])
                        S.op("dve", lambda e, ct=ct: e.tensor_tensor(out=yb[:, ct, :], in0=ps[yps][:], in1=g2[:], op=ALU.mult),
                             reads=[g_tok, ps_tok[yps]], writes=[y_tok[ct]])
                if own:
                    for c2 in range(4):
                        gp = 4 + c2 % 2
                        for kt in range(4):
                            S.op("pe", lambda e, kt=kt, c2=c2, gp=gp: e.matmul(ps[gp][:], lhsT=wglu[:, kt, c2 * 128:(c2 + 1) * 128],
                                                                              rhs=yb[:, kt, :], start=(kt == 0), stop=(kt == 3)),
                                 reads=[w_tok] + y_tok, writes=[ps_tok[gp]])
                        S.op("act", lambda e, c2=c2, gp=gp: e.activation(out=g2[:], in_=ps[gp][:], func=AF.Sigmoid, bias=bglu[:, c2:c2 + 1]),
                             reads=[ps_tok[gp], small_tok], writes=[g_tok])
                        S.op("dve", lambda e, c2=c2, sg=sg: e.tensor_tensor(out=ssmT[:, c2, sg * SEGT:(sg + 1) * SEGT], in0=yb[:, c2, :],
                                                                           in1=g2[:], op=ALU.mult),
                             reads=[g_tok, y_tok[c2]], writes=[ssm_tok[sg]])
            S.barrier()
        S.barrier()


def _rms_rstd(C, S, src_ap, src_tok, junk_ap, junk_tok, ss, ss_tok, rstd, rstd_tok, n=D):
    S.op("act", lambda e: e.activation(out=junk_ap, in_=src_ap, func=AF.Square, accum_out=ss[:]),
         reads=[src_tok], writes=[junk_tok, ss_tok])
    S.op("act", lambda e: e.activation(out=rstd[:], in_=ss[:], func=AF.Ln, bias=C.eps_col[:], scale=1.0 / n),
         reads=[ss_tok, C.const_tok], writes=[rstd_tok])
    S.op("act", lambda e: e.activation(out=rstd[:], in_=rstd[:], func=AF.Exp, scale=-0.5),
         reads=[rstd_tok], writes=[rstd_tok])


def _load_cast(S, nc, dst, src, gcol, gtok, stg, stg_tok, stg_ch, dst_tok, nkt, ncols, cnt):
    for kt in range(nkt):
        for c0 in range(0, ncols, 512):
            sl = cnt[0] % 2
            cnt[0] += 1
            w = min(512, ncols - c0)
            S.dma("sp", stg_ch[sl], stg[sl][:, 0:w], src[:, kt, c0:c0 + w], writes=[stg_tok[sl]])
            eng = "dve" if sl == 0 else "pool"
            if gcol is not None:
                S.op(eng, lambda e, kt=kt, c0=c0, w=w, sl=sl: e.tensor_scalar(dst[:, kt, c0:c0 + w], stg[sl][:, 0:w],
                                                                            gcol[:, kt:kt + 1], None, ALU.mult),
                     reads=[stg_tok[sl], gtok], writes=[dst_tok])
            else:
                S.op(eng, lambda e, kt=kt, c0=c0, w=w, sl=sl: e.tensor_copy(out=dst[:, kt, c0:c0 + w], in_=stg[sl][:, 0:w]),
                     reads=[stg_tok[sl]], writes=[dst_tok])


def phase_post(C, nc, S, ps, ps_tok, I, attnT, attn_tok, ssmT, ssm_tok, y, dbg):
    NT = NOWN * 4
    h2buf = nc.dram_tensor("h2buf", [NOWN * SEGT, D], F32, kind="Internal").ap()
    h2_tok = [Tok() for _ in range(NT)]
    alla = [t for row in attn_tok for t in row]
    bank = [0]

    def nbk():
        b = bank[0] % 6
        bank[0] += 1
        return b

    eo = ExitStack()
    if dbg not in ("h1", "h2"):
        wpq = _sb(nc, eo, "wpq", [128, 8, 2048], BF16)
        skT = _sb(nc, eo, "skT_sb", [128, 16, 128], BF16)
        pw_tok = Tok()
        gffn = _sb(nc, eo, "gffn", [128, 8], F32)
        gffn_rep = _sb(nc, eo, "gffn_rep_sb", [128, D], F32)
        gfin_rep = _sb(nc, eo, "gfin_rep_sb", [128, D], F32)
        iota256 = _sb(nc, eo, "iota256", [128, 256], F32)
        pg_tok = Tok()
        pchg = S.chan()
        S.dma("sp", pchg, gffn[:], I["g_ffn"][:, :], writes=[pg_tok])
        S.dma("sp", pchg, gffn_rep[:], I["gffn_rep"][:, :], writes=[pg_tok])
        S.dma("sp", pchg, gfin_rep[:], I["gfin_rep"][:, :], writes=[pg_tok])
        S.op("pool", lambda e: e.iota(iota256[:], pattern=[[1, 256]], base=0, channel_multiplier=0,
                                      allow_small_or_imprecise_dtypes=True), writes=[pg_tok])
        pstg = [_sb(nc, eo, "qstg%d" % i, [128, 512], F32) for i in range(2)]
        pstg_tok = [Tok(), Tok()]
        pstg_ch = [S.chan(), S.chan()]
        pcnt = [0]
        _load_cast(S, nc, wpq, I["w_pq"], gffn, pg_tok, pstg, pstg_tok, pstg_ch, pw_tok, 8, 2048, pcnt)
        _load_cast(S, nc, skT, I["skT"], None, None, pstg, pstg_tok, pstg_ch, pw_tok, 16, 128, pcnt)

    uvbf = nc.dram_tensor("uvbf", [16384, 2048], BF16, kind="Internal").ap()
    uv_toks = [Tok() for _ in range(32)]
    if dbg not in ("h1", "h2"):
        with ExitStack() as e2:
            cin = [_sb(nc, e2, "cin%d" % i, [128, 8, D], F32) for i in range(2)]
            cin_tok = [Tok(), Tok()]
            cin_ch = [S.chan(), S.chan()]
            cout = [_sb(nc, e2, "cout%d" % i, [128, 8, D], BF16) for i in range(2)]
            cout_tok = [[Tok() for _ in range(8)] for _ in range(2)]
            cout_ch = [S.chan(), S.chan()]
            ci = 0
            engs = ("act", "dve", "pool", "dve", "act", "dve", "act", "dve")
            for tbl, src in enumerate((I["peer_u"], I["peer_v"])):
                for chk in range(16):
                    sl = ci % 2
                    r0 = chk * 1024
                    S.dma("sp", cin_ch[sl], cin[sl][:], src[r0:r0 + 1024, :].rearrange("(p a) d -> p a d", a=8),
                          writes=[cin_tok[sl]])
                    for a in range(8):
                        if engs[a] == "act":
                            S.op("act", lambda e, a=a, sl=sl: e.copy(out=cout[sl][:, a, :], in_=cin[sl][:, a, :]),
                                 reads=[cin_tok[sl]], writes=[cout_tok[sl][a]])
                        else:
                            S.op(engs[a], lambda e, a=a, sl=sl: e.tensor_copy(out=cout[sl][:, a, :], in_=cin[sl][:, a, :]),
                                 reads=[cin_tok[sl]], writes=[cout_tok[sl][a]])
                    S.dma("sp", cout_ch[sl],
                          uvbf[r0:r0 + 1024, tbl * 1024:(tbl + 1) * 1024].rearrange("(p a) d -> p a d", a=8), cout[sl][:],
                          reads=cout_tok[sl], writes=[uv_toks[ci]])
                    ci += 1
            S.barrier()

    with ExitStack() as e2:
        wo = _sb(nc, e2, "wo", [128, 8, D], BF16)
        wxq = _sb(nc, e2, "wxq", [128, 8, D], BF16)
        wxo = _sb(nc, e2, "wxo", [128, 8, D], BF16)
        KmT = _sb(nc, e2, "KmT", [128, 8, 256], BF16)
        Vm = _sb(nc, e2, "Vm", [128, 2, D], BF16)
        w_tok = Tok()
        kv_tok = Tok()
        gcols = _sb(nc, e2, "gcols", [128, 3, 8], F32)
        g_tok = Tok()
        chg = S.chan()
        S.dma("sp", chg, gcols[:, 0, :], I["g_out"][:, :], writes=[g_tok])
        S.dma("sp", chg, gcols[:, 1, :], I["g_xattn"][:, :], writes=[g_tok])
        S.dma("sp", chg, gcols[:, 2, :], I["g_mem"][:, :], writes=[g_tok])
        stg = [_sb(nc, e2, "pstg%d" % i, [128, 512], F32) for i in range(2)]
        stg_tok = [Tok(), Tok()]
        stg_ch = [S.chan(), S.chan()]
        cnt = [0]
        wo_tok, wxq_tok, wxo_tok = Tok(), Tok(), Tok()
        _load_cast(S, nc, wo, I["w_out"], gcols[:, 0, :], g_tok, stg, stg_tok, stg_ch, wo_tok, 8, D, cnt)
        _load_cast(S, nc, wxq, I["w_xq"], gcols[:, 1, :], g_tok, stg, stg_tok, stg_ch, wxq_tok, 8, D, cnt)
        _load_cast(S, nc, wxo, I["w_xo"], None, None, stg, stg_tok, stg_ch, wxo_tok, 8, D, cnt)
        h = _sb(nc, e2, "h", [128, D], F32)
        h_tok = Tok()
        h_ch = S.chan()
        hn = _sb(nc, e2, "hn", [128, D], BF16)
        hn_tok = Tok()
        hnT = _sb(nc, e2, "hnT", [128, 8, 128], BF16)
        hnT_tok = Tok()
        ss = _sb(nc, e2, "pss", [128, 1], F32)
        ss_tok = Tok()
        rstd = _sb(nc, e2, "prstd", [128, 1], F32)
        rstd_tok = Tok()
        with ExitStack() as e3:
            memT = _sb(nc, e3, "memT", [128, 8, 256], BF16)
            memT_tok = Tok()
            wch = _sb(nc, e3, "wch", [128, 8, 512], BF16)
            wch_tok = Tok()
            for mt in range(2):
                S.dma("sp", h_ch, h[:], I["mem"][mt * 128:(mt + 1) * 128, :], writes=[h_tok])
                _rms_rstd(C, S, h[:], h_tok, hn[:], hn_tok, ss, ss_tok, rstd, rstd_tok)
                S.op("dve", lambda e: e.tensor_scalar(hn[:], h[:], rstd[:], None, ALU.mult), reads=[h_tok, rstd_tok], writes=[hn_tok])
                ptb, pt_tok = transpose_to(C, hn, hn_tok, None, None)
                S.op("act", lambda e, mt=mt, ptb=ptb: e.copy(out=memT[:, :, mt * 128:(mt + 1) * 128],
                                                           in_=ptb.rearrange("p (k t) -> p k t", k=8)),
                     reads=[pt_tok], writes=[memT_tok])
            for cc in range(4):
                _load_cast(S, nc, wch, I["w_xkv"][:, :, cc * 512:(cc + 1) * 512], gcols[:, 2, :], g_tok, stg, stg_tok, stg_ch,
                           wch_tok, 8, 512, cnt)
                if cc < 2:
                    for j4 in range(4):
                        b_ = nbk()
                        for kt in range(8):
                            S.op("pe", lambda e, kt=kt, j4=j4, b_=b_: e.matmul(ps[b_][:, 0:256], lhsT=wch[:, kt, j4 * 128:(j4 + 1) * 128],
                                                                             rhs=memT[:, kt, :], start=(kt == 0), stop=(kt == 7)),
                                 reads=[wch_tok, memT_tok], writes=[ps_tok[b_]])
                        S.op("act", lambda e, j4=j4, b_=b_, cc=cc: e.copy(out=KmT[:, cc * 4 + j4, :], in_=ps[b_][:, 0:256]),
                             reads=[ps_tok[b_]], writes=[kv_tok])
                else:
                    for mt in range(2):
                        b_ = nbk()
                        for kt in range(8):
                            S.op("pe", lambda e, kt=kt, mt=mt, b_=b_: e.matmul(ps[b_][:], lhsT=memT[:, kt, mt * 128:(mt + 1) * 128],
                                                                             rhs=wch[:, kt, :], start=(kt == 0), stop=(kt == 7)),
                                 reads=[wch_tok, memT_tok], writes=[ps_tok[b_]])
                        S.op("act", lambda e, mt=mt, b_=b_, cc=cc: e.copy(out=Vm[:, mt, (cc - 2) * 512:(cc - 1) * 512], in_=ps[b_][:]),
                             reads=[ps_tok[b_]], writes=[kv_tok])
            S.barrier()
        def mkset(tag):
            B = {}
            for nm, shp, dt_ in (("sq4", [128, 4, 128], BF16), ("rs2", [128, 2], F32), ("qT", [128, 8, 128], BF16),
                                 ("pp", [128, 4, 256], BF16), ("pT", [128, 8, 128], BF16), ("oT", [128, 8, 128], BF16),
                                 ("mx", [128, 4], F32), ("sm", [128, 4], F32), ("h", [128, D], F32), ("hn", [128, D], BF16),
                                 ("hnT", [128, 8, 128], BF16), ("ss", [128, 1], F32), ("rstd", [128, 1], F32)):
                B[nm] = _sb(nc, e2, nm + tag, shp, dt_)
            for nm in ("sq_tok", "rs2_tok", "qT_tok", "pp_tok", "pT_tok", "oT_tok", "sm_tok", "h_tok", "hn_tok", "hnT_tok",
                       "ss_tok", "rstd_tok"):
                B[nm] = Tok()
            B["h_ch"] = S.chan()
            return B

        class _Rec1:
            def __init__(self):
                self.items = []

            def op(self, *a, **k):
                self.items.append(("op", a, k))

            def dma(self, *a, **k):
                self.items.append(("dma", a, k))

        def emit_tile(S_, tile, nbk_, sq4, rs2, qT, pp, pT, oT, mx, sm, h, hn, hnT, ss, rstd, sq_tok, rs2_tok, qT_tok, pp_tok,
                      pT_tok, oT_tok, sm_tok, h_tok, hn_tok, hnT_tok, ss_tok, rstd_tok, h_ch):
            saved = C.S
            C.S = S_
            t0 = tile * 128
            slot = tile // 4
            tsl = slice(t0, t0 + 128)
            for which, (src, toks) in enumerate(((attnT, attn_tok[slot]), (ssmT, [ssm_tok[slot]]))):
                S_.op("act", lambda e, src=src: e.activation(out=sq4[:], in_=src[:, :, tsl], func=AF.Square),
                     reads=toks, writes=[sq_tok])
                b_ = nbk_()
                for hp in range(4):
                    S_.op("pe", lambda e, hp=hp, b_=b_: e.matmul(ps[b_][:, 0:1], lhsT=sq4[:, hp, :], rhs=C.ones_bf[:, 0:1],
                                                                start=(hp == 0), stop=(hp == 3)),
                         reads=[sq_tok, C.const_tok], writes=[ps_tok[b_]])
                S_.op("act", lambda e, which=which, b_=b_: e.activation(out=rs2[:, which:which + 1], in_=ps[b_][:, 0:1], func=AF.Ln,
                                                                       bias=C.eps_col[:], scale=1.0 / 512),
                     reads=[ps_tok[b_], C.const_tok], writes=[rs2_tok])
            S_.op("act", lambda e: e.activation(out=rs2[:], in_=rs2[:], func=AF.Exp, scale=-0.5), reads=[rs2_tok], writes=[rs2_tok])
            S_.dma("sp", h_ch, h[:], I["x_own"][t0:t0 + 128, :], writes=[h_tok])
            for which, (src, toks) in enumerate(((attnT, attn_tok[slot]), (ssmT, [ssm_tok[slot]]))):
                for n2 in range(2):
                    b_ = nbk_()
                    for hp in range(4):
                        S_.op("pe", lambda e, hp=hp, b_=b_, src=src, which=which, n2=n2: e.matmul(
                            ps[b_][:], lhsT=src[:, hp, tsl], rhs=wo[:, which * 4 + hp, n2 * 512:(n2 + 1) * 512],
                            start=(hp == 0), stop=(hp == 3)), reads=toks + [wo_tok], writes=[ps_tok[b_]])
                    S_.op("dve", lambda e, b_=b_, which=which, n2=n2: e.scalar_tensor_tensor(
                        out=h[:, n2 * 512:(n2 + 1) * 512], in0=ps[b_][:], scalar=rs2[:, which:which + 1],
                        in1=h[:, n2 * 512:(n2 + 1) * 512], op0=ALU.mult, op1=ALU.add),
                        reads=[ps_tok[b_], rs2_tok], writes=[h_tok])
            if dbg == "h1":
                S_.dma("sp", h_ch, y[t0:t0 + 128, :], h[:], reads=[h_tok], writes=[h2_tok[tile]])
                C.S = saved
                return
            _rms_rstd(C, S_, h[:], h_tok, hn[:], hn_tok, ss, ss_tok, rstd, rstd_tok)
            S_.op("dve", lambda e: e.tensor_scalar(hn[:], h[:], rstd[:], None, ALU.mult), reads=[h_tok, rstd_tok], writes=[hn_tok])
            ptb, pt_tok = transpose_to(C, hn, hn_tok, None, None)
            S_.op("act", lambda e, ptb=ptb: e.copy(out=hnT[:], in_=ptb.rearrange("p (k t) -> p k t", k=8)),
                 reads=[pt_tok], writes=[hnT_tok])
            for half in range(2):
                b_ = nbk_()
                for j4 in range(4):
                    hc = half * 4 + j4
                    for kt in range(8):
                        S_.op("pe", lambda e, kt=kt, hc=hc, j4=j4, b_=b_: e.matmul(
                            ps[b_][:, j4 * 128:(j4 + 1) * 128], lhsT=wxq[:, kt, hc * 128:(hc + 1) * 128], rhs=hnT[:, kt, :],
                            start=(kt == 0), stop=(kt == 7)), reads=[wxq_tok, hnT_tok], writes=[ps_tok[b_]])
                S_.op("act", lambda e, half=half, b_=b_: e.mul(out=qT[:, half * 4:(half + 1) * 4, :],
                                                             in_=ps[b_][:].rearrange("p (a b) -> p a b", a=4), mul=0.0625),
                     reads=[ps_tok[b_]], writes=[qT_tok])
            sb_ = [nbk_(), nbk_()]
            for hh in range(4):
                b_ = sb_[hh // 2]
                for c2 in range(2):
                    S_.op("pe", lambda e, hh=hh, c2=c2, b_=b_: e.matmul(
                        ps[b_][:, (hh % 2) * 256:(hh % 2 + 1) * 256], lhsT=qT[:, 2 * hh + c2, :], rhs=KmT[:, 2 * hh + c2, :],
                        start=(c2 == 0), stop=(c2 == 1)), reads=[qT_tok, kv_tok], writes=[ps_tok[b_]])
            for i2 in range(2):
                b_ = sb_[i2]
                S_.op("dve", lambda e, i2=i2, b_=b_: e.tensor_reduce(out=mx[:, 2 * i2:2 * i2 + 2],
                                                                    in_=ps[b_][:].rearrange("p (a b) -> p a b", a=2),
                                                                    axis=AX.X, op=ALU.max),
                     reads=[ps_tok[b_]], writes=[sm_tok])
            S_.op("dve", lambda e: e.tensor_scalar(mx[:], mx[:], -1.0, None, ALU.mult), reads=[sm_tok], writes=[sm_tok])
            for hh in range(4):
                b_ = sb_[hh // 2]
                S_.op("act", lambda e, hh=hh, b_=b_: e.activation(out=pp[:, hh, :], in_=ps[b_][:, (hh % 2) * 256:(hh % 2 + 1) * 256],
                                                                 func=AF.Exp, bias=mx[:, hh:hh + 1], accum_out=sm[:, hh:hh + 1]),
                     reads=[ps_tok[b_], sm_tok], writes=[pp_tok, sm_tok])
            S_.op("dve", lambda e: e.reciprocal(out=sm[:], in_=sm[:]), reads=[sm_tok], writes=[sm_tok])
            for hh in range(4):
                S_.op("dve", lambda e, hh=hh: e.tensor_scalar(pp[:, hh, :], pp[:, hh, :], sm[:, hh:hh + 1], None, ALU.mult),
                     reads=[sm_tok, pp_tok], writes=[pp_tok])
            ptb, pt_tok = transpose_to(C, pp[:].rearrange("p a b -> p (a b)"), pp_tok, None, None)
            S_.op("act", lambda e, ptb=ptb: e.copy(out=pT[:], in_=ptb.rearrange("p (k t) -> p k t", k=8)),
                 reads=[pt_tok], writes=[pT_tok])
            for half in range(2):
                b_ = nbk_()
                for j4 in range(4):
                    hc = half * 4 + j4
                    hh, c2 = hc // 2, hc % 2
                    for mt in range(2):
                        S_.op("pe", lambda e, mt=mt, hh=hh, c2=c2, j4=j4, b_=b_: e.matmul(
                            ps[b_][:, j4 * 128:(j4 + 1) * 128], lhsT=Vm[:, mt, hh * 256 + c2 * 128:hh * 256 + (c2 + 1) * 128],
                            rhs=pT[:, 2 * hh + mt, :], start=(mt == 0), stop=(mt == 1)),
                            reads=[kv_tok, pT_tok], writes=[ps_tok[b_]])
                S_.op("act", lambda e, half=half, b_=b_: e.copy(out=oT[:, half * 4:(half + 1) * 4, :],
                                                              in_=ps[b_][:].rearrange("p (a b) -> p a b", a=4)),
                     reads=[ps_tok[b_]], writes=[oT_tok])
            for n2 in range(2):
                b_ = nbk_()
                for hc in range(8):
                    S_.op("pe", lambda e, hc=hc, n2=n2, b_=b_: e.matmul(ps[b_][:], lhsT=oT[:, hc, :], rhs=wxo[:, hc, n2 * 512:(n2 + 1) * 512],
                                                                      start=(hc == 0), stop=(hc == 7)),
                         reads=[oT_tok, wxo_tok], writes=[ps_tok[b_]])
                S_.op("dve", lambda e, n2=n2, b_=b_: e.tensor_tensor(out=h[:, n2 * 512:(n2 + 1) * 512], in0=ps[b_][:],
                                                                    in1=h[:, n2 * 512:(n2 + 1) * 512], op=ALU.add),
                     reads=[ps_tok[b_]], writes=[h_tok])
            dst = y if dbg == "h2" else h2buf
            S_.dma("sp", h_ch, dst[t0:t0 + 128, :], h[:], reads=[h_tok], writes=[h2_tok[tile]])

            C.S = saved

        sets1 = [mkset("_e"), mkset("_o")]
        bk = [[0], [0]]

        def mk_nbk(par):
            def f():
                b = 3 * par + bk[par][0] % 3
                bk[par][0] += 1
                return b
            return f

        for t2 in range(0, NT, 2):
            recs = []
            for par in range(2):
                r_ = _Rec1()
                C.tp_force = par
                emit_tile(r_, t2 + par, mk_nbk(par), **sets1[par])
                C.tp_force = None
                recs.append(r_.items)
            while recs[0] or recs[1]:
                for par in range(2):
                    for _ in range(6):
                        if recs[par]:
                            kind, a, k = recs[par].pop(0)
                            getattr(S, kind)(*a, **k)
        S.barrier()
    if dbg in ("h1", "h2"):
        S.wait_tok("sp", h2_tok)
        eo.close()
        return

    with ExitStack() as e2:
        h = _sb(nc, e2, "h_b", [128, D], F32)
        h_tok = Tok()
        h_ch = S.chan()
        hn = _sb(nc, e2, "hn_b", [128, D], BF16)
        hn_tok = Tok()
        hnT = _sb(nc, e2, "hnT_b", [128, 8, 128], BF16)
        hnT_tok = Tok()
        hn3 = _sb(nc, e2, "hn3", [128, D], F32)
        hn3_tok = Tok()
        ss = _sb(nc, e2, "qss", [128, 1], F32)
        ss_tok = Tok()
        rstd = _sb(nc, e2, "qrstd", [128, 1], F32)
        rstd_tok = Tok()
        qpT = _sb(nc, e2, "qpT", [128, 16, 128], BF16)
        qpT_tok = Tok()
        sc = _sb(nc, e2, "sc", [128, 16, 128], F32)
        sc_tok = Tok()
        scr = _sb(nc, e2, "scr", [128, 2048], F32)
        scr_tok = Tok()
        hv = _sb(nc, e2, "hv", [128, 16, 16], F32)
        hi = _sb(nc, e2, "hi", [128, 16, 16], U32)
        hif = _sb(nc, e2, "hif", [128, 16, 16], F32)
        hv_tok = Tok()
        cand = _sb(nc, e2, "cand", [128, 8, 256], F32)
        eidx = _sb(nc, e2, "eidx", [128, 8, 256], F32)
        e0 = _sb(nc, e2, "e0", [128, 8, 16], F32)
        cand_tok = Tok()
        bv = _sb(nc, e2, "bv", [128, 8, 16], F32)
        bp = _sb(nc, e2, "bp", [128, 8, 16], U32)
        bpf = _sb(nc, e2, "bpf", [128, 8, 16], F32)
        bv_tok = Tok()
        junk = [_sb(nc, e2, "junk256_%d" % i, [128, 256], F32) for i in range(4)]
        junk_tok = [Tok() for _ in range(4)]
        eidc_tok = [Tok() for _ in range(128)]
        jq = [0]
        eidf = _sb(nc, e2, "eidf", [128, 128], F32)
        eid = _sb(nc, e2, "eid", [128, 128], U32)
        eid_tok = Tok()
        gt = _sb(nc, e2, "gt", [128, 8, 16], F32)
        gs = _sb(nc, e2, "gs", [128, 8], F32)
        nb0 = _sb(nc, e2, "nb0", [128, 8], F32)
        gt_tok = Tok()
        actc = _sb(nc, e2, "actc", [128, 128], F32)
        act_tok = Tok()
        wgt = _sb(nc, e2, "wgt", [128, 128], F32)
        wg2 = _sb(nc, e2, "wg2", [128, 128], F32)
        wgt_tok = Tok()
        NG = 8
        gbuf = [_sb(nc, e2, "gbuf%d" % i, [128, 2 * D], BF16) for i in range(NG)]
        gbuf_tok = [Tok() for _ in range(NG)]
        gbuf_ch = [S.chan() for _ in range(NG)]
        junk2 = [_sb(nc, e2, "junk2_%d" % i, [128, D], BF16) for i in range(2)]
        junk2_tok = [Tok(), Tok()]
        j2 = [0]
        NDG = 4
        dg = [_sb(nc, e2, "dg%d" % i, [128, 128], BF16) for i in range(NDG)]
        dg_tok = [Tok() for _ in range(NDG)]
        slot_tok = [Tok() for _ in range(128)]
        grp_tok = [Tok() for _ in range(32)]
        di = 0
        ytok = Tok()
        gi = 0
        PLAY_N = 15
        bankA = [0]

        def nbkA():
            b = bankA[0] % 4
            bankA[0] += 1
            return b

        class _Rec:
            def __init__(self):
                self.items = []

            def op(self, *a, **k):
                self.items.append(("op", a, k))

            def dma(self, *a, **k):
                self.items.append(("dma", a, k))

        junkF = _sb(nc, e2, "junkF", [128, D], BF16)
        junkF_tok = Tok()
        ss2 = _sb(nc, e2, "ss2", [128, 1], F32)
        ss2_tok = Tok()
        rstd2 = _sb(nc, e2, "rstd2", [128, 1], F32)
        rstd2_tok = Tok()
        obuf = _sb(nc, e2, "obuf", [128, D], F32)
        obuf_tok = Tok()
        o_ch = S.chan()
        h_b2 = _sb(nc, e2, "h_b2", [128, D], F32)
        hn3_b2 = _sb(nc, e2, "hn3_b2", [128, D], F32)
        eid_b2 = _sb(nc, e2, "eid_b2", [128, 128], U32)
        gt_b2 = _sb(nc, e2, "gt_b2", [128, 8, 16], F32)
        sets = [dict(h=h, h_tok=h_tok, h_ch=h_ch, hn3=hn3, hn3_tok=hn3_tok, eid=eid, eid_tok=eid_tok, gt=gt, gt_tok=gt_tok),
                dict(h=h_b2, h_tok=Tok(), h_ch=S.chan(), hn3=hn3_b2, hn3_tok=Tok(), eid=eid_b2, eid_tok=Tok(), gt=gt_b2, gt_tok=Tok())]

        def emitA(S_, tile, h, h_tok, h_ch, hn3, hn3_tok, eid, eid_tok, gt, gt_tok):
            saved = C.S
            C.S = S_
            t0 = tile * 128
            S_.dma("sp", h_ch, h[:], h2buf[t0:t0 + 128, :], reads=[h2_tok[tile]], writes=[h_tok])
            _rms_rstd(C, S_, h[:], h_tok, hn[:], hn_tok, ss, ss_tok, rstd, rstd_tok)
            S_.op("dve", lambda e: e.tensor_scalar(hn[:], h[:], rstd[:], None, ALU.mult), reads=[h_tok, rstd_tok], writes=[hn_tok])
            S_.op("dve", lambda e: e.scalar_tensor_tensor(out=hn3[:], in0=h[:], scalar=rstd[:], in1=gffn_rep[:], op0=ALU.mult, op1=ALU.mult),
                 reads=[h_tok, rstd_tok, pg_tok], writes=[hn3_tok])
            ptb, pt_tok = transpose_to(C, hn, hn_tok, None, None)
            S_.op("act", lambda e, ptb=ptb: e.copy(out=hnT[:], in_=ptb.rearrange("p (k t) -> p k t", k=8)),
                 reads=[pt_tok], writes=[hnT_tok])
            for q4 in range(4):
                b_ = nbkA()
                for j4 in range(4):
                    ch = q4 * 4 + j4
                    for kt in range(8):
                        S_.op("pe", lambda e, kt=kt, ch=ch, j4=j4, b_=b_: e.matmul(
                            ps[b_][:, j4 * 128:(j4 + 1) * 128], lhsT=wpq[:, kt, ch * 128:(ch + 1) * 128], rhs=hnT[:, kt, :],
                            start=(kt == 0), stop=(kt == 7)), reads=[pw_tok, hnT_tok], writes=[ps_tok[b_]])
                S_.op("act", lambda e, q4=q4, b_=b_: e.copy(out=qpT[:, q4 * 4:(q4 + 1) * 4, :],
                                                          in_=ps[b_][:].rearrange("p (a b) -> p a b", a=4)),
                     reads=[ps_tok[b_]], writes=[qpT_tok])
            for q4 in range(4):
                b_ = nbkA()
                for j4 in range(4):
                    ch = q4 * 4 + j4
                    S_.op("pe", lambda e, ch=ch, j4=j4, b_=b_: e.matmul(ps[b_][:, j4 * 128:(j4 + 1) * 128], lhsT=qpT[:, ch, :],
                                                                      rhs=skT[:, ch, :], start=True, stop=True),
                         reads=[pw_tok, qpT_tok], writes=[ps_tok[b_]])
                S_.op("act", lambda e, q4=q4, b_=b_: e.copy(out=sc[:, q4 * 4:(q4 + 1) * 4, :],
                                                          in_=ps[b_][:].rearrange("p (a b) -> p a b", a=4)),
                     reads=[ps_tok[b_]], writes=[sc_tok])
            scr3 = scr[:].rearrange("p (a b) -> p a b", a=16)
            for ch in range(16):
                S_.op("dve", lambda e, ch=ch: e.max(out=hv[:, ch, 0:8], in_=sc[:, ch, :]), reads=[sc_tok], writes=[hv_tok])
                S_.op("dve", lambda e, ch=ch: e.max_index(out=hi[:, ch, 0:8], in_max=hv[:, ch, 0:8], in_values=sc[:, ch, :]),
                     reads=[sc_tok, hv_tok], writes=[hv_tok])
                S_.op("dve", lambda e, ch=ch: e.match_replace(out=scr3[:, ch, :], in_to_replace=hv[:, ch, 0:8], in_values=sc[:, ch, :],
                                                             imm_value=NEG), reads=[sc_tok, hv_tok], writes=[scr_tok])
                S_.op("dve", lambda e, ch=ch: e.max(out=hv[:, ch, 8:16], in_=scr3[:, ch, :]), reads=[scr_tok], writes=[hv_tok])
                S_.op("dve", lambda e, ch=ch: e.max_index(out=hi[:, ch, 8:16], in_max=hv[:, ch, 8:16], in_values=scr3[:, ch, :]),
                     reads=[scr_tok, hv_tok], writes=[hv_tok])
            S_.op("dve", lambda e: e.tensor_copy(out=hif[:], in_=hi[:]), reads=[hv_tok], writes=[hv_tok])
            hv4 = hv[:].rearrange("p (h i) k -> p h i k", i=2)
            hif4 = hif[:].rearrange("p (h i) k -> p h i k", i=2)
            cand4 = cand[:].rearrange("p h (a b) -> p h a b", a=16)
            eidx4 = eidx[:].rearrange("p h (a b) -> p h a b", a=16)
            S_.op("dve", lambda e: e.tensor_tensor(out=cand4, in0=hv4[:, :, 0, :].unsqueeze(3).to_broadcast([128, 8, 16, 16]),
                                                  in1=hv4[:, :, 1, :].unsqueeze(2).to_broadcast([128, 8, 16, 16]), op=ALU.add),
                 reads=[hv_tok], writes=[cand_tok])
            S_.op("dve", lambda e: e.tensor_scalar(e0[:], hif4[:, :, 0, :], 128.0, None, ALU.mult), reads=[hv_tok], writes=[cand_tok])
            S_.op("dve", lambda e: e.tensor_tensor(out=eidx4, in0=e0[:].unsqueeze(3).to_broadcast([128, 8, 16, 16]),
                                                  in1=hif4[:, :, 1, :].unsqueeze(2).to_broadcast([128, 8, 16, 16]), op=ALU.add),
                 reads=[hv_tok, cand_tok], writes=[cand_tok])
            scr8 = scr[:].rearrange("p (a b) -> p a b", a=8)
            for hh in range(8):
                S_.op("dve", lambda e, hh=hh: e.max(out=bv[:, hh, 0:8], in_=cand[:, hh, :]), reads=[cand_tok], writes=[bv_tok])
                S_.op("dve", lambda e, hh=hh: e.max_index(out=bp[:, hh, 0:8], in_max=bv[:, hh, 0:8], in_values=cand[:, hh, :]),
                     reads=[cand_tok, bv_tok], writes=[bv_tok])
                S_.op("dve", lambda e, hh=hh: e.match_replace(out=scr8[:, hh, :], in_to_replace=bv[:, hh, 0:8], in_values=cand[:, hh, :],
                                                             imm_value=NEG), reads=[cand_tok, bv_tok], writes=[scr_tok])
                S_.op("dve", lambda e, hh=hh: e.max(out=bv[:, hh, 8:16], in_=scr8[:, hh, :]), reads=[scr_tok], writes=[bv_tok])
                S_.op("dve", lambda e, hh=hh: e.max_index(out=bp[:, hh, 8:16], in_max=bv[:, hh, 8:16], in_values=scr8[:, hh, :]),
                     reads=[scr_tok, bv_tok], writes=[bv_tok])
            S_.op("dve", lambda e: e.tensor_copy(out=bpf[:], in_=bp[:]), reads=[bv_tok], writes=[bv_tok])
            for hh in range(8):
                for k in range(16):
                    s_ = hh * 16 + k
                    jb = jq[0] % 4
                    jq[0] += 1
                    S_.op("dve", lambda e, hh=hh, k=k, s_=s_, jb=jb: e.scalar_tensor_tensor(
                        out=junk[jb][:], in0=iota256[:], scalar=bpf[:, hh, k:k + 1], in1=eidx[:, hh, :],
                        op0=ALU.is_equal, op1=ALU.mult, accum_out=eidf[:, s_:s_ + 1]),
                        reads=[bv_tok, cand_tok, pg_tok], writes=[junk_tok[jb], eidc_tok[s_]])
            S_.op("dve", lambda e: e.tensor_scalar(eidf[:], eidf[:], 16383.0, 0.0, ALU.min, ALU.max), reads=[eid_tok] + eidc_tok,
                  writes=[eid_tok] + eidc_tok)
            S_.op("dve", lambda e: e.tensor_copy(out=eid[:], in_=eidf[:]), reads=[eid_tok], writes=[eid_tok])
            S_.op("dve", lambda e: e.tensor_scalar(nb0[:], bv[:, :, 0], -1.0, None, ALU.mult), reads=[bv_tok], writes=[gt_tok])
            for hh in range(8):
                S_.op("act", lambda e, hh=hh: e.activation(out=gt[:, hh, :], in_=bv[:, hh, :], func=AF.Exp, bias=nb0[:, hh:hh + 1],
                                                          accum_out=gs[:, hh:hh + 1]), reads=[bv_tok, gt_tok], writes=[gt_tok])
            S_.op("dve", lambda e: e.reciprocal(out=gs[:], in_=gs[:]), reads=[gt_tok], writes=[gt_tok])
            S_.op("dve", lambda e: e.tensor_tensor(out=gt[:], in0=gt[:], in1=gs[:].unsqueeze(2).to_broadcast([128, 8, 16]), op=ALU.mult),
                 reads=[gt_tok], writes=[gt_tok])

            C.S = saved

        def emitB(tile, play, h, h_tok, h_ch, hn3, hn3_tok, eid, eid_tok, gt, gt_tok):
            nonlocal gi, di
            t0 = tile * 128
            pa, pb_ = 4, 5
            gtf = gt[:].rearrange("p a b -> p (a b)")
            for grp in range(32):
                used = []
                for q in range(4):
                    s_ = grp * 4 + q
                    g_ = gi % NG
                    gi += 1
                    used.append(g_)
                    S.dma("pool", gbuf_ch[g_], gbuf[g_][:], uvbf[:, :], reads=[eid_tok] + uv_toks, writes=[gbuf_tok[g_]],
                          indirect=bass.IndirectOffsetOnAxis(ap=eid[:, s_:s_ + 1], axis=0))
                    jb = j2[0] % 2
                    j2[0] += 1
                    S.op("dve", lambda e, g_=g_, s_=s_, jb=jb: e.scalar_tensor_tensor(out=junk2[jb][:], in0=gbuf[g_][:, 0:D], scalar=1.0, in1=hn3[:],
                                                                                     op0=ALU.mult, op1=ALU.mult, accum_out=actc[:, s_:s_ + 1]),
                         reads=[gbuf_tok[g_], hn3_tok], writes=[junk2_tok[jb], slot_tok[s_]])
                cs = slice(grp * 4, grp * 4 + 4)
                gk = grp_tok[grp]
                S.op("dve", lambda e: e.tensor_tensor(out=wg2[:, cs], in0=actc[:, cs], in1=actc[:, cs], op=ALU.mult),
                     reads=[slot_tok[grp * 4 + q] for q in range(4)], writes=[gk])
                S.op("dve", lambda e: e.tensor_scalar(wg2[:, cs], wg2[:, cs], 0.044715, 1.0, ALU.mult, ALU.add), reads=[gk], writes=[gk])
                S.op("dve", lambda e: e.tensor_tensor(out=wg2[:, cs], in0=wg2[:, cs], in1=actc[:, cs], op=ALU.mult),
                     reads=[gk] + [slot_tok[grp * 4 + q] for q in range(4)], writes=[gk])
                S.op("act", lambda e: e.activation(out=wg2[:, cs], in_=wg2[:, cs], func=AF.Sigmoid, scale=1.5957691216057308),
                     reads=[gk], writes=[gk])
                S.op("dve", lambda e: e.tensor_tensor(out=wgt[:, cs], in0=wg2[:, cs], in1=actc[:, cs], op=ALU.mult),
                     reads=[gk] + [slot_tok[grp * 4 + q] for q in range(4)], writes=[gk])
                S.op("dve", lambda e: e.tensor_tensor(out=wgt[:, cs], in0=wgt[:, cs], in1=gtf[:, cs], op=ALU.mult),
                     reads=[gk, gt_tok], writes=[gk])
                for q in range(4):
                    s_ = grp * 4 + q
                    g_ = used[q]
                    d_ = di % NDG
                    di += 1
                    S.op("act", lambda e, d_=d_, s_=s_: e.activation(out=dg[d_][:], in_=C.ident_bf[:], func=AF.Copy,
                                                                    scale=wgt[:, s_:s_ + 1]),
                         reads=[gk, C.const_tok], writes=[dg_tok[d_]])
                    for n2, pbk in enumerate((pa, pb_)):
                        S.op("pe", lambda e, d_=d_, g_=g_, n2=n2, pbk=pbk, s_=s_: e.matmul(
                            ps[pbk][:], lhsT=dg[d_][:], rhs=gbuf[g_][:, D + n2 * 512:D + (n2 + 1) * 512],
                            start=(s_ == 0), stop=(s_ == 127)), reads=[dg_tok[d_], gbuf_tok[g_]], writes=[ps_tok[pbk]])
                play(PLAY_N)
            for n2, pbk in enumerate((pa, pb_)):
                S.op("dve", lambda e, n2=n2, pbk=pbk: e.tensor_tensor(out=h[:, n2 * 512:(n2 + 1) * 512], in0=ps[pbk][:],
                                                                      in1=h[:, n2 * 512:(n2 + 1) * 512], op=ALU.add),
                     reads=[ps_tok[pbk]], writes=[h_tok])
            if dbg == "h3":
                S.dma("sp", h_ch, y[t0:t0 + 128, :], h[:], reads=[h_tok], writes=[ytok])
                play(10 ** 9)
                return
            _rms_rstd(C, S, h[:], h_tok, junkF[:], junkF_tok, ss2, ss2_tok, rstd2, rstd2_tok)
            S.op("dve", lambda e: e.scalar_tensor_tensor(out=obuf[:], in0=h[:], scalar=rstd2[:], in1=gfin_rep[:], op0=ALU.mult, op1=ALU.mult),
                 reads=[h_tok, rstd2_tok, pg_tok], writes=[obuf_tok])
            S.dma("sp", o_ch, y[t0:t0 + 128, :], obuf[:], reads=[obuf_tok], writes=[ytok])
            play(10 ** 9)

        rec = _Rec()
        emitA(rec, 0, **sets[0])
        for kind, a, k in rec.items:
            getattr(S, kind)(*a, **k)
        for tile in range(NT):
            rec = _Rec()
            if tile + 1 < NT:
                emitA(rec, tile + 1, **sets[(tile + 1) % 2])
            items = rec.items

            def play(n, items=items):
                while n > 0 and items:
                    kind, a, k = items.pop(0)
                    getattr(S, kind)(*a, **k)
                    n -= 1

            emitB(tile, play, **sets[tile % 2])
            play(10 ** 9)
        S.wait_tok("sp", [ytok])
        S.barrier()
    eo.close()


def build(dbg=None):
    nc = bass.Bass("TRN2", target_bir_lowering=False)
    C = Ctx()
    C.nc = nc
    C.dbg = dbg

    def din(name, shape, dt=F32):
        return nc.dram_tensor(name, list(shape), dt, kind="ExternalInput").ap()

    x_full = din("x_full", [SEQ, D])
    x_own = din("x_own", [NOWN * SEGT, D])
    qpos = din("qpos", [128, NOWN, SEGT])
    w_in = din("w_in", [128, 8, 2048])
    g_mix = din("g_mix", [128, 8])
    x_rev = din("x_rev", [SEQ, D])
    I = {"x_full": x_full, "x_own": x_own, "w_in": w_in, "x_rev": x_rev}
    for nm, shp in (("a_re", [128, 16]), ("a_im", [128, 16]), ("log_dt", [128, 16]),
                    ("bpad_re", [128, 16, 128]), ("bpad_im", [128, 16, 128]),
                    ("cpad_re", [128, 16, 128]), ("cpad_im", [128, 16, 128]),
                    ("d_skip", [128, 4]), ("w_glu", [128, 4, 512]), ("b_glu", [128, 4]),
                    ("segsel", [128, 4, 16]),
                    ("w_out", [128, 8, D]), ("g_out", [128, 8]), ("mem", [256, D]), ("g_mem", [128, 8]),
                    ("w_xkv", [128, 8, 2048]), ("g_xattn", [128, 8]), ("w_xq", [128, 8, D]), ("w_xo", [128, 8, D]),
                    ("g_ffn", [128, 8]), ("gffn_rep", [128, D]), ("gfin_rep", [128, D]), ("w_pq", [128, 8, 2048]),
                    ("skT", [128, 16, 128]), ("peer_u", [16384, D]), ("peer_v", [16384, D])):
        I[nm] = din(nm, shp)
    y = nc.dram_tensor("y", [NOWN * SEGT, D], F32, kind="ExternalOutput").ap()
    if dbg == "attn":
        dbg_out = nc.dram_tensor("dbg_attn", [128, 4, NOWN * SEGT], F32, kind="ExternalOutput").ap()

    with ExitStack() as es:
        S = Sched(nc, es)
        C.S = S
        C.const_tok = Tok()
        ident_f = _sb(nc, es, "ident_f", [128, 128], F32)
        C.ident_f = ident_f
        C.ident_bf = _sb(nc, es, "ident_bf", [128, 128], BF16)
        ones_f = _sb(nc, es, "ones_f", [128, 128], F32)
        C.ones_bf = _sb(nc, es, "ones_bf", [128, 128], BF16)
        C.tri_bf = _sb(nc, es, "tri_bf", [128, 128], BF16)
        C.atri_bf = _sb(nc, es, "atri_bf", [128, 128], BF16)
        C.eps_col = _sb(nc, es, "eps_col", [128, 1], F32)
        C.kpos = _sb(nc, es, "kpos", [128, 64], F32)
        S.op("pool", lambda e: e.memset(ones_f[:], 1.0), writes=[C.const_tok])
        S.op("pool", lambda e: e.memset(C.eps_col[:], EPS), writes=[C.const_tok])
        S.op("pool", lambda e: e.affine_select(out=ident_f[:], in_=ones_f[:], pattern=[[-1, 128]],
                                               compare_op=ALU.is_equal, fill=0.0, base=0,
                                               channel_multiplier=1),
             reads=[C.const_tok], writes=[C.const_tok])
        S.op("pool", lambda e: e.tensor_copy(out=C.ident_bf[:], in_=ident_f[:]),
             reads=[C.const_tok], writes=[C.const_tok])
        S.op("pool", lambda e: e.tensor_copy(out=C.ones_bf[:], in_=ones_f[:]),
             reads=[C.const_tok], writes=[C.const_tok])
        S.op("pool", lambda e: e.affine_select(out=C.tri_bf[:], in_=ones_f[:], pattern=[[-1, 128]],
                                               compare_op=ALU.is_ge, fill=0.0, base=0,
                                               channel_multiplier=1),
             reads=[C.const_tok], writes=[C.const_tok])
        S.op("pool", lambda e: e.affine_select(out=C.atri_bf[:], in_=ones_f[:], pattern=[[1, 128]],
                                               compare_op=ALU.is_gt, fill=0.0, base=0,
                                               channel_multiplier=-1),
             reads=[C.const_tok], writes=[C.const_tok])
        S.op("pool", lambda e: e.iota(C.kpos[:], pattern=[[128, 64]], base=0, channel_multiplier=1,
                                      allow_small_or_imprecise_dtypes=True),
             writes=[C.const_tok])

        ps = [es.enter_context(nc.psum_tensor("ps%d" % i, [128, 512], F32)) for i in range(8)]
        ps_tok = [Tok() for _ in range(8)]
        C.tp_ps = [ps[6], ps[7]]
        C.tp_tok = [ps_tok[6], ps_tok[7]]
        C.tp_i = 0

        attnT = _sb(nc, es, "attnT", [128, 4, NOWN * SEGT], BF16)
        attn_tok = [[Tok() for _ in range(8)] for _ in range(NOWN)]

        gmix_sb = _sb(nc, es, "gmix", [128, 8], F32)
        gmix_tok = Tok()
        S.dma("sp", S.chan(), gmix_sb[:], g_mix[:, :], writes=[gmix_tok])

        with ExitStack() as e2:
          if dbg != "ssm":
              KT = _sb(nc, e2, "KT", [128, 4, SEQ], BF16)
              V = _sb(nc, e2, "V", [128, 64, 512], BF16)
              QT = _sb(nc, e2, "QT", [128, 4, NOWN * SEGT], BF16)
              kt_tok = [Tok() for _ in range(NSEG)]
              v_tok = [Tok() for _ in range(NSEG)]
              q_tok = [Tok() for _ in range(NOWN)]
              with ExitStack() as e3:
                  wk = _sb(nc, e3, "wk", [128, 8, 512], BF16)
                  wq = wk
                  wv = _sb(nc, e3, "wv", [128, 8, 512], BF16)
                  w_tok = Tok()
                  wk_tok = Tok()
                  wv_tok = Tok()
                  xt = [_sb(nc, e3, "xt%d" % i, [128, D], F32) for i in range(2)]
                  xt_tok = [Tok() for _ in range(2)]
                  xt_ch = [S.chan() for _ in range(2)]
                  wi_box = [0]

                  def load_w(wdst, c0, wtok):
                      for k2 in range(4):
                          sl = wi_box[0] % 2
                          wi_box[0] += 1
                          S.dma("sp", xt_ch[sl], xt[sl][:].rearrange("p (a b) -> p a b", a=2),
                                w_in[:, 2 * k2:2 * k2 + 2, c0:c0 + 512], writes=[xt_tok[sl]])
                          for a2 in range(2):
                              kt = 2 * k2 + a2
                              eng = "dve" if a2 == 0 else "pool"
                              S.op(eng, lambda e, kt=kt, sl=sl, wdst=wdst, a2=a2: e.tensor_scalar(
                                  wdst[:, kt, :], xt[sl][:, a2 * 512:(a2 + 1) * 512], gmix_sb[:, kt:kt + 1], None, ALU.mult),
                                  reads=[xt_tok[sl], gmix_tok], writes=[wtok])

                  load_w(wk, 512, wk_tok)
                  load_w(wv, 1024, wv_tok)

                  xn = [_sb(nc, e3, "xn%d" % i, [128, D], BF16) for i in range(2)]
                  xn_tok = [Tok(), Tok()]
                  ss = [_sb(nc, e3, "ss%d" % i, [128, 1], F32) for i in range(2)]
                  ss_tok = [Tok(), Tok()]
                  rstd = [_sb(nc, e3, "rstd%d" % i, [128, 1], F32) for i in range(2)]
                  rstd_tok = [Tok(), Tok()]
                  xnT = [_sb(nc, e3, "xnT%d" % i, [128, 8, SEGT], BF16) for i in range(2)]
                  xnT_tok = [Tok(), Tok()]
                  ti = 0
                  for sgi in range(NSEG + NOWN):
                      own = sgi >= NSEG
                      sg = sgi - NSEG if own else sgi
                      if sgi == NSEG:
                          load_w(wq, 0, wk_tok)
                      src = x_own if own else x_full
                      xb = sgi % 2
                      for m in range(4):
                          a = ti % 2
                          b = ti % 2
                          ti += 1
                          r0 = sg * SEGT + m * 128
                          S.dma("sp", xt_ch[a], xt[a][:], src[r0:r0 + 128, :], writes=[xt_tok[a]])
                          rms_scale(C, xt[a][:], xt_tok[a], rstd[b][:], rstd_tok[b], xn[b][:], xn_tok[b],
                                    ss[b][:], ss_tok[b])
                          S.op("dve", lambda e, a=a, b=b: e.tensor_scalar(xn[b][:], xt[a][:], rstd[b][:], None, ALU.mult),
                               reads=[xt_tok[a], rstd_tok[b]], writes=[xn_tok[b]])
                          ptb, pt_tok = transpose_to(C, xn[b], xn_tok[b], None, None)
                          S.op("dve", lambda e, xb=xb, m=m, ptb=ptb: e.tensor_copy(
                              out=xnT[xb][:, :, m * 128:(m + 1) * 128],
                              in_=ptb.rearrange("p (k t) -> p k t", k=8)),
                              reads=[pt_tok], writes=[xnT_tok[xb]])
                      if not own:
                          for hp in range(4):
                              pb = hp % 2
                              for kt in range(8):
                                  S.op("pe", lambda e, kt=kt, hp=hp, pb=pb, xb=xb: e.matmul(
                                      ps[pb][:], lhsT=wk[:, kt, hp * 128:(hp + 1) * 128], rhs=xnT[xb][:, kt, :],
                                      start=(kt == 0), stop=(kt == 7)),
                                      reads=[wk_tok, xnT_tok[xb]], writes=[ps_tok[pb]])
                              S.op("act", lambda e, hp=hp, pb=pb, sg=sg: e.copy(
                                  out=KT[:, hp, sg * SEGT:(sg + 1) * SEGT], in_=ps[pb][:]),
                                  reads=[ps_tok[pb]], writes=[kt_tok[sg]])
                          for m in range(4):
                              pb = 2 + m % 2
                              for kt in range(8):
                                  S.op("pe", lambda e, kt=kt, m=m, pb=pb, xb=xb: e.matmul(
                                      ps[pb][:], lhsT=xnT[xb][:, kt, m * 128:(m + 1) * 128], rhs=wv[:, kt, :],
                                      start=(kt == 0), stop=(kt == 7)),
                                      reads=[wv_tok, xnT_tok[xb]], writes=[ps_tok[pb]])
                              S.op("act", lambda e, m=m, pb=pb, sg=sg: e.copy(
                                  out=V[:, sg * 4 + m, :], in_=ps[pb][:]),
                                  reads=[ps_tok[pb]], writes=[v_tok[sg]])
                      else:
                          for hp in range(4):
                              pb = hp % 2
                              for kt in range(8):
                                  S.op("pe", lambda e, kt=kt, hp=hp, pb=pb, xb=xb: e.matmul(
                                      ps[pb][:], lhsT=wq[:, kt, hp * 128:(hp + 1) * 128], rhs=xnT[xb][:, kt, :],
                                      start=(kt == 0), stop=(kt == 7)),
                                      reads=[wk_tok, xnT_tok[xb]], writes=[ps_tok[pb]])
                              S.op("act", lambda e, hp=hp, pb=pb, sg=sg: e.mul(
                                  out=QT[:, hp, sg * SEGT:(sg + 1) * SEGT], in_=ps[pb][:], mul=0.125),
                                  reads=[ps_tok[pb]], writes=[q_tok[sg]])

              S.barrier()
              if dbg == "kv":
                  dch = S.chan()
                  dk = nc.dram_tensor("dbg_kt", [128, 4, SEQ], BF16, kind="ExternalOutput").ap()
                  dv = nc.dram_tensor("dbg_v", [128, 64, 512], BF16, kind="ExternalOutput").ap()
                  dq = nc.dram_tensor("dbg_q", [128, 4, NOWN * SEGT], BF16, kind="ExternalOutput").ap()
                  dtok = Tok()
                  S.dma("sp", dch, dk[:, :, :], KT[:], reads=kt_tok, writes=[dtok])
                  S.dma("sp", dch, dv[:, :, :], V[:], reads=v_tok, writes=[dtok])
                  S.dma("sp", dch, dq[:, :, :], QT[:], reads=q_tok, writes=[dtok])
                  S.wait_tok("sp", [dtok])
              with ExitStack() as e3:
                if dbg != "kv":
                    NE = 3
                    e_sb = [_sb(nc, e3, "e%d" % i, [128, 512], F32) for i in range(NE)]
                    e_tok = [Tok() for _ in range(NE)]
                    sp_sb = [_sb(nc, e3, "sp%d" % i, [128, 512], BF16) for i in range(3)]
                    sp_tok = [Tok(), Tok(), Tok()]
                    ex_sb = [_sb(nc, e3, "ex%d" % i, [128, 512], BF16) for i in range(2)]
                    ex_tok = [Tok(), Tok()]
                    w_sb = [_sb(nc, e3, "w%d" % i, [128, 512], BF16) for i in range(2)]
                    w_tok2 = [Tok(), Tok()]
                    masks = _sb(nc, e3, "masks", [128, 16, 512], BF16)
                    mask_tok = Tok()
                    qp = _sb(nc, e3, "qp", [128, NOWN, SEGT], F32)
                    qp_tok = Tok()
                    S.dma("sp", S.chan(), qp[:], qpos[:, :, :], writes=[qp_tok])
                    zps = [ps[0], ps[1]]
                    zps_tok = [ps_tok[0], ps_tok[1]]
                    cps = [ps[2], ps[3]]
                    cps_tok = [ps_tok[2], ps_tok[3]]
                    ops_ = [ps[4], ps[5]]
                    ops_tok = [ps_tok[4], ps_tok[5]]

                    for slot in range(NOWN):
                        top = KB_TOP[slot]
                        nb = top + 1
                        for r in range(16):
                            kb = top - r
                            S.op("dve", lambda e, r=r, kb=kb, slot=slot: e.tensor_scalar(
                                masks[:, r, :], qp[:, slot, :], C.kpos[:, kb:kb + 1], None, ALU.is_gt),
                                reads=[qp_tok, C.const_tok], writes=[mask_tok])
                        blocks = [(h, top - r, r) for h in range(8) for r in range(nb)]
                        nblk = len(blocks)

                        def s1(i):
                            h, kb, r = blocks[i]
                            hp, hh = h // 2, h % 2
                            zb = i % 2
                            S.op("pe", lambda e: e.matmul(
                                zps[zb][:], lhsT=KT[hh * 64:(hh + 1) * 64, hp, kb * 128:(kb + 1) * 128],
                                rhs=QT[hh * 64:(hh + 1) * 64, hp, slot * SEGT:(slot + 1) * SEGT],
                                start=True, stop=True),
                                reads=[kt_tok[kb // 4], q_tok[slot]], writes=[zps_tok[zb]])

                        def s2(i):
                            h, kb, r = blocks[i]
                            zb, eb, sb = i % 2, i % NE, i % 3
                            S.op("act", lambda e: e.activation(out=e_sb[eb][:], in_=zps[zb][:], func=AF.Exp),
                                 reads=[zps_tok[zb]], writes=[e_tok[eb]])
                            if r < 16:
                                S.op("dve", lambda e: e.tensor_tensor(out=e_sb[eb][:], in0=e_sb[eb][:],
                                                                       in1=masks[:, r, :], op=ALU.mult),
                                     reads=[mask_tok], writes=[e_tok[eb]])
                            S.op("act", lambda e: e.activation(out=sp_sb[sb][:], in_=e_sb[eb][:], func=AF.Ln, bias=1.0),
                                 reads=[e_tok[eb]], writes=[sp_tok[sb]])

                        def s3(i):
                            h, kb, r = blocks[i]
                            sb = i % 3
                            cb = h % 2
                            S.op("pe", lambda e: e.matmul(cps[cb][:], lhsT=C.tri_bf[:], rhs=sp_sb[sb][:],
                                                          start=(r == 0), stop=True, skip_group_check=(r != 0)),
                                 reads=[sp_tok[sb], C.const_tok], writes=[cps_tok[cb]])

                        def s3b(i):
                            h, kb, r = blocks[i]
                            if r == nb - 1:
                                return
                            sb = i % 3
                            cb = h % 2
                            S.op("pe", lambda e: e.matmul(cps[cb][:], lhsT=C.atri_bf[:], rhs=sp_sb[sb][:],
                                                          start=False, stop=True, skip_group_check=True),
                                 reads=[sp_tok[sb], C.const_tok], writes=[cps_tok[cb]])

                        def s4(i):
                            h, kb, r = blocks[i]
                            cb, xb_, eb, wb = h % 2, i % 2, i % NE, i % 2
                            S.op("act", lambda e: e.activation(out=ex_sb[xb_][:], in_=cps[cb][:], func=AF.Exp, scale=-1.0),
                                 reads=[cps_tok[cb]], writes=[ex_tok[xb_]])
                            S.op("dve", lambda e: e.tensor_tensor(out=w_sb[wb][:], in0=e_sb[eb][:], in1=ex_sb[xb_][:],
                                                                   op=ALU.mult),
                                 reads=[e_tok[eb], ex_tok[xb_]], writes=[w_tok2[wb]])

                        def s5(i):
                            h, kb, r = blocks[i]
                            wb = i % 2
                            ob = h % 2
                            hp, hh = h // 2, h % 2
                            S.op("pe", lambda e: e.matmul(ops_[ob][hh * 64:(hh + 1) * 64, :], lhsT=V[:, kb, h * 64:(h + 1) * 64],
                                                          rhs=w_sb[wb][:], start=(r == 0), stop=(r == nb - 1)),
                                 reads=[w_tok2[wb], v_tok[kb // 4]], writes=[ops_tok[ob]])
                            if r == nb - 1:
                                S.op("act", lambda e: e.copy(out=attnT[hh * 64:(hh + 1) * 64, hp, slot * SEGT:(slot + 1) * SEGT],
                                                             in_=ops_[ob][hh * 64:(hh + 1) * 64, :]),
                                     reads=[ops_tok[ob]], writes=[attn_tok[slot][h]])

                        for t in range(nblk + 2):
                            if t < nblk:
                                s1(t)
                                s2(t)
                            if 0 <= t - 2 < nblk:
                                s3b(t - 2)
                            if 0 <= t - 1 < nblk:
                                s3(t - 1)
                                s4(t - 1)
                            if 0 <= t - 2 < nblk:
                                s5(t - 2)

        S.barrier()
        ssmT = _sb(nc, es, "ssmT", [128, 4, NOWN * SEGT], BF16)
        ssm_tok = [Tok() for _ in range(NOWN)]
        if dbg in ("ssm", None, "h1", "h2", "h3"):
            phase_ssm(C, nc, S, ps, ps_tok, I, ssmT, ssm_tok, gmix_sb, gmix_tok)
        S.barrier()
        och = S.chan()
        if dbg == "ssm":
            dbg_ssm = nc.dram_tensor("dbg_ssm", [128, 4, NOWN * SEGT], BF16, kind="ExternalOutput").ap()
            ytok = Tok()
            S.dma("sp", och, dbg_ssm[:, :, :], ssmT[:], reads=ssm_tok, writes=[ytok])
            S.wait_tok("sp", [ytok])
        if dbg == "attn":
            with ExitStack() as e2:
                tmp = _sb(nc, e2, "dbgtmp", [128, 4, NOWN * SEGT], F32)
                tmp_tok = Tok()
                alltoks = [t for row in attn_tok for t in row]
                S.op("dve", lambda e: e.tensor_copy(out=tmp[:], in_=attnT[:]), reads=alltoks, writes=[tmp_tok])
                ytok = Tok()
                S.dma("sp", och, dbg_out[:, :, :], tmp[:], reads=[tmp_tok], writes=[ytok])
                S.wait_tok("sp", [ytok])
        S.barrier()
        if dbg in (None, "h1", "h2", "h3"):
            phase_post(C, nc, S, ps, ps_tok, I, attnT, attn_tok, ssmT, ssm_tok, y, dbg)
            return nc
        with ExitStack() as e2:
            zt = _sb(nc, e2, "zt", [128, D], F32)
            zt_tok = Tok()
            S.op("pool", lambda e: e.memset(zt[:], 0.0), writes=[zt_tok])
            ytok = Tok()
            for i in range(NOWN * 4):
                S.dma("sp", och, y[i * 128:(i + 1) * 128, :], zt[:], reads=[zt_tok], writes=[ytok])
            S.wait_tok("sp", [ytok])
    return nc


def make_in_maps(inputs):
    x = np.ascontiguousarray(inputs["x"], dtype=np.float32)
    w_in = np.ascontiguousarray(inputs["w_in"][0].reshape(8, 128, 2048).transpose(1, 0, 2))
    g_mix = np.ascontiguousarray(inputs["g_mix"][0].reshape(8, 128).T)
    f32 = np.float32
    are = inputs["a_re"][0].reshape(16, 2, 64).transpose(1, 2, 0).reshape(128, 16)
    aim = inputs["a_im"][0].reshape(16, 2, 64).transpose(1, 2, 0).reshape(128, 16)
    ldt = np.repeat(inputs["log_dt"][0].reshape(16, 2).T[:, None, :], 64, axis=1).reshape(128, 16)
    bpr = np.zeros((128, 16, 128), f32)
    bpi = np.zeros((128, 16, 128), f32)
    cpr = np.zeros((128, 16, 128), f32)
    cpi = np.zeros((128, 16, 128), f32)
    for k in range(16):
        for g2 in range(2):
            g = 2 * k + g2
            r0 = (g % 8) * 16
            bpr[r0:r0 + 16, k, g2 * 64:(g2 + 1) * 64] = inputs["b_re"][0][g].T
            bpi[r0:r0 + 16, k, g2 * 64:(g2 + 1) * 64] = inputs["b_im"][0][g].T
            cpr[g2 * 64:(g2 + 1) * 64, k, r0:r0 + 16] = inputs["c_re"][0][g].T
            cpi[g2 * 64:(g2 + 1) * 64, k, r0:r0 + 16] = inputs["c_im"][0][g].T
    dsk = inputs["d_skip"][0].reshape(4, 128).T
    wglu = inputs["w_glu"][0].reshape(4, 128, 512).transpose(1, 0, 2)
    bglu = inputs["b_glu"][0].reshape(4, 128).T
    def kt8(w):
        return w.reshape(8, 128, -1).transpose(1, 0, 2)

    def col8(g):
        return g.reshape(8, 128).T

    post = {"w_out": kt8(inputs["w_out"][0]),
            "g_out": col8(np.concatenate([inputs["g_attn_out"][0], inputs["g_ssm_out"][0]])),
            "g_mem": col8(inputs["g_mem"][0]), "w_xkv": kt8(inputs["w_xkv"][0]),
            "g_xattn": col8(inputs["g_xattn"][0]), "w_xq": kt8(inputs["w_xq"][0]), "w_xo": kt8(inputs["w_xo"][0]),
            "g_ffn": col8(inputs["g_ffn"][0]),
            "gffn_rep": np.broadcast_to(inputs["g_ffn"][0][None, :], (128, D)),
            "gfin_rep": np.broadcast_to(inputs["g_final"][None, :], (128, D)),
            "w_pq": kt8(inputs["w_pq"][0]),
            "skT": inputs["sub_keys"][0].transpose(3, 0, 1, 2).reshape(128, 16, 128),
            "peer_u": inputs["peer_u"][0], "peer_v": inputs["peer_v"][0]}
    common = {"w_in": w_in, "g_mix": g_mix, "a_re": are, "a_im": aim, "log_dt": ldt,
              "bpad_re": bpr, "bpad_im": bpi, "cpad_re": cpr, "cpad_im": cpi,
              "d_skip": dsk, "w_glu": wglu, "b_glu": bglu}
    common.update(post)
    common = {k_: np.ascontiguousarray(v_, dtype=f32) for k_, v_ in common.items()}
    maps = []
    for c in range(8):
        b, j = c // 4, c % 4
        tiles = own_tiles(j)
        segsel = np.zeros((128, NOWN, 16), f32)
        for i_, t_ in enumerate(tiles):
            segsel[:, i_, t_] = 1.0
        x_own = np.concatenate([x[b, t * SEGT:(t + 1) * SEGT] for t in tiles], axis=0)
        qp = np.stack([np.arange(t * SEGT, (t + 1) * SEGT, dtype=np.float32) for t in tiles], axis=0)
        qp = np.ascontiguousarray(np.broadcast_to(qp[None], (128, NOWN, SEGT)))
        x_rev = np.ascontiguousarray(x[b].reshape(NSEG, SEGT, D)[:, ::-1, :].reshape(SEQ, D))
        m = {"x_full": np.ascontiguousarray(x[b]), "x_own": np.ascontiguousarray(x_own), "x_rev": x_rev,
             "qpos": qp, "segsel": segsel, "mem": np.ascontiguousarray(inputs["mem"][b], dtype=f32)}
        m.update(common)
        maps.append(m)
    return maps


def kernel(**inputs):
    nc = build()
    maps = make_in_maps(inputs)
    res = run_bass_kernel_spmd(nc, maps, core_ids=list(range(8)))
    out = np.zeros((2, SEQ, D), np.float32)
    for c in range(8):
        b, j = c // 4, c % 4
        yo = res.results[c]["y"]
        for i, t in enumerate(own_tiles(j)):
            out[b, t * SEGT:(t + 1) * SEGT] = yo[i * SEGT:(i + 1) * SEGT]
    return out
```

```python
from contextlib import ExitStack
import numpy as np
import concourse.bass as bass
import concourse.mybir as mybir
from concourse.bass_utils import run_bass_kernel_spmd

F32 = mybir.dt.float32
BF16 = mybir.dt.bfloat16
U32 = mybir.dt.uint32
I32 = mybir.dt.int32
AF = mybir.ActivationFunctionType
ALU = mybir.AluOpType
AX = mybir.AxisListType

D = 1024
SEQ = 8192
NSEG = 16
SEGT = 512
NOWN = 4
EPS = 1e-6
KB_TOP = [15, 31, 47, 63]
NEG = -1.0e30


def own_tiles(j):
    return [j, 7 - j, 8 + j, 15 - j]


class Tok:
    __slots__ = ("w", "r")

    def __init__(self):
        self.w = None
        self.r = {}


class _Eng:
    def __init__(self, name, h, sem):
        self.name = name
        self.h = h
        self.sem = sem
        self.n = 0
        self.waited = {}


class _Chan:
    def __init__(self, sem):
        self.sem = sem
        self.n = 0


class Sched:
    def __init__(self, nc, es):
        self.nc = nc
        self.es = es
        self.E = {}
        for name, h in (("pe", nc.tensor), ("act", nc.scalar), ("dve", nc.vector),
                        ("pool", nc.gpsimd), ("sp", nc.sync)):
            sem = es.enter_context(nc.semaphore("sem_" + name))
            self.E[name] = _Eng(name, h, sem)
        self.nchan = 0
        self.chans = []

    def chan(self):
        self.nchan += 1
        c = _Chan(self.es.enter_context(self.nc.semaphore("ch%d" % self.nchan)))
        self.chans.append(c)
        return c

    def barrier(self):
        for E in self.E.values():
            for F in self.E.values():
                if F is E or F.n == 0:
                    continue
                if E.waited.get(id(F.sem), 0) < F.n:
                    E.h.wait_ge(F.sem, F.n)
                    E.waited[id(F.sem)] = F.n
            for c in self.chans:
                if c.n > 0 and E.waited.get(id(c.sem), 0) < c.n:
                    E.h.wait_ge(c.sem, c.n)
                    E.waited[id(c.sem)] = c.n

    def _wait(self, E, reads, writes, weak=()):
        deps = {}
        for t in weak:
            for d in [t.w] + list(t.r.values()):
                if d is not None and d[0] is not E.sem:
                    k_ = id(d[0])
                    if k_ not in deps or deps[k_][1] < d[1]:
                        deps[k_] = d

        def add(d):
            if d is None:
                return
            s, v = d
            k = id(s)
            if k not in deps or deps[k][1] < v:
                deps[k] = (s, v)

        for t in reads:
            add(t.w)
        for t in writes:
            add(t.w)
            for d in t.r.values():
                add(d)
        for k, (s, v) in deps.items():
            if E.name == "pe" and s is E.sem:
                continue
            if E.waited.get(k, 0) < v:
                E.h.wait_ge(s, v)
                E.waited[k] = v

    def op(self, eng, fn, reads=(), writes=(), weak=()):
        E = self.E[eng]
        self._wait(E, reads, writes, weak)
        ins = fn(E.h)
        E.n += 1
        ins.then_inc(E.sem, 1)
        me = (E.sem, E.n)
        for t in reads:
            t.r[id(E.sem)] = me
        for t in writes:
            t.w = me
            t.r = {}
        for t in weak:
            t.w = me
            t.r = {}
        return ins

    def dma(self, queue, ch, out, in_, reads=(), writes=(), indirect=None, **kw):
        Q = self.E[queue]
        self._wait(Q, reads, writes)
        if indirect is not None:
            ins = Q.h.indirect_dma_start(out=out, out_offset=None, in_=in_, in_offset=indirect)
        else:
            ins = Q.h.dma_start(out=out, in_=in_, **kw)
        ch.n += 16
        ins.then_inc(ch.sem, 16)
        me = (ch.sem, ch.n)
        for t in reads:
            t.r[id(ch.sem)] = me
        for t in writes:
            t.w = me
            t.r = {}
        return ins

    def wait_tok(self, eng, toks):
        self._wait(self.E[eng], toks, ())


class Ctx:
    pass


_NAME_CNT = [0]


def _sb(nc, es, name, shape, dt):
    _NAME_CNT[0] += 1
    return es.enter_context(nc.sbuf_tensor("%s_%d" % (name, _NAME_CNT[0]), list(shape), dt))


def rms_scale(C, xt, xt_tok, rstd, rstd_tok, junk, junk_tok, ss, ss_tok):
    S = C.S
    S.op("act", lambda e: e.activation(out=junk, in_=xt, func=AF.Square, accum_out=ss),
         reads=[xt_tok], writes=[junk_tok, ss_tok])
    S.op("act", lambda e: e.activation(out=rstd, in_=ss, func=AF.Ln, bias=C.eps_col[:], scale=1.0 / D),
         reads=[ss_tok, C.const_tok], writes=[rstd_tok])
    S.op("act", lambda e: e.activation(out=rstd, in_=rstd, func=AF.Exp, scale=-0.5),
         reads=[rstd_tok], writes=[rstd_tok])


def transpose_to(C, xn, xn_tok, dst_fn, dst_tok, nkt=8):
    S = C.S
    force = getattr(C, "tp_force", None)
    if force is not None:
        pt, pt_tok = C.tp_ps[force], C.tp_tok[force]
    else:
        pt, pt_tok = C.tp_ps[C.tp_i % 2], C.tp_tok[C.tp_i % 2]
        C.tp_i += 1
    ptb = pt[:].bitcast(BF16)
    for kt in range(nkt):
        S.op("pe", lambda e, kt=kt: e.transpose(out=ptb[:, kt * 128:(kt + 1) * 128],
                                                in_=xn[:, kt * 128:(kt + 1) * 128],
                                                identity=C.ident_bf[:]),
             reads=[xn_tok, C.const_tok], writes=[pt_tok])
    return ptb, pt_tok


TWO_PI = 6.283185307179586
CW1 = 6.28125
CW2 = TWO_PI - CW1


def phase_ssm(C, nc, S, ps, ps_tok, I, ssmT, ssm_tok, gmix_sb, gmix_tok):
    with ExitStack() as e2:
        cosT = _sb(nc, e2, "cosT", [128, 16, SEGT], F32)
        sinT = _sb(nc, e2, "sinT", [128, 16, SEGT], F32)
        tab_tok = Tok()
        bre = _sb(nc, e2, "bre", [128, 16, 128], BF16)
        bim = _sb(nc, e2, "bim", [128, 16, 128], BF16)
        cre = _sb(nc, e2, "cre", [128, 16, 128], BF16)
        ncim = _sb(nc, e2, "ncim", [128, 16, 128], BF16)
        bc_tok = Tok()
        wu = _sb(nc, e2, "wu", [128, 8, 512], BF16)
        wglu = _sb(nc, e2, "wglu", [128, 4, 512], BF16)
        diagD = _sb(nc, e2, "diagD", [128, 4, 128], BF16)
        w_tok = Tok()
        P = {}
        for nm in ("are", "aim", "ldt", "dt", "rho", "th", "gre", "gim", "ngim", "pa", "pb", "pc", "pd", "adt", "nadt",
                   "ilr", "ili", "q1", "q2", "q3", "q4"):
            P[nm] = _sb(nc, e2, "p_" + nm, [128, 16], F32)
        p_tok = Tok()
        bglu = _sb(nc, e2, "bglu", [128, 4], F32)
        dsk = _sb(nc, e2, "dsk", [128, 4], F32)
        segsel = _sb(nc, e2, "segsel_sb", [128, 4, 16], F32)
        halfpi = _sb(nc, e2, "halfpi", [128, 1], F32)
        Xst = _sb(nc, e2, "Xst", [128, 2, 16, 17], F32)
        tau1p = _sb(nc, e2, "tau1p", [128, SEGT], F32)
        magt = _sb(nc, e2, "magt", [128, SEGT], F32)
        mag_tok = Tok()
        S.op("pool", lambda e: e.iota(tau1p[:], pattern=[[1, SEGT]], base=1, channel_multiplier=0,
                                      allow_small_or_imprecise_dtypes=True), writes=[mag_tok])
        xst_tok = Tok()
        small_tok = Tok()
        ch0 = S.chan()
        for dst, src in ((P["are"], I["a_re"]), (P["aim"], I["a_im"]), (P["ldt"], I["log_dt"]),
                         (bglu, I["b_glu"]), (dsk, I["d_skip"])):
            S.dma("sp", ch0, dst[:], src[:, :], writes=[small_tok])
        S.dma("sp", ch0, segsel[:], I["segsel"][:, :, :], writes=[small_tok])
        S.op("pool", lambda e: e.memset(halfpi[:], float(np.pi / 2)), writes=[small_tok])
        S.op("pool", lambda e: e.memset(Xst[:], 0.0), writes=[xst_tok])
        S.op("act", lambda e: e.activation(out=P["dt"][:], in_=P["ldt"][:], func=AF.Exp), reads=[small_tok], writes=[p_tok])
        S.op("dve", lambda e: e.tensor_tensor(out=P["pa"][:], in0=P["are"][:], in1=P["dt"][:], op=ALU.mult),
             reads=[small_tok, p_tok], writes=[p_tok])
        S.op("act", lambda e: e.activation(out=P["rho"][:], in_=P["pa"][:], func=AF.Exp), reads=[p_tok], writes=[p_tok])
        S.op("dve", lambda e: e.tensor_copy(out=P["adt"][:], in_=P["pa"][:]), reads=[p_tok], writes=[p_tok])
        S.op("dve", lambda e: e.tensor_scalar(P["nadt"][:], P["pa"][:], -1.0, None, ALU.mult), reads=[p_tok], writes=[p_tok])
        S.op("dve", lambda e: e.tensor_tensor(out=P["th"][:], in0=P["aim"][:], in1=P["dt"][:], op=ALU.mult),
             reads=[small_tok, p_tok], writes=[p_tok])
        with ExitStack() as e3:
            tau1 = _sb(nc, e3, "tau1", [128, SEGT], F32)
            ang = _sb(nc, e3, "ang", [128, 4, SEGT], F32)
            kf = _sb(nc, e3, "kf", [128, 4, SEGT], F32)
            ki = _sb(nc, e3, "ki", [128, 4, SEGT], I32)
            s1 = _sb(nc, e3, "s1", [128, 4, SEGT], F32)
            t_tok = Tok()
            S.op("pool", lambda e: e.iota(tau1[:], pattern=[[1, SEGT]], base=1, channel_multiplier=0,
                                          allow_small_or_imprecise_dtypes=True), writes=[t_tok])
            for gq in range(4):
                for kk in range(4):
                    k = gq * 4 + kk
                    S.op("dve", lambda e, kk=kk, k=k: e.tensor_scalar(ang[:, kk, :], tau1[:], P["th"][:, k:k + 1], None, ALU.mult),
                         reads=[p_tok, t_tok], writes=[t_tok])
                S.op("dve", lambda e: e.tensor_scalar(kf[:], ang[:], 1.0 / TWO_PI, None, ALU.mult), reads=[t_tok], writes=[t_tok])
                S.op("dve", lambda e: e.tensor_copy(out=ki[:], in_=kf[:]), reads=[t_tok], writes=[t_tok])
                S.op("dve", lambda e: e.tensor_copy(out=kf[:], in_=ki[:]), reads=[t_tok], writes=[t_tok])
                S.op("dve", lambda e: e.scalar_tensor_tensor(out=ang[:], in0=kf[:], scalar=-CW1, in1=ang[:], op0=ALU.mult, op1=ALU.add),
                     reads=[t_tok], writes=[t_tok])
                S.op("dve", lambda e: e.scalar_tensor_tensor(out=ang[:], in0=kf[:], scalar=-CW2, in1=ang[:], op0=ALU.mult, op1=ALU.add),
                     reads=[t_tok], writes=[t_tok])
                S.op("act", lambda e: e.activation(out=s1[:], in_=ang[:], func=AF.Sin, scale=0.25), reads=[t_tok], writes=[t_tok])
                S.op("act", lambda e: e.activation(out=kf[:], in_=ang[:], func=AF.Sin, scale=0.25, bias=halfpi[:]),
                     reads=[t_tok, small_tok], writes=[t_tok])
                S.op("dve", lambda e: e.scalar_tensor_tensor(out=ang[:], in0=s1[:], scalar=2.0, in1=kf[:], op0=ALU.mult, op1=ALU.mult),
                     reads=[t_tok], writes=[t_tok])
                S.op("dve", lambda e: e.tensor_tensor(out=s1[:], in0=s1[:], in1=s1[:], op=ALU.mult), reads=[t_tok], writes=[t_tok])
                S.op("dve", lambda e: e.tensor_scalar(s1[:], s1[:], -2.0, 1.0, ALU.mult, ALU.add), reads=[t_tok], writes=[t_tok])
                S.op("dve", lambda e, gq=gq: e.scalar_tensor_tensor(out=sinT[:, gq * 4:(gq + 1) * 4, :], in0=ang[:], scalar=2.0, in1=s1[:],
                                                                    op0=ALU.mult, op1=ALU.mult),
                     reads=[t_tok], writes=[tab_tok])
                S.op("dve", lambda e: e.tensor_tensor(out=ang[:], in0=ang[:], in1=ang[:], op=ALU.mult), reads=[t_tok], writes=[t_tok])
                S.op("dve", lambda e, gq=gq: e.tensor_scalar(cosT[:, gq * 4:(gq + 1) * 4, :], ang[:], -2.0, 1.0, ALU.mult, ALU.add),
                     reads=[t_tok], writes=[tab_tok])
            c0 = cosT[:, :, 0]
            s0 = sinT[:, :, 0]
            tt = lambda o, a, b, op: S.op("dve", lambda e: e.tensor_tensor(out=o, in0=a, in1=b, op=op),
                                          reads=[p_tok, tab_tok, small_tok], writes=[p_tok])
            tt(P["pa"][:], P["rho"][:], c0, ALU.mult)
            S.op("dve", lambda e: e.tensor_scalar(P["pa"][:], P["pa"][:], -1.0, None, ALU.add), reads=[p_tok], writes=[p_tok])
            tt(P["pb"][:], P["rho"][:], s0, ALU.mult)
            tt(P["pc"][:], P["are"][:], P["are"][:], ALU.mult)
            tt(P["pd"][:], P["aim"][:], P["aim"][:], ALU.mult)
            tt(P["pc"][:], P["pc"][:], P["pd"][:], ALU.add)
            S.op("dve", lambda e: e.reciprocal(out=P["pc"][:], in_=P["pc"][:]), reads=[p_tok], writes=[p_tok])
            tt(P["gre"][:], P["pa"][:], P["are"][:], ALU.mult)
            tt(P["pd"][:], P["pb"][:], P["aim"][:], ALU.mult)
            tt(P["gre"][:], P["gre"][:], P["pd"][:], ALU.add)
            tt(P["gre"][:], P["gre"][:], P["pc"][:], ALU.mult)
            tt(P["gim"][:], P["pb"][:], P["are"][:], ALU.mult)
            tt(P["pd"][:], P["pa"][:], P["aim"][:], ALU.mult)
            tt(P["gim"][:], P["gim"][:], P["pd"][:], ALU.subtract)
            tt(P["gim"][:], P["gim"][:], P["pc"][:], ALU.mult)
            S.op("dve", lambda e: e.tensor_scalar(P["ngim"][:], P["gim"][:], -1.0, None, ALU.mult), reads=[p_tok], writes=[p_tok])
            S.barrier()
        with ExitStack() as e3:
            f1 = _sb(nc, e3, "f1", [128, 16, 128], F32)
            f2 = _sb(nc, e3, "f2", [128, 16, 128], F32)
            f3 = _sb(nc, e3, "f3", [128, 128], F32)
            f_tok = Tok()
            chf = S.chan()
            for src, dst in ((I["bpad_re"], bre), (I["bpad_im"], bim)):
                S.dma("sp", chf, f1[:], src[:, :, :], writes=[f_tok])
                S.op("dve", lambda e, dst=dst: e.tensor_copy(out=dst[:], in_=f1[:]), reads=[f_tok], writes=[bc_tok, f_tok])
            S.dma("sp", chf, f1[:], I["cpad_re"][:, :, :], writes=[f_tok])
            S.dma("sp", chf, f2[:], I["cpad_im"][:, :, :], writes=[f_tok])
            for k in range(16):
                S.op("dve", lambda e, k=k: e.tensor_scalar(f3[:], f2[:, k, :], P["gim"][:, k:k + 1], None, ALU.mult),
                     reads=[f_tok, p_tok], writes=[f_tok])
                S.op("dve", lambda e, k=k: e.scalar_tensor_tensor(out=cre[:, k, :], in0=f1[:, k, :], scalar=P["gre"][:, k:k + 1],
                                                                  in1=f3[:], op0=ALU.mult, op1=ALU.subtract),
                     reads=[f_tok, p_tok], writes=[bc_tok])
                S.op("dve", lambda e, k=k: e.tensor_scalar(f3[:], f2[:, k, :], P["gre"][:, k:k + 1], None, ALU.mult),
                     reads=[f_tok, p_tok, bc_tok], writes=[f_tok])
                S.op("dve", lambda e, k=k: e.scalar_tensor_tensor(out=ncim[:, k, :], in0=f1[:, k, :], scalar=P["ngim"][:, k:k + 1],
                                                                  in1=f3[:], op0=ALU.mult, op1=ALU.subtract),
                     reads=[f_tok, p_tok], writes=[bc_tok])
            f1v = f1[:].rearrange("p a b -> p (a b)").rearrange("p (k c) -> p k c", k=4)
            for half in range(2):
                S.dma("sp", chf, f1v, I["w_in"][:, 4 * half:4 * half + 4, 1536:2048], writes=[f_tok])
                for k4 in range(4):
                    kt = 4 * half + k4
                    S.op("dve" if k4 % 2 == 0 else "pool", lambda e, kt=kt, k4=k4: e.tensor_scalar(
                        wu[:, kt, :], f1v[:, k4, :], gmix_sb[:, kt:kt + 1], None, ALU.mult),
                        reads=[f_tok, gmix_tok], writes=[w_tok])
            for kt in range(4):
                S.dma("sp", chf, f1[:, 0:4, :], I["w_glu"][:, kt, :].rearrange("p (a b) -> p a b", a=4), writes=[f_tok])
                S.op("dve", lambda e, kt=kt: e.tensor_copy(out=wglu[:, kt, :].rearrange("p (a b) -> p a b", a=4), in_=f1[:, 0:4, :]),
                     reads=[f_tok], writes=[w_tok, f_tok])
            for ct in range(4):
                S.op("dve", lambda e, ct=ct: e.tensor_scalar(diagD[:, ct, :], C.ident_f[:], dsk[:, ct:ct + 1], None, ALU.mult),
                     reads=[small_tok, C.const_tok], writes=[w_tok])
            S.barrier()

        def scale_tables(sign_key):
            for k in range(16):
                S.op("act", lambda e, k=k: e.activation(out=magt[:], in_=tau1p[:], func=AF.Exp, scale=P[sign_key][:, k:k + 1]),
                     reads=[mag_tok, p_tok], writes=[mag_tok])
                S.op("dve", lambda e, k=k: e.tensor_tensor(out=cosT[:, k, :], in0=cosT[:, k, :], in1=magt[:], op=ALU.mult),
                     reads=[mag_tok, tab_tok], writes=[tab_tok])
                S.op("dve", lambda e, k=k: e.tensor_tensor(out=sinT[:, k, :], in0=sinT[:, k, :], in1=magt[:], op=ALU.mult),
                     reads=[mag_tok, tab_tok], writes=[tab_tok])

        scale_tables("adt")
        with ExitStack() as e3:
            xt = [_sb(nc, e3, "axt%d" % i, [128, D], F32) for i in range(2)]
            xt_tok = [Tok(), Tok()]
            xt_ch = [S.chan(), S.chan()]
            xn = [_sb(nc, e3, "axn%d" % i, [128, D], BF16) for i in range(2)]
            xn_tok = [Tok(), Tok()]
            ss = [_sb(nc, e3, "ass%d" % i, [128, 1], F32) for i in range(2)]
            ss_tok = [Tok(), Tok()]
            rstd = [_sb(nc, e3, "arstd%d" % i, [128, 1], F32) for i in range(2)]
            rstd_tok = [Tok(), Tok()]
            xnT = [_sb(nc, e3, "axnT%d" % i, [128, 8, SEGT], BF16) for i in range(2)]
            xnT_tok = [Tok(), Tok()]
            uT = [_sb(nc, e3, "auT%d" % i, [128, 4, SEGT], BF16) for i in range(2)]
            uT_tok = [Tok(), Tok()]
            acc4 = [_sb(nc, e3, "acc4_%d" % i, [128, 4, 16], F32) for i in range(2)]
            acc4_tok = [[[Tok() for _ in range(16)] for _ in range(4)] for _ in range(2)]
            junkS = [_sb(nc, e3, "junkS%d" % i, [128, SEGT], BF16) for i in range(4)]
            junkS_tok = [Tok() for _ in range(4)]
            jc = [0]
            sm_ = {nm: _sb(nc, e3, "sm_" + nm, [128, 16], F32) for nm in ("a", "b", "c", "d", "sr", "si")}
            sm_tok = Tok()
            t0r, t0i = cosT[:, :, 0], sinT[:, :, 0]
            Lr, Li = cosT[:, :, SEGT - 1], sinT[:, :, SEGT - 1]
            tt2 = lambda o, a, b, op: S.op("dve", lambda e: e.tensor_tensor(out=o, in0=a, in1=b, op=op),
                                           reads=[p_tok, tab_tok, sm_tok], writes=[p_tok])
            tt2(P["q1"][:], t0r, t0r, ALU.mult)
            tt2(P["q2"][:], t0i, t0i, ALU.mult)
            tt2(P["q1"][:], P["q1"][:], P["q2"][:], ALU.add)
            S.op("dve", lambda e: e.reciprocal(out=P["q1"][:], in_=P["q1"][:]), reads=[p_tok], writes=[p_tok])
            tt2(P["ilr"][:], t0r, P["q1"][:], ALU.mult)
            tt2(P["ili"][:], t0i, P["q1"][:], ALU.mult)
            S.op("dve", lambda e: e.tensor_scalar(P["ili"][:], P["ili"][:], -1.0, None, ALU.mult), reads=[p_tok], writes=[p_tok])
            ti = 0
            for sg in range(NSEG):
                xb = sg % 2
                for m in range(4):
                    a = ti % 2
                    ti += 1
                    r0 = sg * SEGT + m * 128
                    S.dma("sp", xt_ch[a], xt[a][:], I["x_rev"][r0:r0 + 128, :], writes=[xt_tok[a]])
                    rms_scale(C, xt[a][:], xt_tok[a], rstd[a][:], rstd_tok[a], xn[a][:], xn_tok[a], ss[a][:], ss_tok[a])
                    S.op("dve", lambda e, a=a: e.tensor_scalar(xn[a][:], xt[a][:], rstd[a][:], None, ALU.mult),
                         reads=[xt_tok[a], rstd_tok[a]], writes=[xn_tok[a]])
                    ptb, pt_tok = transpose_to(C, xn[a], xn_tok[a], None, None)
                    S.op("act", lambda e, xb=xb, m=m, ptb=ptb: e.copy(out=xnT[xb][:, :, m * 128:(m + 1) * 128],
                                                                      in_=ptb.rearrange("p (k t) -> p k t", k=8)),
                         reads=[pt_tok], writes=[xnT_tok[xb]])
                for ct in range(4):
                    pb = 4 + ct % 2
                    for kt in range(8):
                        S.op("pe", lambda e, kt=kt, ct=ct, pb=pb, xb=xb: e.matmul(
                            ps[pb][:], lhsT=wu[:, kt, ct * 128:(ct + 1) * 128], rhs=xnT[xb][:, kt, :],
                            start=(kt == 0), stop=(kt == 7)), reads=[w_tok, xnT_tok[xb]], writes=[ps_tok[pb]])
                    S.op("act", lambda e, ct=ct, pb=pb, xb=xb: e.copy(out=uT[xb][:, ct, :], in_=ps[pb][:]),
                         reads=[ps_tok[pb]], writes=[uT_tok[xb]])
                ab = sg % 2
                for k in range(16):
                    ct = k // 4
                    bur, bui = (0, 1) if k % 2 == 0 else (2, 3)
                    S.op("pe", lambda e, k=k, ct=ct, bur=bur: e.matmul(ps[bur][:], lhsT=bre[:, k, :], rhs=uT[xb][:, ct, :], start=True, stop=True),
                         reads=[bc_tok, uT_tok[xb]], writes=[ps_tok[bur]])
                    S.op("pe", lambda e, k=k, ct=ct, bui=bui: e.matmul(ps[bui][:], lhsT=bim[:, k, :], rhs=uT[xb][:, ct, :], start=True, stop=True),
                         reads=[bc_tok, uT_tok[xb]], writes=[ps_tok[bui]])
                    for j, (bk, tab) in enumerate(((bur, cosT), (bui, sinT), (bur, sinT), (bui, cosT))):
                        jb = jc[0] % 4
                        jc[0] += 1
                        S.op("dve", lambda e, j=j, bk=bk, tab=tab, k=k, jb=jb: e.scalar_tensor_tensor(
                            out=junkS[jb][:], in0=ps[bk][:], scalar=1.0, in1=tab[:, k, :], op0=ALU.mult, op1=ALU.mult,
                            accum_out=acc4[ab][:, j, k:k + 1]),
                            reads=[ps_tok[bk], tab_tok], writes=[junkS_tok[jb], acc4_tok[ab][j][k]])
                A_ = acc4[ab]
                rd = [t_ for row_ in acc4_tok[ab] for t_ in row_] + [p_tok, tab_tok, xst_tok, sm_tok]
                tt3 = lambda o, a_, b_, op: S.op("dve", lambda e: e.tensor_tensor(out=o, in0=a_, in1=b_, op=op), reads=rd, writes=[sm_tok])
                tt3(sm_["a"][:], A_[:, 0, :], A_[:, 1, :], ALU.subtract)
                tt3(sm_["b"][:], A_[:, 2, :], A_[:, 3, :], ALU.add)
                tt3(sm_["c"][:], P["ilr"][:], sm_["a"][:], ALU.mult)
                tt3(sm_["d"][:], P["ili"][:], sm_["b"][:], ALU.mult)
                tt3(sm_["sr"][:], sm_["c"][:], sm_["d"][:], ALU.subtract)
                tt3(sm_["c"][:], P["ilr"][:], sm_["b"][:], ALU.mult)
                tt3(sm_["d"][:], P["ili"][:], sm_["a"][:], ALU.mult)
                tt3(sm_["si"][:], sm_["c"][:], sm_["d"][:], ALU.add)
                Xr, Xi = Xst[:, 0, :, sg], Xst[:, 1, :, sg]
                tt3(sm_["a"][:], Lr, Xr, ALU.mult)
                tt3(sm_["b"][:], Li, Xi, ALU.mult)
                tt3(sm_["a"][:], sm_["a"][:], sm_["b"][:], ALU.subtract)
                S.op("dve", lambda e, sg=sg: e.tensor_tensor(out=Xst[:, 0, :, sg + 1], in0=sm_["a"][:], in1=sm_["sr"][:], op=ALU.add),
                     reads=[sm_tok], writes=[xst_tok])
                tt3(sm_["c"][:], Lr, Xi, ALU.mult)
                tt3(sm_["d"][:], Li, Xr, ALU.mult)
                tt3(sm_["c"][:], sm_["c"][:], sm_["d"][:], ALU.add)
                S.op("dve", lambda e, sg=sg: e.tensor_tensor(out=Xst[:, 1, :, sg + 1], in0=sm_["c"][:], in1=sm_["si"][:], op=ALU.add),
                     reads=[sm_tok], writes=[xst_tok])
            S.barrier()
        scale_tables("nadt")
        S.barrier()
        with ExitStack() as e3:
            xt = [_sb(nc, e3, "sxt%d" % i, [128, D], F32) for i in range(2)]
            xt_tok = [Tok(), Tok()]
            xt_ch = [S.chan(), S.chan()]
            xn = [_sb(nc, e3, "sxn%d" % i, [128, D], BF16) for i in range(2)]
            xn_tok = [Tok(), Tok()]
            ss = [_sb(nc, e3, "sss%d" % i, [128, 1], F32) for i in range(2)]
            ss_tok = [Tok(), Tok()]
            rstd = [_sb(nc, e3, "srstd%d" % i, [128, 1], F32) for i in range(2)]
            rstd_tok = [Tok(), Tok()]
            xnT = [_sb(nc, e3, "sxnT", [128, 8, SEGT], BF16)] * 2
            xnT_tok = [Tok()] * 2
            uT = [_sb(nc, e3, "uT%d" % i, [128, 4, SEGT], BF16) for i in range(2)]
            uT_tok = [Tok(), Tok()]
            tmp4 = [_sb(nc, e3, "tm%d" % i, [128, SEGT], F32) for i in range(4)]
            tmp4_tok = [Tok() for _ in range(4)]
            Wr = [_sb(nc, e3, "Wr%d" % i, [128, SEGT], F32) for i in range(2)]
            Wi = [_sb(nc, e3, "Wi%d" % i, [128, SEGT], F32) for i in range(2)]
            Vr = [_sb(nc, e3, "Vr%d" % i, [128, SEGT], F32) for i in range(2)]
            Vi = [_sb(nc, e3, "Vi%d" % i, [128, SEGT], F32) for i in range(2)]
            W_tok = [Tok(), Tok()]
            V_tok = [Tok(), Tok()]
            Xb = [_sb(nc, e3, "Xb%d" % i, [128, 2, SEGT], BF16) for i in range(2)]
            Xb_tok = [Tok(), Tok()]
            xin = _sb(nc, e3, "xin", [128, 32], F32)
            xin_tok = Tok()
            selt = _sb(nc, e3, "selt", [128, 32, 16], F32)
            rot = _sb(nc, e3, "rot", [128, 4], F32)
            rot_tok = Tok()
            yb = _sb(nc, e3, "yb", [128, 4, SEGT], BF16)
            y_tok = [Tok() for _ in range(4)]
            g1 = _sb(nc, e3, "g1", [128, SEGT], F32)
            g2 = _sb(nc, e3, "g2", [128, SEGT], F32)
            g_tok = Tok()
            ti = 0
            pcount = 0
            for sgi in range(NSEG, NSEG + NOWN):
                own = sgi >= NSEG
                sg = sgi - NSEG if own else sgi
                src = I["x_own"] if own else I["x_full"]
                xb = sgi % 2
                for m in range(4):
                    a = ti % 2
                    ti += 1
                    r0 = sg * SEGT + m * 128
                    S.dma("sp", xt_ch[a], xt[a][:], src[r0:r0 + 128, :], writes=[xt_tok[a]])
                    rms_scale(C, xt[a][:], xt_tok[a], rstd[a][:], rstd_tok[a], xn[a][:], xn_tok[a], ss[a][:], ss_tok[a])
                    S.op("dve", lambda e, a=a: e.tensor_scalar(xn[a][:], xt[a][:], rstd[a][:], None, ALU.mult),
                         reads=[xt_tok[a], rstd_tok[a]], writes=[xn_tok[a]])
                    ptb, pt_tok = transpose_to(C, xn[a], xn_tok[a], None, None)
                    S.op("act", lambda e, xb=xb, m=m, ptb=ptb: e.copy(out=xnT[xb][:, :, m * 128:(m + 1) * 128],
                                                                      in_=ptb.rearrange("p (k t) -> p k t", k=8)),
                         reads=[pt_tok], writes=[xnT_tok[xb]])
                for ct in range(4):
                    pb = ct % 2
                    for kt in range(8):
                        S.op("pe", lambda e, kt=kt, ct=ct, pb=pb, xb=xb: e.matmul(
                            ps[pb][:], lhsT=wu[:, kt, ct * 128:(ct + 1) * 128], rhs=xnT[xb][:, kt, :],
                            start=(kt == 0), stop=(kt == 7)), reads=[w_tok, xnT_tok[xb]], writes=[ps_tok[pb]])
                    S.op("act", lambda e, ct=ct, pb=pb, xb=xb: e.copy(out=uT[xb][:, ct, :], in_=ps[pb][:]),
                         reads=[ps_tok[pb]], writes=[uT_tok[xb]])
                if own:
                    S.op("dve", lambda e, sg=sg: e.tensor_tensor(out=selt[:], in0=Xst[:].rearrange("p r k s -> p (r k) s")[:, :, 0:16],
                                                                in1=segsel[:, sg:sg + 1, :].to_broadcast([128, 32, 16]), op=ALU.mult),
                         reads=[xst_tok, small_tok], writes=[xin_tok])
                    S.op("dve", lambda e: e.tensor_reduce(out=xin[:], in_=selt[:], axis=AX.X, op=ALU.add),
                         reads=[xin_tok], writes=[xin_tok])
                for ct in range(4):
                    yps = 4 + ct % 2
                    for kk in range(4):
                        k = ct * 4 + kk
                        pp = pcount % 2
                        pcount += 1
                        bur, bui = 2 * pp, 2 * pp + 1
                        bur, bui = (2, 3) if pp == 0 else (0, 1)
                        S.op("pe", lambda e: e.matmul(ps[bur][:], lhsT=bre[:, k, :], rhs=uT[xb][:, ct, :], start=True, stop=True),
                             reads=[bc_tok, uT_tok[xb]], writes=[ps_tok[bur]])
                        S.op("pe", lambda e: e.matmul(ps[bui][:], lhsT=bim[:, k, :], rhs=uT[xb][:, ct, :], start=True, stop=True),
                             reads=[bc_tok, uT_tok[xb]], writes=[ps_tok[bui]])
                        ck, sk = cosT[:, k, :], sinT[:, k, :]
                        S.op("dve", lambda e: e.tensor_tensor(out=tmp4[0][:], in0=ps[bur][:], in1=ck, op=ALU.mult),
                             reads=[ps_tok[bur], tab_tok], writes=[tmp4_tok[0]])
                        S.op("dve", lambda e: e.tensor_tensor(out=tmp4[1][:], in0=ps[bui][:], in1=sk, op=ALU.mult),
                             reads=[ps_tok[bui], tab_tok], writes=[tmp4_tok[1]])
                        S.op("dve", lambda e: e.tensor_tensor(out=Wr[pp][:], in0=tmp4[0][:], in1=tmp4[1][:], op=ALU.add),
                             reads=[tmp4_tok[0], tmp4_tok[1]], writes=[W_tok[pp]])
                        S.op("dve", lambda e: e.tensor_tensor(out=tmp4[2][:], in0=ps[bui][:], in1=ck, op=ALU.mult),
                             reads=[ps_tok[bui], tab_tok], writes=[tmp4_tok[2]])
                        S.op("dve", lambda e: e.tensor_tensor(out=tmp4[3][:], in0=ps[bur][:], in1=sk, op=ALU.mult),
                             reads=[ps_tok[bur], tab_tok], writes=[tmp4_tok[3]])
                        S.op("dve", lambda e: e.tensor_tensor(out=Wi[pp][:], in0=tmp4[2][:], in1=tmp4[3][:], op=ALU.subtract),
                             reads=[tmp4_tok[2], tmp4_tok[3]], writes=[W_tok[pp]])
                        if own:
                            ir, ii = xin[:, k:k + 1], xin[:, 16 + k:16 + k + 1]
                            itoks = [xin_tok]
                        else:
                            ir, ii = Xst[:, 2 * k, sg:sg + 1], Xst[:, 2 * k + 1, sg:sg + 1]
                            itoks = [xst_tok]
                        rb = P["rho"][:, k:k + 1].to_broadcast([128, SEGT])
                        S.op("dve", lambda e: e.tensor_tensor_scan(out=Vr[pp][:], data0=rb, data1=Wr[pp][:], initial=ir,
                                                                   op0=ALU.mult, op1=ALU.add),
                             reads=[W_tok[pp], p_tok] + itoks, writes=[V_tok[pp]])
                        S.op("dve", lambda e: e.tensor_tensor_scan(out=Vi[pp][:], data0=rb, data1=Wi[pp][:], initial=ii,
                                                                   op0=ALU.mult, op1=ALU.add),
                             reads=[W_tok[pp], p_tok] + itoks, writes=[V_tok[pp]])
                        if not own:
                            cl, sl_ = cosT[:, k, SEGT - 1:SEGT], sinT[:, k, SEGT - 1:SEGT]
                            vr, vi = Vr[pp][:, SEGT - 1:SEGT], Vi[pp][:, SEGT - 1:SEGT]
                            S.op("dve", lambda e: e.tensor_tensor(out=rot[:, 0:1], in0=vi, in1=sl_, op=ALU.mult),
                                 reads=[V_tok[pp], tab_tok], writes=[rot_tok])
                            S.op("dve", lambda e: e.scalar_tensor_tensor(out=Xst[:, 2 * k, sg + 1:sg + 2], in0=vr, scalar=cl, in1=rot[:, 0:1],
                                                                         op0=ALU.mult, op1=ALU.subtract),
                                 reads=[V_tok[pp], tab_tok, rot_tok], writes=[xst_tok])
                            S.op("dve", lambda e: e.tensor_tensor(out=rot[:, 1:2], in0=vr, in1=sl_, op=ALU.mult),
                                 reads=[V_tok[pp], tab_tok], writes=[rot_tok])
                            S.op("dve", lambda e: e.scalar_tensor_tensor(out=Xst[:, 2 * k + 1, sg + 1:sg + 2], in0=vi, scalar=cl, in1=rot[:, 1:2],
                                                                         op0=ALU.mult, op1=ALU.add),
                                 reads=[V_tok[pp], tab_tok, rot_tok], writes=[xst_tok])
                        else:
                            S.op("dve", lambda e: e.tensor_tensor(out=tmp4[0][:], in0=Vr[pp][:], in1=ck, op=ALU.mult),
                                 reads=[V_tok[pp], tab_tok], writes=[tmp4_tok[0]])
                            S.op("dve", lambda e: e.tensor_tensor(out=tmp4[1][:], in0=Vi[pp][:], in1=sk, op=ALU.mult),
                                 reads=[V_tok[pp], tab_tok], writes=[tmp4_tok[1]])
                            S.op("dve", lambda e: e.tensor_tensor(out=Xb[pp][:, 0, :], in0=tmp4[0][:], in1=tmp4[1][:], op=ALU.subtract),
                                 reads=[tmp4_tok[0], tmp4_tok[1]], writes=[Xb_tok[pp]])
                            S.op("dve", lambda e: e.tensor_tensor(out=tmp4[2][:], in0=Vr[pp][:], in1=sk, op=ALU.mult),
                                 reads=[V_tok[pp], tab_tok], writes=[tmp4_tok[2]])
                            S.op("dve", lambda e: e.tensor_tensor(out=tmp4[3][:], in0=Vi[pp][:], in1=ck, op=ALU.mult),
                                 reads=[V_tok[pp], tab_tok], writes=[tmp4_tok[3]])
                            S.op("dve", lambda e: e.tensor_tensor(out=Xb[pp][:, 1, :], in0=tmp4[2][:], in1=tmp4[3][:], op=ALU.add),
                                 reads=[tmp4_tok[2], tmp4_tok[3]], writes=[Xb_tok[pp]])
                            S.op("pe", lambda e: e.matmul(ps[yps][:], lhsT=cre[:, k, :], rhs=Xb[pp][:, 0, :], start=(kk == 0), stop=False),
                                 reads=[bc_tok, Xb_tok[pp]], writes=[ps_tok[yps]])
                            S.op("pe", lambda e: e.matmul(ps[yps][:], lhsT=ncim[:, k, :], rhs=Xb[pp][:, 1, :], start=False, stop=False),
                                 reads=[bc_tok, Xb_tok[pp]], writes=[ps_tok[yps]])
                    if own:
                        S.op("pe", lambda e: e.matmul(ps[yps][:], lhsT=diagD[:, ct, :], rhs=uT[xb][:, ct, :], start=False, stop=True),
                             reads=[w_tok, uT_tok[xb]], writes=[ps_tok[yps]])
                        S.op("act", lambda e: e.activation(out=g1[:], in_=ps[yps][:], func=AF.Square), reads=[ps_tok[yps]], writes=[g_tok])
                        S.op("dve", lambda e: e.tensor_scalar(g1[:], g1[:], 0.044715, 1.0, ALU.mult, ALU.add), reads=[g_tok], writes=[g_tok])
                        S.op("dve", lambda e: e.tensor_tensor(out=g1[:], in0=ps[yps][:], in1=g1[:], op=ALU.mult),
                             reads=[g_tok, ps_tok[yps]], writes=[g_tok])
                        S.op("act", lambda e: e.activation(out=g2[:], in_=g1[:], func=AF.Sigmoid, scale=1.5957691216057308),
                             reads=[g_tok], writes=[g_tok])
                        S.op("dve", lambda e, ct=ct: e.tensor_tensor(out=yb[:, ct, :], in0=ps[yps][:], in1=g2[:], op=ALU.mult),
                             reads=[g_tok, ps_tok[yps]], writes=[y_tok[ct]])
                if own:
                    for c2 in range(4):
                        gp = 4 + c2 % 2
                        for kt in range(4):
                            S.op("pe", lambda e, kt=kt, c2=c2, gp=gp: e.matmul(ps[gp][:], lhsT=wglu[:, kt, c2 * 128:(c2 + 1) * 128],
                                                                              rhs=yb[:, kt, :], start=(kt == 0), stop=(kt == 3)),
                                 reads=[w_tok] + y_tok, writes=[ps_tok[gp]])
                        S.op("act", lambda e, c2=c2, gp=gp: e.activation(out=g2[:], in_=ps[gp][:], func=AF.Sigmoid, bias=bglu[:, c2:c2 + 1]),
                             reads=[ps_tok[gp], small_tok], writes=[g_tok])
                        S.op("dve", lambda e, c2=c2, sg=sg: e.tensor_tensor(out=ssmT[:, c2, sg * SEGT:(sg + 1) * SEGT], in0=yb[:, c2, :],
                                                                           in1=g2[:], op=ALU.mult),
                             reads=[g_tok, y_tok[c2]], writes=[ssm_tok[sg]])
            S.barrier()
        S.barrier()


def _rms_rstd(C, S, src_ap, src_tok, junk_ap, junk_tok, ss, ss_tok, rstd, rstd_tok, n=D):
    S.op("act", lambda e: e.activation(out=junk_ap, in_=src_ap, func=AF.Square, accum_out=ss[:]),
         reads=[src_tok], writes=[junk_tok, ss_tok])
    S.op("act", lambda e: e.activation(out=rstd[:], in_=ss[:], func=AF.Ln, bias=C.eps_col[:], scale=1.0 / n),
         reads=[ss_tok, C.const_tok], writes=[rstd_tok])
    S.op("act", lambda e: e.activation(out=rstd[:], in_=rstd[:], func=AF.Exp, scale=-0.5),
         reads=[rstd_tok], writes=[rstd_tok])


def _load_cast(S, nc, dst, src, gcol, gtok, stg, stg_tok, stg_ch, dst_tok, nkt, ncols, cnt):
    for kt in range(nkt):
        for c0 in range(0, ncols, 512):
            sl = cnt[0] % 2
            cnt[0] += 1
            w = min(512, ncols - c0)
            S.dma("sp", stg_ch[sl], stg[sl][:, 0:w], src[:, kt, c0:c0 + w], writes=[stg_tok[sl]])
            eng = "dve" if sl == 0 else "pool"
            if gcol is not None:
                S.op(eng, lambda e, kt=kt, c0=c0, w=w, sl=sl: e.tensor_scalar(dst[:, kt, c0:c0 + w], stg[sl][:, 0:w],
                                                                            gcol[:, kt:kt + 1], None, ALU.mult),
                     reads=[stg_tok[sl], gtok], writes=[dst_tok])
            else:
                S.op(eng, lambda e, kt=kt, c0=c0, w=w, sl=sl: e.tensor_copy(out=dst[:, kt, c0:c0 + w], in_=stg[sl][:, 0:w]),
                     reads=[stg_tok[sl]], writes=[dst_tok])


def phase_post(C, nc, S, ps, ps_tok, I, attnT, attn_tok, ssmT, ssm_tok, y, dbg):
    NT = NOWN * 4
    h2buf = nc.dram_tensor("h2buf", [NOWN * SEGT, D], F32, kind="Internal").ap()
    h2_tok = [Tok() for _ in range(NT)]
    alla = [t for row in attn_tok for t in row]
    bank = [0]

    def nbk():
        b = bank[0] % 6
        bank[0] += 1
        return b

    eo = ExitStack()
    if dbg not in ("h1", "h2"):
        wpq = _sb(nc, eo, "wpq", [128, 8, 2048], BF16)
        skT = _sb(nc, eo, "skT_sb", [128, 16, 128], BF16)
        pw_tok = Tok()
        gffn = _sb(nc, eo, "gffn", [128, 8], F32)
        gffn_rep = _sb(nc, eo, "gffn_rep_sb", [128, D], F32)
        gfin_rep = _sb(nc, eo, "gfin_rep_sb", [128, D], F32)
        iota256 = _sb(nc, eo, "iota256", [128, 256], F32)
        pg_tok = Tok()
        pchg = S.chan()
        S.dma("sp", pchg, gffn[:], I["g_ffn"][:, :], writes=[pg_tok])
        S.dma("sp", pchg, gffn_rep[:], I["gffn_rep"][:, :], writes=[pg_tok])
        S.dma("sp", pchg, gfin_rep[:], I["gfin_rep"][:, :], writes=[pg_tok])
        S.op("pool", lambda e: e.iota(iota256[:], pattern=[[1, 256]], base=0, channel_multiplier=0,
                                      allow_small_or_imprecise_dtypes=True), writes=[pg_tok])
        pstg = [_sb(nc, eo, "qstg%d" % i, [128, 512], F32) for i in range(2)]
        pstg_tok = [Tok(), Tok()]
        pstg_ch = [S.chan(), S.chan()]
        pcnt = [0]
        _load_cast(S, nc, wpq, I["w_pq"], gffn, pg_tok, pstg, pstg_tok, pstg_ch, pw_tok, 8, 2048, pcnt)
        _load_cast(S, nc, skT, I["skT"], None, None, pstg, pstg_tok, pstg_ch, pw_tok, 16, 128, pcnt)

    uvbf = nc.dram_tensor("uvbf", [16384, 2048], BF16, kind="Internal").ap()
    uv_toks = [Tok() for _ in range(32)]
    if dbg not in ("h1", "h2"):
        with ExitStack() as e2:
            cin = [_sb(nc, e2, "cin%d" % i, [128, 8, D], F32) for i in range(2)]
            cin_tok = [Tok(), Tok()]
            cin_ch = [S.chan(), S.chan()]
            cout = [_sb(nc, e2, "cout%d" % i, [128, 8, D], BF16) for i in range(2)]
            cout_tok = [[Tok() for _ in range(8)] for _ in range(2)]
            cout_ch = [S.chan(), S.chan()]
            ci = 0
            engs = ("act", "dve", "pool", "dve", "act", "dve", "act", "dve")
            for tbl, src in enumerate((I["peer_u"], I["peer_v"])):
                for chk in range(16):
                    sl = ci % 2
                    r0 = chk * 1024
                    S.dma("sp", cin_ch[sl], cin[sl][:], src[r0:r0 + 1024, :].rearrange("(p a) d -> p a d", a=8),
                          writes=[cin_tok[sl]])
                    for a in range(8):
                        if engs[a] == "act":
                            S.op("act", lambda e, a=a, sl=sl: e.copy(out=cout[sl][:, a, :], in_=cin[sl][:, a, :]),
                                 reads=[cin_tok[sl]], writes=[cout_tok[sl][a]])
                        else:
                            S.op(engs[a], lambda e, a=a, sl=sl: e.tensor_copy(out=cout[sl][:, a, :], in_=cin[sl][:, a, :]),
                                 reads=[cin_tok[sl]], writes=[cout_tok[sl][a]])
                    S.dma("sp", cout_ch[sl],
                          uvbf[r0:r0 + 1024, tbl * 1024:(tbl + 1) * 1024].rearrange("(p a) d -> p a d", a=8), cout[sl][:],
                          reads=cout_tok[sl], writes=[uv_toks[ci]])
                    ci += 1
            S.barrier()

    with ExitStack() as e2:
        wo = _sb(nc, e2, "wo", [128, 8, D], BF16)
        wxq = _sb(nc, e2, "wxq", [128, 8, D], BF16)
        wxo = _sb(nc, e2, "wxo", [128, 8, D], BF16)
        KmT = _sb(nc, e2, "KmT", [128, 8, 256], BF16)
        Vm = _sb(nc, e2, "Vm", [128, 2, D], BF16)
        w_tok = Tok()
        kv_tok = Tok()
        gcols = _sb(nc, e2, "gcols", [128, 3, 8], F32)
        g_tok = Tok()
        chg = S.chan()
        S.dma("sp", chg, gcols[:, 0, :], I["g_out"][:, :], writes=[g_tok])
        S.dma("sp", chg, gcols[:, 1, :], I["g_xattn"][:, :], writes=[g_tok])
        S.dma("sp", chg, gcols[:, 2, :], I["g_mem"][:, :], writes=[g_tok])
        stg = [_sb(nc, e2, "pstg%d" % i, [128, 512], F32) for i in range(2)]
        stg_tok = [Tok(), Tok()]
        stg_ch = [S.chan(), S.chan()]
        cnt = [0]
        wo_tok, wxq_tok, wxo_tok = Tok(), Tok(), Tok()
        _load_cast(S, nc, wo, I["w_out"], gcols[:, 0, :], g_tok, stg, stg_tok, stg_ch, wo_tok, 8, D, cnt)
        _load_cast(S, nc, wxq, I["w_xq"], gcols[:, 1, :], g_tok, stg, stg_tok, stg_ch, wxq_tok, 8, D, cnt)
        _load_cast(S, nc, wxo, I["w_xo"], None, None, stg, stg_tok, stg_ch, wxo_tok, 8, D, cnt)
        h = _sb(nc, e2, "h", [128, D], F32)
        h_tok = Tok()
        h_ch = S.chan()
        hn = _sb(nc, e2, "hn", [128, D], BF16)
        hn_tok = Tok()
        hnT = _sb(nc, e2, "hnT", [128, 8, 128], BF16)
        hnT_tok = Tok()
        ss = _sb(nc, e2, "pss", [128, 1], F32)
        ss_tok = Tok()
        rstd = _sb(nc, e2, "prstd", [128, 1], F32)
        rstd_tok = Tok()
        with ExitStack() as e3:
            memT = _sb(nc, e3, "memT", [128, 8, 256], BF16)
            memT_tok = Tok()
            wch = _sb(nc, e3, "wch", [128, 8, 512], BF16)
            wch_tok = Tok()
            for mt in range(2):
                S.dma("sp", h_ch, h[:], I["mem"][mt * 128:(mt + 1) * 128, :], writes=[h_tok])
                _rms_rstd(C, S, h[:], h_tok, hn[:], hn_tok, ss, ss_tok, rstd, rstd_tok)
                S.op("dve", lambda e: e.tensor_scalar(hn[:], h[:], rstd[:], None, ALU.mult), reads=[h_tok, rstd_tok], writes=[hn_tok])
                ptb, pt_tok = transpose_to(C, hn, hn_tok, None, None)
                S.op("act", lambda e, mt=mt, ptb=ptb: e.copy(out=memT[:, :, mt * 128:(mt + 1) * 128],
                                                           in_=ptb.rearrange("p (k t) -> p k t", k=8)),
                     reads=[pt_tok], writes=[memT_tok])
            for cc in range(4):
                _load_cast(S, nc, wch, I["w_xkv"][:, :, cc * 512:(cc + 1) * 512], gcols[:, 2, :], g_tok, stg, stg_tok, stg_ch,
                           wch_tok, 8, 512, cnt)
                if cc < 2:
                    for j4 in range(4):
                        b_ = nbk()
                        for kt in range(8):
                            S.op("pe", lambda e, kt=kt, j4=j4, b_=b_: e.matmul(ps[b_][:, 0:256], lhsT=wch[:, kt, j4 * 128:(j4 + 1) * 128],
                                                                             rhs=memT[:, kt, :], start=(kt == 0), stop=(kt == 7)),
                                 reads=[wch_tok, memT_tok], writes=[ps_tok[b_]])
                        S.op("act", lambda e, j4=j4, b_=b_, cc=cc: e.copy(out=KmT[:, cc * 4 + j4, :], in_=ps[b_][:, 0:256]),
                             reads=[ps_tok[b_]], writes=[kv_tok])
                else:
                    for mt in range(2):
                        b_ = nbk()
                        for kt in range(8):
                            S.op("pe", lambda e, kt=kt, mt=mt, b_=b_: e.matmul(ps[b_][:], lhsT=memT[:, kt, mt * 128:(mt + 1) * 128],
                                                                             rhs=wch[:, kt, :], start=(kt == 0), stop=(kt == 7)),
                                 reads=[wch_tok, memT_tok], writes=[ps_tok[b_]])
                        S.op("act", lambda e, mt=mt, b_=b_, cc=cc: e.copy(out=Vm[:, mt, (cc - 2) * 512:(cc - 1) * 512], in_=ps[b_][:]),
                             reads=[ps_tok[b_]], writes=[kv_tok])
            S.barrier()
        def mkset(tag):
            B = {}
            for nm, shp, dt_ in (("sq4", [128, 4, 128], BF16), ("rs2", [128, 2], F32), ("qT", [128, 8, 128], BF16),
                                 ("pp", [128, 4, 256], BF16), ("pT", [128, 8, 128], BF16), ("oT", [128, 8, 128], BF16),
                                 ("mx", [128, 4], F32), ("sm", [128, 4], F32), ("h", [128, D], F32), ("hn", [128, D], BF16),
                                 ("hnT", [128, 8, 128], BF16), ("ss", [128, 1], F32), ("rstd", [128, 1], F32)):
                B[nm] = _sb(nc, e2, nm + tag, shp, dt_)
            for nm in ("sq_tok", "rs2_tok", "qT_tok", "pp_tok", "pT_tok", "oT_tok", "sm_tok", "h_tok", "hn_tok", "hnT_tok",
                       "ss_tok", "rstd_tok"):
                B[nm] = Tok()
            B["h_ch"] = S.chan()
            return B

        class _Rec1:
            def __init__(self):
                self.items = []

            def op(self, *a, **k):
                self.items.append(("op", a, k))

            def dma(self, *a, **k):
                self.items.append(("dma", a, k))

        def emit_tile(S_, tile, nbk_, sq4, rs2, qT, pp, pT, oT, mx, sm, h, hn, hnT, ss, rstd, sq_tok, rs2_tok, qT_tok, pp_tok,
                      pT_tok, oT_tok, sm_tok, h_tok, hn_tok, hnT_tok, ss_tok, rstd_tok, h_ch):
            saved = C.S
            C.S = S_
            t0 = tile * 128
            slot = tile // 4
            tsl = slice(t0, t0 + 128)
            for which, (src, toks) in enumerate(((attnT, attn_tok[slot]), (ssmT, [ssm_tok[slot]]))):
                S_.op("act", lambda e, src=src: e.activation(out=sq4[:], in_=src[:, :, tsl], func=AF.Square),
                     reads=toks, writes=[sq_tok])
                b_ = nbk_()
                for hp in range(4):
                    S_.op("pe", lambda e, hp=hp, b_=b_: e.matmul(ps[b_][:, 0:1], lhsT=sq4[:, hp, :], rhs=C.ones_bf[:, 0:1],
                                                                start=(hp == 0), stop=(hp == 3)),
                         reads=[sq_tok, C.const_tok], writes=[ps_tok[b_]])
                S_.op("act", lambda e, which=which, b_=b_: e.activation(out=rs2[:, which:which + 1], in_=ps[b_][:, 0:1], func=AF.Ln,
                                                                       bias=C.eps_col[:], scale=1.0 / 512),
                     reads=[ps_tok[b_], C.const_tok], writes=[rs2_tok])
            S_.op("act", lambda e: e.activation(out=rs2[:], in_=rs2[:], func=AF.Exp, scale=-0.5), reads=[rs2_tok], writes=[rs2_tok])
            S_.dma("sp", h_ch, h[:], I["x_own"][t0:t0 + 128, :], writes=[h_tok])
            for which, (src, toks) in enumerate(((attnT, attn_tok[slot]), (ssmT, [ssm_tok[slot]]))):
                for n2 in range(2):
                    b_ = nbk_()
                    for hp in range(4):
                        S_.op("pe", lambda e, hp=hp, b_=b_, src=src, which=which, n2=n2: e.matmul(
                            ps[b_][:], lhsT=src[:, hp, tsl], rhs=wo[:, which * 4 + hp, n2 * 512:(n2 + 1) * 512],
                            start=(hp == 0), stop=(hp == 3)), reads=toks + [wo_tok], writes=[ps_tok[b_]])
                    S_.op("dve", lambda e, b_=b_, which=which, n2=n2: e.scalar_tensor_tensor(
                        out=h[:, n2 * 512:(n2 + 1) * 512], in0=ps[b_][:], scalar=rs2[:, which:which + 1],
                        in1=h[:, n2 * 512:(n2 + 1) * 512], op0=ALU.mult, op1=ALU.add),
                        reads=[ps_tok[b_], rs2_tok], writes=[h_tok])
            if dbg == "h1":
                S_.dma("sp", h_ch, y[t0:t0 + 128, :], h[:], reads=[h_tok], writes=[h2_tok[tile]])
                C.S = saved
                return
            _rms_rstd(C, S_, h[:], h_tok, hn[:], hn_tok, ss, ss_tok, rstd, rstd_tok)
            S_.op("dve", lambda e: e.tensor_scalar(hn[:], h[:], rstd[:], None, ALU.mult), reads=[h_tok, rstd_tok], writes=[hn_tok])
            ptb, pt_tok = transpose_to(C, hn, hn_tok, None, None)
            S_.op("act", lambda e, ptb=ptb: e.copy(out=hnT[:], in_=ptb.rearrange("p (k t) -> p k t", k=8)),
                 reads=[pt_tok], writes=[hnT_tok])
            for half in range(2):
                b_ = nbk_()
                for j4 in range(4):
                    hc = half * 4 + j4
                    for kt in range(8):
                        S_.op("pe", lambda e, kt=kt, hc=hc, j4=j4, b_=b_: e.matmul(
                            ps[b_][:, j4 * 128:(j4 + 1) * 128], lhsT=wxq[:, kt, hc * 128:(hc + 1) * 128], rhs=hnT[:, kt, :],
                            start=(kt == 0), stop=(kt == 7)), reads=[wxq_tok, hnT_tok], writes=[ps_tok[b_]])
                S_.op("act", lambda e, half=half, b_=b_: e.mul(out=qT[:, half * 4:(half + 1) * 4, :],
                                                             in_=ps[b_][:].rearrange("p (a b) -> p a b", a=4), mul=0.0625),
                     reads=[ps_tok[b_]], writes=[qT_tok])
            sb_ = [nbk_(), nbk_()]
            for hh in range(4):
                b_ = sb_[hh // 2]
                for c2 in range(2):
                    S_.op("pe", lambda e, hh=hh, c2=c2, b_=b_: e.matmul(
                        ps[b_][:, (hh % 2) * 256:(hh % 2 + 1) * 256], lhsT=qT[:, 2 * hh + c2, :], rhs=KmT[:, 2 * hh + c2, :],
                        start=(c2 == 0), stop=(c2 == 1)), reads=[qT_tok, kv_tok], writes=[ps_tok[b_]])
            for i2 in range(2):
                b_ = sb_[i2]
                S_.op("dve", lambda e, i2=i2, b_=b_: e.tensor_reduce(out=mx[:, 2 * i2:2 * i2 + 2],
                                                                    in_=ps[b_][:].rearrange("p (a b) -> p a b", a=2),
                                                                    axis=AX.X, op=ALU.max),
                     reads=[ps_tok[b_]], writes=[sm_tok])
            S_.op("dve", lambda e: e.tensor_scalar(mx[:], mx[:], -1.0, None, ALU.mult), reads=[sm_tok], writes=[sm_tok])
            for hh in range(4):
                b_ = sb_[hh // 2]
                S_.op("act", lambda e, hh=hh, b_=b_: e.activation(out=pp[:, hh, :], in_=ps[b_][:, (hh % 2) * 256:(hh % 2 + 1) * 256],
                                                                 func=AF.Exp, bias=mx[:, hh:hh + 1], accum_out=sm[:, hh:hh + 1]),
                     reads=[ps_tok[b_], sm_tok], writes=[pp_tok, sm_tok])
            S_.op("dve", lambda e: e.reciprocal(out=sm[:], in_=sm[:]), reads=[sm_tok], writes=[sm_tok])
            for hh in range(4):
                S_.op("dve", lambda e, hh=hh: e.tensor_scalar(pp[:, hh, :], pp[:, hh, :], sm[:, hh:hh + 1], None, ALU.mult),
                     reads=[sm_tok, pp_tok], writes=[pp_tok])
            ptb, pt_tok = transpose_to(C, pp[:].rearrange("p a b -> p (a b)"), pp_tok, None, None)
            S_.op("act", lambda e, ptb=ptb: e.copy(out=pT[:], in_=ptb.rearrange("p (k t) -> p k t", k=8)),
                 reads=[pt_tok], writes=[pT_tok])
            for half in range(2):
                b_ = nbk_()
                for j4 in range(4):
                    hc = half * 4 + j4
                    hh, c2 = hc // 2, hc % 2
                    for mt in range(2):
                        S_.op("pe", lambda e, mt=mt, hh=hh, c2=c2, j4=j4, b_=b_: e.matmul(
                            ps[b_][:, j4 * 128:(j4 + 1) * 128], lhsT=Vm[:, mt, hh * 256 + c2 * 128:hh * 256 + (c2 + 1) * 128],
                            rhs=pT[:, 2 * hh + mt, :], start=(mt == 0), stop=(mt == 1)),
                            reads=[kv_tok, pT_tok], writes=[ps_tok[b_]])
                S_.op("act", lambda e, half=half, b_=b_: e.copy(out=oT[:, half * 4:(half + 1) * 4, :],
                                                              in_=ps[b_][:].rearrange("p (a b) -> p a b", a=4)),
                     reads=[ps_tok[b_]], writes=[oT_tok])
            for n2 in range(2):
                b_ = nbk_()
                for hc in range(8):
                    S_.op("pe", lambda e, hc=hc, n2=n2, b_=b_: e.matmul(ps[b_][:], lhsT=oT[:, hc, :], rhs=wxo[:, hc, n2 * 512:(n2 + 1) * 512],
                                                                      start=(hc == 0), stop=(hc == 7)),
                         reads=[oT_tok, wxo_tok], writes=[ps_tok[b_]])
                S_.op("dve", lambda e, n2=n2, b_=b_: e.tensor_tensor(out=h[:, n2 * 512:(n2 + 1) * 512], in0=ps[b_][:],
                                                                    in1=h[:, n2 * 512:(n2 + 1) * 512], op=ALU.add),
                     reads=[ps_tok[b_]], writes=[h_tok])
            dst = y if dbg == "h2" else h2buf
            S_.dma("sp", h_ch, dst[t0:t0 + 128, :], h[:], reads=[h_tok], writes=[h2_tok[tile]])

            C.S = saved

        sets1 = [mkset("_e"), mkset("_o")]
        bk = [[0], [0]]

        def mk_nbk(par):
            def f():
                b = 3 * par + bk[par][0] % 3
                bk[par][0] += 1
                return b
            return f

        for t2 in range(0, NT, 2):
            recs = []
            for par in range(2):
                r_ = _Rec1()
                C.tp_force = par
                emit_tile(r_, t2 + par, mk_nbk(par), **sets1[par])
                C.tp_force = None
                recs.append(r_.items)
            while recs[0] or recs[1]:
                for par in range(2):
                    for _ in range(6):
                        if recs[par]:
                            kind, a, k = recs[par].pop(0)
                            getattr(S, kind)(*a, **k)
        S.barrier()
    if dbg in ("h1", "h2"):
        S.wait_tok("sp", h2_tok)
        eo.close()
        return

    with ExitStack() as e2:
        h = _sb(nc, e2, "h_b", [128, D], F32)
        h_tok = Tok()
        h_ch = S.chan()
        hn = _sb(nc, e2, "hn_b", [128, D], BF16)
        hn_tok = Tok()
        hnT = _sb(nc, e2, "hnT_b", [128, 8, 128], BF16)
        hnT_tok = Tok()
        hn3 = _sb(nc, e2, "hn3", [128, D], F32)
        hn3_tok = Tok()
        ss = _sb(nc, e2, "qss", [128, 1], F32)
        ss_tok = Tok()
        rstd = _sb(nc, e2, "qrstd", [128, 1], F32)
        rstd_tok = Tok()
        qpT = _sb(nc, e2, "qpT", [128, 16, 128], BF16)
        qpT_tok = Tok()
        sc = _sb(nc, e2, "sc", [128, 16, 128], F32)
        sc_tok = Tok()
        scr = _sb(nc, e2, "scr", [128, 2048], F32)
        scr_tok = Tok()
        hv = _sb(nc, e2, "hv", [128, 16, 16], F32)
        hi = _sb(nc, e2, "hi", [128, 16, 16], U32)
        hif = _sb(nc, e2, "hif", [128, 16, 16], F32)
        hv_tok = Tok()
        cand = _sb(nc, e2, "cand", [128, 8, 256], F32)
        eidx = _sb(nc, e2, "eidx", [128, 8, 256], F32)
        e0 = _sb(nc, e2, "e0", [128, 8, 16], F32)
        cand_tok = Tok()
        bv = _sb(nc, e2, "bv", [128, 8, 16], F32)
        bp = _sb(nc, e2, "bp", [128, 8, 16], U32)
        bpf = _sb(nc, e2, "bpf", [128, 8, 16], F32)
        bv_tok = Tok()
        junk = [_sb(nc, e2, "junk256_%d" % i, [128, 256], F32) for i in range(4)]
        junk_tok = [Tok() for _ in range(4)]
        eidc_tok = [Tok() for _ in range(128)]
        jq = [0]
        eidf = _sb(nc, e2, "eidf", [128, 128], F32)
        eid = _sb(nc, e2, "eid", [128, 128], U32)
        eid_tok = Tok()
        gt = _sb(nc, e2, "gt", [128, 8, 16], F32)
        gs = _sb(nc, e2, "gs", [128, 8], F32)
        nb0 = _sb(nc, e2, "nb0", [128, 8], F32)
        gt_tok = Tok()
        actc = _sb(nc, e2, "actc", [128, 128], F32)
        act_tok = Tok()
        wgt = _sb(nc, e2, "wgt", [128, 128], F32)
        wg2 = _sb(nc, e2, "wg2", [128, 128], F32)
        wgt_tok = Tok()
        NG = 8
        gbuf = [_sb(nc, e2, "gbuf%d" % i, [128, 2 * D], BF16) for i in range(NG)]
        gbuf_tok = [Tok() for _ in range(NG)]
        gbuf_ch = [S.chan() for _ in range(NG)]
        junk2 = [_sb(nc, e2, "junk2_%d" % i, [128, D], BF16) for i in range(2)]
        junk2_tok = [Tok(), Tok()]
        j2 = [0]
        NDG = 4
        dg = [_sb(nc, e2, "dg%d" % i, [128, 128], BF16) for i in range(NDG)]
        dg_tok = [Tok() for _ in range(NDG)]
        slot_tok = [Tok() for _ in range(128)]
        grp_tok = [Tok() for _ in range(32)]
        di = 0
        ytok = Tok()
        gi = 0
        PLAY_N = 15
        bankA = [0]

        def nbkA():
            b = bankA[0] % 4
            bankA[0] += 1
            return b

        class _Rec:
            def __init__(self):
                self.items = []

            def op(self, *a, **k):
                self.items.append(("op", a, k))

            def dma(self, *a, **k):
                self.items.append(("dma", a, k))

        junkF = _sb(nc, e2, "junkF", [128, D], BF16)
        junkF_tok = Tok()
        ss2 = _sb(nc, e2, "ss2", [128, 1], F32)
        ss2_tok = Tok()
        rstd2 = _sb(nc, e2, "rstd2", [128, 1], F32)
        rstd2_tok = Tok()
        obuf = _sb(nc, e2, "obuf", [128, D], F32)
        obuf_tok = Tok()
        o_ch = S.chan()
        h_b2 = _sb(nc, e2, "h_b2", [128, D], F32)
        hn3_b2 = _sb(nc, e2, "hn3_b2", [128, D], F32)
        eid_b2 = _sb(nc, e2, "eid_b2", [128, 128], U32)
        gt_b2 = _sb(nc, e2, "gt_b2", [128, 8, 16], F32)
        sets = [dict(h=h, h_tok=h_tok, h_ch=h_ch, hn3=hn3, hn3_tok=hn3_tok, eid=eid, eid_tok=eid_tok, gt=gt, gt_tok=gt_tok),
                dict(h=h_b2, h_tok=Tok(), h_ch=S.chan(), hn3=hn3_b2, hn3_tok=Tok(), eid=eid_b2, eid_tok=Tok(), gt=gt_b2, gt_tok=Tok())]

        def emitA(S_, tile, h, h_tok, h_ch, hn3, hn3_tok, eid, eid_tok, gt, gt_tok):
            saved = C.S
            C.S = S_
            t0 = tile * 128
            S_.dma("sp", h_ch, h[:], h2buf[t0:t0 + 128, :], reads=[h2_tok[tile]], writes=[h_tok])
            _rms_rstd(C, S_, h[:], h_tok, hn[:], hn_tok, ss, ss_tok, rstd, rstd_tok)
            S_.op("dve", lambda e: e.tensor_scalar(hn[:], h[:], rstd[:], None, ALU.mult), reads=[h_tok, rstd_tok], writes=[hn_tok])
            S_.op("dve", lambda e: e.scalar_tensor_tensor(out=hn3[:], in0=h[:], scalar=rstd[:], in1=gffn_rep[:], op0=ALU.mult, op1=ALU.mult),
                 reads=[h_tok, rstd_tok, pg_tok], writes=[hn3_tok])
            ptb, pt_tok = transpose_to(C, hn, hn_tok, None, None)
            S_.op("act", lambda e, ptb=ptb: e.copy(out=hnT[:], in_=ptb.rearrange("p (k t) -> p k t", k=8)),
                 reads=[pt_tok], writes=[hnT_tok])
            for q4 in range(4):
                b_ = nbkA()
                for j4 in range(4):
                    ch = q4 * 4 + j4
                    for kt in range(8):
                        S_.op("pe", lambda e, kt=kt, ch=ch, j4=j4, b_=b_: e.matmul(
                            ps[b_][:, j4 * 128:(j4 + 1) * 128], lhsT=wpq[:, kt, ch * 128:(ch + 1) * 128], rhs=hnT[:, kt, :],
                            start=(kt == 0), stop=(kt == 7)), reads=[pw_tok, hnT_tok], writes=[ps_tok[b_]])
                S_.op("act", lambda e, q4=q4, b_=b_: e.copy(out=qpT[:, q4 * 4:(q4 + 1) * 4, :],
                                                          in_=ps[b_][:].rearrange("p (a b) -> p a b", a=4)),
                     reads=[ps_tok[b_]], writes=[qpT_tok])
            for q4 in range(4):
                b_ = nbkA()
                for j4 in range(4):
                    ch = q4 * 4 + j4
                    S_.op("pe", lambda e, ch=ch, j4=j4, b_=b_: e.matmul(ps[b_][:, j4 * 128:(j4 + 1) * 128], lhsT=qpT[:, ch, :],
                                                                      rhs=skT[:, ch, :], start=True, stop=True),
                         reads=[pw_tok, qpT_tok], writes=[ps_tok[b_]])
                S_.op("act", lambda e, q4=q4, b_=b_: e.copy(out=sc[:, q4 * 4:(q4 + 1) * 4, :],
                                                          in_=ps[b_][:].rearrange("p (a b) -> p a b", a=4)),
                     reads=[ps_tok[b_]], writes=[sc_tok])
            scr3 = scr[:].rearrange("p (a b) -> p a b", a=16)
            for ch in range(16):
                S_.op("dve", lambda e, ch=ch: e.max(out=hv[:, ch, 0:8], in_=sc[:, ch, :]), reads=[sc_tok], writes=[hv_tok])
                S_.op("dve", lambda e, ch=ch: e.max_index(out=hi[:, ch, 0:8], in_max=hv[:, ch, 0:8], in_values=sc[:, ch, :]),
                     reads=[sc_tok, hv_tok], writes=[hv_tok])
                S_.op("dve", lambda e, ch=ch: e.match_replace(out=scr3[:, ch, :], in_to_replace=hv[:, ch, 0:8], in_values=sc[:, ch, :],
                                                             imm_value=NEG), reads=[sc_tok, hv_tok], writes=[scr_tok])
                S_.op("dve", lambda e, ch=ch: e.max(out=hv[:, ch, 8:16], in_=scr3[:, ch, :]), reads=[scr_tok], writes=[hv_tok])
                S_.op("dve", lambda e, ch=ch: e.max_index(out=hi[:, ch, 8:16], in_max=hv[:, ch, 8:16], in_values=scr3[:, ch, :]),
                     reads=[scr_tok, hv_tok], writes=[hv_tok])
            S_.op("dve", lambda e: e.tensor_copy(out=hif[:], in_=hi[:]), reads=[hv_tok], writes=[hv_tok])
            hv4 = hv[:].rearrange("p (h i) k -> p h i k", i=2)
            hif4 = hif[:].rearrange("p (h i) k -> p h i k", i=2)
            cand4 = cand[:].rearrange("p h (a b) -> p h a b", a=16)
            eidx4 = eidx[:].rearrange("p h (a b) -> p h a b", a=16)
            S_.op("dve", lambda e: e.tensor_tensor(out=cand4, in0=hv4[:, :, 0, :].unsqueeze(3).to_broadcast([128, 8, 16, 16]),
                                                  in1=hv4[:, :, 1, :].unsqueeze(2).to_broadcast([128, 8, 16, 16]), op=ALU.add),
                 reads=[hv_tok], writes=[cand_tok])
            S_.op("dve", lambda e: e.tensor_scalar(e0[:], hif4[:, :, 0, :], 128.0, None, ALU.mult), reads=[hv_tok], writes=[cand_tok])
            S_.op("dve", lambda e: e.tensor_tensor(out=eidx4, in0=e0[:].unsqueeze(3).to_broadcast([128, 8, 16, 16]),
                                                  in1=hif4[:, :, 1, :].unsqueeze(2).to_broadcast([128, 8, 16, 16]), op=ALU.add),
                 reads=[hv_tok, cand_tok], writes=[cand_tok])
            scr8 = scr[:].rearrange("p (a b) -> p a b", a=8)
            for hh in range(8):
                S_.op("dve", lambda e, hh=hh: e.max(out=bv[:, hh, 0:8], in_=cand[:, hh, :]), reads=[cand_tok], writes=[bv_tok])
                S_.op("dve", lambda e, hh=hh: e.max_index(out=bp[:, hh, 0:8], in_max=bv[:, hh, 0:8], in_values=cand[:, hh, :]),
                     reads=[cand_tok, bv_tok], writes=[bv_tok])
                S_.op("dve", lambda e, hh=hh: e.match_replace(out=scr8[:, hh, :], in_to_replace=bv[:, hh, 0:8], in_values=cand[:, hh, :],
                                                             imm_value=NEG), reads=[cand_tok, bv_tok], writes=[scr_tok])
                S_.op("dve", lambda e, hh=hh: e.max(out=bv[:, hh, 8:16], in_=scr8[:, hh, :]), reads=[scr_tok], writes=[bv_tok])
                S_.op("dve", lambda e, hh=hh: e.max_index(out=bp[:, hh, 8:16], in_max=bv[:, hh, 8:16], in_values=scr8[:, hh, :]),
                     reads=[scr_tok, bv_tok], writes=[bv_tok])
            S_.op("dve", lambda e: e.tensor_copy(out=bpf[:], in_=bp[:]), reads=[bv_tok], writes=[bv_tok])
            for hh in range(8):
                for k in range(16):
                    s_ = hh * 16 + k
                    jb = jq[0] % 4
                    jq[0] += 1
                    S_.op("dve", lambda e, hh=hh, k=k, s_=s_, jb=jb: e.scalar_tensor_tensor(
                        out=junk[jb][:], in0=iota256[:], scalar=bpf[:, hh, k:k + 1], in1=eidx[:, hh, :],
                        op0=ALU.is_equal, op1=ALU.mult, accum_out=eidf[:, s_:s_ + 1]),
                        reads=[bv_tok, cand_tok, pg_tok], writes=[junk_tok[jb], eidc_tok[s_]])
            S_.op("dve", lambda e: e.tensor_scalar(eidf[:], eidf[:], 16383.0, 0.0, ALU.min, ALU.max), reads=[eid_tok] + eidc_tok,
                  writes=[eid_tok] + eidc_tok)
            S_.op("dve", lambda e: e.tensor_copy(out=eid[:], in_=eidf[:]), reads=[eid_tok], writes=[eid_tok])
            S_.op("dve", lambda e: e.tensor_scalar(nb0[:], bv[:, :, 0], -1.0, None, ALU.mult), reads=[bv_tok], writes=[gt_tok])
            for hh in range(8):
                S_.op("act", lambda e, hh=hh: e.activation(out=gt[:, hh, :], in_=bv[:, hh, :], func=AF.Exp, bias=nb0[:, hh:hh + 1],
                                                          accum_out=gs[:, hh:hh + 1]), reads=[bv_tok, gt_tok], writes=[gt_tok])
            S_.op("dve", lambda e: e.reciprocal(out=gs[:], in_=gs[:]), reads=[gt_tok], writes=[gt_tok])
            S_.op("dve", lambda e: e.tensor_tensor(out=gt[:], in0=gt[:], in1=gs[:].unsqueeze(2).to_broadcast([128, 8, 16]), op=ALU.mult),
                 reads=[gt_tok], writes=[gt_tok])

            C.S = saved

        def emitB(tile, play, h, h_tok, h_ch, hn3, hn3_tok, eid, eid_tok, gt, gt_tok):
            nonlocal gi, di
            t0 = tile * 128
            pa, pb_ = 4, 5
            gtf = gt[:].rearrange("p a b -> p (a b)")
            for grp in range(32):
                used = []
                for q in range(4):
                    s_ = grp * 4 + q
                    g_ = gi % NG
                    gi += 1
                    used.append(g_)
                    S.dma("pool", gbuf_ch[g_], gbuf[g_][:], uvbf[:, :], reads=[eid_tok] + uv_toks, writes=[gbuf_tok[g_]],
                          indirect=bass.IndirectOffsetOnAxis(ap=eid[:, s_:s_ + 1], axis=0))
                    jb = j2[0] % 2
                    j2[0] += 1
                    S.op("dve", lambda e, g_=g_, s_=s_, jb=jb: e.scalar_tensor_tensor(out=junk2[jb][:], in0=gbuf[g_][:, 0:D], scalar=1.0, in1=hn3[:],
                                                                                     op0=ALU.mult, op1=ALU.mult, accum_out=actc[:, s_:s_ + 1]),
                         reads=[gbuf_tok[g_], hn3_tok], writes=[junk2_tok[jb], slot_tok[s_]])
                cs = slice(grp * 4, grp * 4 + 4)
                gk = grp_tok[grp]
                S.op("dve", lambda e: e.tensor_tensor(out=wg2[:, cs], in0=actc[:, cs], in1=actc[:, cs], op=ALU.mult),
                     reads=[slot_tok[grp * 4 + q] for q in range(4)], writes=[gk])
                S.op("dve", lambda e: e.tensor_scalar(wg2[:, cs], wg2[:, cs], 0.044715, 1.0, ALU.mult, ALU.add), reads=[gk], writes=[gk])
                S.op("dve", lambda e: e.tensor_tensor(out=wg2[:, cs], in0=wg2[:, cs], in1=actc[:, cs], op=ALU.mult),
                     reads=[gk] + [slot_tok[grp * 4 + q] for q in range(4)], writes=[gk])
                S.op("act", lambda e: e.activation(out=wg2[:, cs], in_=wg2[:, cs], func=AF.Sigmoid, scale=1.5957691216057308),
                     reads=[gk], writes=[gk])
                S.op("dve", lambda e: e.tensor_tensor(out=wgt[:, cs], in0=wg2[:, cs], in1=actc[:, cs], op=ALU.mult),
                     reads=[gk] + [slot_tok[grp * 4 + q] for q in range(4)], writes=[gk])
                S.op("dve", lambda e: e.tensor_tensor(out=wgt[:, cs], in0=wgt[:, cs], in1=gtf[:, cs], op=ALU.mult),
                     reads=[gk, gt_tok], writes=[gk])
                for q in range(4):
                    s_ = grp * 4 + q
                    g_ = used[q]
                    d_ = di % NDG
                    di += 1
                    S.op("act", lambda e, d_=d_, s_=s_: e.activation(out=dg[d_][:], in_=C.ident_bf[:], func=AF.Copy,
                                                                    scale=wgt[:, s_:s_ + 1]),
                         reads=[gk, C.const_tok], writes=[dg_tok[d_]])
                    for n2, pbk in enumerate((pa, pb_)):
                        S.op("pe", lambda e, d_=d_, g_=g_, n2=n2, pbk=pbk, s_=s_: e.matmul(
                            ps[pbk][:], lhsT=dg[d_][:], rhs=gbuf[g_][:, D + n2 * 512:D + (n2 + 1) * 512],
                            start=(s_ == 0), stop=(s_ == 127)), reads=[dg_tok[d_], gbuf_tok[g_]], writes=[ps_tok[pbk]])
                play(PLAY_N)
            for n2, pbk in enumerate((pa, pb_)):
                S.op("dve", lambda e, n2=n2, pbk=pbk: e.tensor_tensor(out=h[:, n2 * 512:(n2 + 1) * 512], in0=ps[pbk][:],
                                                                      in1=h[:, n2 * 512:(n2 + 1) * 512], op=ALU.add),
                     reads=[ps_tok[pbk]], writes=[h_tok])
            if dbg == "h3":
                S.dma("sp", h_ch, y[t0:t0 + 128, :], h[:], reads=[h_tok], writes=[ytok])
                play(10 ** 9)
                return
            _rms_rstd(C, S, h[:], h_tok, junkF[:], junkF_tok, ss2, ss2_tok, rstd2, rstd2_tok)
            S.op("dve", lambda e: e.scalar_tensor_tensor(out=obuf[:], in0=h[:], scalar=rstd2[:], in1=gfin_rep[:], op0=ALU.mult, op1=ALU.mult),
                 reads=[h_tok, rstd2_tok, pg_tok], writes=[obuf_tok])
            S.dma("sp", o_ch, y[t0:t0 + 128, :], obuf[:], reads=[obuf_tok], writes=[ytok])
            play(10 ** 9)

        rec = _Rec()
        emitA(rec, 0, **sets[0])
        for kind, a, k in rec.items:
            getattr(S, kind)(*a, **k)
        for tile in range(NT):
            rec = _Rec()
            if tile + 1 < NT:
                emitA(rec, tile + 1, **sets[(tile + 1) % 2])
            items = rec.items

            def play(n, items=items):
                while n > 0 and items:
                    kind, a, k = items.pop(0)
                    getattr(S, kind)(*a, **k)
                    n -= 1

            emitB(tile, play, **sets[tile % 2])
            play(10 ** 9)
        S.wait_tok("sp", [ytok])
        S.barrier()
    eo.close()


def build(dbg=None):
    nc = bass.Bass("TRN2", target_bir_lowering=False)
    C = Ctx()
    C.nc = nc
    C.dbg = dbg

    def din(name, shape, dt=F32):
        return nc.dram_tensor(name, list(shape), dt, kind="ExternalInput").ap()

    x_full = din("x_full", [SEQ, D])
    x_own = din("x_own", [NOWN * SEGT, D])
    qpos = din("qpos", [128, NOWN, SEGT])
    w_in = din("w_in", [128, 8, 2048])
    g_mix = din("g_mix", [128, 8])
    x_rev = din("x_rev", [SEQ, D])
    I = {"x_full": x_full, "x_own": x_own, "w_in": w_in, "x_rev": x_rev}
    for nm, shp in (("a_re", [128, 16]), ("a_im", [128, 16]), ("log_dt", [128, 16]),
                    ("bpad_re", [128, 16, 128]), ("bpad_im", [128, 16, 128]),
                    ("cpad_re", [128, 16, 128]), ("cpad_im", [128, 16, 128]),
                    ("d_skip", [128, 4]), ("w_glu", [128, 4, 512]), ("b_glu", [128, 4]),
                    ("segsel", [128, 4, 16]),
                    ("w_out", [128, 8, D]), ("g_out", [128, 8]), ("mem", [256, D]), ("g_mem", [128, 8]),
                    ("w_xkv", [128, 8, 2048]), ("g_xattn", [128, 8]), ("w_xq", [128, 8, D]), ("w_xo", [128, 8, D]),
                    ("g_ffn", [128, 8]), ("gffn_rep", [128, D]), ("gfin_rep", [128, D]), ("w_pq", [128, 8, 2048]),
                    ("skT", [128, 16, 128]), ("peer_u", [16384, D]), ("peer_v", [16384, D])):
        I[nm] = din(nm, shp)
    y = nc.dram_tensor("y", [NOWN * SEGT, D], F32, kind="ExternalOutput").ap()
    if dbg == "attn":
        dbg_out = nc.dram_tensor("dbg_attn", [128, 4, NOWN * SEGT], F32, kind="ExternalOutput").ap()

    with ExitStack() as es:
        S = Sched(nc, es)
        C.S = S
        C.const_tok = Tok()
        ident_f = _sb(nc, es, "ident_f", [128, 128], F32)
        C.ident_f = ident_f
        C.ident_bf = _sb(nc, es, "ident_bf", [128, 128], BF16)
        ones_f = _sb(nc, es, "ones_f", [128, 128], F32)
        C.ones_bf = _sb(nc, es, "ones_bf", [128, 128], BF16)
        C.tri_bf = _sb(nc, es, "tri_bf", [128, 128], BF16)
        C.atri_bf = _sb(nc, es, "atri_bf", [128, 128], BF16)
        C.eps_col = _sb(nc, es, "eps_col", [128, 1], F32)
        C.kpos = _sb(nc, es, "kpos", [128, 64], F32)
        S.op("pool", lambda e: e.memset(ones_f[:], 1.0), writes=[C.const_tok])
        S.op("pool", lambda e: e.memset(C.eps_col[:], EPS), writes=[C.const_tok])
        S.op("pool", lambda e: e.affine_select(out=ident_f[:], in_=ones_f[:], pattern=[[-1, 128]],
                                               compare_op=ALU.is_equal, fill=0.0, base=0,
                                               channel_multiplier=1),
             reads=[C.const_tok], writes=[C.const_tok])
        S.op("pool", lambda e: e.tensor_copy(out=C.ident_bf[:], in_=ident_f[:]),
             reads=[C.const_tok], writes=[C.const_tok])
        S.op("pool", lambda e: e.tensor_copy(out=C.ones_bf[:], in_=ones_f[:]),
             reads=[C.const_tok], writes=[C.const_tok])
        S.op("pool", lambda e: e.affine_select(out=C.tri_bf[:], in_=ones_f[:], pattern=[[-1, 128]],
                                               compare_op=ALU.is_ge, fill=0.0, base=0,
                                               channel_multiplier=1),
             reads=[C.const_tok], writes=[C.const_tok])
        S.op("pool", lambda e: e.affine_select(out=C.atri_bf[:], in_=ones_f[:], pattern=[[1, 128]],
                                               compare_op=ALU.is_gt, fill=0.0, base=0,
                                               channel_multiplier=-1),
             reads=[C.const_tok], writes=[C.const_tok])
        S.op("pool", lambda e: e.iota(C.kpos[:], pattern=[[128, 64]], base=0, channel_multiplier=1,
                                      allow_small_or_imprecise_dtypes=True),
             writes=[C.const_tok])

        ps = [es.enter_context(nc.psum_tensor("ps%d" % i, [128, 512], F32)) for i in range(8)]
        ps_tok = [Tok() for _ in range(8)]
        C.tp_ps = [ps[6], ps[7]]
        C.tp_tok = [ps_tok[6], ps_tok[7]]
        C.tp_i = 0

        attnT = _sb(nc, es, "attnT", [128, 4, NOWN * SEGT], BF16)
        attn_tok = [[Tok() for _ in range(8)] for _ in range(NOWN)]

        gmix_sb = _sb(nc, es, "gmix", [128, 8], F32)
        gmix_tok = Tok()
        S.dma("sp", S.chan(), gmix_sb[:], g_mix[:, :], writes=[gmix_tok])

        with ExitStack() as e2:
          if dbg != "ssm":
              KT = _sb(nc, e2, "KT", [128, 4, SEQ], BF16)
              V = _sb(nc, e2, "V", [128, 64, 512], BF16)
              QT = _sb(nc, e2, "QT", [128, 4, NOWN * SEGT], BF16)
              kt_tok = [Tok() for _ in range(NSEG)]
              v_tok = [Tok() for _ in range(NSEG)]
              q_tok = [Tok() for _ in range(NOWN)]
              with ExitStack() as e3:
                  wk = _sb(nc, e3, "wk", [128, 8, 512], BF16)
                  wq = wk
                  wv = _sb(nc, e3, "wv", [128, 8, 512], BF16)
                  w_tok = Tok()
                  wk_tok = Tok()
                  wv_tok = Tok()
                  xt = [_sb(nc, e3, "xt%d" % i, [128, D], F32) for i in range(2)]
                  xt_tok = [Tok() for _ in range(2)]
                  xt_ch = [S.chan() for _ in range(2)]
                  wi_box = [0]

                  def load_w(wdst, c0, wtok):
                      for k2 in range(4):
                          sl = wi_box[0] % 2
                          wi_box[0] += 1
                          S.dma("sp", xt_ch[sl], xt[sl][:].rearrange("p (a b) -> p a b", a=2),
                                w_in[:, 2 * k2:2 * k2 + 2, c0:c0 + 512], writes=[xt_tok[sl]])
                          for a2 in range(2):
                              kt = 2 * k2 + a2
                              eng = "dve" if a2 == 0 else "pool"
                              S.op(eng, lambda e, kt=kt, sl=sl, wdst=wdst, a2=a2: e.tensor_scalar(
                                  wdst[:, kt, :], xt[sl][:, a2 * 512:(a2 + 1) * 512], gmix_sb[:, kt:kt + 1], None, ALU.mult),
                                  reads=[xt_tok[sl], gmix_tok], writes=[wtok])

                  load_w(wk, 512, wk_tok)
                  load_w(wv, 1024, wv_tok)

                  xn = [_sb(nc, e3, "xn%d" % i, [128, D], BF16) for i in range(2)]
                  xn_tok = [Tok(), Tok()]
                  ss = [_sb(nc, e3, "ss%d" % i, [128, 1], F32) for i in range(2)]
                  ss_tok = [Tok(), Tok()]
                  rstd = [_sb(nc, e3, "rstd%d" % i, [128, 1], F32) for i in range(2)]
                  rstd_tok = [Tok(), Tok()]
                  xnT = [_sb(nc, e3, "xnT%d" % i, [128, 8, SEGT], BF16) for i in range(2)]
                  xnT_tok = [Tok(), Tok()]
                  ti = 0
                  for sgi in range(NSEG + NOWN):
                      own = sgi >= NSEG
                      sg = sgi - NSEG if own else sgi
                      if sgi == NSEG:
                          load_w(wq, 0, wk_tok)
                      src = x_own if own else x_full
                      xb = sgi % 2
                      for m in range(4):
                          a = ti % 2
                          b = ti % 2
                          ti += 1
                          r0 = sg * SEGT + m * 128
                          S.dma("sp", xt_ch[a], xt[a][:], src[r0:r0 + 128, :], writes=[xt_tok[a]])
                          rms_scale(C, xt[a][:], xt_tok[a], rstd[b][:], rstd_tok[b], xn[b][:], xn_tok[b],
                                    ss[b][:], ss_tok[b])
                          S.op("dve", lambda e, a=a, b=b: e.tensor_scalar(xn[b][:], xt[a][:], rstd[b][:], None, ALU.mult),
                               reads=[xt_tok[a], rstd_tok[b]], writes=[xn_tok[b]])
                          ptb, pt_tok = transpose_to(C, xn[b], xn_tok[b], None, None)
                          S.op("dve", lambda e, xb=xb, m=m, ptb=ptb: e.tensor_copy(
                              out=xnT[xb][:, :, m * 128:(m + 1) * 128],
                              in_=ptb.rearrange("p (k t) -> p k t", k=8)),
                              reads=[pt_tok], writes=[xnT_tok[xb]])
                      if not own:
                          for hp in range(4):
                              pb = hp % 2
                              for kt in range(8):
                                  S.op("pe", lambda e, kt=kt, hp=hp, pb=pb, xb=xb: e.matmul(
                                      ps[pb][:], lhsT=wk[:, kt, hp * 128:(hp + 1) * 128], rhs=xnT[xb][:, kt, :],
                                      start=(kt == 0), stop=(kt == 7)),
                                      reads=[wk_tok, xnT_tok[xb]], writes=[ps_tok[pb]])
                              S.op("act", lambda e, hp=hp, pb=pb, sg=sg: e.copy(
                                  out=KT[:, hp, sg * SEGT:(sg + 1) * SEGT], in_=ps[pb][:]),
                                  reads=[ps_tok[pb]], writes=[kt_tok[sg]])
                          for m in range(4):
                              pb = 2 + m % 2
                              for kt in range(8):
                                  S.op("pe", lambda e, kt=kt, m=m, pb=pb, xb=xb: e.matmul(
                                      ps[pb][:], lhsT=xnT[xb][:, kt, m * 128:(m + 1) * 128], rhs=wv[:, kt, :],
                                      start=(kt == 0), stop=(kt == 7)),
                                      reads=[wv_tok, xnT_tok[xb]], writes=[ps_tok[pb]])
                              S.op("act", lambda e, m=m, pb=pb, sg=sg: e.copy(
                                  out=V[:, sg * 4 + m, :], in_=ps[pb][:]),
                                  reads=[ps_tok[pb]], writes=[v_tok[sg]])
                      else:
                          for hp in range(4):
                              pb = hp % 2
                              for kt in range(8):
                                  S.op("pe", lambda e, kt=kt, hp=hp, pb=pb, xb=xb: e.matmul(
                                      ps[pb][:], lhsT=wq[:, kt, hp * 128:(hp + 1) * 128], rhs=xnT[xb][:, kt, :],
                                      start=(kt == 0), stop=(kt == 7)),
                                      reads=[wk_tok, xnT_tok[xb]], writes=[ps_tok[pb]])
                              S.op("act", lambda e, hp=hp, pb=pb, sg=sg: e.mul(
                                  out=QT[:, hp, sg * SEGT:(sg + 1) * SEGT], in_=ps[pb][:], mul=0.125),
                                  reads=[ps_tok[pb]], writes=[q_tok[sg]])

              S.barrier()
              if dbg == "kv":
                  dch = S.chan()
                  dk = nc.dram_tensor("dbg_kt", [128, 4, SEQ], BF16, kind="ExternalOutput").ap()
                  dv = nc.dram_tensor("dbg_v", [128, 64, 512], BF16, kind="ExternalOutput").ap()
                  dq = nc.dram_tensor("dbg_q", [128, 4, NOWN * SEGT], BF16, kind="ExternalOutput").ap()
                  dtok = Tok()
                  S.dma("sp", dch, dk[:, :, :], KT[:], reads=kt_tok, writes=[dtok])
                  S.dma("sp", dch, dv[:, :, :], V[:], reads=v_tok, writes=[dtok])
                  S.dma("sp", dch, dq[:, :, :], QT[:], reads=q_tok, writes=[dtok])
                  S.wait_tok("sp", [dtok])
              with ExitStack() as e3:
                if dbg != "kv":
                    NE = 3
                    e_sb = [_sb(nc, e3, "e%d" % i, [128, 512], F32) for i in range(NE)]
                    e_tok = [Tok() for _ in range(NE)]
                    sp_sb = [_sb(nc, e3, "sp%d" % i, [128, 512], BF16) for i in range(3)]
                    sp_tok = [Tok(), Tok(), Tok()]
                    ex_sb = [_sb(nc, e3, "ex%d" % i, [128, 512], BF16) for i in range(2)]
                    ex_tok = [Tok(), Tok()]
                    w_sb = [_sb(nc, e3, "w%d" % i, [128, 512], BF16) for i in range(2)]
                    w_tok2 = [Tok(), Tok()]
                    masks = _sb(nc, e3, "masks", [128, 16, 512], BF16)
                    mask_tok = Tok()
                    qp = _sb(nc, e3, "qp", [128, NOWN, SEGT], F32)
                    qp_tok = Tok()
                    S.dma("sp", S.chan(), qp[:], qpos[:, :, :], writes=[qp_tok])
                    zps = [ps[0], ps[1]]
                    zps_tok = [ps_tok[0], ps_tok[1]]
                    cps = [ps[2], ps[3]]
                    cps_tok = [ps_tok[2], ps_tok[3]]
                    ops_ = [ps[4], ps[5]]
                    ops_tok = [ps_tok[4], ps_tok[5]]

                    for slot in range(NOWN):
                        top = KB_TOP[slot]
                        nb = top + 1
                        for r in range(16):
                            kb = top - r
                            S.op("dve", lambda e, r=r, kb=kb, slot=slot: e.tensor_scalar(
                                masks[:, r, :], qp[:, slot, :], C.kpos[:, kb:kb + 1], None, ALU.is_gt),
                                reads=[qp_tok, C.const_tok], writes=[mask_tok])
                        blocks = [(h, top - r, r) for h in range(8) for r in range(nb)]
                        nblk = len(blocks)

                        def s1(i):
                            h, kb, r = blocks[i]
                            hp, hh = h // 2, h % 2
                            zb = i % 2
                            S.op("pe", lambda e: e.matmul(
                                zps[zb][:], lhsT=KT[hh * 64:(hh + 1) * 64, hp, kb * 128:(kb + 1) * 128],
                                rhs=QT[hh * 64:(hh + 1) * 64, hp, slot * SEGT:(slot + 1) * SEGT],
                                start=True, stop=True),
                                reads=[kt_tok[kb // 4], q_tok[slot]], writes=[zps_tok[zb]])

                        def s2(i):
                            h, kb, r = blocks[i]
                            zb, eb, sb = i % 2, i % NE, i % 3
                            S.op("act", lambda e: e.activation(out=e_sb[eb][:], in_=zps[zb][:], func=AF.Exp),
                                 reads=[zps_tok[zb]], writes=[e_tok[eb]])
                            if r < 16:
                                S.op("dve", lambda e: e.tensor_tensor(out=e_sb[eb][:], in0=e_sb[eb][:],
                                                                       in1=masks[:, r, :], op=ALU.mult),
                                     reads=[mask_tok], writes=[e_tok[eb]])

                        def s2b(i):
                            h, kb, r = blocks[i]
                            zb, eb, sb = i % 2, i % NE, i % 3
                            S.op("act", lambda e: e.activation(out=sp_sb[sb][:], in_=e_sb[eb][:], func=AF.Ln, bias=1.0),
                                 reads=[e_tok[eb]], writes=[sp_tok[sb]])

                        def s3(i):
                            h, kb, r = blocks[i]
                            sb = i % 3
                            cb = h % 2
                            S.op("pe", lambda e: e.matmul(cps[cb][:], lhsT=C.tri_bf[:], rhs=sp_sb[sb][:],
                                                          start=(r == 0), stop=True, skip_group_check=(r != 0)),
                                 reads=[sp_tok[sb], C.const_tok], writes=[cps_tok[cb]])

                        def s3b(i):
                            h, kb, r = blocks[i]
                            if r == nb - 1:
                                return
                            sb = i % 3
                            cb = h % 2
                            S.op("pe", lambda e: e.matmul(cps[cb][:], lhsT=C.atri_bf[:], rhs=sp_sb[sb][:],
                                                          start=False, stop=True, skip_group_check=True),
                                 reads=[sp_tok[sb], C.const_tok], writes=[cps_tok[cb]])

                        def s4(i):
                            h, kb, r = blocks[i]
                            cb, xb_, eb, wb = h % 2, i % 2, i % NE, i % 2
                            S.op("act", lambda e: e.activation(out=ex_sb[xb_][:], in_=cps[cb][:], func=AF.Exp, scale=-1.0),
                                 reads=[cps_tok[cb]], writes=[ex_tok[xb_]])
                            S.op("dve", lambda e: e.tensor_tensor(out=w_sb[wb][:], in0=e_sb[eb][:], in1=ex_sb[xb_][:],
                                                                   op=ALU.mult),
                                 reads=[e_tok[eb], ex_tok[xb_]], writes=[w_tok2[wb]])

                        def s5(i):
                            h, kb, r = blocks[i]
                            wb = i % 2
                            ob = h % 2
                            hp, hh = h // 2, h % 2
                            S.op("pe", lambda e: e.matmul(ops_[ob][hh * 64:(hh + 1) * 64, :], lhsT=V[:, kb, h * 64:(h + 1) * 64],
                                                          rhs=w_sb[wb][:], start=(r == 0), stop=(r == nb - 1)),
                                 reads=[w_tok2[wb], v_tok[kb // 4]], writes=[ops_tok[ob]])
                            if r == nb - 1:
                                S.op("act", lambda e: e.copy(out=attnT[hh * 64:(hh + 1) * 64, hp, slot * SEGT:(slot + 1) * SEGT],
                                                             in_=ops_[ob][hh * 64:(hh + 1) * 64, :]),
                                     reads=[ops_tok[ob]], writes=[attn_tok[slot][h]])

                        for t in range(nblk + 2):
                            if t < nblk:
                                s1(t)
                                s2(t)
                            if 0 <= t - 2 < nblk:
                                s3b(t - 2)
                            if 0 <= t - 1 < nblk:
                                s3(t - 1)
                                s4(t - 1)
                            if t < nblk:
                                s2b(t)
                            if 0 <= t - 2 < nblk:
                                s5(t - 2)

        S.barrier()
        ssmT = _sb(nc, es, "ssmT", [128, 4, NOWN * SEGT], BF16)
        ssm_tok = [Tok() for _ in range(NOWN)]
        if dbg in ("ssm", None, "h1", "h2", "h3"):
            phase_ssm(C, nc, S, ps, ps_tok, I, ssmT, ssm_tok, gmix_sb, gmix_tok)
        S.barrier()
        och = S.chan()
        if dbg == "ssm":
            dbg_ssm = nc.dram_tensor("dbg_ssm", [128, 4, NOWN * SEGT], BF16, kind="ExternalOutput").ap()
            ytok = Tok()
            S.dma("sp", och, dbg_ssm[:, :, :], ssmT[:], reads=ssm_tok, writes=[ytok])
            S.wait_tok("sp", [ytok])
        if dbg == "attn":
            with ExitStack() as e2:
                tmp = _sb(nc, e2, "dbgtmp", [128, 4, NOWN * SEGT], F32)
                tmp_tok = Tok()
                alltoks = [t for row in attn_tok for t in row]
                S.op("dve", lambda e: e.tensor_copy(out=tmp[:], in_=attnT[:]), reads=alltoks, writes=[tmp_tok])
                ytok = Tok()
                S.dma("sp", och, dbg_out[:, :, :], tmp[:], reads=[tmp_tok], writes=[ytok])
                S.wait_tok("sp", [ytok])
        S.barrier()
        if dbg in (None, "h1", "h2", "h3"):
            phase_post(C, nc, S, ps, ps_tok, I, attnT, attn_tok, ssmT, ssm_tok, y, dbg)
            return nc
        with ExitStack() as e2:
            zt = _sb(nc, e2, "zt", [128, D], F32)
            zt_tok = Tok()
            S.op("pool", lambda e: e.memset(zt[:], 0.0), writes=[zt_tok])
            ytok = Tok()
            for i in range(NOWN * 4):
                S.dma("sp", och, y[i * 128:(i + 1) * 128, :], zt[:], reads=[zt_tok], writes=[ytok])
            S.wait_tok("sp", [ytok])
    return nc


def make_in_maps(inputs):
    x = np.ascontiguousarray(inputs["x"], dtype=np.float32)
    w_in = np.ascontiguousarray(inputs["w_in"][0].reshape(8, 128, 2048).transpose(1, 0, 2))
    g_mix = np.ascontiguousarray(inputs["g_mix"][0].reshape(8, 128).T)
    f32 = np.float32
    are = inputs["a_re"][0].reshape(16, 2, 64).transpose(1, 2, 0).reshape(128, 16)
    aim = inputs["a_im"][0].reshape(16, 2, 64).transpose(1, 2, 0).reshape(128, 16)
    ldt = np.repeat(inputs["log_dt"][0].reshape(16, 2).T[:, None, :], 64, axis=1).reshape(128, 16)
    bpr = np.zeros((128, 16, 128), f32)
    bpi = np.zeros((128, 16, 128), f32)
    cpr = np.zeros((128, 16, 128), f32)
    cpi = np.zeros((128, 16, 128), f32)
    for k in range(16):
        for g2 in range(2):
            g = 2 * k + g2
            r0 = (g % 8) * 16
            bpr[r0:r0 + 16, k, g2 * 64:(g2 + 1) * 64] = inputs["b_re"][0][g].T
            bpi[r0:r0 + 16, k, g2 * 64:(g2 + 1) * 64] = inputs["b_im"][0][g].T
            cpr[g2 * 64:(g2 + 1) * 64, k, r0:r0 + 16] = inputs["c_re"][0][g].T
            cpi[g2 * 64:(g2 + 1) * 64, k, r0:r0 + 16] = inputs["c_im"][0][g].T
    dsk = inputs["d_skip"][0].reshape(4, 128).T
    wglu = inputs["w_glu"][0].reshape(4, 128, 512).transpose(1, 0, 2)
    bglu = inputs["b_glu"][0].reshape(4, 128).T
    def kt8(w):
        return w.reshape(8, 128, -1).transpose(1, 0, 2)

    def col8(g):
        return g.reshape(8, 128).T

    post = {"w_out": kt8(inputs["w_out"][0]),
            "g_out": col8(np.concatenate([inputs["g_attn_out"][0], inputs["g_ssm_out"][0]])),
            "g_mem": col8(inputs["g_mem"][0]), "w_xkv": kt8(inputs["w_xkv"][0]),
            "g_xattn": col8(inputs["g_xattn"][0]), "w_xq": kt8(inputs["w_xq"][0]), "w_xo": kt8(inputs["w_xo"][0]),
            "g_ffn": col8(inputs["g_ffn"][0]),
            "gffn_rep": np.broadcast_to(inputs["g_ffn"][0][None, :], (128, D)),
            "gfin_rep": np.broadcast_to(inputs["g_final"][None, :], (128, D)),
            "w_pq": kt8(inputs["w_pq"][0]),
            "skT": inputs["sub_keys"][0].transpose(3, 0, 1, 2).reshape(128, 16, 128),
            "peer_u": inputs["peer_u"][0], "peer_v": inputs["peer_v"][0]}
    common = {"w_in": w_in, "g_mix": g_mix, "a_re": are, "a_im": aim, "log_dt": ldt,
              "bpad_re": bpr, "bpad_im": bpi, "cpad_re": cpr, "cpad_im": cpi,
              "d_skip": dsk, "w_glu": wglu, "b_glu": bglu}
    common.update(post)
    common = {k_: np.ascontiguousarray(v_, dtype=f32) for k_, v_ in common.items()}
    maps = []
    for c in range(8):
        b, j = c // 4, c % 4
        tiles = own_tiles(j)
        segsel = np.zeros((128, NOWN, 16), f32)
        for i_, t_ in enumerate(tiles):
            segsel[:, i_, t_] = 1.0
        x_own = np.concatenate([x[b, t * SEGT:(t + 1) * SEGT] for t in tiles], axis=0)
        qp = np.stack([np.arange(t * SEGT, (t + 1) * SEGT, dtype=np.float32) for t in tiles], axis=0)
        qp = np.ascontiguousarray(np.broadcast_to(qp[None], (128, NOWN, SEGT)))
        x_rev = np.ascontiguousarray(x[b].reshape(NSEG, SEGT, D)[:, ::-1, :].reshape(SEQ, D))
        m = {"x_full": np.ascontiguousarray(x[b]), "x_own": np.ascontiguousarray(x_own), "x_rev": x_rev,
             "qpos": qp, "segsel": segsel, "mem": np.ascontiguousarray(inputs["mem"][b], dtype=f32)}
        m.update(common)
        maps.append(m)
    return maps


def kernel(**inputs):
    nc = build()
    maps = make_in_maps(inputs)
    res = run_bass_kernel_spmd(nc, maps, core_ids=list(range(8)))
    out = np.zeros((2, SEQ, D), np.float32)
    for c in range(8):
        b, j = c // 4, c % 4
        yo = res.results[c]["y"]
        for i, t in enumerate(own_tiles(j)):
            out[b, t * SEGT:(t + 1) * SEGT] = yo[i * SEGT:(i + 1) * SEGT]
    return out
```
